# Optimizing a Trainium2 kernel written in Bass

```python
import jax, jax.numpy as jnp
from jax import lax
import numpy as np

D_MODEL = 1024
BATCH = 8
SEQ = 2048
DEPTH = 4

CHUNK = 64
N_MIXERS = 2

MLSTM_HEADS = 8
QK_HEAD_DIM = D_MODEL // 2 // MLSTM_HEADS
V_HEAD_DIM = D_MODEL // MLSTM_HEADS
QK_WIDTH = MLSTM_HEADS * QK_HEAD_DIM
V_WIDTH = MLSTM_HEADS * V_HEAD_DIM
Q_END = QK_WIDTH
K_END = 2 * QK_WIDTH
V_END = K_END + V_WIDTH
O_END = V_END + V_WIDTH
MLSTM_IN_COLS = O_END + 2 * MLSTM_HEADS
GATE_SOFTCAP = 15.0

POOL_WINDOWS = (2, 4, 8, 16)
POOL_GROUPS = len(POOL_WINDOWS)
POOL_GROUP_DIM = D_MODEL // POOL_GROUPS

D_FF = ((8 * D_MODEL + 3 * 256 - 1) // (3 * 256)) * 256

EPS = 1e-6

kernel_name = "hybrid_mlstm_pool_swiglu_trunk"


def rmsnorm(x, gain):
    xf = x.astype(jnp.float32)
    y = xf * lax.rsqrt(jnp.mean(xf * xf, axis=-1, keepdims=True) + EPS)
    return (y * gain.astype(jnp.float32)).astype(x.dtype)


def mlstm_chunkwise(q, k, v, i_pre, f_pre):
    B, H, S, dk = q.shape
    dv = v.shape[-1]
    nc = S // CHUNK
    q = q.reshape(B, H, nc, CHUNK, dk)
    k = k.reshape(B, H, nc, CHUNK, dk)
    v = v.reshape(B, H, nc, CHUNK, dv)
    i_pre = i_pre.reshape(B, H, nc, CHUNK)
    log_f = jax.nn.log_sigmoid(f_pre).reshape(B, H, nc, CHUNK)
    b = jnp.cumsum(log_f, axis=-1)
    b_last = b[..., -1]

    a = b_last[..., None] - b + i_pre
    m_loc = jnp.max(a, axis=-1)
    w_loc = jnp.exp(a - m_loc[..., None])
    c_loc = jnp.einsum('bhcl,bhclv,bhclk->bhcvk', w_loc, v, k)
    n_loc = jnp.einsum('bhcl,bhclk->bhck', w_loc, k)

    def step(carry, xs):
        c_st, n_st, m_st = carry
        c_l, n_l, m_l, b_l = xs
        m_new = jnp.maximum(b_l + m_st, m_l)
        s_old = jnp.exp(b_l + m_st - m_new)
        s_new = jnp.exp(m_l - m_new)
        c_next = s_old[..., None, None] * c_st + s_new[..., None, None] * c_l
        n_next = s_old[..., None] * n_st + s_new[..., None] * n_l
        return (c_next, n_next, m_new), (c_st, n_st, m_st)

    init = (jnp.zeros((B, H, dv, dk), jnp.float32),
            jnp.zeros((B, H, dk), jnp.float32),
            jnp.zeros((B, H), jnp.float32))
    xs = (jnp.moveaxis(c_loc, 2, 0), jnp.moveaxis(n_loc, 2, 0),
          jnp.moveaxis(m_loc, 2, 0), jnp.moveaxis(b_last, 2, 0))
    _, (c0, n0, m0) = lax.scan(step, init, xs)
    c0 = jnp.moveaxis(c0, 0, 2)
    n0 = jnp.moveaxis(n0, 0, 2)
    m0 = jnp.moveaxis(m0, 0, 2)

    causal = jnp.tril(jnp.ones((CHUNK, CHUNK), dtype=bool))
    d_log = b[..., :, None] - b[..., None, :] + i_pre[..., None, :]
    d_log = jnp.where(causal, d_log, -jnp.inf)
    inter_log = b + m0[..., None]
    m = jnp.maximum(inter_log, jnp.max(d_log, axis=-1))
    d_w = jnp.exp(d_log - m[..., None])
    inter_w = jnp.exp(inter_log - m)
    qk = jnp.einsum('bhcsk,bhcrk->bhcsr', q, k) * d_w
    num = (jnp.einsum('bhcsr,bhcrv->bhcsv', qk, v)
           + inter_w[..., None] * jnp.einsum('bhcvk,bhcsk->bhcsv', c0, q))
    den = jnp.sum(qk, axis=-1) + inter_w * jnp.einsum('bhck,bhcsk->bhcs', n0, q)
    h = num / jnp.maximum(jnp.abs(den), jnp.exp(-m))[..., None]
    return h.reshape(B, H, S, dv)


def mlstm_mixer(h, w_in, gate_bias, head_gain, w_out):
    B, S, _ = h.shape
    proj = h @ w_in
    q, k, v, o, gates = jnp.split(proj, [Q_END, K_END, V_END, O_END], axis=-1)

    def heads(t, d):
        return t.reshape(B, S, MLSTM_HEADS, d).transpose(0, 2, 1, 3).astype(jnp.float32)

    gates = (gates + gate_bias).astype(jnp.float32)
    gates = GATE_SOFTCAP * jnp.tanh(gates / GATE_SOFTCAP)
    i_pre = gates[..., :MLSTM_HEADS].transpose(0, 2, 1)
    f_pre = gates[..., MLSTM_HEADS:].transpose(0, 2, 1)
    hc = mlstm_chunkwise(heads(q, QK_HEAD_DIM),
                         heads(k, QK_HEAD_DIM) * (QK_HEAD_DIM ** -0.5),
                         heads(v, V_HEAD_DIM), i_pre, f_pre)
    hc = hc * lax.rsqrt(jnp.mean(hc * hc, axis=-1, keepdims=True) + EPS)
    hc = hc.transpose(0, 2, 1, 3).reshape(B, S, V_WIDTH) * head_gain.astype(jnp.float32)
    out = (hc * jax.nn.sigmoid(o.astype(jnp.float32))).astype(h.dtype)
    return out @ w_out


def pool_mixer(h, w_group, scale):
    B, S, _ = h.shape
    hf = h.astype(jnp.float32)
    csum = jnp.cumsum(hf, axis=1)
    t = jnp.arange(1, S + 1, dtype=jnp.float32)
    outs = []
    for g, win in enumerate(POOL_WINDOWS):
        sl = slice(g * POOL_GROUP_DIM, (g + 1) * POOL_GROUP_DIM)
        cg = csum[..., sl]
        lag = jnp.pad(cg, ((0, 0), (win, 0), (0, 0)))[:, :S]
        mean = (cg - lag) / jnp.minimum(t, float(win))[None, :, None]
        y = (mean - hf[..., sl]).astype(h.dtype)
        outs.append(jnp.einsum('bsc,cd->bsd', y, w_group[g]))
    return jnp.concatenate(outs, axis=-1) * scale


def swiglu(h, w_gate_up, w_down):
    gu = h @ w_gate_up
    g, u = jnp.split(gu, 2, axis=-1)
    return (jax.nn.silu(g) * u) @ w_down


def setup_inputs(seed: int = 0) -> dict:
    key = jax.random.key(seed)
    ks = jax.random.split(key, 16)
    n_a = (DEPTH + N_MIXERS - 1) // N_MIXERS
    n_b = DEPTH // N_MIXERS
    f32 = jnp.float32

    x = jax.random.normal(ks[0], (BATCH, SEQ, D_MODEL), f32)
    norm_mix = 1.0 + 0.02 * jax.random.normal(ks[1], (DEPTH, D_MODEL), f32)
    norm_ffn = 1.0 + 0.02 * jax.random.normal(ks[2], (DEPTH, D_MODEL), f32)

    mlstm_w_in = jax.random.normal(ks[3], (n_a, D_MODEL, MLSTM_IN_COLS), f32) * D_MODEL ** -0.5
    i_bias = 0.1 * jax.random.normal(ks[4], (n_a, MLSTM_HEADS), f32)
    f_bias = (jnp.linspace(3.0, 6.0, MLSTM_HEADS, dtype=f32)[None, :]
              + 0.1 * jax.random.normal(ks[5], (n_a, MLSTM_HEADS), f32))
    mlstm_gate_bias = jnp.concatenate([i_bias, f_bias], axis=-1)
    mlstm_head_gain = 1.0 + 0.02 * jax.random.normal(ks[6], (n_a, V_WIDTH), f32)
    mlstm_w_out = jax.random.normal(ks[7], (n_a, V_WIDTH, D_MODEL), f32) * V_WIDTH ** -0.5

    pool_w_group = (jax.random.normal(ks[8], (n_b, POOL_GROUPS, POOL_GROUP_DIM, POOL_GROUP_DIM), f32)
                    * POOL_GROUP_DIM ** -0.5)
    pool_scale = 1.0 + 0.1 * jax.random.normal(ks[9], (n_b, D_MODEL), f32)

    ffn_w_gate_up = jax.random.normal(ks[10], (DEPTH, D_MODEL, 2 * D_FF), f32) * D_MODEL ** -0.5
    ffn_w_down = jax.random.normal(ks[11], (DEPTH, D_FF, D_MODEL), f32) * D_FF ** -0.5
    final_norm = 1.0 + 0.02 * jax.random.normal(ks[12], (D_MODEL,), f32)

    return {"x": x, "norm_mix": norm_mix, "norm_ffn": norm_ffn,
            "mlstm_w_in": mlstm_w_in, "mlstm_gate_bias": mlstm_gate_bias,
            "mlstm_head_gain": mlstm_head_gain, "mlstm_w_out": mlstm_w_out,
            "pool_w_group": pool_w_group, "pool_scale": pool_scale,
            "ffn_w_gate_up": ffn_w_gate_up, "ffn_w_down": ffn_w_down,
            "final_norm": final_norm}


def reference(x, norm_mix, norm_ffn, mlstm_w_in, mlstm_gate_bias, mlstm_head_gain,
              mlstm_w_out, pool_w_group, pool_scale, ffn_w_gate_up, ffn_w_down, final_norm):
    for layer in range(DEPTH):
        slot = layer // N_MIXERS
        h = rmsnorm(x, norm_mix[layer])
        if layer % N_MIXERS == 0:
            x = x + mlstm_mixer(h, mlstm_w_in[slot], mlstm_gate_bias[slot],
                                mlstm_head_gain[slot], mlstm_w_out[slot])
        else:
            x = x + pool_mixer(h, pool_w_group[slot], pool_scale[slot])
        x = x + swiglu(rmsnorm(x, norm_ffn[layer]), ffn_w_gate_up[layer], ffn_w_down[layer])
    return rmsnorm(x, final_norm)
```

```python
import numpy as np
from contextlib import ExitStack
import concourse.bass as bass
import concourse.mybir as mybir
from concourse.bass_utils import run_bass_kernel_spmd

F32 = mybir.dt.float32
BF16 = mybir.dt.bfloat16
U8 = mybir.dt.uint8
AF = mybir.ActivationFunctionType
ALU = mybir.AluOpType
AX = mybir.AxisListType

S = 2048
D = 1024
NT = 16
KC = 8
H = 8
DFF = 2816
NJ = 22
EPS = 1e-6
N_CORES = 8
DEPTH = 4
WINS = (2, 4, 8, 16)


class _Op:
    __slots__ = ("eng", "fn", "waits", "sig", "pos", "gid", "slot", "cnt", "known", "sidx")


class Prog:
    ENGS = ("pe", "act", "dve", "pool", "sp")

    def __init__(self, self_sync=True):
        self.streams = {e: [] for e in self.ENGS}
        self.known = {e: {} for e in self.ENGS}
        self.last_w = {}
        self.readers = {}
        self.slot_cnt = {}
        self.slot_last = {}
        self.gid = 0
        self.self_sync = self_sync

    def add(self, eng, fn, R=(), W=(), slot=None, extra=()):
        op = _Op()
        op.eng, op.fn, op.waits, op.sig, op.slot, op.cnt, op.sidx = eng, fn, [], False, slot, 0, 0
        op.gid = self.gid
        self.gid += 1
        st = self.streams[eng]
        op.pos = len(st)
        st.append(op)
        psum_r = [k for k in R if isinstance(k, tuple) and k[0] == "B"]
        if psum_r:
            R = [k for k in R if not (isinstance(k, tuple) and k[0] == "B")]
            W = list(W) + psum_r
        deps = {}
        for k in R:
            lw = self.last_w.get(k)
            if lw is not None:
                deps[lw.gid] = lw
        for k in W:
            lw = self.last_w.get(k)
            if lw is not None:
                deps[lw.gid] = lw
            for r in self.readers.get(k, ()):
                deps[r.gid] = r
        for d in extra:
            deps[d.gid] = d
        kn = self.known[eng]
        for g in sorted(deps, reverse=True):
            d = deps[g]
            if d.slot is not None:
                src, val = d.slot, d.cnt
            else:
                if d.eng == eng and (eng == "pe" or not self.self_sync):
                    continue
                src, val = d.eng, d.pos
            if kn.get(src, -1) >= val:
                continue
            assert d.fn is not None
            op.waits.append(d)
            d.sig = True
            for s_, v_ in d.known.items():
                if kn.get(s_, -1) < v_:
                    kn[s_] = v_
        if slot is not None:
            c = self.slot_cnt.get(slot, 0) + 1
            self.slot_cnt[slot] = c
            op.cnt = c
            prev = self.slot_last.get(slot)
            if prev is not None:
                assert kn.get(slot, -1) >= prev.cnt, ("two DMAs in flight on slot", slot)
            self.slot_last[slot] = op
            op.known = dict(kn)
            op.known[slot] = c
        else:
            op.known = dict(kn)
            op.known[eng] = op.pos
        for k in R:
            self.readers.setdefault(k, []).append(op)
        for k in W:
            self.last_w[k] = op
            self.readers[k] = []
        return op

    def barrier(self):
        lasts = []
        for e in self.ENGS:
            for op in reversed(self.streams[e]):
                if op.fn is not None and op.slot is None:
                    lasts.append(op)
                    break
        lasts += list(self.slot_last.values())
        for e in self.ENGS:
            self.add(e, None, extra=lasts)

    def emit(self, block, sems, slot_sems):
        for e in self.ENGS:
            n = 0
            for op in self.streams[e]:
                if op.slot is None and op.sig:
                    n += 1
                    op.sidx = n

        def body_for(e):
            st = self.streams[e]

            def body(eng):
                for op in st:
                    for d in op.waits:
                        if d.slot is not None:
                            eng.wait_ge(slot_sems[d.slot], 16 * d.cnt)
                        else:
                            eng.wait_ge(sems[d.eng], d.sidx)
                    if op.fn is not None:
                        ins = op.fn(eng)
                        if op.slot is not None:
                            ins.then_inc(slot_sems[op.slot], 16)
                        elif op.sig:
                            ins.then_inc(sems[e], 1)
            return body

        block.tensor(body_for("pe"))
        block.scalar(body_for("act"))
        block.vector(body_for("dve"))
        block.gpsimd(body_for("pool"))
        block.sync(body_for("sp"))


def _dsize(dt):
    return {F32: 4, BF16: 2, U8: 1}[dt]


class Builder:
    def __init__(self, layers, final_norm, self_sync=True, parts=("mixer", "ffn"), ntiles=NT):
        self.parts = parts
        self.ntiles = ntiles
        self.layers = list(layers)
        self.final_norm = final_norm
        self.nc = bass.Bass("TRN2", target_bir_lowering=False)
        self.P = Prog(self_sync=self_sync)
        self.dram = {}
        self.slots = set()

    def din(self, name, shape):
        if name not in self.dram:
            self.dram[name] = self.nc.dram_tensor(name, list(shape), F32, kind="ExternalInput").ap()
        return self.dram[name]

    def view(self, off, shape, dt):
        n = 1
        for s_ in shape[1:]:
            n *= s_
        nb = n * _dsize(dt)
        assert off % 32 == 0 and off + nb <= self.arena_bytes, (off, nb, self.arena_bytes)
        ap = self.arena[:, off:off + nb].bitcast(dt)
        if len(shape) == 3:
            ap = ap.rearrange("p (a b) -> p a b", a=shape[1])
        elif len(shape) == 4:
            ap = ap.rearrange("p (a b c) -> p a b c", a=shape[1], b=shape[2])
        return ap

    def carve(self, shape, dt):
        n = 1
        for s_ in shape[1:]:
            n *= s_
        nb = (n * _dsize(dt) + 31) // 32 * 32
        v = self.view(self.off, shape, dt)
        self.off += nb
        return v

    def dma(self, eng, out, in_, slot, R=(), W=()):
        self.slots.add(slot)
        return self.P.add(eng, lambda e: e.dma_start(out=out, in_=in_), R=R, W=W, slot=slot)

    def mm(self, out, lhsT, rhs, start, stop, R=(), W=()):
        return self.P.add("pe", lambda e: e.matmul(out, lhsT=lhsT, rhs=rhs, start=start, stop=stop), R=R, W=W)

    def tr(self, out, in_, ident, R=(), W=()):
        return self.P.add("pe", lambda e: e.transpose(out, in_, ident), R=R, W=W)

    def act(self, out, in_, func, R=(), W=(), **kw):
        return self.P.add("act", lambda e: e.activation(out=out, in_=in_, func=func, **kw), R=R, W=W)

    def tt(self, eng, out, in0, in1, op, R=(), W=()):
        return self.P.add(eng, lambda e: e.tensor_tensor(out=out, in0=in0, in1=in1, op=op), R=R, W=W)

    def ts(self, eng, out, in0, s1, s2, op0, op1=None, R=(), W=()):
        if op1 is None:
            return self.P.add(eng, lambda e: e.tensor_scalar(out=out, in0=in0, scalar1=s1, scalar2=None, op0=op0), R=R, W=W)
        return self.P.add(eng, lambda e: e.tensor_scalar(out=out, in0=in0, scalar1=s1, scalar2=s2, op0=op0, op1=op1), R=R, W=W)

    def stt(self, eng, out, in0, scalar, in1, op0, op1, R=(), W=()):
        return self.P.add(eng, lambda e: e.scalar_tensor_tensor(out=out, in0=in0, scalar=scalar, in1=in1, op0=op0, op1=op1), R=R, W=W)

    def copy(self, eng, out, in_, R=(), W=()):
        return self.P.add(eng, lambda e: e.tensor_copy(out, in_), R=R, W=W)

    def memset(self, eng, ap, val, R=(), W=()):
        return self.P.add(eng, lambda e: e.memset(ap, val), R=R, W=W)

    def build(self):
        nc = self.nc
        P = self.P
        with ExitStack() as es:
            def sb(name, shape, dt):
                return es.enter_context(nc.sbuf_tensor(name, shape, dt))

            self.x = sb("x_res", [128, NT, D], F32)
            self.ident_bf = sb("ident_bf", [128, 128], BF16)
            self.ident_f = sb("ident_f", [128, 128], F32)
            self.causal = sb("causal", [128, 128], BF16)
            self.negtri = sb("negtri", [128, 128], F32)
            self.negones = sb("negones", [128, 128], F32)
            self.invcnt = sb("invcnt", [128, 4, 16], F32)
            self.small = sb("small", [128, 512], F32)
            self.gain_bc = [sb(f"gain_bc{i}", [128, D], F32) for i in range(2)]
            self.gain_n = 0
            self.arena_bytes = (nc.sbuf_bytes_remaining - 256) // 64 * 64
            self.arena = sb("arena", [128, self.arena_bytes], U8)
            self.B = [es.enter_context(nc.psum_tensor(f"B{i}", [128, 512], F32)) for i in range(8)]
            self.out = nc.dram_tensor("out", [S, D], F32, kind="ExternalOutput").ap()
            xin = self.din("x", [S, D])

            sm = self.small
            self.ss = sm[:, 0:16]
            self.lnv = sm[:, 16:32]
            self.rstd = sm[:, 32:48]

            self.setup_consts()
            for t in range(NT):
                self.dma("sp", self.x[:, t, :], xin[t * 128:(t + 1) * 128, :], slot=("xld", t), W=[("x", t)])

            for l in self.layers:
                if "mixer" in self.parts:
                    if l % 2 == 0:
                        self.mlstm_phase(l)
                    else:
                        self.pool_phase(l)
                if "ffn" in self.parts:
                    self.ffn_phase(l)
            self.final_phase()

            sems = {e: es.enter_context(nc.semaphore(f"s_{e}")) for e in Prog.ENGS}
            slot_sems = {}
            for i, sl in enumerate(sorted(self.slots, key=str)):
                slot_sems[sl] = es.enter_context(nc.semaphore(f"d_{i}"))
            block = es.enter_context(nc.Block())
            P.emit(block, sems, slot_sems)
        return nc

    def setup_consts(self):
        def sel(ap, cmp, pattern, cm, key):
            self.P.add("pool", lambda e: e.affine_select(out=ap, in_=ap, compare_op=cmp, fill=0.0, base=0,
                                                         pattern=pattern, channel_multiplier=cm), W=[key])
        self.memset("pool", self.ident_bf[:], 1.0, W=["ident_bf"])
        sel(self.ident_bf[:], ALU.is_equal, [[-1, 128]], 1, "ident_bf")
        self.memset("pool", self.ident_f[:], 1.0, W=["ident_f"])
        sel(self.ident_f[:], ALU.is_equal, [[-1, 128]], 1, "ident_f")
        self.memset("pool", self.causal[:], 1.0, W=["causal"])
        sel(self.causal[:], ALU.is_ge, [[1, 128]], -1, "causal")
        self.memset("pool", self.negtri[:], -1.0, W=["negtri"])
        sel(self.negtri[:], ALU.is_ge, [[1, 128]], -1, "negtri")
        self.memset("pool", self.negones[:], -1.0, W=["negones"])
        for wi, w in enumerate(WINS):
            self.memset("pool", self.invcnt[:, wi, :], 1.0 / w, W=["invcnt"])
            for t in range(w - 1):
                self.memset("pool", self.invcnt[:, wi, t:t + 1], 1.0 / (t + 1), W=["invcnt"])

    def load_gain(self, row_ap):
        i = self.gain_n % 2
        self.gain_n += 1
        self.dma("sp", self.gain_bc[i][:], row_ap.partition_broadcast(128), slot=("gain", i), W=[("gain", i)])
        return i

    def norm_stats(self, junk):
        for t in range(NT):
            self.act(junk, self.x[:, t, :], AF.Square, R=[("x", t)], W=["junk", "ss"], accum_out=self.ss[:, t:t + 1])
        self.act(self.lnv, self.ss, AF.Ln, R=["ss"], W=["lnv"], scale=1.0 / D, bias=EPS)
        self.act(self.rstd, self.lnv, AF.Exp, R=["lnv"], W=["rstd"], scale=-0.5)

    def norm_to_hT(self, gain_row, hT, hb):
        gi = self.load_gain(gain_row)
        self.norm_stats(hb[0])
        psT = self.B[7][:].bitcast(BF16)
        for t in range(NT):
            hbt = hb[t % 2]
            self.stt("dve", hbt, self.x[:, t, :], self.rstd[:, t:t + 1], self.gain_bc[gi][:], ALU.mult, ALU.mult,
                     R=[("x", t), "rstd", ("gain", gi)], W=[("hb", t % 2), "junk"] if t % 2 == 0 else [("hb", t % 2)])
            for kc in range(KC):
                self.tr(psT[:, kc * 128:(kc + 1) * 128], hbt[:, kc * 128:(kc + 1) * 128], self.ident_bf[:],
                        R=[("hb", t % 2), "ident_bf"], W=[("B", 7)])
            self.act(hT[:, :, t * 128:(t + 1) * 128], psT.rearrange("p (k s) -> p k s", k=KC), AF.Copy,
                     R=[("B", 7)], W=[("hT", t)])

    def mlstm_phase(self, l):
        P = self.P
        slot = l // 2
        P.barrier()
        self.off = 0
        hT = self.carve([128, KC, S], BF16)
        win = self.carve([128, KC, 3088], BF16)
        wout = self.carve([128, KC, D], BF16)
        hb = [self.carve([128, D], BF16) for _ in range(2)]
        q_bf = self.carve([128, 512], BF16)
        k_bf = self.carve([128, 512], BF16)
        kE = self.carve([128, 512], BF16)
        kO = self.carve([128, 512], BF16)
        qkT = self.carve([128, 8, 128], BF16)
        vs = self.carve([128, H, 128], BF16)
        PT = self.carve([128, H, 128], BF16)
        sig = self.carve([128, D], F32)
        hc = self.carve([128, D], F32)
        outT = self.carve([128, 8, 128], BF16)
        C32 = self.carve([128, 4, 132], F32)
        Cbf = self.carve([128, 4, 132], BF16)
        hgain = self.carve([128, D], F32)
        bias_bc = self.carve([128, 16], F32)
        eabf = self.carve([128, 8], BF16)
        jk = self.carve([128, 128], BF16)
        outb = hb[0]
        sm = self.small
        g1, th, gs, ee, lfn = sm[:, 64:80], sm[:, 80:96], sm[:, 96:112], sm[:, 112:120], sm[:, 120:128]
        a_, ea, eb, gp, gsel = sm[:, 128:136], sm[:, 136:144], sm[:, 144:152], sm[:, 152:156], sm[:, 156:160]
        d1, d2, scl, ssh, l2, rs = sm[:, 160:168], sm[:, 168:176], sm[:, 176:184], sm[:, 184:192], sm[:, 192:200], sm[:, 200:208]
        B = self.B

        w_in = self.din("mlstm_w_in", [2, D, 3088])[slot].rearrange("(k p) n -> p k n", p=128)
        w_out = self.din("mlstm_w_out", [2, D, D])[slot].rearrange("(k p) n -> p k n", p=128)
        pieces = [(0, 1024), (1024, 2048), (2048, 3072), (3072, 3088)]
        for i, (c0, c1) in enumerate(pieces):
            self.dma("pool", win[:, :, c0:c1], w_in[:, :, c0:c1], slot=("win", i), W=[("win", i)])
        self.dma("pool", wout, w_out, slot=("wout",), W=["wout"])
        self.dma("sp", hgain, self.din("mlstm_head_gain", [2, D])[slot:slot + 1, :].partition_broadcast(128),
                 slot=("hgain",), W=["hgain"])
        self.dma("sp", bias_bc, self.din("mlstm_gate_bias", [2, 16])[slot:slot + 1, :].partition_broadcast(128),
                 slot=("gbias",), W=["gbias"])
        self.memset("dve", C32, 0.0, W=["C32"])
        self.memset("dve", Cbf, 0.0, W=["Cbf"])
        self.memset("dve", kE, 0.0, W=["kE"])
        self.memset("dve", kO, 0.0, W=["kO"])

        self.norm_to_hT(self.din("norm_mix", [DEPTH, D])[l:l + 1, :], hT, hb)

        psT = B[7][:].bitcast(BF16)
        psT3 = psT.rearrange("p (k s) -> p k s", k=8)

        def b4(i):
            return B[i][:].rearrange("p (h s) -> p h s", h=4)

        wpiece = {0: 0, 1: 0, 2: 1, 3: 1, 4: 2, 5: 2}
        for t in range(self.ntiles):
            tok = slice(t * 128, (t + 1) * 128)
            for cb in range(6):
                for kc in range(KC):
                    self.mm(B[cb][:], hT[:, kc, tok], win[:, kc, cb * 512:(cb + 1) * 512], kc == 0, kc == KC - 1,
                            R=[("hT", t), ("win", wpiece[cb])], W=[("B", cb)])
            for kc in range(KC):
                self.mm(B[6][:, 0:16], hT[:, kc, tok], win[:, kc, 3072:3088], kc == 0, kc == KC - 1,
                        R=[("hT", t), ("win", 3)], W=[("B", 6)])
            self.tt("dve", g1, B[6][:, 0:16], bias_bc, ALU.add, R=[("B", 6), "gbias"], W=["g1"])
            self.act(th, g1, AF.Tanh, R=["g1"], W=["th"], scale=1.0 / 15.0)
            self.ts("dve", gs, th, 15.0, None, ALU.mult, R=["th"], W=["gs"])
            self.act(ee, gs[:, 8:16], AF.Exp, R=["gs"], W=["ee"], scale=-1.0)
            self.act(lfn, ee, AF.Ln, R=["ee"], W=["lfn"], bias=1.0)
            self.mm(B[6][:, 16:24], self.negtri[:], lfn, True, True, R=["lfn", "negtri"], W=[("B", 6)])
            self.mm(B[6][:, 24:32], self.negones[:], lfn, True, True, R=["lfn", "negones"], W=[("B", 6)])
            self.tt("dve", a_, gs[:, 0:8], B[6][:, 16:24], ALU.subtract, R=["gs", ("B", 6)], W=["a"])
            self.act(ea, a_, AF.Exp, R=["a"], W=["ea"])
            self.copy("dve", eabf, ea, R=["ea"], W=["eabf"])
            self.act(eb, B[6][:, 16:24], AF.Exp, R=[("B", 6)], W=["eb"])
            bl2 = B[6][:, 24:32].rearrange("p (j two) -> p j two", two=2)
            self.copy("dve", gp[0:64, :], bl2[0:64, :, 0], R=[("B", 6)], W=["gp0"])
            self.copy("dve", gp[64:128, :], bl2[64:128, :, 1], R=[("B", 6)], W=["gp1"])
            self.act(gsel, gp, AF.Exp, R=["gp0", "gp1"], W=["gsel"])
            self.act(q_bf, B[0][:], AF.Copy, R=[("B", 0)], W=["q_bf"])
            self.act(k_bf, B[1][:], AF.Copy, R=[("B", 1)], W=["k_bf"], scale=0.125)
            k4 = B[1][:].rearrange("p (j two c) -> p j two c", two=2, c=64)
            kE4 = kE.rearrange("p (j two c) -> p j two c", two=2, c=64)
            kO4 = kO.rearrange("p (j two c) -> p j two c", two=2, c=64)
            self.ts("dve", kE4[:, :, 0, :], k4[:, :, 0, :], 0.125, None, ALU.mult, R=[("B", 1)], W=["kE"])
            self.ts("dve", kO4[:, :, 1, :], k4[:, :, 1, :], 0.125, None, ALU.mult, R=[("B", 1)], W=["kO"])
            for j in range(4):
                self.tr(psT[:, j * 128:(j + 1) * 128], q_bf[:, j * 128:(j + 1) * 128], self.ident_bf[:],
                        R=["q_bf", "ident_bf"], W=[("B", 7)])
            for j in range(4):
                self.tr(psT[:, (4 + j) * 128:(5 + j) * 128], k_bf[:, j * 128:(j + 1) * 128], self.ident_bf[:],
                        R=["k_bf", "ident_bf"], W=[("B", 7)])
            self.act(qkT, psT3, AF.Copy, R=[("B", 7)], W=["qkT"])
            for i in range(2):
                self.tt("dve", vs[:, 4 * i:4 * i + 4, :], b4(2 + i),
                        ea[:, 4 * i:4 * i + 4].unsqueeze(2).to_broadcast([128, 4, 128]), ALU.mult,
                        R=[("B", 2 + i), "ea"], W=[("vs", i)])
            for i in range(2):
                self.act(sig[:, i * 512:(i + 1) * 512], B[4 + i][:], AF.Tanh, R=[("B", 4 + i)], W=[("sig", i)], scale=0.5)
            for h in range(H):
                p0 = (h % 2) * 64
                self.mm(b4(h % 2)[:, h // 2, :], qkT[p0:p0 + 64, 4 + h // 2, :], qkT[p0:p0 + 64, h // 2, :], True, True,
                        R=["qkT"], W=[("B", h % 2)])
            PT4 = PT.rearrange("p (j two) s -> p j two s", two=2)
            for i in range(2):
                self.tt("dve", PT4[:, :, i, :], b4(i),
                        self.causal[:].unsqueeze(1).to_broadcast([128, 4, 128]), ALU.mult,
                        R=[("B", i), "causal"], W=[("PT", i)])
            for h in range(H):
                p0 = (h % 2) * 64
                qTh = qkT[p0:p0 + 64, h // 2, :]
                self.mm(b4(2 + h // 4)[:, h % 4, :], PT[:, h, :], vs[:, h, :], True, False,
                        R=[("PT", h % 2), ("vs", h // 4)], W=[("B", 2 + h // 4)])
                self.mm(b4(2 + h // 4)[:, h % 4, :], qTh, Cbf[p0:p0 + 64, h // 2, 0:128], False, True,
                        R=["qkT", "Cbf"], W=[("B", 2 + h // 4)])
                self.mm(B[6][:, 32 + h:33 + h], PT[:, h, :], eabf[:, h:h + 1], True, False,
                        R=[("PT", h % 2), "eabf"], W=[("B", 6)])
                self.mm(B[6][:, 32 + h:33 + h], qTh, Cbf[p0:p0 + 64, h // 2, 128:129], False, True,
                        R=["qkT", "Cbf"], W=[("B", 6)])
            for j in range(4):
                self.mm(b4(4)[:, j, :], kE[:, j * 128:(j + 1) * 128], vs[:, 2 * j, :], True, False,
                        R=["kE", ("vs", j // 2)], W=[("B", 4)])
                self.mm(b4(4)[:, j, :], kO[:, j * 128:(j + 1) * 128], vs[:, 2 * j + 1, :], False, True,
                        R=["kO", ("vs", j // 2)], W=[("B", 4)])
                self.mm(B[6][:, 40 + j:41 + j], kE[:, j * 128:(j + 1) * 128], eabf[:, 2 * j:2 * j + 1], True, False,
                        R=["kE", "eabf"], W=[("B", 6)])
                self.mm(B[6][:, 40 + j:41 + j], kO[:, j * 128:(j + 1) * 128], eabf[:, 2 * j + 1:2 * j + 2], False, True,
                        R=["kO", "eabf"], W=[("B", 6)])
            self.tt("dve", C32[:, :, 0:128], C32[:, :, 0:128], b4(4), ALU.add, R=["C32", ("B", 4)], W=["C32"])
            self.tt("dve", C32[:, :, 128], C32[:, :, 128], B[6][:, 40:44], ALU.add, R=["C32", ("B", 6)], W=["C32"])
            self.tt("dve", C32, C32, gsel.unsqueeze(2).to_broadcast([128, 4, 132]), ALU.mult, R=["C32", "gsel"], W=["C32"])
            self.act(Cbf, C32, AF.Copy, R=["C32"], W=["Cbf"])
            self.tt("dve", d1, B[6][:, 32:40], eb, ALU.mult, R=[("B", 6), "eb"], W=["d1"])
            self.stt("dve", d2, d1, -1.0, d1, ALU.mult, ALU.max, R=["d1"], W=["d2"])
            self.ts("dve", d2, d2, 1.0, None, ALU.max, R=["d2"], W=["d2"])
            self.P.add("dve", lambda e: e.reciprocal(out=d2, in_=d2), R=["d2"], W=["d2"])
            self.tt("dve", scl, d2, eb, ALU.mult, R=["d2", "eb"], W=["scl"])
            hc3 = hc.rearrange("p (h s) -> p h s", h=H)
            for i in range(2):
                self.tt("dve", hc3[:, 4 * i:4 * i + 4, :], b4(2 + i),
                        scl[:, 4 * i:4 * i + 4].unsqueeze(2).to_broadcast([128, 4, 128]), ALU.mult,
                        R=[("B", 2 + i), "scl"], W=[("hc", i)])
            for h in range(H):
                self.act(jk, hc3[:, h, :], AF.Square, R=[("hc", h // 4)], W=["jk", "ssh"], accum_out=ssh[:, h:h + 1])
            self.act(l2, ssh, AF.Ln, R=["ssh"], W=["l2"], scale=1.0 / 128.0, bias=EPS)
            self.act(rs, l2, AF.Exp, R=["l2"], W=["rs"], scale=-0.5, bias=float(np.log(0.5)))
            self.tt("dve", hc3, hc3, rs.unsqueeze(2).to_broadcast([128, H, 128]), ALU.mult,
                    R=[("hc", 0), ("hc", 1), "rs"], W=[("hc", 0), ("hc", 1)])
            self.tt("pool", hc, hc, hgain, ALU.mult, R=[("hc", 0), ("hc", 1), "hgain"], W=[("hc", 0), ("hc", 1)])
            self.stt("dve", outb, sig, 1.0, hc, ALU.add, ALU.mult,
                     R=[("sig", 0), ("sig", 1), ("hc", 0), ("hc", 1)], W=[("hb", 0)])
            for j in range(8):
                self.tr(psT[:, j * 128:(j + 1) * 128], outb[:, j * 128:(j + 1) * 128], self.ident_bf[:],
                        R=[("hb", 0), "ident_bf"], W=[("B", 7)])
            self.act(outT, psT3, AF.Copy, R=[("B", 7)], W=["outT"])
            for half in range(2):
                bk = 5 if half == 0 else 1
                for vc in range(8):
                    self.mm(B[bk][:], outT[:, vc, :], wout[:, vc, half * 512:(half + 1) * 512], vc == 0, vc == 7,
                            R=["outT", "wout"], W=[("B", bk)])
                xs = self.x[:, t, half * 512:(half + 1) * 512]
                self.tt("dve", xs, xs, B[bk][:], ALU.add, R=[("B", bk), ("x", t)], W=[("x", t)])

    def pool_phase(self, l):
        P = self.P
        slot = l // 2
        P.barrier()
        self.off = 0
        hT32 = self.carve([128, KC, S], F32)
        yT = self.carve([128, KC, S], BF16)
        sbuf_ = [self.carve([128, S], F32) for _ in range(2)]
        wp = self.carve([128, 4, 2, 256], BF16)
        pscale = self.carve([128, D], F32)
        h32 = [self.carve([128, D], F32) for _ in range(2)]
        t16 = self.carve([128, 16], F32)
        B = self.B
        wsrc = self.din("pool_w_group", [2, 4, 256, 256])[slot]
        for g in range(4):
            self.dma("pool", wp[:, g, :, :], wsrc[g].rearrange("(k p) d -> p k d", p=128), slot=("wp", g), W=[("wp", g)])
        self.dma("sp", pscale, self.din("pool_scale", [2, D])[slot:slot + 1, :].partition_broadcast(128),
                 slot=("pscale",), W=["pscale"])
        gi = self.load_gain(self.din("norm_mix", [DEPTH, D])[l:l + 1, :])
        self.norm_stats(h32[0])
        for t in range(NT):
            ht = h32[t % 2]
            self.stt("dve", ht, self.x[:, t, :], self.rstd[:, t:t + 1], self.gain_bc[gi][:], ALU.mult, ALU.mult,
                     R=[("x", t), "rstd", ("gain", gi)], W=[("h32", t % 2), "junk"] if t % 2 == 0 else [("h32", t % 2)])
            for kc in range(KC):
                bk = 6 + kc // 4
                self.tr(B[bk][:, (kc % 4) * 128:(kc % 4 + 1) * 128], ht[:, kc * 128:(kc + 1) * 128], self.ident_f[:],
                        R=[("h32", t % 2), "ident_f"], W=[("B", bk)])
            self.act(hT32[:, 0:4, t * 128:(t + 1) * 128], B[6][:].rearrange("p (k s) -> p k s", k=4), AF.Copy,
                     R=[("B", 6)], W=[("hT32", c) for c in range(0, 4)])
            self.copy("dve", hT32[:, 4:8, t * 128:(t + 1) * 128], B[7][:].rearrange("p (k s) -> p k s", k=4),
                      R=[("B", 7)], W=[("hT32", c) for c in range(4, 8)])
        for c in range(KC):
            wi = c // 2
            win_ = WINS[wi]
            src = hT32[:, c, :]
            cur, ckey = src, ("hT32", c)
            sh, n = 1, 0
            while sh < win_:
                dst = sbuf_[n % 2]
                dkey = ("sb", n % 2)
                self.tt("dve", dst[:, sh:], cur[:, sh:], cur[:, 0:S - sh], ALU.add, R=[ckey], W=[dkey])
                self.copy("dve", dst[:, 0:sh], cur[:, 0:sh], R=[ckey], W=[dkey])
                cur, ckey = dst, dkey
                sh *= 2
                n += 1
            self.stt("dve", yT[:, c, :], cur, 1.0 / win_, src, ALU.mult, ALU.subtract, R=[ckey, ("hT32", c)], W=[("yT", c)])
            self.tt("dve", t16, cur[:, 0:16], self.invcnt[:, wi, :], ALU.mult, R=[ckey, "invcnt"], W=["t16"])
            self.tt("dve", yT[:, c, 0:16], t16, src[:, 0:16], ALU.subtract, R=["t16", ("hT32", c)], W=[("yT", c)])
        for t in range(NT):
            tok = slice(t * 128, (t + 1) * 128)
            for g in range(4):
                bk = (t % 2) * 2 + g // 2
                for kc in range(2):
                    self.mm(B[bk][:, (g % 2) * 256:(g % 2 + 1) * 256], yT[:, 2 * g + kc, tok], wp[:, g, kc, :], kc == 0, kc == 1,
                            R=[("yT", 2 * g + kc), ("wp", g)], W=[("B", bk)])
            for half in range(2):
                bk = (t % 2) * 2 + half
                tmp = h32[half][:, 0:512]
                self.tt("dve", tmp, B[bk][:], pscale[:, half * 512:(half + 1) * 512], ALU.mult,
                        R=[("B", bk), "pscale"], W=[("h32", half)])
                xs = self.x[:, t, half * 512:(half + 1) * 512]
                self.tt("dve", xs, xs, tmp, ALU.add, R=[("h32", half), ("x", t)], W=[("x", t)])

    def ffn_phase(self, l):
        P = self.P
        P.barrier()
        self.off = 0
        hT = self.carve([128, KC, S], BF16)
        abuf = [self.carve([128, 4, S], BF16) for _ in range(2)]
        wgu = [self.carve([128, KC, 2, 256], BF16) for _ in range(3)]
        wd = [self.carve([128, 4, D], BF16) for _ in range(2)]
        hb = [self.carve([128, D], BF16) for _ in range(2)]
        sl = [self.carve([128, 512], BF16) for _ in range(2)]
        B = self.B
        wgu_src = self.din("ffn_w_gate_up", [DEPTH, D, 2 * DFF])[l].rearrange("(k p) n -> p k n", p=128)
        wd_src = self.din("ffn_w_down", [DEPTH, DFF, D])[l].rearrange("(j p) d -> p j d", p=128)
        groups = [(j0, min(4, NJ - j0)) for j0 in range(0, NJ, 4)]

        def load_gu(n):
            s_ = n % 3
            self.dma("pool", wgu[s_][:, :, 0, :], wgu_src[:, :, n * 256:(n + 1) * 256], slot=("wg", s_), W=[("wg", s_)])
            self.dma("pool", wgu[s_][:, :, 1, :], wgu_src[:, :, DFF + n * 256:DFF + (n + 1) * 256], slot=("wu", s_), W=[("wu", s_)])

        def load_d(gi):
            j0, nj = groups[gi]
            self.dma("pool", wd[gi % 2][:, 0:nj, :], wd_src[:, j0:j0 + nj, :], slot=("wd", gi % 2), W=[("wd", gi % 2)])

        load_gu(0)
        load_gu(1)
        load_gu(2)
        load_d(0)
        self.norm_to_hT(self.din("norm_ffn", [DEPTH, D])[l:l + 1, :], hT, hb)

        cnt = [0]

        def gateup(gi):
            j0, nj = groups[gi]
            ab = abuf[gi % 2]
            for jj in range(nj):
                j = j0 + jj
                n, sub = j // 2, j % 2
                s_ = n % 3
                for tg in range(4):
                    pr = cnt[0] % 2
                    cnt[0] += 1
                    bg, bu = B[2 * pr], B[2 * pr + 1]
                    rk = [("hT", 4 * tg + i) for i in range(4)]
                    for kc in range(KC):
                        self.mm(bg[:], wgu[s_][:, kc, 0, sub * 128:(sub + 1) * 128], hT[:, kc, tg * 512:(tg + 1) * 512],
                                kc == 0, kc == KC - 1, R=rk + [("wg", s_)], W=[("B", 2 * pr)])
                    for kc in range(KC):
                        self.mm(bu[:], wgu[s_][:, kc, 1, sub * 128:(sub + 1) * 128], hT[:, kc, tg * 512:(tg + 1) * 512],
                                kc == 0, kc == KC - 1, R=rk + [("wu", s_)], W=[("B", 2 * pr + 1)])
                    self.act(sl[pr], bg[:], AF.Silu, R=[("B", 2 * pr)], W=[("sl", pr)])
                    self.tt("dve", ab[:, jj, tg * 512:(tg + 1) * 512], sl[pr], bu[:], ALU.mult,
                            R=[("sl", pr), ("B", 2 * pr + 1)], W=[("ab", gi % 2, tg)])
                if sub == 1 or j == NJ - 1:
                    if n + 3 < NJ // 2:
                        load_gu(n + 3)

        def down(gi):
            j0, nj = groups[gi]
            ab = abuf[gi % 2]
            for t in range(NT):
                for half in range(2):
                    bk = 4 + (2 * t + half) % 4
                    for jj in range(nj):
                        self.mm(B[bk][:], ab[:, jj, t * 128:(t + 1) * 128], wd[gi % 2][:, jj, half * 512:(half + 1) * 512],
                                jj == 0, jj == nj - 1, R=[("ab", gi % 2, t // 4), ("wd", gi % 2)], W=[("B", bk)])
                    xs = self.x[:, t, half * 512:(half + 1) * 512]
                    self.tt("dve", xs, xs, B[bk][:], ALU.add, R=[("B", bk), ("x", t)], W=[("x", t)])
            if gi + 2 < len(groups):
                load_d(gi + 2)

        gateup(0)
        load_d(1)
        for gi in range(len(groups)):
            if gi + 1 < len(groups):
                gateup(gi + 1)
            down(gi)

    def final_phase(self):
        P = self.P
        P.barrier()
        self.off = 0
        ob = [self.carve([128, D], F32) for _ in range(2)]
        if self.final_norm:
            gi = self.load_gain(self.din("final_norm", [1, D])[0:1, :])
            self.norm_stats(ob[0])
        stores = []
        for t in range(NT):
            o = ob[t % 2]
            if self.final_norm:
                self.stt("dve", o, self.x[:, t, :], self.rstd[:, t:t + 1], self.gain_bc[gi][:], ALU.mult, ALU.mult,
                         R=[("x", t), "rstd", ("gain", gi)], W=[("ob", t % 2), "junk"] if t % 2 == 0 else [("ob", t % 2)])
                src, rk = o, [("ob", t % 2)]
            else:
                src, rk = self.x[:, t, :], [("x", t)]
            stores.append(self.dma("sp", self.out[t * 128:(t + 1) * 128, :], src, slot=("st", t), R=rk, W=[("outd", t)]))
        P.add("sp", None, extra=stores)


_CACHE = {}


def _get_prog(layers, final_norm):
    key = (tuple(layers), final_norm)
    if key not in _CACHE:
        b = Builder(layers, final_norm)
        nc = b.build()
        _CACHE[key] = (nc, sorted(b.dram.keys()))
    return _CACHE[key]


def _run(layers, final_norm, xs, inputs):
    nc, names = _get_prog(layers, final_norm)
    in_maps = []
    for c in range(N_CORES):
        m = {}
        for n in names:
            if n == "x":
                m[n] = np.ascontiguousarray(xs[c])
            elif n == "final_norm":
                m[n] = np.ascontiguousarray(inputs[n]).reshape(1, D)
            else:
                m[n] = np.ascontiguousarray(inputs[n])
        in_maps.append(m)
    res = run_bass_kernel_spmd(nc, in_maps, core_ids=list(range(N_CORES)))
    return [res.results[c]["out"] for c in range(N_CORES)]


LAUNCH_GROUPS = [[0], [1], [2], [3]]


def kernel(**inputs):
    inputs = {k: np.asarray(v, dtype=np.float32) for k, v in inputs.items()}
    xs = [inputs["x"][b] for b in range(N_CORES)]
    for gi, grp in enumerate(LAUNCH_GROUPS):
        xs = _run(grp, gi == len(LAUNCH_GROUPS) - 1, xs, inputs)
    return np.stack(xs, axis=0).astype(np.float32)
```

```python
import numpy as np
from contextlib import ExitStack
import concourse.bass as bass
import concourse.mybir as mybir
from concourse.bass_utils import run_bass_kernel_spmd

F32 = mybir.dt.float32
BF16 = mybir.dt.bfloat16
U8 = mybir.dt.uint8
AF = mybir.ActivationFunctionType
ALU = mybir.AluOpType
AX = mybir.AxisListType

S = 2048
D = 1024
NT = 16
KC = 8
H = 8
DFF = 2816
NJ = 22
EPS = 1e-6
N_CORES = 8
DEPTH = 4
WINS = (2, 4, 8, 16)


class _Op:
    __slots__ = ("eng", "fn", "waits", "sig", "pos", "gid", "slot", "cnt", "known", "sidx")


class Prog:
    ENGS = ("pe", "act", "dve", "pool", "sp")

    def __init__(self, self_sync=True):
        self.streams = {e: [] for e in self.ENGS}
        self.known = {e: {} for e in self.ENGS}
        self.last_w = {}
        self.readers = {}
        self.slot_cnt = {}
        self.slot_last = {}
        self.gid = 0
        self.self_sync = self_sync

    def add(self, eng, fn, R=(), W=(), slot=None, extra=()):
        op = _Op()
        op.eng, op.fn, op.waits, op.sig, op.slot, op.cnt, op.sidx = eng, fn, [], False, slot, 0, 0
        op.gid = self.gid
        self.gid += 1
        st = self.streams[eng]
        op.pos = len(st)
        st.append(op)
        psum_r = [k for k in R if isinstance(k, tuple) and k[0] == "B"]
        if psum_r:
            R = [k for k in R if not (isinstance(k, tuple) and k[0] == "B")]
            W = list(W) + psum_r
        deps = {}
        for k in R:
            lw = self.last_w.get(k)
            if lw is not None:
                deps[lw.gid] = lw
        for k in W:
            lw = self.last_w.get(k)
            if lw is not None:
                deps[lw.gid] = lw
            for r in self.readers.get(k, ()):
                deps[r.gid] = r
        for d in extra:
            deps[d.gid] = d
        kn = self.known[eng]
        for g in sorted(deps, reverse=True):
            d = deps[g]
            if d.slot is not None:
                src, val = d.slot, d.cnt
            else:
                if d.eng == eng and (eng == "pe" or not self.self_sync):
                    continue
                src, val = d.eng, d.pos
            if kn.get(src, -1) >= val:
                continue
            assert d.fn is not None
            op.waits.append(d)
            d.sig = True
            for s_, v_ in d.known.items():
                if kn.get(s_, -1) < v_:
                    kn[s_] = v_
        if slot is not None:
            c = self.slot_cnt.get(slot, 0) + 1
            self.slot_cnt[slot] = c
            op.cnt = c
            prev = self.slot_last.get(slot)
            if prev is not None:
                assert kn.get(slot, -1) >= prev.cnt, ("two DMAs in flight on slot", slot)
            self.slot_last[slot] = op
            op.known = dict(kn)
            op.known[slot] = c
        else:
            op.known = dict(kn)
            op.known[eng] = op.pos
        for k in R:
            self.readers.setdefault(k, []).append(op)
        for k in W:
            self.last_w[k] = op
            self.readers[k] = []
        return op

    def barrier(self):
        lasts = []
        for e in self.ENGS:
            for op in reversed(self.streams[e]):
                if op.fn is not None and op.slot is None:
                    lasts.append(op)
                    break
        lasts += list(self.slot_last.values())
        for e in self.ENGS:
            self.add(e, None, extra=lasts)

    def emit(self, block, sems, slot_sems):
        for e in self.ENGS:
            n = 0
            for op in self.streams[e]:
                if op.slot is None and op.sig:
                    n += 1
                    op.sidx = n

        def body_for(e):
            st = self.streams[e]

            def body(eng):
                for op in st:
                    for d in op.waits:
                        if d.slot is not None:
                            eng.wait_ge(slot_sems[d.slot], 16 * d.cnt)
                        else:
                            eng.wait_ge(sems[d.eng], d.sidx)
                    if op.fn is not None:
                        ins = op.fn(eng)
                        if op.slot is not None:
                            ins.then_inc(slot_sems[op.slot], 16)
                        elif op.sig:
                            ins.then_inc(sems[e], 1)
            return body

        block.tensor(body_for("pe"))
        block.scalar(body_for("act"))
        block.vector(body_for("dve"))
        block.gpsimd(body_for("pool"))
        block.sync(body_for("sp"))


def _dsize(dt):
    return {F32: 4, BF16: 2, U8: 1}[dt]


class Builder:
    def __init__(self, layers, final_norm, self_sync=True, parts=("mixer", "ffn"), ntiles=NT):
        self.parts = parts
        self.ntiles = ntiles
        self.layers = list(layers)
        self.final_norm = final_norm
        self.nc = bass.Bass("TRN2", target_bir_lowering=False)
        self.P = Prog(self_sync=self_sync)
        self.dram = {}
        self.slots = set()

    def din(self, name, shape):
        if name not in self.dram:
            self.dram[name] = self.nc.dram_tensor(name, list(shape), F32, kind="ExternalInput").ap()
        return self.dram[name]

    def view(self, off, shape, dt):
        n = 1
        for s_ in shape[1:]:
            n *= s_
        nb = n * _dsize(dt)
        assert off % 32 == 0 and off + nb <= self.arena_bytes, (off, nb, self.arena_bytes)
        ap = self.arena[:, off:off + nb].bitcast(dt)
        if len(shape) == 3:
            ap = ap.rearrange("p (a b) -> p a b", a=shape[1])
        elif len(shape) == 4:
            ap = ap.rearrange("p (a b c) -> p a b c", a=shape[1], b=shape[2])
        return ap

    def carve(self, shape, dt):
        n = 1
        for s_ in shape[1:]:
            n *= s_
        nb = (n * _dsize(dt) + 31) // 32 * 32
        v = self.view(self.off, shape, dt)
        self.off += nb
        return v

    def dma(self, eng, out, in_, slot, R=(), W=()):
        self.slots.add(slot)
        return self.P.add(eng, lambda e: e.dma_start(out=out, in_=in_), R=R, W=W, slot=slot)

    def mm(self, out, lhsT, rhs, start, stop, R=(), W=()):
        return self.P.add("pe", lambda e: e.matmul(out, lhsT=lhsT, rhs=rhs, start=start, stop=stop), R=R, W=W)

    def tr(self, out, in_, ident, R=(), W=()):
        return self.P.add("pe", lambda e: e.transpose(out, in_, ident), R=R, W=W)

    def act(self, out, in_, func, R=(), W=(), **kw):
        return self.P.add("act", lambda e: e.activation(out=out, in_=in_, func=func, **kw), R=R, W=W)

    def tt(self, eng, out, in0, in1, op, R=(), W=()):
        return self.P.add(eng, lambda e: e.tensor_tensor(out=out, in0=in0, in1=in1, op=op), R=R, W=W)

    def ts(self, eng, out, in0, s1, s2, op0, op1=None, R=(), W=()):
        if op1 is None:
            return self.P.add(eng, lambda e: e.tensor_scalar(out=out, in0=in0, scalar1=s1, scalar2=None, op0=op0), R=R, W=W)
        return self.P.add(eng, lambda e: e.tensor_scalar(out=out, in0=in0, scalar1=s1, scalar2=s2, op0=op0, op1=op1), R=R, W=W)

    def stt(self, eng, out, in0, scalar, in1, op0, op1, R=(), W=()):
        return self.P.add(eng, lambda e: e.scalar_tensor_tensor(out=out, in0=in0, scalar=scalar, in1=in1, op0=op0, op1=op1), R=R, W=W)

    def copy(self, eng, out, in_, R=(), W=()):
        return self.P.add(eng, lambda e: e.tensor_copy(out, in_), R=R, W=W)

    def memset(self, eng, ap, val, R=(), W=()):
        return self.P.add(eng, lambda e: e.memset(ap, val), R=R, W=W)

    def build(self):
        nc = self.nc
        P = self.P
        with ExitStack() as es:
            def sb(name, shape, dt):
                return es.enter_context(nc.sbuf_tensor(name, shape, dt))

            self.x = sb("x_res", [128, NT, D], F32)
            self.ident_bf = sb("ident_bf", [128, 128], BF16)
            self.ident_f = sb("ident_f", [128, 128], F32)
            self.causal = sb("causal", [128, 128], BF16)
            self.negtri = sb("negtri", [128, 128], F32)
            self.negones = sb("negones", [128, 128], F32)
            self.invcnt = sb("invcnt", [128, 4, 16], F32)
            self.small = sb("small", [128, 512], F32)
            self.gain_bc = [sb(f"gain_bc{i}", [128, D], F32) for i in range(2)]
            self.gain_n = 0
            self.arena_bytes = (nc.sbuf_bytes_remaining - 256) // 64 * 64
            self.arena = sb("arena", [128, self.arena_bytes], U8)
            self.B = [es.enter_context(nc.psum_tensor(f"B{i}", [128, 512], F32)) for i in range(8)]
            self.out = nc.dram_tensor("out", [S, D], F32, kind="ExternalOutput").ap()
            xin = self.din("x", [S, D])

            sm = self.small
            self.ss = sm[:, 0:16]
            self.lnv = sm[:, 16:32]
            self.rstd = sm[:, 32:48]

            self.setup_consts()
            for t in range(NT):
                self.dma("sp", self.x[:, t, :], xin[t * 128:(t + 1) * 128, :], slot=("xld", t), W=[("x", t)])

            for l in self.layers:
                if "mixer" in self.parts:
                    if l % 2 == 0:
                        self.mlstm_phase(l)
                    else:
                        self.pool_phase(l)
                if "ffn" in self.parts:
                    self.ffn_phase(l)
            self.final_phase()

            sems = {e: es.enter_context(nc.semaphore(f"s_{e}")) for e in Prog.ENGS}
            slot_sems = {}
            for i, sl in enumerate(sorted(self.slots, key=str)):
                slot_sems[sl] = es.enter_context(nc.semaphore(f"d_{i}"))
            block = es.enter_context(nc.Block())
            P.emit(block, sems, slot_sems)
        return nc

    def setup_consts(self):
        def sel(ap, cmp, pattern, cm, key):
            self.P.add("pool", lambda e: e.affine_select(out=ap, in_=ap, compare_op=cmp, fill=0.0, base=0,
                                                         pattern=pattern, channel_multiplier=cm), W=[key])
        self.memset("pool", self.ident_bf[:], 1.0, W=["ident_bf"])
        sel(self.ident_bf[:], ALU.is_equal, [[-1, 128]], 1, "ident_bf")
        self.memset("pool", self.ident_f[:], 1.0, W=["ident_f"])
        sel(self.ident_f[:], ALU.is_equal, [[-1, 128]], 1, "ident_f")
        self.memset("pool", self.causal[:], 1.0, W=["causal"])
        sel(self.causal[:], ALU.is_ge, [[1, 128]], -1, "causal")
        self.memset("pool", self.negtri[:], -1.0, W=["negtri"])
        sel(self.negtri[:], ALU.is_ge, [[1, 128]], -1, "negtri")
        self.memset("pool", self.negones[:], -1.0, W=["negones"])
        for wi, w in enumerate(WINS):
            self.memset("pool", self.invcnt[:, wi, :], 1.0 / w, W=["invcnt"])
            for t in range(w - 1):
                self.memset("pool", self.invcnt[:, wi, t:t + 1], 1.0 / (t + 1), W=["invcnt"])

    def load_gain(self, row_ap):
        i = self.gain_n % 2
        self.gain_n += 1
        self.dma("sp", self.gain_bc[i][:], row_ap.partition_broadcast(128), slot=("gain", i), W=[("gain", i)])
        return i

    def norm_stats(self, junk):
        for t in range(NT):
            self.act(junk, self.x[:, t, :], AF.Square, R=[("x", t)], W=["junk", "ss"], accum_out=self.ss[:, t:t + 1])
        self.act(self.lnv, self.ss, AF.Ln, R=["ss"], W=["lnv"], scale=1.0 / D, bias=EPS)
        self.act(self.rstd, self.lnv, AF.Exp, R=["lnv"], W=["rstd"], scale=-0.5)

    def norm_to_hT(self, gain_row, hT, hb):
        gi = self.load_gain(gain_row)
        self.norm_stats(hb[0])
        psT = self.B[7][:].bitcast(BF16)
        for t in range(NT):
            hbt = hb[t % 2]
            self.stt("dve", hbt, self.x[:, t, :], self.rstd[:, t:t + 1], self.gain_bc[gi][:], ALU.mult, ALU.mult,
                     R=[("x", t), "rstd", ("gain", gi)], W=[("hb", t % 2), "junk"] if t % 2 == 0 else [("hb", t % 2)])
            for kc in range(KC):
                self.tr(psT[:, kc * 128:(kc + 1) * 128], hbt[:, kc * 128:(kc + 1) * 128], self.ident_bf[:],
                        R=[("hb", t % 2), "ident_bf"], W=[("B", 7)])
            self.act(hT[:, :, t * 128:(t + 1) * 128], psT.rearrange("p (k s) -> p k s", k=KC), AF.Copy,
                     R=[("B", 7)], W=[("hT", t)])

    def mlstm_phase(self, l):
        P = self.P
        slot = l // 2
        P.barrier()
        self.off = 0
        NTL = self.ntiles
        win = self.carve([128, KC, 3088], BF16)
        wout = self.carve([128, KC, D], BF16)
        hgain = self.carve([128, D], F32)
        bias_bc = self.carve([128, 16], F32)
        hb = [self.carve([128, D], BF16) for _ in range(2)]
        hTt = [self.carve([128, KC, 128], BF16) for _ in range(2)]
        q_bf = self.carve([128, 512], BF16)
        k_bf = self.carve([128, 512], BF16)
        kE = [self.carve([128, 512], BF16) for _ in range(2)]
        kO = [self.carve([128, 512], BF16) for _ in range(2)]
        qkT = [self.carve([128, 8, 128], BF16) for _ in range(2)]
        vs = [self.carve([128, H, 128], BF16) for _ in range(2)]
        eo = [self.carve([128, D], F32) for _ in range(2)]
        PT = self.carve([128, H, 128], BF16)
        hcs = [self.carve([128, D], F32) for _ in range(2)]
        outbs = [self.carve([128, D], BF16) for _ in range(2)]
        outTs = [self.carve([128, 8, 128], BF16) for _ in range(2)]
        C32 = self.carve([128, 4, 132], F32)
        Cbf = [self.carve([128, 4, 132], BF16) for _ in range(2)]
        eabf = [self.carve([128, 16], BF16) for _ in range(2)]
        sqb = self.carve([128, D], F32)
        sm = self.small
        def smv(p):
            o = 64 + p * 120
            names = [("ssA", 1), ("lnA", 1), ("rsA", 1), ("g1", 16), ("e2", 16), ("dd", 16), ("gs", 16), ("ee", 8),
                     ("lfn", 8), ("a", 8), ("ea", 8), ("eb", 8), ("gp", 4), ("gsel", 4)]
            d_ = {}
            for n_, w_ in names:
                d_[n_] = sm[:, o:o + w_]
                o += w_
            return d_
        SV = [smv(0), smv(1)]
        d1, d2, scl, ssh, l2, rs = (sm[:, 320:328], sm[:, 328:336], sm[:, 336:344], sm[:, 344:352], sm[:, 352:360], sm[:, 360:368])
        B = self.B

        w_in = self.din("mlstm_w_in", [2, D, 3088])[slot].rearrange("(k p) n -> p k n", p=128)
        w_out = self.din("mlstm_w_out", [2, D, D])[slot].rearrange("(k p) n -> p k n", p=128)
        pieces = [(0, 1024), (3072, 3088), (1024, 2048), (2048, 3072)]
        pkey = {0: 0, 1: 0, 2: 2, 3: 2, 4: 3, 5: 3}
        for i, (c0, c1) in enumerate(pieces):
            self.dma("pool", win[:, :, c0:c1], w_in[:, :, c0:c1], slot=("win", i), W=[("win", i)])
        self.dma("pool", wout, w_out, slot=("wout",), W=["wout"])
        self.dma("sp", hgain, self.din("mlstm_head_gain", [2, D])[slot:slot + 1, :].partition_broadcast(128),
                 slot=("hgain",), W=["hgain"])
        self.dma("sp", bias_bc, self.din("mlstm_gate_bias", [2, 16])[slot:slot + 1, :].partition_broadcast(128),
                 slot=("gbias",), W=["gbias"])
        gi = self.load_gain(self.din("norm_mix", [DEPTH, D])[l:l + 1, :])
        self.memset("dve", C32, 0.0, W=["C32"])
        self.memset("dve", Cbf[0], 0.0, W=[("Cbf", 0)])
        for p in range(2):
            self.memset("dve", kE[p], 0.0, W=[("kE", p)])
            self.memset("dve", kO[p], 0.0, W=[("kO", p)])

        psT = B[7][:].bitcast(BF16)
        psT3 = psT.rearrange("p (k s) -> p k s", k=8)

        def b4(i):
            return B[i][:].rearrange("p (h s) -> p h s", h=4)

        def A0(t):
            p = t % 2
            v = SV[p]
            self.act(hb[p], self.x[:, t, :], AF.Square, R=[("x", t)], W=[("hb", p)], accum_out=v["ssA"])
            self.act(v["lnA"], v["ssA"], AF.Ln, R=[("hb", p)], W=[("lnA", p)], scale=1.0 / D, bias=EPS)
            self.act(v["rsA"], v["lnA"], AF.Exp, R=[("lnA", p)], W=[("rsA", p)], scale=-0.5)
            self.stt("dve", hb[p], self.x[:, t, :], v["rsA"], self.gain_bc[gi][:], ALU.mult, ALU.mult,
                     R=[("x", t), ("rsA", p), ("gain", gi)], W=[("hb", p)])

        def A1(t):
            p = t % 2
            for kc in range(KC):
                self.tr(psT[:, kc * 128:(kc + 1) * 128], hb[p][:, kc * 128:(kc + 1) * 128], self.ident_bf[:],
                        R=[("hb", p), "ident_bf"], W=[("B", 7)])
            self.act(hTt[p], psT3, AF.Copy, R=[("B", 7)], W=[("hTt", p)])

        def proj(t, cb, bank):
            p = t % 2
            for kc in range(KC):
                self.mm(B[bank][:], hTt[p][:, kc, :], win[:, kc, cb * 512:(cb + 1) * 512], kc == 0, kc == KC - 1,
                        R=[("hTt", p), ("win", pkey[cb])], W=[("B", bank)])

        def A2(t, late_fn=None):
            p = t % 2
            v = SV[p]
            proj(t, 0, 4)
            self.act(q_bf, B[4][:], AF.Copy, R=[("B", 4)], W=["q_bf"])
            proj(t, 1, 5)
            for kc in range(KC):
                self.mm(B[6][:, 0:16], hTt[p][:, kc, :], win[:, kc, 3072:3088], kc == 0, kc == KC - 1,
                        R=[("hTt", p), ("win", 1)], W=[("B", 6)])
            self.act(k_bf, B[5][:], AF.Copy, R=[("B", 5)], W=["k_bf"], scale=0.125)
            k4 = B[5][:].rearrange("p (j two c) -> p j two c", two=2, c=64)
            kE4 = kE[p].rearrange("p (j two c) -> p j two c", two=2, c=64)
            kO4 = kO[p].rearrange("p (j two c) -> p j two c", two=2, c=64)
            self.ts("dve", kE4[:, :, 0, :], k4[:, :, 0, :], 0.125, None, ALU.mult, R=[("B", 5)], W=[("kE", p)])
            self.ts("dve", kO4[:, :, 1, :], k4[:, :, 1, :], 0.125, None, ALU.mult, R=[("B", 5)], W=[("kO", p)])
            self.tt("dve", v["g1"], B[6][:, 0:16], bias_bc, ALU.add, R=[("B", 6), "gbias"], W=[("g1", p)])
            self.act(v["e2"], v["g1"], AF.Exp, R=[("g1", p)], W=[("e2", p)], scale=2.0 / 15.0)
            self.ts("dve", v["dd"], v["e2"], 1.0, None, ALU.add, R=[("e2", p)], W=[("dd", p)])
            self.P.add("dve", (lambda o_, i_: (lambda e: e.reciprocal(out=o_, in_=i_)))(v["dd"], v["dd"]), R=[("dd", p)], W=[("dd", p)])
            self.ts("dve", v["gs"], v["dd"], -30.0, 15.0, ALU.mult, ALU.add, R=[("dd", p)], W=[("gs", p)])
            self.act(v["ee"], v["gs"][:, 8:16], AF.Exp, R=[("gs", p)], W=[("ee", p)], scale=-1.0)
            self.act(v["lfn"], v["ee"], AF.Ln, R=[("ee", p)], W=[("lfn", p)], bias=1.0)
            if late_fn is not None:
                late_fn()
            self.mm(B[6][:, 16:24], self.negtri[:], v["lfn"], True, True, R=[("lfn", p), "negtri"], W=[("B", 6)])
            self.mm(B[6][:, 24:32], self.negones[:], v["lfn"], True, True, R=[("lfn", p), "negones"], W=[("B", 6)])
            self.tt("dve", v["a"], v["gs"][:, 0:8], B[6][:, 16:24], ALU.subtract, R=[("gs", p), ("B", 6)], W=[("a", p)])
            self.act(v["eb"], B[6][:, 16:24], AF.Exp, R=[("B", 6)], W=[("eb", p)])
            bl2 = B[6][:, 24:32].rearrange("p (j two) -> p j two", two=2)
            self.copy("dve", v["gp"][0:64, :], bl2[0:64, :, 0], R=[("B", 6)], W=[("gp0", p)])
            self.copy("dve", v["gp"][64:128, :], bl2[64:128, :, 1], R=[("B", 6)], W=[("gp1", p)])
            self.act(v["ea"], v["a"], AF.Exp, R=[("a", p)], W=[("ea", p)])
            self.copy("dve", eabf[p][:, 0:8], v["ea"], R=[("ea", p)], W=[("eabf", p)])
            self.act(v["gsel"], v["gp"], AF.Exp, R=[("gp0", p), ("gp1", p)], W=[("gsel", p)])

        def A3(t):
            p = t % 2
            v = SV[p]
            for j in range(4):
                self.tr(psT[:, j * 128:(j + 1) * 128], q_bf[:, j * 128:(j + 1) * 128], self.ident_bf[:],
                        R=["q_bf", "ident_bf"], W=[("B", 7)])
            for j in range(4):
                self.tr(psT[:, (4 + j) * 128:(5 + j) * 128], k_bf[:, j * 128:(j + 1) * 128], self.ident_bf[:],
                        R=["k_bf", "ident_bf"], W=[("B", 7)])
            self.act(qkT[p], psT3, AF.Copy, R=[("B", 7)], W=[("qkT", p)])
            for i in range(2):
                proj(t, 2 + i, 4 + i)
                self.tt("dve", vs[p][:, 4 * i:4 * i + 4, :], b4(4 + i),
                        v["ea"][:, 4 * i:4 * i + 4].unsqueeze(2).to_broadcast([128, 4, 128]), ALU.mult,
                        R=[("B", 4 + i), ("ea", p)], W=[("vs", p, i)])
            for i in range(2):
                proj(t, 4 + i, 4 + i)
                self.act(eo[p][:, i * 512:(i + 1) * 512], B[4 + i][:], AF.Exp, R=[("B", 4 + i)], W=[("eo", p)], scale=-1.0)

        def A3b(t):
            p = t % 2
            self.act(eo[p], eo[p], AF.Ln, R=[("eo", p)], W=[("eo", p)], bias=1.0)
            self.act(eo[p], eo[p], AF.Exp, R=[("eo", p)], W=[("eo", p)], scale=-1.0)
            self.tt("dve", eo[p], eo[p], hgain, ALU.mult, R=[("eo", p), "hgain"], W=[("eo", p)])

        def B1(t):
            p = t % 2
            for h in range(H):
                p0 = (h % 2) * 64
                self.mm(b4(h % 2)[:, h // 2, :], qkT[p][p0:p0 + 64, 4 + h // 2, :], qkT[p][p0:p0 + 64, h // 2, :], True, True,
                        R=[("qkT", p)], W=[("B", h % 2)])
            PT4 = PT.rearrange("p (j two) s -> p j two s", two=2)
            for i in range(2):
                self.tt("dve", PT4[:, :, i, :], b4(i), self.causal[:].unsqueeze(1).to_broadcast([128, 4, 128]), ALU.mult,
                        R=[("B", i), "causal"], W=[("PT", i)])

        def B2(t):
            p = t % 2
            v = SV[p]
            cb_ = Cbf[p]
            for h in range(H):
                p0 = (h % 2) * 64
                qTh = qkT[p][p0:p0 + 64, h // 2, :]
                self.mm(b4(2 + h // 4)[:, h % 4, :], PT[:, h, :], vs[p][:, h, :], True, False,
                        R=[("PT", h % 2), ("vs", p, h // 4)], W=[("B", 2 + h // 4)])
                self.mm(b4(2 + h // 4)[:, h % 4, :], qTh, cb_[p0:p0 + 64, h // 2, 0:128], False, True,
                        R=[("qkT", p), ("Cbf", p)], W=[("B", 2 + h // 4)])
                self.mm(B[6][:, 32 + h:33 + h], PT[:, h, :], eabf[p][:, h:h + 1], True, False,
                        R=[("PT", h % 2), ("eabf", p)], W=[("B", 6)])
                self.mm(B[6][:, 32 + h:33 + h], qTh, cb_[p0:p0 + 64, h // 2, 128:129], False, True,
                        R=[("qkT", p), ("Cbf", p)], W=[("B", 6)])
            for j in range(4):
                self.mm(b4(0)[:, j, :], kE[p][:, j * 128:(j + 1) * 128], vs[p][:, 2 * j, :], True, False,
                        R=[("kE", p), ("vs", p, j // 2)], W=[("B", 0)])
                self.mm(b4(0)[:, j, :], kO[p][:, j * 128:(j + 1) * 128], vs[p][:, 2 * j + 1, :], False, True,
                        R=[("kO", p), ("vs", p, j // 2)], W=[("B", 0)])
                self.mm(B[6][:, 40 + j:41 + j], kE[p][:, j * 128:(j + 1) * 128], eabf[p][:, 2 * j:2 * j + 1], True, False,
                        R=[("kE", p), ("eabf", p)], W=[("B", 6)])
                self.mm(B[6][:, 40 + j:41 + j], kO[p][:, j * 128:(j + 1) * 128], eabf[p][:, 2 * j + 1:2 * j + 2], False, True,
                        R=[("kO", p), ("eabf", p)], W=[("B", 6)])
            self.tt("dve", d1, B[6][:, 32:40], v["eb"], ALU.mult, R=[("B", 6), ("eb", p)], W=["d1"])
            self.stt("dve", d2, d1, -1.0, d1, ALU.mult, ALU.max, R=["d1"], W=["d2"])
            self.ts("dve", d2, d2, 1.0, None, ALU.max, R=["d2"], W=["d2"])
            self.P.add("dve", lambda e: e.reciprocal(out=d2, in_=d2), R=["d2"], W=["d2"])
            self.tt("dve", scl, d2, v["eb"], ALU.mult, R=["d2", ("eb", p)], W=["scl"])
            hc3 = hcs[p].rearrange("p (h s) -> p h s", h=H)
            for i in range(2):
                self.tt("dve", hc3[:, 4 * i:4 * i + 4, :], b4(2 + i),
                        scl[:, 4 * i:4 * i + 4].unsqueeze(2).to_broadcast([128, 4, 128]), ALU.mult,
                        R=[("B", 2 + i), "scl"], W=[("hc", p)])
            self.tt("dve", C32[:, :, 0:128], C32[:, :, 0:128], b4(0), ALU.add, R=["C32", ("B", 0)], W=["C32"])
            self.tt("dve", C32[:, :, 128], C32[:, :, 128], B[6][:, 40:44], ALU.add, R=["C32", ("B", 6)], W=["C32"])
            self.tt("dve", C32, C32, v["gsel"].unsqueeze(2).to_broadcast([128, 4, 132]), ALU.mult, R=["C32", ("gsel", p)], W=["C32"])
            self.act(Cbf[1 - p], C32, AF.Copy, R=["C32"], W=[("Cbf", 1 - p)])

        def B3(t):
            p = t % 2
            hc = hcs[p]
            hc3 = hc.rearrange("p (h s) -> p h s", h=H)
            self.act(sqb, hc, AF.Square, R=[("hc", p)], W=["sqb"])
            self.P.add("dve", lambda e: e.reduce_sum(out=ssh, in_=sqb.rearrange("p (h s) -> p h s", h=H), axis=AX.X),
                       R=["sqb"], W=["ssh"])
            self.act(l2, ssh, AF.Ln, R=["ssh"], W=["l2"], scale=1.0 / 128.0, bias=EPS)
            self.act(rs, l2, AF.Exp, R=["l2"], W=["rs"], scale=-0.5)
            self.tt("dve", hc3, hc3, rs.unsqueeze(2).to_broadcast([128, H, 128]), ALU.mult,
                    R=[("hc", p), "rs"], W=[("hc", p)])
            self.tt("dve", outbs[p], hc, eo[p], ALU.mult, R=[("hc", p), ("eo", p)], W=[("outb", p)])

        def B3pe(t):
            p = t % 2
            for j in range(8):
                self.tr(psT[:, j * 128:(j + 1) * 128], outbs[p][:, j * 128:(j + 1) * 128], self.ident_bf[:],
                        R=[("outb", p), "ident_bf"], W=[("B", 7)])
            self.act(outTs[p], psT3, AF.Copy, R=[("B", 7)], W=[("outT", p)])

        def B4(t):
            p = t % 2
            for half in range(2):
                bk = half
                for vc in range(8):
                    self.mm(B[bk][:], outTs[p][:, vc, :], wout[:, vc, half * 512:(half + 1) * 512], vc == 0, vc == 7,
                            R=[("outT", p), "wout"], W=[("B", bk)])
                xs = self.x[:, t, half * 512:(half + 1) * 512]
                self.tt("dve", xs, xs, B[bk][:], ALU.add, R=[("B", bk), ("x", t)], W=[("x", t)])

        A0(0)
        if NTL > 1:
            A0(1)
        A1(0)
        A2(0)
        A3(0)
        for t in range(NTL + 1):
            cur = t < NTL
            nxt = t + 1 < NTL
            late = t >= 1
            if cur:
                B1(t)
            if nxt:
                A1(t + 1)
            if t + 2 < NTL:
                A0(t + 2)
            if late:
                B3(t - 1)
            if cur:
                B2(t)
            if late:
                B3pe(t - 1)
            if nxt:
                A2(t + 1, late_fn=(lambda tt=t: B4(tt - 1)) if late else None)
            elif late:
                B4(t - 1)
            if nxt:
                A3(t + 1)
            if cur:
                A3b(t)

    def mlstm_phase_v1(self, l):
        P = self.P
        slot = l // 2
        P.barrier()
        self.off = 0
        hT = self.carve([128, KC, S], BF16)
        win = self.carve([128, KC, 3088], BF16)
        wout = self.carve([128, KC, D], BF16)
        hb = [self.carve([128, D], BF16) for _ in range(2)]
        q_bf = self.carve([128, 512], BF16)
        k_bf = self.carve([128, 512], BF16)
        kE = self.carve([128, 512], BF16)
        kO = self.carve([128, 512], BF16)
        qkT = self.carve([128, 8, 128], BF16)
        vs = self.carve([128, H, 128], BF16)
        PT = self.carve([128, H, 128], BF16)
        sig = self.carve([128, D], F32)
        hc = self.carve([128, D], F32)
        outT = self.carve([128, 8, 128], BF16)
        C32 = self.carve([128, 4, 132], F32)
        Cbf = self.carve([128, 4, 132], BF16)
        hgain = self.carve([128, D], F32)
        bias_bc = self.carve([128, 16], F32)
        eabf = self.carve([128, 8], BF16)
        jk = self.carve([128, 128], BF16)
        outb = hb[0]
        sm = self.small
        g1, th, gs, ee, lfn = sm[:, 64:80], sm[:, 80:96], sm[:, 96:112], sm[:, 112:120], sm[:, 120:128]
        a_, ea, eb, gp, gsel = sm[:, 128:136], sm[:, 136:144], sm[:, 144:152], sm[:, 152:156], sm[:, 156:160]
        d1, d2, scl, ssh, l2, rs = sm[:, 160:168], sm[:, 168:176], sm[:, 176:184], sm[:, 184:192], sm[:, 192:200], sm[:, 200:208]
        B = self.B

        w_in = self.din("mlstm_w_in", [2, D, 3088])[slot].rearrange("(k p) n -> p k n", p=128)
        w_out = self.din("mlstm_w_out", [2, D, D])[slot].rearrange("(k p) n -> p k n", p=128)
        pieces = [(0, 1024), (1024, 2048), (2048, 3072), (3072, 3088)]
        for i, (c0, c1) in enumerate(pieces):
            self.dma("pool", win[:, :, c0:c1], w_in[:, :, c0:c1], slot=("win", i), W=[("win", i)])
        self.dma("pool", wout, w_out, slot=("wout",), W=["wout"])
        self.dma("sp", hgain, self.din("mlstm_head_gain", [2, D])[slot:slot + 1, :].partition_broadcast(128),
                 slot=("hgain",), W=["hgain"])
        self.dma("sp", bias_bc, self.din("mlstm_gate_bias", [2, 16])[slot:slot + 1, :].partition_broadcast(128),
                 slot=("gbias",), W=["gbias"])
        self.memset("dve", C32, 0.0, W=["C32"])
        self.memset("dve", Cbf, 0.0, W=["Cbf"])
        self.memset("dve", kE, 0.0, W=["kE"])
        self.memset("dve", kO, 0.0, W=["kO"])

        self.norm_to_hT(self.din("norm_mix", [DEPTH, D])[l:l + 1, :], hT, hb)

        psT = B[7][:].bitcast(BF16)
        psT3 = psT.rearrange("p (k s) -> p k s", k=8)

        def b4(i):
            return B[i][:].rearrange("p (h s) -> p h s", h=4)

        wpiece = {0: 0, 1: 0, 2: 1, 3: 1, 4: 2, 5: 2}
        for t in range(self.ntiles):
            tok = slice(t * 128, (t + 1) * 128)
            for cb in range(6):
                for kc in range(KC):
                    self.mm(B[cb][:], hT[:, kc, tok], win[:, kc, cb * 512:(cb + 1) * 512], kc == 0, kc == KC - 1,
                            R=[("hT", t), ("win", wpiece[cb])], W=[("B", cb)])
            for kc in range(KC):
                self.mm(B[6][:, 0:16], hT[:, kc, tok], win[:, kc, 3072:3088], kc == 0, kc == KC - 1,
                        R=[("hT", t), ("win", 3)], W=[("B", 6)])
            self.tt("dve", g1, B[6][:, 0:16], bias_bc, ALU.add, R=[("B", 6), "gbias"], W=["g1"])
            self.act(th, g1, AF.Tanh, R=["g1"], W=["th"], scale=1.0 / 15.0)
            self.ts("dve", gs, th, 15.0, None, ALU.mult, R=["th"], W=["gs"])
            self.act(ee, gs[:, 8:16], AF.Exp, R=["gs"], W=["ee"], scale=-1.0)
            self.act(lfn, ee, AF.Ln, R=["ee"], W=["lfn"], bias=1.0)
            self.mm(B[6][:, 16:24], self.negtri[:], lfn, True, True, R=["lfn", "negtri"], W=[("B", 6)])
            self.mm(B[6][:, 24:32], self.negones[:], lfn, True, True, R=["lfn", "negones"], W=[("B", 6)])
            self.tt("dve", a_, gs[:, 0:8], B[6][:, 16:24], ALU.subtract, R=["gs", ("B", 6)], W=["a"])
            self.act(ea, a_, AF.Exp, R=["a"], W=["ea"])
            self.copy("dve", eabf, ea, R=["ea"], W=["eabf"])
            self.act(eb, B[6][:, 16:24], AF.Exp, R=[("B", 6)], W=["eb"])
            bl2 = B[6][:, 24:32].rearrange("p (j two) -> p j two", two=2)
            self.copy("dve", gp[0:64, :], bl2[0:64, :, 0], R=[("B", 6)], W=["gp0"])
            self.copy("dve", gp[64:128, :], bl2[64:128, :, 1], R=[("B", 6)], W=["gp1"])
            self.act(gsel, gp, AF.Exp, R=["gp0", "gp1"], W=["gsel"])
            self.act(q_bf, B[0][:], AF.Copy, R=[("B", 0)], W=["q_bf"])
            self.act(k_bf, B[1][:], AF.Copy, R=[("B", 1)], W=["k_bf"], scale=0.125)
            k4 = B[1][:].rearrange("p (j two c) -> p j two c", two=2, c=64)
            kE4 = kE.rearrange("p (j two c) -> p j two c", two=2, c=64)
            kO4 = kO.rearrange("p (j two c) -> p j two c", two=2, c=64)
            self.ts("dve", kE4[:, :, 0, :], k4[:, :, 0, :], 0.125, None, ALU.mult, R=[("B", 1)], W=["kE"])
            self.ts("dve", kO4[:, :, 1, :], k4[:, :, 1, :], 0.125, None, ALU.mult, R=[("B", 1)], W=["kO"])
            for j in range(4):
                self.tr(psT[:, j * 128:(j + 1) * 128], q_bf[:, j * 128:(j + 1) * 128], self.ident_bf[:],
                        R=["q_bf", "ident_bf"], W=[("B", 7)])
            for j in range(4):
                self.tr(psT[:, (4 + j) * 128:(5 + j) * 128], k_bf[:, j * 128:(j + 1) * 128], self.ident_bf[:],
                        R=["k_bf", "ident_bf"], W=[("B", 7)])
            self.act(qkT, psT3, AF.Copy, R=[("B", 7)], W=["qkT"])
            for i in range(2):
                self.tt("dve", vs[:, 4 * i:4 * i + 4, :], b4(2 + i),
                        ea[:, 4 * i:4 * i + 4].unsqueeze(2).to_broadcast([128, 4, 128]), ALU.mult,
                        R=[("B", 2 + i), "ea"], W=[("vs", i)])
            for i in range(2):
                self.act(sig[:, i * 512:(i + 1) * 512], B[4 + i][:], AF.Tanh, R=[("B", 4 + i)], W=[("sig", i)], scale=0.5)
            for h in range(H):
                p0 = (h % 2) * 64
                self.mm(b4(h % 2)[:, h // 2, :], qkT[p0:p0 + 64, 4 + h // 2, :], qkT[p0:p0 + 64, h // 2, :], True, True,
                        R=["qkT"], W=[("B", h % 2)])
            PT4 = PT.rearrange("p (j two) s -> p j two s", two=2)
            for i in range(2):
                self.tt("dve", PT4[:, :, i, :], b4(i),
                        self.causal[:].unsqueeze(1).to_broadcast([128, 4, 128]), ALU.mult,
                        R=[("B", i), "causal"], W=[("PT", i)])
            for h in range(H):
                p0 = (h % 2) * 64
                qTh = qkT[p0:p0 + 64, h // 2, :]
                self.mm(b4(2 + h // 4)[:, h % 4, :], PT[:, h, :], vs[:, h, :], True, False,
                        R=[("PT", h % 2), ("vs", h // 4)], W=[("B", 2 + h // 4)])
                self.mm(b4(2 + h // 4)[:, h % 4, :], qTh, Cbf[p0:p0 + 64, h // 2, 0:128], False, True,
                        R=["qkT", "Cbf"], W=[("B", 2 + h // 4)])
                self.mm(B[6][:, 32 + h:33 + h], PT[:, h, :], eabf[:, h:h + 1], True, False,
                        R=[("PT", h % 2), "eabf"], W=[("B", 6)])
                self.mm(B[6][:, 32 + h:33 + h], qTh, Cbf[p0:p0 + 64, h // 2, 128:129], False, True,
                        R=["qkT", "Cbf"], W=[("B", 6)])
            for j in range(4):
                self.mm(b4(4)[:, j, :], kE[:, j * 128:(j + 1) * 128], vs[:, 2 * j, :], True, False,
                        R=["kE", ("vs", j // 2)], W=[("B", 4)])
                self.mm(b4(4)[:, j, :], kO[:, j * 128:(j + 1) * 128], vs[:, 2 * j + 1, :], False, True,
                        R=["kO", ("vs", j // 2)], W=[("B", 4)])
                self.mm(B[6][:, 40 + j:41 + j], kE[:, j * 128:(j + 1) * 128], eabf[:, 2 * j:2 * j + 1], True, False,
                        R=["kE", "eabf"], W=[("B", 6)])
                self.mm(B[6][:, 40 + j:41 + j], kO[:, j * 128:(j + 1) * 128], eabf[:, 2 * j + 1:2 * j + 2], False, True,
                        R=["kO", "eabf"], W=[("B", 6)])
            self.tt("dve", C32[:, :, 0:128], C32[:, :, 0:128], b4(4), ALU.add, R=["C32", ("B", 4)], W=["C32"])
            self.tt("dve", C32[:, :, 128], C32[:, :, 128], B[6][:, 40:44], ALU.add, R=["C32", ("B", 6)], W=["C32"])
            self.tt("dve", C32, C32, gsel.unsqueeze(2).to_broadcast([128, 4, 132]), ALU.mult, R=["C32", "gsel"], W=["C32"])
            self.act(Cbf, C32, AF.Copy, R=["C32"], W=["Cbf"])
            self.tt("dve", d1, B[6][:, 32:40], eb, ALU.mult, R=[("B", 6), "eb"], W=["d1"])
            self.stt("dve", d2, d1, -1.0, d1, ALU.mult, ALU.max, R=["d1"], W=["d2"])
            self.ts("dve", d2, d2, 1.0, None, ALU.max, R=["d2"], W=["d2"])
            self.P.add("dve", lambda e: e.reciprocal(out=d2, in_=d2), R=["d2"], W=["d2"])
            self.tt("dve", scl, d2, eb, ALU.mult, R=["d2", "eb"], W=["scl"])
            hc3 = hc.rearrange("p (h s) -> p h s", h=H)
            for i in range(2):
                self.tt("dve", hc3[:, 4 * i:4 * i + 4, :], b4(2 + i),
                        scl[:, 4 * i:4 * i + 4].unsqueeze(2).to_broadcast([128, 4, 128]), ALU.mult,
                        R=[("B", 2 + i), "scl"], W=[("hc", i)])
            for h in range(H):
                self.act(jk, hc3[:, h, :], AF.Square, R=[("hc", h // 4)], W=["jk", "ssh"], accum_out=ssh[:, h:h + 1])
            self.act(l2, ssh, AF.Ln, R=["ssh"], W=["l2"], scale=1.0 / 128.0, bias=EPS)
            self.act(rs, l2, AF.Exp, R=["l2"], W=["rs"], scale=-0.5, bias=float(np.log(0.5)))
            self.tt("dve", hc3, hc3, rs.unsqueeze(2).to_broadcast([128, H, 128]), ALU.mult,
                    R=[("hc", 0), ("hc", 1), "rs"], W=[("hc", 0), ("hc", 1)])
            self.tt("pool", hc, hc, hgain, ALU.mult, R=[("hc", 0), ("hc", 1), "hgain"], W=[("hc", 0), ("hc", 1)])
            self.stt("dve", outb, sig, 1.0, hc, ALU.add, ALU.mult,
                     R=[("sig", 0), ("sig", 1), ("hc", 0), ("hc", 1)], W=[("hb", 0)])
            for j in range(8):
                self.tr(psT[:, j * 128:(j + 1) * 128], outb[:, j * 128:(j + 1) * 128], self.ident_bf[:],
                        R=[("hb", 0), "ident_bf"], W=[("B", 7)])
            self.act(outT, psT3, AF.Copy, R=[("B", 7)], W=["outT"])
            for half in range(2):
                bk = 5 if half == 0 else 1
                for vc in range(8):
                    self.mm(B[bk][:], outT[:, vc, :], wout[:, vc, half * 512:(half + 1) * 512], vc == 0, vc == 7,
                            R=["outT", "wout"], W=[("B", bk)])
                xs = self.x[:, t, half * 512:(half + 1) * 512]
                self.tt("dve", xs, xs, B[bk][:], ALU.add, R=[("B", bk), ("x", t)], W=[("x", t)])

    def pool_phase(self, l):
        P = self.P
        slot = l // 2
        P.barrier()
        self.off = 0
        hT32 = self.carve([128, KC, S], F32)
        yT = self.carve([128, KC, S], BF16)
        sbuf_ = [self.carve([128, S], F32) for _ in range(2)]
        wp = self.carve([128, 4, 2, 256], BF16)
        pscale = self.carve([128, D], F32)
        h32 = [self.carve([128, D], F32) for _ in range(2)]
        t16 = self.carve([128, 16], F32)
        B = self.B
        wsrc = self.din("pool_w_group", [2, 4, 256, 256])[slot]
        for g in range(4):
            self.dma("pool", wp[:, g, :, :], wsrc[g].rearrange("(k p) d -> p k d", p=128), slot=("wp", g), W=[("wp", g)])
        self.dma("sp", pscale, self.din("pool_scale", [2, D])[slot:slot + 1, :].partition_broadcast(128),
                 slot=("pscale",), W=["pscale"])
        gi = self.load_gain(self.din("norm_mix", [DEPTH, D])[l:l + 1, :])
        self.norm_stats(h32[0])
        for t in range(NT):
            ht = h32[t % 2]
            self.stt("dve", ht, self.x[:, t, :], self.rstd[:, t:t + 1], self.gain_bc[gi][:], ALU.mult, ALU.mult,
                     R=[("x", t), "rstd", ("gain", gi)], W=[("h32", t % 2), "junk"] if t % 2 == 0 else [("h32", t % 2)])
            for kc in range(KC):
                bk = 6 + kc // 4
                self.tr(B[bk][:, (kc % 4) * 128:(kc % 4 + 1) * 128], ht[:, kc * 128:(kc + 1) * 128], self.ident_f[:],
                        R=[("h32", t % 2), "ident_f"], W=[("B", bk)])
            self.act(hT32[:, 0:4, t * 128:(t + 1) * 128], B[6][:].rearrange("p (k s) -> p k s", k=4), AF.Copy,
                     R=[("B", 6)], W=[("hT32", c) for c in range(0, 4)])
            self.copy("dve", hT32[:, 4:8, t * 128:(t + 1) * 128], B[7][:].rearrange("p (k s) -> p k s", k=4),
                      R=[("B", 7)], W=[("hT32", c) for c in range(4, 8)])
        for c in range(KC):
            wi = c // 2
            win_ = WINS[wi]
            src = hT32[:, c, :]
            cur, ckey = src, ("hT32", c)
            sh, n = 1, 0
            while sh < win_:
                dst = sbuf_[n % 2]
                dkey = ("sb", n % 2)
                self.tt("dve", dst[:, sh:], cur[:, sh:], cur[:, 0:S - sh], ALU.add, R=[ckey], W=[dkey])
                self.copy("dve", dst[:, 0:sh], cur[:, 0:sh], R=[ckey], W=[dkey])
                cur, ckey = dst, dkey
                sh *= 2
                n += 1
            self.stt("dve", yT[:, c, :], cur, 1.0 / win_, src, ALU.mult, ALU.subtract, R=[ckey, ("hT32", c)], W=[("yT", c)])
            self.tt("dve", t16, cur[:, 0:16], self.invcnt[:, wi, :], ALU.mult, R=[ckey, "invcnt"], W=["t16"])
            self.tt("dve", yT[:, c, 0:16], t16, src[:, 0:16], ALU.subtract, R=["t16", ("hT32", c)], W=[("yT", c)])
        for t in range(NT):
            tok = slice(t * 128, (t + 1) * 128)
            for g in range(4):
                bk = (t % 2) * 2 + g // 2
                for kc in range(2):
                    self.mm(B[bk][:, (g % 2) * 256:(g % 2 + 1) * 256], yT[:, 2 * g + kc, tok], wp[:, g, kc, :], kc == 0, kc == 1,
                            R=[("yT", 2 * g + kc), ("wp", g)], W=[("B", bk)])
            for half in range(2):
                bk = (t % 2) * 2 + half
                tmp = h32[half][:, 0:512]
                self.tt("dve", tmp, B[bk][:], pscale[:, half * 512:(half + 1) * 512], ALU.mult,
                        R=[("B", bk), "pscale"], W=[("h32", half)])
                xs = self.x[:, t, half * 512:(half + 1) * 512]
                self.tt("dve", xs, xs, tmp, ALU.add, R=[("h32", half), ("x", t)], W=[("x", t)])

    def ffn_phase(self, l):
        P = self.P
        P.barrier()
        self.off = 0
        hT = self.carve([128, KC, S], BF16)
        abuf = [self.carve([128, 4, S], BF16) for _ in range(2)]
        wgu = [self.carve([128, KC, 2, 256], BF16) for _ in range(3)]
        wd = [self.carve([128, 4, D], BF16) for _ in range(2)]
        hb = [self.carve([128, D], BF16) for _ in range(2)]
        sl = [self.carve([128, 512], BF16) for _ in range(2)]
        B = self.B
        wgu_src = self.din("ffn_w_gate_up", [DEPTH, D, 2 * DFF])[l].rearrange("(k p) n -> p k n", p=128)
        wd_src = self.din("ffn_w_down", [DEPTH, DFF, D])[l].rearrange("(j p) d -> p j d", p=128)
        groups = [(j0, min(4, NJ - j0)) for j0 in range(0, NJ, 4)]

        def load_gu(n):
            s_ = n % 3
            self.dma("pool", wgu[s_][:, :, 0, :], wgu_src[:, :, n * 256:(n + 1) * 256], slot=("wg", s_), W=[("wg", s_)])
            self.dma("pool", wgu[s_][:, :, 1, :], wgu_src[:, :, DFF + n * 256:DFF + (n + 1) * 256], slot=("wu", s_), W=[("wu", s_)])

        def load_d(gi):
            j0, nj = groups[gi]
            self.dma("pool", wd[gi % 2][:, 0:nj, :], wd_src[:, j0:j0 + nj, :], slot=("wd", gi % 2), W=[("wd", gi % 2)])

        load_gu(0)
        load_gu(1)
        load_gu(2)
        load_d(0)
        self.norm_to_hT(self.din("norm_ffn", [DEPTH, D])[l:l + 1, :], hT, hb)

        cnt = [0]

        def gateup(gi):
            j0, nj = groups[gi]
            ab = abuf[gi % 2]
            for jj in range(nj):
                j = j0 + jj
                n, sub = j // 2, j % 2
                s_ = n % 3
                for tg in range(4):
                    pr = cnt[0] % 2
                    cnt[0] += 1
                    bg, bu = B[2 * pr], B[2 * pr + 1]
                    rk = [("hT", 4 * tg + i) for i in range(4)]
                    for kc in range(KC):
                        self.mm(bg[:], wgu[s_][:, kc, 0, sub * 128:(sub + 1) * 128], hT[:, kc, tg * 512:(tg + 1) * 512],
                                kc == 0, kc == KC - 1, R=rk + [("wg", s_)], W=[("B", 2 * pr)])
                    for kc in range(KC):
                        self.mm(bu[:], wgu[s_][:, kc, 1, sub * 128:(sub + 1) * 128], hT[:, kc, tg * 512:(tg + 1) * 512],
                                kc == 0, kc == KC - 1, R=rk + [("wu", s_)], W=[("B", 2 * pr + 1)])
                    self.act(sl[pr], bg[:], AF.Silu, R=[("B", 2 * pr)], W=[("sl", pr)])
                    self.tt("dve", ab[:, jj, tg * 512:(tg + 1) * 512], sl[pr], bu[:], ALU.mult,
                            R=[("sl", pr), ("B", 2 * pr + 1)], W=[("ab", gi % 2, tg)])
                if sub == 1 or j == NJ - 1:
                    if n + 3 < NJ // 2:
                        load_gu(n + 3)

        def down(gi):
            j0, nj = groups[gi]
            ab = abuf[gi % 2]
            for t in range(NT):
                for half in range(2):
                    bk = 4 + (2 * t + half) % 4
                    for jj in range(nj):
                        self.mm(B[bk][:], ab[:, jj, t * 128:(t + 1) * 128], wd[gi % 2][:, jj, half * 512:(half + 1) * 512],
                                jj == 0, jj == nj - 1, R=[("ab", gi % 2, t // 4), ("wd", gi % 2)], W=[("B", bk)])
                    xs = self.x[:, t, half * 512:(half + 1) * 512]
                    self.tt("dve", xs, xs, B[bk][:], ALU.add, R=[("B", bk), ("x", t)], W=[("x", t)])
            if gi + 2 < len(groups):
                load_d(gi + 2)

        gateup(0)
        load_d(1)
        for gi in range(len(groups)):
            if gi + 1 < len(groups):
                gateup(gi + 1)
            down(gi)

    def final_phase(self):
        P = self.P
        P.barrier()
        self.off = 0
        ob = [self.carve([128, D], F32) for _ in range(2)]
        if self.final_norm:
            gi = self.load_gain(self.din("final_norm", [1, D])[0:1, :])
            self.norm_stats(ob[0])
        stores = []
        for t in range(NT):
            o = ob[t % 2]
            if self.final_norm:
                self.stt("dve", o, self.x[:, t, :], self.rstd[:, t:t + 1], self.gain_bc[gi][:], ALU.mult, ALU.mult,
                         R=[("x", t), "rstd", ("gain", gi)], W=[("ob", t % 2), "junk"] if t % 2 == 0 else [("ob", t % 2)])
                src, rk = o, [("ob", t % 2)]
            else:
                src, rk = self.x[:, t, :], [("x", t)]
            stores.append(self.dma("sp", self.out[t * 128:(t + 1) * 128, :], src, slot=("st", t), R=rk, W=[("outd", t)]))
        P.add("sp", None, extra=stores)


_CACHE = {}


def _get_prog(layers, final_norm):
    key = (tuple(layers), final_norm)
    if key not in _CACHE:
        b = Builder(layers, final_norm)
        nc = b.build()
        _CACHE[key] = (nc, sorted(b.dram.keys()))
    return _CACHE[key]


def _run(layers, final_norm, xs, inputs):
    nc, names = _get_prog(layers, final_norm)
    in_maps = []
    for c in range(N_CORES):
        m = {}
        for n in names:
            if n == "x":
                m[n] = np.ascontiguousarray(xs[c])
            elif n == "final_norm":
                m[n] = np.ascontiguousarray(inputs[n]).reshape(1, D)
            else:
                m[n] = np.ascontiguousarray(inputs[n])
        in_maps.append(m)
    res = run_bass_kernel_spmd(nc, in_maps, core_ids=list(range(N_CORES)))
    return [res.results[c]["out"] for c in range(N_CORES)]


LAUNCH_GROUPS = [[0, 1, 2, 3]]


def kernel(**inputs):
    inputs = {k: np.asarray(v, dtype=np.float32) for k, v in inputs.items()}
    xs = [inputs["x"][b] for b in range(N_CORES)]
    for gi, grp in enumerate(LAUNCH_GROUPS):
        xs = _run(grp, gi == len(LAUNCH_GROUPS) - 1, xs, inputs)
    return np.stack(xs, axis=0).astype(np.float32)
```

```python
import numpy as np
from contextlib import ExitStack
import concourse.bass as bass
import concourse.mybir as mybir
from concourse.bass_utils import run_bass_kernel_spmd

F32 = mybir.dt.float32
BF16 = mybir.dt.bfloat16
U8 = mybir.dt.uint8
AF = mybir.ActivationFunctionType
ALU = mybir.AluOpType
AX = mybir.AxisListType

S = 2048
D = 1024
NT = 16
KC = 8
H = 8
DFF = 2816
NJ = 22
EPS = 1e-6
N_CORES = 8
DEPTH = 4
WINS = (2, 4, 8, 16)


class _Op:
    __slots__ = ("eng", "fn", "waits", "sig", "pos", "gid", "slot", "cnt", "known", "sidx")


class Prog:
    ENGS = ("pe", "act", "dve", "pool", "sp")

    def __init__(self, self_sync=True):
        self.streams = {e: [] for e in self.ENGS}
        self.known = {e: {} for e in self.ENGS}
        self.last_w = {}
        self.readers = {}
        self.slot_cnt = {}
        self.slot_last = {}
        self.gid = 0
        self.self_sync = self_sync

    def add(self, eng, fn, R=(), W=(), slot=None, extra=()):
        op = _Op()
        op.eng, op.fn, op.waits, op.sig, op.slot, op.cnt, op.sidx = eng, fn, [], False, slot, 0, 0
        op.gid = self.gid
        self.gid += 1
        st = self.streams[eng]
        op.pos = len(st)
        st.append(op)
        psum_r = [k for k in R if isinstance(k, tuple) and k[0] == "B"]
        if psum_r:
            R = [k for k in R if not (isinstance(k, tuple) and k[0] == "B")]
            W = list(W) + psum_r
        deps = {}
        for k in R:
            lw = self.last_w.get(k)
            if lw is not None:
                deps[lw.gid] = lw
        for k in W:
            lw = self.last_w.get(k)
            if lw is not None:
                deps[lw.gid] = lw
            for r in self.readers.get(k, ()):
                deps[r.gid] = r
        for d in extra:
            deps[d.gid] = d
        kn = self.known[eng]
        for g in sorted(deps, reverse=True):
            d = deps[g]
            if d.slot is not None:
                src, val = d.slot, d.cnt
            else:
                if d.eng == eng and (eng == "pe" or not self.self_sync):
                    continue
                src, val = d.eng, d.pos
            if kn.get(src, -1) >= val:
                continue
            assert d.fn is not None
            op.waits.append(d)
            d.sig = True
            for s_, v_ in d.known.items():
                if kn.get(s_, -1) < v_:
                    kn[s_] = v_
        if slot is not None:
            c = self.slot_cnt.get(slot, 0) + 1
            self.slot_cnt[slot] = c
            op.cnt = c
            prev = self.slot_last.get(slot)
            if prev is not None:
                assert kn.get(slot, -1) >= prev.cnt, ("two DMAs in flight on slot", slot)
            self.slot_last[slot] = op
            op.known = dict(kn)
            op.known[slot] = c
        else:
            op.known = dict(kn)
            op.known[eng] = op.pos
        for k in R:
            self.readers.setdefault(k, []).append(op)
        for k in W:
            self.last_w[k] = op
            self.readers[k] = []
        return op

    def barrier(self):
        lasts = []
        for e in self.ENGS:
            for op in reversed(self.streams[e]):
                if op.fn is not None and op.slot is None:
                    lasts.append(op)
                    break
        lasts += list(self.slot_last.values())
        for e in self.ENGS:
            self.add(e, None, extra=lasts)

    def emit(self, block, sems, slot_sems):
        for e in self.ENGS:
            n = 0
            for op in self.streams[e]:
                if op.slot is None and op.sig:
                    n += 1
                    op.sidx = n

        def body_for(e):
            st = self.streams[e]

            def body(eng):
                for op in st:
                    for d in op.waits:
                        if d.slot is not None:
                            eng.wait_ge(slot_sems[d.slot], 16 * d.cnt)
                        else:
                            eng.wait_ge(sems[d.eng], d.sidx)
                    if op.fn is not None:
                        ins = op.fn(eng)
                        if op.slot is not None:
                            ins.then_inc(slot_sems[op.slot], 16)
                        elif op.sig:
                            ins.then_inc(sems[e], 1)
            return body

        block.tensor(body_for("pe"))
        block.scalar(body_for("act"))
        block.vector(body_for("dve"))
        block.gpsimd(body_for("pool"))
        block.sync(body_for("sp"))


def _dsize(dt):
    return {F32: 4, BF16: 2, U8: 1}[dt]


class Builder:
    def __init__(self, layers, final_norm, self_sync=True, parts=("mixer", "ffn"), ntiles=NT):
        self.parts = parts
        self.ntiles = ntiles
        self.layers = list(layers)
        self.final_norm = final_norm
        self.nc = bass.Bass("TRN2", target_bir_lowering=False)
        self.P = Prog(self_sync=self_sync)
        self.dram = {}
        self.slots = set()

    def din(self, name, shape):
        if name not in self.dram:
            self.dram[name] = self.nc.dram_tensor(name, list(shape), F32, kind="ExternalInput").ap()
        return self.dram[name]

    def view(self, off, shape, dt):
        n = 1
        for s_ in shape[1:]:
            n *= s_
        nb = n * _dsize(dt)
        assert off % 32 == 0 and off + nb <= self.arena_bytes, (off, nb, self.arena_bytes)
        ap = self.arena[:, off:off + nb].bitcast(dt)
        if len(shape) == 3:
            ap = ap.rearrange("p (a b) -> p a b", a=shape[1])
        elif len(shape) == 4:
            ap = ap.rearrange("p (a b c) -> p a b c", a=shape[1], b=shape[2])
        return ap

    def carve(self, shape, dt):
        n = 1
        for s_ in shape[1:]:
            n *= s_
        nb = (n * _dsize(dt) + 31) // 32 * 32
        v = self.view(self.off, shape, dt)
        self.off += nb
        return v

    def dma(self, eng, out, in_, slot, R=(), W=()):
        self.slots.add(slot)
        return self.P.add(eng, lambda e: e.dma_start(out=out, in_=in_), R=R, W=W, slot=slot)

    def mm(self, out, lhsT, rhs, start, stop, R=(), W=()):
        return self.P.add("pe", lambda e: e.matmul(out, lhsT=lhsT, rhs=rhs, start=start, stop=stop), R=R, W=W)

    def tr(self, out, in_, ident, R=(), W=()):
        return self.P.add("pe", lambda e: e.transpose(out, in_, ident), R=R, W=W)

    def act(self, out, in_, func, R=(), W=(), **kw):
        return self.P.add("act", lambda e: e.activation(out=out, in_=in_, func=func, **kw), R=R, W=W)

    def tt(self, eng, out, in0, in1, op, R=(), W=()):
        return self.P.add(eng, lambda e: e.tensor_tensor(out=out, in0=in0, in1=in1, op=op), R=R, W=W)

    def ts(self, eng, out, in0, s1, s2, op0, op1=None, R=(), W=()):
        if op1 is None:
            return self.P.add(eng, lambda e: e.tensor_scalar(out=out, in0=in0, scalar1=s1, scalar2=None, op0=op0), R=R, W=W)
        return self.P.add(eng, lambda e: e.tensor_scalar(out=out, in0=in0, scalar1=s1, scalar2=s2, op0=op0, op1=op1), R=R, W=W)

    def stt(self, eng, out, in0, scalar, in1, op0, op1, R=(), W=()):
        return self.P.add(eng, lambda e: e.scalar_tensor_tensor(out=out, in0=in0, scalar=scalar, in1=in1, op0=op0, op1=op1), R=R, W=W)

    def copy(self, eng, out, in_, R=(), W=()):
        return self.P.add(eng, lambda e: e.tensor_copy(out, in_), R=R, W=W)

    def memset(self, eng, ap, val, R=(), W=()):
        return self.P.add(eng, lambda e: e.memset(ap, val), R=R, W=W)

    def build(self):
        nc = self.nc
        P = self.P
        with ExitStack() as es:
            def sb(name, shape, dt):
                return es.enter_context(nc.sbuf_tensor(name, shape, dt))

            self.x = sb("x_res", [128, NT, D], F32)
            self.ident_bf = sb("ident_bf", [128, 128], BF16)
            self.ident_f = sb("ident_f", [128, 128], F32)
            self.causal = sb("causal", [128, 128], BF16)
            self.negtri = sb("negtri", [128, 128], F32)
            self.negones = sb("negones", [128, 128], F32)
            self.invcnt = sb("invcnt", [128, 4, 16], F32)
            self.small = sb("small", [128, 512], F32)
            self.gain_bc = [sb(f"gain_bc{i}", [128, D], F32) for i in range(2)]
            self.gain_n = 0
            self.arena_bytes = (nc.sbuf_bytes_remaining - 256) // 64 * 64
            self.arena = sb("arena", [128, self.arena_bytes], U8)
            self.B = [es.enter_context(nc.psum_tensor(f"B{i}", [128, 512], F32)) for i in range(8)]
            self.out = nc.dram_tensor("out", [S, D], F32, kind="ExternalOutput").ap()
            xin = self.din("x", [S, D])

            sm = self.small
            self.ss = sm[:, 0:16]
            self.lnv = sm[:, 16:32]
            self.rstd = sm[:, 32:48]

            self.setup_consts()
            for t in range(NT):
                self.dma("sp", self.x[:, t, :], xin[t * 128:(t + 1) * 128, :], slot=("xld", t), W=[("x", t)])

            for l in self.layers:
                if "mixer" in self.parts:
                    if l % 2 == 0:
                        self.mlstm_phase(l)
                    else:
                        self.pool_phase(l)
                if "ffn" in self.parts:
                    self.ffn_phase(l)
            self.final_phase()

            sems = {e: es.enter_context(nc.semaphore(f"s_{e}")) for e in Prog.ENGS}
            slot_sems = {}
            for i, sl in enumerate(sorted(self.slots, key=str)):
                slot_sems[sl] = es.enter_context(nc.semaphore(f"d_{i}"))
            block = es.enter_context(nc.Block())
            P.emit(block, sems, slot_sems)
        return nc

    def setup_consts(self):
        def sel(ap, cmp, pattern, cm, key):
            self.P.add("pool", lambda e: e.affine_select(out=ap, in_=ap, compare_op=cmp, fill=0.0, base=0,
                                                         pattern=pattern, channel_multiplier=cm), W=[key])
        self.memset("pool", self.ident_bf[:], 1.0, W=["ident_bf"])
        sel(self.ident_bf[:], ALU.is_equal, [[-1, 128]], 1, "ident_bf")
        self.memset("pool", self.ident_f[:], 1.0, W=["ident_f"])
        sel(self.ident_f[:], ALU.is_equal, [[-1, 128]], 1, "ident_f")
        self.memset("pool", self.causal[:], 1.0, W=["causal"])
        sel(self.causal[:], ALU.is_ge, [[1, 128]], -1, "causal")
        self.memset("pool", self.negtri[:], -1.0, W=["negtri"])
        sel(self.negtri[:], ALU.is_ge, [[1, 128]], -1, "negtri")
        self.memset("pool", self.negones[:], -1.0, W=["negones"])
        for wi, w in enumerate(WINS):
            self.memset("pool", self.invcnt[:, wi, :], 1.0 / w, W=["invcnt"])
            for t in range(w - 1):
                self.memset("pool", self.invcnt[:, wi, t:t + 1], 1.0 / (t + 1), W=["invcnt"])

    def load_gain(self, row_ap):
        i = self.gain_n % 2
        self.gain_n += 1
        self.dma("sp", self.gain_bc[i][:], row_ap.partition_broadcast(128), slot=("gain", i), W=[("gain", i)])
        return i

    def norm_stats(self, junk):
        for t in range(NT):
            self.act(junk, self.x[:, t, :], AF.Square, R=[("x", t)], W=["junk", "ss"], accum_out=self.ss[:, t:t + 1])
        self.act(self.lnv, self.ss, AF.Ln, R=["ss"], W=["lnv"], scale=1.0 / D, bias=EPS)
        self.act(self.rstd, self.lnv, AF.Exp, R=["lnv"], W=["rstd"], scale=-0.5)

    def norm_to_hT(self, gain_row, hT, hb):
        gi = self.load_gain(gain_row)
        self.norm_stats(hb[0])
        psT = self.B[7][:].bitcast(BF16)
        for t in range(NT):
            hbt = hb[t % 2]
            self.stt("dve", hbt, self.x[:, t, :], self.rstd[:, t:t + 1], self.gain_bc[gi][:], ALU.mult, ALU.mult,
                     R=[("x", t), "rstd", ("gain", gi)], W=[("hb", t % 2), "junk"] if t % 2 == 0 else [("hb", t % 2)])
            for kc in range(KC):
                self.tr(psT[:, kc * 128:(kc + 1) * 128], hbt[:, kc * 128:(kc + 1) * 128], self.ident_bf[:],
                        R=[("hb", t % 2), "ident_bf"], W=[("B", 7)])
            self.act(hT[:, :, t * 128:(t + 1) * 128], psT.rearrange("p (k s) -> p k s", k=KC), AF.Copy,
                     R=[("B", 7)], W=[("hT", t)])

    def mlstm_phase(self, l):
        P = self.P
        slot = l // 2
        P.barrier()
        self.off = 0
        NTL = self.ntiles
        win = self.carve([128, KC, 3088], BF16)
        wout = self.carve([128, KC, D], BF16)
        hgain = self.carve([128, D], F32)
        bias_bc = self.carve([128, 16], F32)
        hb = [self.carve([128, D], BF16) for _ in range(2)]
        hTt = [self.carve([128, KC, 128], BF16) for _ in range(2)]
        q_bf = self.carve([128, 512], BF16)
        k_bf = self.carve([128, 512], BF16)
        kE = [self.carve([128, 512], BF16) for _ in range(2)]
        kO = [self.carve([128, 512], BF16) for _ in range(2)]
        qkT = [self.carve([128, 8, 128], BF16) for _ in range(2)]
        vs = [self.carve([128, H, 128], BF16) for _ in range(2)]
        eo = [self.carve([128, D], F32) for _ in range(2)]
        PT = self.carve([128, H, 128], BF16)
        hcs = [self.carve([128, D], F32) for _ in range(2)]
        outbs = [self.carve([128, D], BF16) for _ in range(2)]
        outTs = [self.carve([128, 8, 128], BF16) for _ in range(2)]
        C32 = self.carve([128, 4, 132], F32)
        Cbf = [self.carve([128, 4, 132], BF16) for _ in range(2)]
        eabf = [self.carve([128, 16], BF16) for _ in range(2)]
        sqb = self.carve([128, D], F32)
        sm = self.small
        def smv(p):
            o = 64 + p * 120
            names = [("ssA", 1), ("lnA", 1), ("rsA", 1), ("g1", 16), ("e2", 16), ("dd", 16), ("gs", 16), ("ee", 8),
                     ("lfn", 8), ("a", 8), ("ea", 8), ("eb", 8), ("gp", 4), ("gsel", 4)]
            d_ = {}
            for n_, w_ in names:
                d_[n_] = sm[:, o:o + w_]
                o += w_
            return d_
        SV = [smv(0), smv(1)]
        d1, d2, scl, ssh, l2, rs = (sm[:, 320:328], sm[:, 328:336], sm[:, 336:344], sm[:, 344:352], sm[:, 352:360], sm[:, 360:368])
        B = self.B

        w_in = self.din("mlstm_w_in", [2, D, 3088])[slot].rearrange("(k p) n -> p k n", p=128)
        w_out = self.din("mlstm_w_out", [2, D, D])[slot].rearrange("(k p) n -> p k n", p=128)
        pieces = [(0, 1024), (3072, 3088), (1024, 2048), (2048, 3072)]
        pkey = {0: 0, 1: 0, 2: 2, 3: 2, 4: 3, 5: 3}
        for i, (c0, c1) in enumerate(pieces):
            self.dma("pool", win[:, :, c0:c1], w_in[:, :, c0:c1], slot=("win", i), W=[("win", i)])
        self.dma("pool", wout, w_out, slot=("wout",), W=["wout"])
        self.dma("sp", hgain, self.din("mlstm_head_gain", [2, D])[slot:slot + 1, :].partition_broadcast(128),
                 slot=("hgain",), W=["hgain"])
        self.dma("sp", bias_bc, self.din("mlstm_gate_bias", [2, 16])[slot:slot + 1, :].partition_broadcast(128),
                 slot=("gbias",), W=["gbias"])
        gi = self.load_gain(self.din("norm_mix", [DEPTH, D])[l:l + 1, :])
        self.memset("dve", C32, 0.0, W=["C32"])
        self.memset("dve", Cbf[0], 0.0, W=[("Cbf", 0)])
        for p in range(2):
            self.memset("dve", kE[p], 0.0, W=[("kE", p)])
            self.memset("dve", kO[p], 0.0, W=[("kO", p)])

        psT = B[7][:].bitcast(BF16)
        psT3 = psT.rearrange("p (k s) -> p k s", k=8)

        def b4(i):
            return B[i][:].rearrange("p (h s) -> p h s", h=4)

        def A0(t):
            p = t % 2
            v = SV[p]
            self.act(hb[p], self.x[:, t, :], AF.Square, R=[("x", t)], W=[("hb", p)], accum_out=v["ssA"])
            self.act(v["lnA"], v["ssA"], AF.Ln, R=[("hb", p)], W=[("lnA", p)], scale=1.0 / D, bias=EPS)
            self.act(v["rsA"], v["lnA"], AF.Exp, R=[("lnA", p)], W=[("rsA", p)], scale=-0.5)
            self.stt("dve", hb[p], self.x[:, t, :], v["rsA"], self.gain_bc[gi][:], ALU.mult, ALU.mult,
                     R=[("x", t), ("rsA", p), ("gain", gi)], W=[("hb", p)])

        def A1(t):
            p = t % 2
            for kc in range(KC):
                self.tr(psT[:, kc * 128:(kc + 1) * 128], hb[p][:, kc * 128:(kc + 1) * 128], self.ident_bf[:],
                        R=[("hb", p), "ident_bf"], W=[("B", 7)])
            self.act(hTt[p], psT3, AF.Copy, R=[("B", 7)], W=[("hTt", p)])

        def proj(t, cb, bank):
            p = t % 2
            for kc in range(KC):
                self.mm(B[bank][:], hTt[p][:, kc, :], win[:, kc, cb * 512:(cb + 1) * 512], kc == 0, kc == KC - 1,
                        R=[("hTt", p), ("win", pkey[cb])], W=[("B", bank)])

        def A2(t, late_fn=None):
            p = t % 2
            v = SV[p]
            proj(t, 0, 4)
            self.act(q_bf, B[4][:], AF.Copy, R=[("B", 4)], W=["q_bf"])
            proj(t, 1, 5)
            for kc in range(KC):
                self.mm(B[6][:, 0:16], hTt[p][:, kc, :], win[:, kc, 3072:3088], kc == 0, kc == KC - 1,
                        R=[("hTt", p), ("win", 1)], W=[("B", 6)])
            self.act(k_bf, B[5][:], AF.Copy, R=[("B", 5)], W=["k_bf"], scale=0.125)
            k4 = B[5][:].rearrange("p (j two c) -> p j two c", two=2, c=64)
            kE4 = kE[p].rearrange("p (j two c) -> p j two c", two=2, c=64)
            kO4 = kO[p].rearrange("p (j two c) -> p j two c", two=2, c=64)
            self.ts("dve", kE4[:, :, 0, :], k4[:, :, 0, :], 0.125, None, ALU.mult, R=[("B", 5)], W=[("kE", p)])
            self.ts("dve", kO4[:, :, 1, :], k4[:, :, 1, :], 0.125, None, ALU.mult, R=[("B", 5)], W=[("kO", p)])
            self.tt("dve", v["g1"], B[6][:, 0:16], bias_bc, ALU.add, R=[("B", 6), "gbias"], W=[("g1", p)])
            self.act(v["e2"], v["g1"], AF.Exp, R=[("g1", p)], W=[("e2", p)], scale=2.0 / 15.0)
            self.ts("dve", v["dd"], v["e2"], 1.0, None, ALU.add, R=[("e2", p)], W=[("dd", p)])
            self.P.add("dve", (lambda o_, i_: (lambda e: e.reciprocal(out=o_, in_=i_)))(v["dd"], v["dd"]), R=[("dd", p)], W=[("dd", p)])
            self.ts("dve", v["gs"], v["dd"], -30.0, 15.0, ALU.mult, ALU.add, R=[("dd", p)], W=[("gs", p)])
            self.act(v["ee"], v["gs"][:, 8:16], AF.Exp, R=[("gs", p)], W=[("ee", p)], scale=-1.0)
            self.act(v["lfn"], v["ee"], AF.Ln, R=[("ee", p)], W=[("lfn", p)], bias=1.0)
            if late_fn is not None:
                late_fn()
            self.mm(B[6][:, 16:24], self.negtri[:], v["lfn"], True, True, R=[("lfn", p), "negtri"], W=[("B", 6)])
            self.mm(B[6][:, 24:32], self.negones[:], v["lfn"], True, True, R=[("lfn", p), "negones"], W=[("B", 6)])
            self.tt("dve", v["a"], v["gs"][:, 0:8], B[6][:, 16:24], ALU.subtract, R=[("gs", p), ("B", 6)], W=[("a", p)])
            self.act(v["eb"], B[6][:, 16:24], AF.Exp, R=[("B", 6)], W=[("eb", p)])
            bl2 = B[6][:, 24:32].rearrange("p (j two) -> p j two", two=2)
            self.copy("dve", v["gp"][0:64, :], bl2[0:64, :, 0], R=[("B", 6)], W=[("gp0", p)])
            self.copy("dve", v["gp"][64:128, :], bl2[64:128, :, 1], R=[("B", 6)], W=[("gp1", p)])
            self.act(v["ea"], v["a"], AF.Exp, R=[("a", p)], W=[("ea", p)])
            self.copy("dve", eabf[p][:, 0:8], v["ea"], R=[("ea", p)], W=[("eabf", p)])
            self.act(v["gsel"], v["gp"], AF.Exp, R=[("gp0", p), ("gp1", p)], W=[("gsel", p)])

        def A3(t):
            p = t % 2
            v = SV[p]
            for j in range(4):
                self.tr(psT[:, j * 128:(j + 1) * 128], q_bf[:, j * 128:(j + 1) * 128], self.ident_bf[:],
                        R=["q_bf", "ident_bf"], W=[("B", 7)])
            for j in range(4):
                self.tr(psT[:, (4 + j) * 128:(5 + j) * 128], k_bf[:, j * 128:(j + 1) * 128], self.ident_bf[:],
                        R=["k_bf", "ident_bf"], W=[("B", 7)])
            self.act(qkT[p], psT3, AF.Copy, R=[("B", 7)], W=[("qkT", p)])
            for i in range(2):
                proj(t, 2 + i, 4 + i)
                self.tt("dve", vs[p][:, 4 * i:4 * i + 4, :], b4(4 + i),
                        v["ea"][:, 4 * i:4 * i + 4].unsqueeze(2).to_broadcast([128, 4, 128]), ALU.mult,
                        R=[("B", 4 + i), ("ea", p)], W=[("vs", p, i)])
            for i in range(2):
                proj(t, 4 + i, 4 + i)
                self.act(eo[p][:, i * 512:(i + 1) * 512], B[4 + i][:], AF.Exp, R=[("B", 4 + i)], W=[("eo", p)], scale=-1.0)

        def A3b(t):
            p = t % 2
            self.act(eo[p], eo[p], AF.Ln, R=[("eo", p)], W=[("eo", p)], bias=1.0)
            self.act(eo[p], eo[p], AF.Exp, R=[("eo", p)], W=[("eo", p)], scale=-1.0)
            self.tt("dve", eo[p], eo[p], hgain, ALU.mult, R=[("eo", p), "hgain"], W=[("eo", p)])

        def B1(t):
            p = t % 2
            for h in range(H):
                p0 = (h % 2) * 64
                self.mm(b4(h % 2)[:, h // 2, :], qkT[p][p0:p0 + 64, 4 + h // 2, :], qkT[p][p0:p0 + 64, h // 2, :], True, True,
                        R=[("qkT", p)], W=[("B", h % 2)])
            PT4 = PT.rearrange("p (j two) s -> p j two s", two=2)
            for i in range(2):
                self.tt("dve", PT4[:, :, i, :], b4(i), self.causal[:].unsqueeze(1).to_broadcast([128, 4, 128]), ALU.mult,
                        R=[("B", i), "causal"], W=[("PT", i)])

        def B2(t):
            p = t % 2
            v = SV[p]
            cb_ = Cbf[p]
            for h in range(H):
                p0 = (h % 2) * 64
                qTh = qkT[p][p0:p0 + 64, h // 2, :]
                self.mm(b4(2 + h // 4)[:, h % 4, :], PT[:, h, :], vs[p][:, h, :], True, False,
                        R=[("PT", h % 2), ("vs", p, h // 4)], W=[("B", 2 + h // 4)])
                self.mm(b4(2 + h // 4)[:, h % 4, :], qTh, cb_[p0:p0 + 64, h // 2, 0:128], False, True,
                        R=[("qkT", p), ("Cbf", p)], W=[("B", 2 + h // 4)])
                self.mm(B[6][:, 32 + h:33 + h], PT[:, h, :], eabf[p][:, h:h + 1], True, False,
                        R=[("PT", h % 2), ("eabf", p)], W=[("B", 6)])
                self.mm(B[6][:, 32 + h:33 + h], qTh, cb_[p0:p0 + 64, h // 2, 128:129], False, True,
                        R=[("qkT", p), ("Cbf", p)], W=[("B", 6)])
            for j in range(4):
                self.mm(b4(0)[:, j, :], kE[p][:, j * 128:(j + 1) * 128], vs[p][:, 2 * j, :], True, False,
                        R=[("kE", p), ("vs", p, j // 2)], W=[("B", 0)])
                self.mm(b4(0)[:, j, :], kO[p][:, j * 128:(j + 1) * 128], vs[p][:, 2 * j + 1, :], False, True,
                        R=[("kO", p), ("vs", p, j // 2)], W=[("B", 0)])
                self.mm(B[6][:, 40 + j:41 + j], kE[p][:, j * 128:(j + 1) * 128], eabf[p][:, 2 * j:2 * j + 1], True, False,
                        R=[("kE", p), ("eabf", p)], W=[("B", 6)])
                self.mm(B[6][:, 40 + j:41 + j], kO[p][:, j * 128:(j + 1) * 128], eabf[p][:, 2 * j + 1:2 * j + 2], False, True,
                        R=[("kO", p), ("eabf", p)], W=[("B", 6)])
            self.tt("dve", d1, B[6][:, 32:40], v["eb"], ALU.mult, R=[("B", 6), ("eb", p)], W=["d1"])
            self.stt("dve", d2, d1, -1.0, d1, ALU.mult, ALU.max, R=["d1"], W=["d2"])
            self.ts("dve", d2, d2, 1.0, None, ALU.max, R=["d2"], W=["d2"])
            self.P.add("dve", lambda e: e.reciprocal(out=d2, in_=d2), R=["d2"], W=["d2"])
            self.tt("dve", scl, d2, v["eb"], ALU.mult, R=["d2", ("eb", p)], W=["scl"])
            hc3 = hcs[p].rearrange("p (h s) -> p h s", h=H)
            for i in range(2):
                self.tt("dve", hc3[:, 4 * i:4 * i + 4, :], b4(2 + i),
                        scl[:, 4 * i:4 * i + 4].unsqueeze(2).to_broadcast([128, 4, 128]), ALU.mult,
                        R=[("B", 2 + i), "scl"], W=[("hc", p)])
            self.tt("dve", C32[:, :, 0:128], C32[:, :, 0:128], b4(0), ALU.add, R=["C32", ("B", 0)], W=["C32"])
            self.tt("dve", C32[:, :, 128], C32[:, :, 128], B[6][:, 40:44], ALU.add, R=["C32", ("B", 6)], W=["C32"])
            self.tt("dve", C32, C32, v["gsel"].unsqueeze(2).to_broadcast([128, 4, 132]), ALU.mult, R=["C32", ("gsel", p)], W=["C32"])
            self.act(Cbf[1 - p], C32, AF.Copy, R=["C32"], W=[("Cbf", 1 - p)])

        def B3(t):
            p = t % 2
            hc = hcs[p]
            hc3 = hc.rearrange("p (h s) -> p h s", h=H)
            self.act(sqb, hc, AF.Square, R=[("hc", p)], W=["sqb"])
            self.P.add("dve", lambda e: e.reduce_sum(out=ssh, in_=sqb.rearrange("p (h s) -> p h s", h=H), axis=AX.X),
                       R=["sqb"], W=["ssh"])
            self.act(l2, ssh, AF.Ln, R=["ssh"], W=["l2"], scale=1.0 / 128.0, bias=EPS)
            self.act(rs, l2, AF.Exp, R=["l2"], W=["rs"], scale=-0.5)
            self.tt("dve", hc3, hc3, rs.unsqueeze(2).to_broadcast([128, H, 128]), ALU.mult,
                    R=[("hc", p), "rs"], W=[("hc", p)])
            self.tt("dve", outbs[p], hc, eo[p], ALU.mult, R=[("hc", p), ("eo", p)], W=[("outb", p)])

        def B3pe(t):
            p = t % 2
            for j in range(8):
                self.tr(psT[:, j * 128:(j + 1) * 128], outbs[p][:, j * 128:(j + 1) * 128], self.ident_bf[:],
                        R=[("outb", p), "ident_bf"], W=[("B", 7)])
            self.act(outTs[p], psT3, AF.Copy, R=[("B", 7)], W=[("outT", p)])

        def B4(t):
            p = t % 2
            for half in range(2):
                bk = half
                for vc in range(8):
                    self.mm(B[bk][:], outTs[p][:, vc, :], wout[:, vc, half * 512:(half + 1) * 512], vc == 0, vc == 7,
                            R=[("outT", p), "wout"], W=[("B", bk)])
                xs = self.x[:, t, half * 512:(half + 1) * 512]
                self.tt("dve", xs, xs, B[bk][:], ALU.add, R=[("B", bk), ("x", t)], W=[("x", t)])

        A0(0)
        if NTL > 1:
            A0(1)
        A1(0)
        A2(0)
        A3(0)
        for t in range(NTL + 1):
            cur = t < NTL
            nxt = t + 1 < NTL
            late = t >= 1
            if cur:
                B1(t)
            if nxt:
                A1(t + 1)
            if t + 2 < NTL:
                A0(t + 2)
            if late:
                B3(t - 1)
            if cur:
                B2(t)
            if late:
                B3pe(t - 1)
            if nxt:
                A2(t + 1, late_fn=(lambda tt=t: B4(tt - 1)) if late else None)
            elif late:
                B4(t - 1)
            if nxt:
                A3(t + 1)
            if cur:
                A3b(t)

    def mlstm_phase_v1(self, l):
        P = self.P
        slot = l // 2
        P.barrier()
        self.off = 0
        hT = self.carve([128, KC, S], BF16)
        win = self.carve([128, KC, 3088], BF16)
        wout = self.carve([128, KC, D], BF16)
        hb = [self.carve([128, D], BF16) for _ in range(2)]
        q_bf = self.carve([128, 512], BF16)
        k_bf = self.carve([128, 512], BF16)
        kE = self.carve([128, 512], BF16)
        kO = self.carve([128, 512], BF16)
        qkT = self.carve([128, 8, 128], BF16)
        vs = self.carve([128, H, 128], BF16)
        PT = self.carve([128, H, 128], BF16)
        sig = self.carve([128, D], F32)
        hc = self.carve([128, D], F32)
        outT = self.carve([128, 8, 128], BF16)
        C32 = self.carve([128, 4, 132], F32)
        Cbf = self.carve([128, 4, 132], BF16)
        hgain = self.carve([128, D], F32)
        bias_bc = self.carve([128, 16], F32)
        eabf = self.carve([128, 8], BF16)
        jk = self.carve([128, 128], BF16)
        outb = hb[0]
        sm = self.small
        g1, th, gs, ee, lfn = sm[:, 64:80], sm[:, 80:96], sm[:, 96:112], sm[:, 112:120], sm[:, 120:128]
        a_, ea, eb, gp, gsel = sm[:, 128:136], sm[:, 136:144], sm[:, 144:152], sm[:, 152:156], sm[:, 156:160]
        d1, d2, scl, ssh, l2, rs = sm[:, 160:168], sm[:, 168:176], sm[:, 176:184], sm[:, 184:192], sm[:, 192:200], sm[:, 200:208]
        B = self.B

        w_in = self.din("mlstm_w_in", [2, D, 3088])[slot].rearrange("(k p) n -> p k n", p=128)
        w_out = self.din("mlstm_w_out", [2, D, D])[slot].rearrange("(k p) n -> p k n", p=128)
        pieces = [(0, 1024), (1024, 2048), (2048, 3072), (3072, 3088)]
        for i, (c0, c1) in enumerate(pieces):
            self.dma("pool", win[:, :, c0:c1], w_in[:, :, c0:c1], slot=("win", i), W=[("win", i)])
        self.dma("pool", wout, w_out, slot=("wout",), W=["wout"])
        self.dma("sp", hgain, self.din("mlstm_head_gain", [2, D])[slot:slot + 1, :].partition_broadcast(128),
                 slot=("hgain",), W=["hgain"])
        self.dma("sp", bias_bc, self.din("mlstm_gate_bias", [2, 16])[slot:slot + 1, :].partition_broadcast(128),
                 slot=("gbias",), W=["gbias"])
        self.memset("dve", C32, 0.0, W=["C32"])
        self.memset("dve", Cbf, 0.0, W=["Cbf"])
        self.memset("dve", kE, 0.0, W=["kE"])
        self.memset("dve", kO, 0.0, W=["kO"])

        self.norm_to_hT(self.din("norm_mix", [DEPTH, D])[l:l + 1, :], hT, hb)

        psT = B[7][:].bitcast(BF16)
        psT3 = psT.rearrange("p (k s) -> p k s", k=8)

        def b4(i):
            return B[i][:].rearrange("p (h s) -> p h s", h=4)

        wpiece = {0: 0, 1: 0, 2: 1, 3: 1, 4: 2, 5: 2}
        for t in range(self.ntiles):
            tok = slice(t * 128, (t + 1) * 128)
            for cb in range(6):
                for kc in range(KC):
                    self.mm(B[cb][:], hT[:, kc, tok], win[:, kc, cb * 512:(cb + 1) * 512], kc == 0, kc == KC - 1,
                            R=[("hT", t), ("win", wpiece[cb])], W=[("B", cb)])
            for kc in range(KC):
                self.mm(B[6][:, 0:16], hT[:, kc, tok], win[:, kc, 3072:3088], kc == 0, kc == KC - 1,
                        R=[("hT", t), ("win", 3)], W=[("B", 6)])
            self.tt("dve", g1, B[6][:, 0:16], bias_bc, ALU.add, R=[("B", 6), "gbias"], W=["g1"])
            self.act(th, g1, AF.Tanh, R=["g1"], W=["th"], scale=1.0 / 15.0)
            self.ts("dve", gs, th, 15.0, None, ALU.mult, R=["th"], W=["gs"])
            self.act(ee, gs[:, 8:16], AF.Exp, R=["gs"], W=["ee"], scale=-1.0)
            self.act(lfn, ee, AF.Ln, R=["ee"], W=["lfn"], bias=1.0)
            self.mm(B[6][:, 16:24], self.negtri[:], lfn, True, True, R=["lfn", "negtri"], W=[("B", 6)])
            self.mm(B[6][:, 24:32], self.negones[:], lfn, True, True, R=["lfn", "negones"], W=[("B", 6)])
            self.tt("dve", a_, gs[:, 0:8], B[6][:, 16:24], ALU.subtract, R=["gs", ("B", 6)], W=["a"])
            self.act(ea, a_, AF.Exp, R=["a"], W=["ea"])
            self.copy("dve", eabf, ea, R=["ea"], W=["eabf"])
            self.act(eb, B[6][:, 16:24], AF.Exp, R=[("B", 6)], W=["eb"])
            bl2 = B[6][:, 24:32].rearrange("p (j two) -> p j two", two=2)
            self.copy("dve", gp[0:64, :], bl2[0:64, :, 0], R=[("B", 6)], W=["gp0"])
            self.copy("dve", gp[64:128, :], bl2[64:128, :, 1], R=[("B", 6)], W=["gp1"])
            self.act(gsel, gp, AF.Exp, R=["gp0", "gp1"], W=["gsel"])
            self.act(q_bf, B[0][:], AF.Copy, R=[("B", 0)], W=["q_bf"])
            self.act(k_bf, B[1][:], AF.Copy, R=[("B", 1)], W=["k_bf"], scale=0.125)
            k4 = B[1][:].rearrange("p (j two c) -> p j two c", two=2, c=64)
            kE4 = kE.rearrange("p (j two c) -> p j two c", two=2, c=64)
            kO4 = kO.rearrange("p (j two c) -> p j two c", two=2, c=64)
            self.ts("dve", kE4[:, :, 0, :], k4[:, :, 0, :], 0.125, None, ALU.mult, R=[("B", 1)], W=["kE"])
            self.ts("dve", kO4[:, :, 1, :], k4[:, :, 1, :], 0.125, None, ALU.mult, R=[("B", 1)], W=["kO"])
            for j in range(4):
                self.tr(psT[:, j * 128:(j + 1) * 128], q_bf[:, j * 128:(j + 1) * 128], self.ident_bf[:],
                        R=["q_bf", "ident_bf"], W=[("B", 7)])
            for j in range(4):
                self.tr(psT[:, (4 + j) * 128:(5 + j) * 128], k_bf[:, j * 128:(j + 1) * 128], self.ident_bf[:],
                        R=["k_bf", "ident_bf"], W=[("B", 7)])
            self.act(qkT, psT3, AF.Copy, R=[("B", 7)], W=["qkT"])
            for i in range(2):
                self.tt("dve", vs[:, 4 * i:4 * i + 4, :], b4(2 + i),
                        ea[:, 4 * i:4 * i + 4].unsqueeze(2).to_broadcast([128, 4, 128]), ALU.mult,
                        R=[("B", 2 + i), "ea"], W=[("vs", i)])
            for i in range(2):
                self.act(sig[:, i * 512:(i + 1) * 512], B[4 + i][:], AF.Tanh, R=[("B", 4 + i)], W=[("sig", i)], scale=0.5)
            for h in range(H):
                p0 = (h % 2) * 64
                self.mm(b4(h % 2)[:, h // 2, :], qkT[p0:p0 + 64, 4 + h // 2, :], qkT[p0:p0 + 64, h // 2, :], True, True,
                        R=["qkT"], W=[("B", h % 2)])
            PT4 = PT.rearrange("p (j two) s -> p j two s", two=2)
            for i in range(2):
                self.tt("dve", PT4[:, :, i, :], b4(i),
                        self.causal[:].unsqueeze(1).to_broadcast([128, 4, 128]), ALU.mult,
                        R=[("B", i), "causal"], W=[("PT", i)])
            for h in range(H):
                p0 = (h % 2) * 64
                qTh = qkT[p0:p0 + 64, h // 2, :]
                self.mm(b4(2 + h // 4)[:, h % 4, :], PT[:, h, :], vs[:, h, :], True, False,
                        R=[("PT", h % 2), ("vs", h // 4)], W=[("B", 2 + h // 4)])
                self.mm(b4(2 + h // 4)[:, h % 4, :], qTh, Cbf[p0:p0 + 64, h // 2, 0:128], False, True,
                        R=["qkT", "Cbf"], W=[("B", 2 + h // 4)])
                self.mm(B[6][:, 32 + h:33 + h], PT[:, h, :], eabf[:, h:h + 1], True, False,
                        R=[("PT", h % 2), "eabf"], W=[("B", 6)])
                self.mm(B[6][:, 32 + h:33 + h], qTh, Cbf[p0:p0 + 64, h // 2, 128:129], False, True,
                        R=["qkT", "Cbf"], W=[("B", 6)])
            for j in range(4):
                self.mm(b4(4)[:, j, :], kE[:, j * 128:(j + 1) * 128], vs[:, 2 * j, :], True, False,
                        R=["kE", ("vs", j // 2)], W=[("B", 4)])
                self.mm(b4(4)[:, j, :], kO[:, j * 128:(j + 1) * 128], vs[:, 2 * j + 1, :], False, True,
                        R=["kO", ("vs", j // 2)], W=[("B", 4)])
                self.mm(B[6][:, 40 + j:41 + j], kE[:, j * 128:(j + 1) * 128], eabf[:, 2 * j:2 * j + 1], True, False,
                        R=["kE", "eabf"], W=[("B", 6)])
                self.mm(B[6][:, 40 + j:41 + j], kO[:, j * 128:(j + 1) * 128], eabf[:, 2 * j + 1:2 * j + 2], False, True,
                        R=["kO", "eabf"], W=[("B", 6)])
            self.tt("dve", C32[:, :, 0:128], C32[:, :, 0:128], b4(4), ALU.add, R=["C32", ("B", 4)], W=["C32"])
            self.tt("dve", C32[:, :, 128], C32[:, :, 128], B[6][:, 40:44], ALU.add, R=["C32", ("B", 6)], W=["C32"])
            self.tt("dve", C32, C32, gsel.unsqueeze(2).to_broadcast([128, 4, 132]), ALU.mult, R=["C32", "gsel"], W=["C32"])
            self.act(Cbf, C32, AF.Copy, R=["C32"], W=["Cbf"])
            self.tt("dve", d1, B[6][:, 32:40], eb, ALU.mult, R=[("B", 6), "eb"], W=["d1"])
            self.stt("dve", d2, d1, -1.0, d1, ALU.mult, ALU.max, R=["d1"], W=["d2"])
            self.ts("dve", d2, d2, 1.0, None, ALU.max, R=["d2"], W=["d2"])
            self.P.add("dve", lambda e: e.reciprocal(out=d2, in_=d2), R=["d2"], W=["d2"])
            self.tt("dve", scl, d2, eb, ALU.mult, R=["d2", "eb"], W=["scl"])
            hc3 = hc.rearrange("p (h s) -> p h s", h=H)
            for i in range(2):
                self.tt("dve", hc3[:, 4 * i:4 * i + 4, :], b4(2 + i),
                        scl[:, 4 * i:4 * i + 4].unsqueeze(2).to_broadcast([128, 4, 128]), ALU.mult,
                        R=[("B", 2 + i), "scl"], W=[("hc", i)])
            for h in range(H):
                self.act(jk, hc3[:, h, :], AF.Square, R=[("hc", h // 4)], W=["jk", "ssh"], accum_out=ssh[:, h:h + 1])
            self.act(l2, ssh, AF.Ln, R=["ssh"], W=["l2"], scale=1.0 / 128.0, bias=EPS)
            self.act(rs, l2, AF.Exp, R=["l2"], W=["rs"], scale=-0.5, bias=float(np.log(0.5)))
            self.tt("dve", hc3, hc3, rs.unsqueeze(2).to_broadcast([128, H, 128]), ALU.mult,
                    R=[("hc", 0), ("hc", 1), "rs"], W=[("hc", 0), ("hc", 1)])
            self.tt("pool", hc, hc, hgain, ALU.mult, R=[("hc", 0), ("hc", 1), "hgain"], W=[("hc", 0), ("hc", 1)])
            self.stt("dve", outb, sig, 1.0, hc, ALU.add, ALU.mult,
                     R=[("sig", 0), ("sig", 1), ("hc", 0), ("hc", 1)], W=[("hb", 0)])
            for j in range(8):
                self.tr(psT[:, j * 128:(j + 1) * 128], outb[:, j * 128:(j + 1) * 128], self.ident_bf[:],
                        R=[("hb", 0), "ident_bf"], W=[("B", 7)])
            self.act(outT, psT3, AF.Copy, R=[("B", 7)], W=["outT"])
            for half in range(2):
                bk = 5 if half == 0 else 1
                for vc in range(8):
                    self.mm(B[bk][:], outT[:, vc, :], wout[:, vc, half * 512:(half + 1) * 512], vc == 0, vc == 7,
                            R=["outT", "wout"], W=[("B", bk)])
                xs = self.x[:, t, half * 512:(half + 1) * 512]
                self.tt("dve", xs, xs, B[bk][:], ALU.add, R=[("B", bk), ("x", t)], W=[("x", t)])

    def pool_phase(self, l):
        P = self.P
        slot = l // 2
        P.barrier()
        self.off = 0
        band = [self.carve([128, 128], F32) for _ in range(4)]
        band0 = [self.carve([128, 128], F32) for _ in range(4)]
        offm = [self.carve([128, 128], F32) for _ in range(4)]
        sc16 = self.carve([128, 4, 16], F32)
        h32 = [self.carve([128, D], F32) for _ in range(3)]
        yT = [self.carve([128, KC, 128], BF16) for _ in range(2)]
        wp = self.carve([128, 4, 2, 256], BF16)
        pscale = self.carve([128, D], F32)
        tmp = [self.carve([128, 512], F32) for _ in range(2)]
        B = self.B
        wsrc = self.din("pool_w_group", [2, 4, 256, 256])[slot]
        for g in range(4):
            self.dma("pool", wp[:, g, :, :], wsrc[g].rearrange("(k p) d -> p k d", p=128), slot=("wp", g), W=[("wp", g)])
        self.dma("sp", pscale, self.din("pool_scale", [2, D])[slot:slot + 1, :].partition_broadcast(128),
                 slot=("pscale",), W=["pscale"])
        gi = self.load_gain(self.din("norm_mix", [DEPTH, D])[l:l + 1, :])

        def sel(ap, pattern, cm, base, key):
            self.P.add("pool", lambda e: e.affine_select(out=ap, in_=ap, compare_op=ALU.is_ge, fill=0.0, base=base,
                                                         pattern=pattern, channel_multiplier=cm), W=[key])
        for wi, w in enumerate(WINS):
            self.memset("pool", band[wi], 1.0 / w, W=[("band", wi)])
            sel(band[wi], [[1, 128]], -1, 0, ("band", wi))
            sel(band[wi], [[-1, 128]], 1, w - 1, ("band", wi))
            self.memset("pool", offm[wi], 1.0 / w, W=[("off", wi)])
            sel(offm[wi], [[-1, 128]], 1, w - 129, ("off", wi))
            self.ts("dve", sc16[:, wi, :], self.invcnt[:, wi, :], float(w), None, ALU.mult, R=["invcnt"], W=[("sc16", wi)])
            self.copy("dve", band0[wi], band[wi], R=[("band", wi)], W=[("band0", wi)])
            self.tt("dve", band0[wi][:, 0:16], band[wi][:, 0:16], sc16[:, wi, :], ALU.mult,
                    R=[("band", wi), ("sc16", wi)], W=[("band0", wi)])
            self.tt("dve", band0[wi], band0[wi], self.ident_f[:], ALU.subtract, R=[("band0", wi), "ident_f"], W=[("band0", wi)])
            self.tt("dve", band[wi], band[wi], self.ident_f[:], ALU.subtract, R=[("band", wi), "ident_f"], W=[("band", wi)])

        self.norm_stats(tmp[0].bitcast(BF16))

        def b4(i):
            return B[i][:].rearrange("p (h s) -> p h s", h=4)

        def norm_tile(t):
            self.stt("dve", h32[t % 3], self.x[:, t, :], self.rstd[:, t:t + 1], self.gain_bc[gi][:], ALU.mult, ALU.mult,
                     R=[("x", t), "rstd", ("gain", gi)], W=[("h32", t % 3)] + (["junk"] if t == 0 else []))

        def band_mm(t):
            par = t % 2
            for c in range(KC):
                wi = c // 2
                bk = 2 * par + c // 4
                out = b4(bk)[:, c % 4, :]
                cs = slice(c * 128, (c + 1) * 128)
                if t == 0:
                    self.mm(out, h32[0][:, cs], band0[wi], True, True, R=[("h32", 0), ("band0", wi)], W=[("B", bk)])
                else:
                    self.mm(out, h32[t % 3][:, cs], band[wi], True, False, R=[("h32", t % 3), ("band", wi)], W=[("B", bk)])
                    self.mm(out[:, 0:16], h32[(t - 1) % 3][:, cs], offm[wi][:, 0:16], False, True,
                            R=[("h32", (t - 1) % 3), ("off", wi)], W=[("B", bk)])

        def evac_y(t):
            par = t % 2
            self.act(yT[par][:, 0:4, :], b4(2 * par), AF.Copy, R=[("B", 2 * par)], W=[("yT", par, 0)])
            self.copy("dve", yT[par][:, 4:8, :], b4(2 * par + 1), R=[("B", 2 * par + 1)], W=[("yT", par, 1)])

        def pool_mm(t):
            par = t % 2
            for g in range(4):
                bk = 4 + 2 * par + g // 2
                for kc in range(2):
                    self.mm(B[bk][:, (g % 2) * 256:(g % 2 + 1) * 256], yT[par][:, 2 * g + kc, :], wp[:, g, kc, :], kc == 0, kc == 1,
                            R=[("yT", par, g // 2), ("wp", g)], W=[("B", bk)])

        def final(t):
            par = t % 2
            for half in range(2):
                bk = 4 + 2 * par + half
                self.tt("dve", tmp[half], B[bk][:], pscale[:, half * 512:(half + 1) * 512], ALU.mult,
                        R=[("B", bk), "pscale"], W=[("tmp", half)])
                xs = self.x[:, t, half * 512:(half + 1) * 512]
                self.tt("dve", xs, xs, tmp[half], ALU.add, R=[("tmp", half), ("x", t)], W=[("x", t)])

        norm_tile(0)
        norm_tile(1)
        band_mm(0)
        evac_y(0)
        for t in range(NT):
            if t + 2 < NT:
                norm_tile(t + 2)
            if t + 1 < NT:
                band_mm(t + 1)
            pool_mm(t)
            if t + 1 < NT:
                evac_y(t + 1)
            final(t)

    def pool_phase_v1(self, l):
        P = self.P
        slot = l // 2
        P.barrier()
        self.off = 0
        hT32 = self.carve([128, KC, S], F32)
        yT = self.carve([128, KC, S], BF16)
        sbuf_ = [self.carve([128, S], F32) for _ in range(2)]
        wp = self.carve([128, 4, 2, 256], BF16)
        pscale = self.carve([128, D], F32)
        h32 = [self.carve([128, D], F32) for _ in range(2)]
        t16 = self.carve([128, 16], F32)
        B = self.B
        wsrc = self.din("pool_w_group", [2, 4, 256, 256])[slot]
        for g in range(4):
            self.dma("pool", wp[:, g, :, :], wsrc[g].rearrange("(k p) d -> p k d", p=128), slot=("wp", g), W=[("wp", g)])
        self.dma("sp", pscale, self.din("pool_scale", [2, D])[slot:slot + 1, :].partition_broadcast(128),
                 slot=("pscale",), W=["pscale"])
        gi = self.load_gain(self.din("norm_mix", [DEPTH, D])[l:l + 1, :])
        self.norm_stats(h32[0])
        for t in range(NT):
            ht = h32[t % 2]
            self.stt("dve", ht, self.x[:, t, :], self.rstd[:, t:t + 1], self.gain_bc[gi][:], ALU.mult, ALU.mult,
                     R=[("x", t), "rstd", ("gain", gi)], W=[("h32", t % 2), "junk"] if t % 2 == 0 else [("h32", t % 2)])
            for kc in range(KC):
                bk = 6 + kc // 4
                self.tr(B[bk][:, (kc % 4) * 128:(kc % 4 + 1) * 128], ht[:, kc * 128:(kc + 1) * 128], self.ident_f[:],
                        R=[("h32", t % 2), "ident_f"], W=[("B", bk)])
            self.act(hT32[:, 0:4, t * 128:(t + 1) * 128], B[6][:].rearrange("p (k s) -> p k s", k=4), AF.Copy,
                     R=[("B", 6)], W=[("hT32", c) for c in range(0, 4)])
            self.copy("dve", hT32[:, 4:8, t * 128:(t + 1) * 128], B[7][:].rearrange("p (k s) -> p k s", k=4),
                      R=[("B", 7)], W=[("hT32", c) for c in range(4, 8)])
        for c in range(KC):
            wi = c // 2
            win_ = WINS[wi]
            src = hT32[:, c, :]
            cur, ckey = src, ("hT32", c)
            sh, n = 1, 0
            while sh < win_:
                dst = sbuf_[n % 2]
                dkey = ("sb", n % 2)
                self.tt("dve", dst[:, sh:], cur[:, sh:], cur[:, 0:S - sh], ALU.add, R=[ckey], W=[dkey])
                self.copy("dve", dst[:, 0:sh], cur[:, 0:sh], R=[ckey], W=[dkey])
                cur, ckey = dst, dkey
                sh *= 2
                n += 1
            self.stt("dve", yT[:, c, :], cur, 1.0 / win_, src, ALU.mult, ALU.subtract, R=[ckey, ("hT32", c)], W=[("yT", c)])
            self.tt("dve", t16, cur[:, 0:16], self.invcnt[:, wi, :], ALU.mult, R=[ckey, "invcnt"], W=["t16"])
            self.tt("dve", yT[:, c, 0:16], t16, src[:, 0:16], ALU.subtract, R=["t16", ("hT32", c)], W=[("yT", c)])
        for t in range(NT):
            tok = slice(t * 128, (t + 1) * 128)
            for g in range(4):
                bk = (t % 2) * 2 + g // 2
                for kc in range(2):
                    self.mm(B[bk][:, (g % 2) * 256:(g % 2 + 1) * 256], yT[:, 2 * g + kc, tok], wp[:, g, kc, :], kc == 0, kc == 1,
                            R=[("yT", 2 * g + kc), ("wp", g)], W=[("B", bk)])
            for half in range(2):
                bk = (t % 2) * 2 + half
                tmp = h32[half][:, 0:512]
                self.tt("dve", tmp, B[bk][:], pscale[:, half * 512:(half + 1) * 512], ALU.mult,
                        R=[("B", bk), "pscale"], W=[("h32", half)])
                xs = self.x[:, t, half * 512:(half + 1) * 512]
                self.tt("dve", xs, xs, tmp, ALU.add, R=[("h32", half), ("x", t)], W=[("x", t)])

    def ffn_phase(self, l):
        P = self.P
        P.barrier()
        self.off = 0
        hT = self.carve([128, KC, S], BF16)
        abuf = [self.carve([128, 4, S], BF16) for _ in range(2)]
        wgu = [self.carve([128, KC, 2, 256], BF16) for _ in range(3)]
        wd = [self.carve([128, 4, D], BF16) for _ in range(2)]
        hb = [self.carve([128, D], BF16) for _ in range(2)]
        sl = [self.carve([128, 512], BF16) for _ in range(2)]
        B = self.B
        wgu_src = self.din("ffn_w_gate_up", [DEPTH, D, 2 * DFF])[l].rearrange("(k p) n -> p k n", p=128)
        wd_src = self.din("ffn_w_down", [DEPTH, DFF, D])[l].rearrange("(j p) d -> p j d", p=128)
        groups = [(j0, min(4, NJ - j0)) for j0 in range(0, NJ, 4)]

        def load_gu(n):
            s_ = n % 3
            self.dma("pool", wgu[s_][:, :, 0, :], wgu_src[:, :, n * 256:(n + 1) * 256], slot=("wg", s_), W=[("wg", s_)])
            self.dma("pool", wgu[s_][:, :, 1, :], wgu_src[:, :, DFF + n * 256:DFF + (n + 1) * 256], slot=("wu", s_), W=[("wu", s_)])

        def load_d(gi):
            j0, nj = groups[gi]
            self.dma("pool", wd[gi % 2][:, 0:nj, :], wd_src[:, j0:j0 + nj, :], slot=("wd", gi % 2), W=[("wd", gi % 2)])

        load_gu(0)
        load_gu(1)
        load_gu(2)
        load_d(0)
        self.norm_to_hT(self.din("norm_ffn", [DEPTH, D])[l:l + 1, :], hT, hb)

        cnt = [0]

        def gateup(gi):
            j0, nj = groups[gi]
            ab = abuf[gi % 2]
            for jj in range(nj):
                j = j0 + jj
                n, sub = j // 2, j % 2
                s_ = n % 3
                for tg in range(4):
                    pr = cnt[0] % 2
                    cnt[0] += 1
                    bg, bu = B[2 * pr], B[2 * pr + 1]
                    rk = [("hT", 4 * tg + i) for i in range(4)]
                    for kc in range(KC):
                        self.mm(bg[:], wgu[s_][:, kc, 0, sub * 128:(sub + 1) * 128], hT[:, kc, tg * 512:(tg + 1) * 512],
                                kc == 0, kc == KC - 1, R=rk + [("wg", s_)], W=[("B", 2 * pr)])
                    for kc in range(KC):
                        self.mm(bu[:], wgu[s_][:, kc, 1, sub * 128:(sub + 1) * 128], hT[:, kc, tg * 512:(tg + 1) * 512],
                                kc == 0, kc == KC - 1, R=rk + [("wu", s_)], W=[("B", 2 * pr + 1)])
                    self.act(sl[pr], bg[:], AF.Silu, R=[("B", 2 * pr)], W=[("sl", pr)])
                    self.tt("dve", ab[:, jj, tg * 512:(tg + 1) * 512], sl[pr], bu[:], ALU.mult,
                            R=[("sl", pr), ("B", 2 * pr + 1)], W=[("ab", gi % 2, tg)])
                if sub == 1 or j == NJ - 1:
                    if n + 3 < NJ // 2:
                        load_gu(n + 3)

        def down(gi):
            j0, nj = groups[gi]
            ab = abuf[gi % 2]
            for t in range(NT):
                for half in range(2):
                    bk = 4 + (2 * t + half) % 4
                    for jj in range(nj):
                        self.mm(B[bk][:], ab[:, jj, t * 128:(t + 1) * 128], wd[gi % 2][:, jj, half * 512:(half + 1) * 512],
                                jj == 0, jj == nj - 1, R=[("ab", gi % 2, t // 4), ("wd", gi % 2)], W=[("B", bk)])
                    xs = self.x[:, t, half * 512:(half + 1) * 512]
                    self.tt("dve", xs, xs, B[bk][:], ALU.add, R=[("B", bk), ("x", t)], W=[("x", t)])
            if gi + 2 < len(groups):
                load_d(gi + 2)

        gateup(0)
        load_d(1)
        for gi in range(len(groups)):
            if gi + 1 < len(groups):
                gateup(gi + 1)
            down(gi)

    def final_phase(self):
        P = self.P
        P.barrier()
        self.off = 0
        ob = [self.carve([128, D], F32) for _ in range(2)]
        if self.final_norm:
            gi = self.load_gain(self.din("final_norm", [1, D])[0:1, :])
            self.norm_stats(ob[0])
        stores = []
        for t in range(NT):
            o = ob[t % 2]
            if self.final_norm:
                self.stt("dve", o, self.x[:, t, :], self.rstd[:, t:t + 1], self.gain_bc[gi][:], ALU.mult, ALU.mult,
                         R=[("x", t), "rstd", ("gain", gi)], W=[("ob", t % 2), "junk"] if t % 2 == 0 else [("ob", t % 2)])
                src, rk = o, [("ob", t % 2)]
            else:
                src, rk = self.x[:, t, :], [("x", t)]
            stores.append(self.dma("sp", self.out[t * 128:(t + 1) * 128, :], src, slot=("st", t), R=rk, W=[("outd", t)]))
        P.add("sp", None, extra=stores)


_CACHE = {}


def _get_prog(layers, final_norm):
    key = (tuple(layers), final_norm)
    if key not in _CACHE:
        b = Builder(layers, final_norm)
        nc = b.build()
        _CACHE[key] = (nc, sorted(b.dram.keys()))
    return _CACHE[key]


def _run(layers, final_norm, xs, inputs):
    nc, names = _get_prog(layers, final_norm)
    in_maps = []
    for c in range(N_CORES):
        m = {}
        for n in names:
            if n == "x":
                m[n] = np.ascontiguousarray(xs[c])
            elif n == "final_norm":
                m[n] = np.ascontiguousarray(inputs[n]).reshape(1, D)
            else:
                m[n] = np.ascontiguousarray(inputs[n])
        in_maps.append(m)
    res = run_bass_kernel_spmd(nc, in_maps, core_ids=list(range(N_CORES)))
    return [res.results[c]["out"] for c in range(N_CORES)]


LAUNCH_GROUPS = [[0, 1, 2, 3]]


def kernel(**inputs):
    inputs = {k: np.asarray(v, dtype=np.float32) for k, v in inputs.items()}
    xs = [inputs["x"][b] for b in range(N_CORES)]
    for gi, grp in enumerate(LAUNCH_GROUPS):
        xs = _run(grp, gi == len(LAUNCH_GROUPS) - 1, xs, inputs)
    return np.stack(xs, axis=0).astype(np.float32)
```

```python
import numpy as np
from contextlib import ExitStack
import concourse.bass as bass
import concourse.mybir as mybir
from concourse.bass_utils import run_bass_kernel_spmd

F32 = mybir.dt.float32
BF16 = mybir.dt.bfloat16
U8 = mybir.dt.uint8
AF = mybir.ActivationFunctionType
ALU = mybir.AluOpType
AX = mybir.AxisListType

S = 2048
D = 1024
NT = 16
KC = 8
H = 8
DFF = 2816
NJ = 22
EPS = 1e-6
N_CORES = 8
DEPTH = 4
WINS = (2, 4, 8, 16)


class _Op:
    __slots__ = ("eng", "fn", "waits", "sig", "pos", "gid", "slot", "cnt", "known", "sidx")


class Prog:
    ENGS = ("pe", "act", "dve", "pool", "sp")

    def __init__(self, self_sync=True):
        self.streams = {e: [] for e in self.ENGS}
        self.known = {e: {} for e in self.ENGS}
        self.last_w = {}
        self.readers = {}
        self.slot_cnt = {}
        self.slot_last = {}
        self.gid = 0
        self.self_sync = self_sync

    def add(self, eng, fn, R=(), W=(), slot=None, extra=()):
        op = _Op()
        op.eng, op.fn, op.waits, op.sig, op.slot, op.cnt, op.sidx = eng, fn, [], False, slot, 0, 0
        op.gid = self.gid
        self.gid += 1
        st = self.streams[eng]
        op.pos = len(st)
        st.append(op)
        psum_r = [k for k in R if isinstance(k, tuple) and k[0] == "B"]
        if psum_r:
            R = [k for k in R if not (isinstance(k, tuple) and k[0] == "B")]
            W = list(W) + psum_r
        deps = {}
        for k in R:
            lw = self.last_w.get(k)
            if lw is not None:
                deps[lw.gid] = lw
        for k in W:
            lw = self.last_w.get(k)
            if lw is not None:
                deps[lw.gid] = lw
            for r in self.readers.get(k, ()):
                deps[r.gid] = r
        for d in extra:
            deps[d.gid] = d
        kn = self.known[eng]
        for g in sorted(deps, reverse=True):
            d = deps[g]
            if d.slot is not None:
                src, val = d.slot, d.cnt
            else:
                if d.eng == eng and (eng == "pe" or not self.self_sync):
                    continue
                src, val = d.eng, d.pos
            if kn.get(src, -1) >= val:
                continue
            assert d.fn is not None
            op.waits.append(d)
            d.sig = True
            for s_, v_ in d.known.items():
                if kn.get(s_, -1) < v_:
                    kn[s_] = v_
        if slot is not None:
            c = self.slot_cnt.get(slot, 0) + 1
            self.slot_cnt[slot] = c
            op.cnt = c
            prev = self.slot_last.get(slot)
            if prev is not None:
                assert kn.get(slot, -1) >= prev.cnt, ("two DMAs in flight on slot", slot)
            self.slot_last[slot] = op
            op.known = dict(kn)
            op.known[slot] = c
        else:
            op.known = dict(kn)
            op.known[eng] = op.pos
        for k in R:
            self.readers.setdefault(k, []).append(op)
        for k in W:
            self.last_w[k] = op
            self.readers[k] = []
        return op

    def barrier(self):
        lasts = []
        for e in self.ENGS:
            for op in reversed(self.streams[e]):
                if op.fn is not None and op.slot is None:
                    lasts.append(op)
                    break
        lasts += list(self.slot_last.values())
        for e in self.ENGS:
            self.add(e, None, extra=lasts)

    def emit(self, block, sems, slot_sems):
        for e in self.ENGS:
            n = 0
            for op in self.streams[e]:
                if op.slot is None and op.sig:
                    n += 1
                    op.sidx = n

        def body_for(e):
            st = self.streams[e]

            def body(eng):
                for op in st:
                    for d in op.waits:
                        if d.slot is not None:
                            eng.wait_ge(slot_sems[d.slot], 16 * d.cnt)
                        else:
                            eng.wait_ge(sems[d.eng], d.sidx)
                    if op.fn is not None:
                        ins = op.fn(eng)
                        if op.slot is not None:
                            ins.then_inc(slot_sems[op.slot], 16)
                        elif op.sig:
                            ins.then_inc(sems[e], 1)
            return body

        block.tensor(body_for("pe"))
        block.scalar(body_for("act"))
        block.vector(body_for("dve"))
        block.gpsimd(body_for("pool"))
        block.sync(body_for("sp"))


def _dsize(dt):
    return {F32: 4, BF16: 2, U8: 1}[dt]


class Builder:
    def __init__(self, layers, final_norm, self_sync=True, parts=("mixer", "ffn"), ntiles=NT):
        self.parts = parts
        self.ntiles = ntiles
        self.layers = list(layers)
        self.final_norm = final_norm
        self.nc = bass.Bass("TRN2", target_bir_lowering=False)
        self.P = Prog(self_sync=self_sync)
        self.dram = {}
        self.slots = set()

    def din(self, name, shape):
        if name not in self.dram:
            self.dram[name] = self.nc.dram_tensor(name, list(shape), F32, kind="ExternalInput").ap()
        return self.dram[name]

    def view(self, off, shape, dt):
        n = 1
        for s_ in shape[1:]:
            n *= s_
        nb = n * _dsize(dt)
        assert off % 32 == 0 and off + nb <= self.arena_bytes, (off, nb, self.arena_bytes)
        ap = self.arena[:, off:off + nb].bitcast(dt)
        if len(shape) == 3:
            ap = ap.rearrange("p (a b) -> p a b", a=shape[1])
        elif len(shape) == 4:
            ap = ap.rearrange("p (a b c) -> p a b c", a=shape[1], b=shape[2])
        return ap

    def carve(self, shape, dt):
        n = 1
        for s_ in shape[1:]:
            n *= s_
        nb = (n * _dsize(dt) + 31) // 32 * 32
        v = self.view(self.off, shape, dt)
        self.off += nb
        return v

    def dma(self, eng, out, in_, slot, R=(), W=()):
        self.slots.add(slot)
        return self.P.add(eng, lambda e: e.dma_start(out=out, in_=in_), R=R, W=W, slot=slot)

    def mm(self, out, lhsT, rhs, start, stop, R=(), W=()):
        return self.P.add("pe", lambda e: e.matmul(out, lhsT=lhsT, rhs=rhs, start=start, stop=stop), R=R, W=W)

    def tr(self, out, in_, ident, R=(), W=()):
        return self.P.add("pe", lambda e: e.transpose(out, in_, ident), R=R, W=W)

    def act(self, out, in_, func, R=(), W=(), **kw):
        return self.P.add("act", lambda e: e.activation(out=out, in_=in_, func=func, **kw), R=R, W=W)

    def tt(self, eng, out, in0, in1, op, R=(), W=()):
        return self.P.add(eng, lambda e: e.tensor_tensor(out=out, in0=in0, in1=in1, op=op), R=R, W=W)

    def ts(self, eng, out, in0, s1, s2, op0, op1=None, R=(), W=()):
        if op1 is None:
            return self.P.add(eng, lambda e: e.tensor_scalar(out=out, in0=in0, scalar1=s1, scalar2=None, op0=op0), R=R, W=W)
        return self.P.add(eng, lambda e: e.tensor_scalar(out=out, in0=in0, scalar1=s1, scalar2=s2, op0=op0, op1=op1), R=R, W=W)

    def stt(self, eng, out, in0, scalar, in1, op0, op1, R=(), W=()):
        return self.P.add(eng, lambda e: e.scalar_tensor_tensor(out=out, in0=in0, scalar=scalar, in1=in1, op0=op0, op1=op1), R=R, W=W)

    def copy(self, eng, out, in_, R=(), W=()):
        return self.P.add(eng, lambda e: e.tensor_copy(out, in_), R=R, W=W)

    def memset(self, eng, ap, val, R=(), W=()):
        return self.P.add(eng, lambda e: e.memset(ap, val), R=R, W=W)

    def build(self):
        nc = self.nc
        P = self.P
        with ExitStack() as es:
            def sb(name, shape, dt):
                return es.enter_context(nc.sbuf_tensor(name, shape, dt))

            self.x = sb("x_res", [128, NT, D], F32)
            self.ident_bf = sb("ident_bf", [128, 128], BF16)
            self.ident_f = sb("ident_f", [128, 128], F32)
            self.causal = sb("causal", [128, 128], BF16)
            self.negtri = sb("negtri", [128, 128], F32)
            self.negones = sb("negones", [128, 128], F32)
            self.invcnt = sb("invcnt", [128, 4, 16], F32)
            self.small = sb("small", [128, 512], F32)
            self.gain_bc = [sb(f"gain_bc{i}", [128, D], F32) for i in range(2)]
            self.gain_n = 0
            self.arena_bytes = (nc.sbuf_bytes_remaining - 256) // 64 * 64
            self.arena = sb("arena", [128, self.arena_bytes], U8)
            self.B = [es.enter_context(nc.psum_tensor(f"B{i}", [128, 512], F32)) for i in range(8)]
            self.out = nc.dram_tensor("out", [S, D], F32, kind="ExternalOutput").ap()
            xin = self.din("x", [S, D])

            sm = self.small
            self.ss = sm[:, 0:16]
            self.lnv = sm[:, 16:32]
            self.rstd = sm[:, 32:48]

            self.setup_consts()
            for t in range(NT):
                self.dma("sp", self.x[:, t, :], xin[t * 128:(t + 1) * 128, :], slot=("xld", t), W=[("x", t)])

            for l in self.layers:
                if "mixer" in self.parts:
                    if l % 2 == 0:
                        self.mlstm_phase(l)
                    else:
                        self.pool_phase(l)
                if "ffn" in self.parts:
                    self.ffn_phase(l)
            self.final_phase()

            sems = {e: es.enter_context(nc.semaphore(f"s_{e}")) for e in Prog.ENGS}
            slot_sems = {}
            for i, sl in enumerate(sorted(self.slots, key=str)):
                slot_sems[sl] = es.enter_context(nc.semaphore(f"d_{i}"))
            block = es.enter_context(nc.Block())
            P.emit(block, sems, slot_sems)
        return nc

    def setup_consts(self):
        def sel(ap, cmp, pattern, cm, key):
            self.P.add("pool", lambda e: e.affine_select(out=ap, in_=ap, compare_op=cmp, fill=0.0, base=0,
                                                         pattern=pattern, channel_multiplier=cm), W=[key])
        self.memset("pool", self.ident_bf[:], 1.0, W=["ident_bf"])
        sel(self.ident_bf[:], ALU.is_equal, [[-1, 128]], 1, "ident_bf")
        self.memset("pool", self.ident_f[:], 1.0, W=["ident_f"])
        sel(self.ident_f[:], ALU.is_equal, [[-1, 128]], 1, "ident_f")
        self.memset("pool", self.causal[:], 1.0, W=["causal"])
        sel(self.causal[:], ALU.is_ge, [[1, 128]], -1, "causal")
        self.memset("pool", self.negtri[:], -1.0, W=["negtri"])
        sel(self.negtri[:], ALU.is_ge, [[1, 128]], -1, "negtri")
        self.memset("pool", self.negones[:], -1.0, W=["negones"])
        for wi, w in enumerate(WINS):
            self.memset("pool", self.invcnt[:, wi, :], 1.0 / w, W=["invcnt"])
            for t in range(w - 1):
                self.memset("pool", self.invcnt[:, wi, t:t + 1], 1.0 / (t + 1), W=["invcnt"])

    def load_gain(self, row_ap):
        i = self.gain_n % 2
        self.gain_n += 1
        self.dma("sp", self.gain_bc[i][:], row_ap.partition_broadcast(128), slot=("gain", i), W=[("gain", i)])
        return i

    def emit_ss(self, t, junk):
        self.act(junk, self.x[:, t, :], AF.Square, R=[("x", t)], W=["junk", ("ss", t)], accum_out=self.ss[:, t:t + 1])

    def norm_stats(self, junk):
        if not getattr(self, "stats_ready", False):
            for t in range(NT):
                self.emit_ss(t, junk)
        self.stats_ready = False
        self.act(self.lnv, self.ss, AF.Ln, R=[("ss", t) for t in range(NT)], W=["lnv"], scale=1.0 / D, bias=EPS)
        self.act(self.rstd, self.lnv, AF.Exp, R=["lnv"], W=["rstd"], scale=-0.5)

    def norm_to_hT(self, gain_row, hT, hb):
        gi = self.load_gain(gain_row)
        self.norm_stats(hb[0])
        psT = self.B[7][:].bitcast(BF16)
        for t in range(NT):
            hbt = hb[t % 2]
            self.stt("dve", hbt, self.x[:, t, :], self.rstd[:, t:t + 1], self.gain_bc[gi][:], ALU.mult, ALU.mult,
                     R=[("x", t), "rstd", ("gain", gi)], W=[("hb", t % 2), "junk"] if t % 2 == 0 else [("hb", t % 2)])
            for kc in range(KC):
                self.tr(psT[:, kc * 128:(kc + 1) * 128], hbt[:, kc * 128:(kc + 1) * 128], self.ident_bf[:],
                        R=[("hb", t % 2), "ident_bf"], W=[("B", 7)])
            self.act(hT[:, :, t * 128:(t + 1) * 128], psT.rearrange("p (k s) -> p k s", k=KC), AF.Copy,
                     R=[("B", 7)], W=[("hT", t)])

    def mlstm_phase(self, l):
        P = self.P
        slot = l // 2
        P.barrier()
        self.off = 0
        NTL = self.ntiles
        win = self.carve([128, KC, 3088], BF16)
        wout = self.carve([128, KC, D], BF16)
        hgain = self.carve([128, D], F32)
        bias_bc = self.carve([128, 16], F32)
        hb = [self.carve([128, D], BF16) for _ in range(2)]
        hTt = [self.carve([128, KC, 128], BF16) for _ in range(2)]
        q_bf = self.carve([128, 512], BF16)
        k_bf = self.carve([128, 512], BF16)
        kE = [self.carve([128, 512], BF16) for _ in range(2)]
        kO = [self.carve([128, 512], BF16) for _ in range(2)]
        qkT = [self.carve([128, 8, 128], BF16) for _ in range(2)]
        vs = [self.carve([128, H, 128], BF16) for _ in range(2)]
        eo = [self.carve([128, D], F32) for _ in range(2)]
        PT = self.carve([128, H, 128], BF16)
        hcs = [self.carve([128, D], F32) for _ in range(2)]
        outbs = [self.carve([128, D], BF16) for _ in range(2)]
        outTs = [self.carve([128, 8, 128], BF16) for _ in range(2)]
        C32 = self.carve([128, 4, 132], F32)
        Cbf = [self.carve([128, 4, 132], BF16) for _ in range(2)]
        eabf = [self.carve([128, 16], BF16) for _ in range(2)]
        sqb = self.carve([128, D], F32)
        jkD = self.carve([128, D], BF16)
        sm = self.small
        def smv(p):
            o = 64 + p * 120
            names = [("ssA", 1), ("lnA", 1), ("rsA", 1), ("g1", 16), ("e2", 16), ("dd", 16), ("gs", 16), ("ee", 8),
                     ("lfn", 8), ("a", 8), ("ea", 8), ("eb", 8), ("gp", 4), ("gsel", 4)]
            d_ = {}
            for n_, w_ in names:
                d_[n_] = sm[:, o:o + w_]
                o += w_
            return d_
        SV = [smv(0), smv(1)]
        d1, d2, scl, ssh, l2, rs = (sm[:, 320:328], sm[:, 328:336], sm[:, 336:344], sm[:, 344:352], sm[:, 352:360], sm[:, 360:368])
        B = self.B

        w_in = self.din("mlstm_w_in", [2, D, 3088])[slot].rearrange("(k p) n -> p k n", p=128)
        w_out = self.din("mlstm_w_out", [2, D, D])[slot].rearrange("(k p) n -> p k n", p=128)
        pieces = [(0, 1024), (3072, 3088), (1024, 2048), (2048, 3072)]
        pkey = {0: 0, 1: 0, 2: 2, 3: 2, 4: 3, 5: 3}
        for i, (c0, c1) in enumerate(pieces):
            self.dma("pool", win[:, :, c0:c1], w_in[:, :, c0:c1], slot=("win", i), W=[("win", i)])
        self.dma("pool", wout, w_out, slot=("wout",), W=["wout"])
        self.dma("sp", hgain, self.din("mlstm_head_gain", [2, D])[slot:slot + 1, :].partition_broadcast(128),
                 slot=("hgain",), W=["hgain"])
        self.dma("sp", bias_bc, self.din("mlstm_gate_bias", [2, 16])[slot:slot + 1, :].partition_broadcast(128),
                 slot=("gbias",), W=["gbias"])
        gi = self.load_gain(self.din("norm_mix", [DEPTH, D])[l:l + 1, :])
        self.memset("dve", C32, 0.0, W=["C32"])
        self.memset("dve", Cbf[0], 0.0, W=[("Cbf", 0)])
        for p in range(2):
            self.memset("dve", kE[p], 0.0, W=[("kE", p)])
            self.memset("dve", kO[p], 0.0, W=[("kO", p)])

        psT = B[7][:].bitcast(BF16)
        psT3 = psT.rearrange("p (k s) -> p k s", k=8)

        def b4(i):
            return B[i][:].rearrange("p (h s) -> p h s", h=4)

        def A0(t):
            p = t % 2
            v = SV[p]
            self.act(hb[p], self.x[:, t, :], AF.Square, R=[("x", t)], W=[("hb", p)], accum_out=v["ssA"])
            self.act(v["lnA"], v["ssA"], AF.Ln, R=[("hb", p)], W=[("lnA", p)], scale=1.0 / D, bias=EPS)
            self.act(v["rsA"], v["lnA"], AF.Exp, R=[("lnA", p)], W=[("rsA", p)], scale=-0.5)
            self.stt("dve", hb[p], self.x[:, t, :], v["rsA"], self.gain_bc[gi][:], ALU.mult, ALU.mult,
                     R=[("x", t), ("rsA", p), ("gain", gi)], W=[("hb", p)])

        def A1(t):
            p = t % 2
            for kc in range(KC):
                self.tr(psT[:, kc * 128:(kc + 1) * 128], hb[p][:, kc * 128:(kc + 1) * 128], self.ident_bf[:],
                        R=[("hb", p), "ident_bf"], W=[("B", 7)])
            self.act(hTt[p], psT3, AF.Copy, R=[("B", 7)], W=[("hTt", p)])

        def proj(t, cb, bank):
            p = t % 2
            for kc in range(KC):
                self.mm(B[bank][:], hTt[p][:, kc, :], win[:, kc, cb * 512:(cb + 1) * 512], kc == 0, kc == KC - 1,
                        R=[("hTt", p), ("win", pkey[cb])], W=[("B", bank)])

        def A2(t, late_fn=None):
            p = t % 2
            v = SV[p]
            proj(t, 0, 4)
            self.act(q_bf, B[4][:], AF.Copy, R=[("B", 4)], W=["q_bf"])
            proj(t, 1, 5)
            for kc in range(KC):
                self.mm(B[6][:, 0:16], hTt[p][:, kc, :], win[:, kc, 3072:3088], kc == 0, kc == KC - 1,
                        R=[("hTt", p), ("win", 1)], W=[("B", 6)])
            self.act(k_bf, B[5][:], AF.Copy, R=[("B", 5)], W=["k_bf"], scale=0.125)
            k4 = B[5][:].rearrange("p (j two c) -> p j two c", two=2, c=64)
            kE4 = kE[p].rearrange("p (j two c) -> p j two c", two=2, c=64)
            kO4 = kO[p].rearrange("p (j two c) -> p j two c", two=2, c=64)
            self.ts("dve", kE4[:, :, 0, :], k4[:, :, 0, :], 0.125, None, ALU.mult, R=[("B", 5)], W=[("kE", p)])
            self.ts("dve", kO4[:, :, 1, :], k4[:, :, 1, :], 0.125, None, ALU.mult, R=[("B", 5)], W=[("kO", p)])
            self.tt("dve", v["g1"], B[6][:, 0:16], bias_bc, ALU.add, R=[("B", 6), "gbias"], W=[("g1", p)])
            self.act(v["e2"], v["g1"], AF.Exp, R=[("g1", p)], W=[("e2", p)], scale=2.0 / 15.0)
            self.ts("dve", v["dd"], v["e2"], 1.0, None, ALU.add, R=[("e2", p)], W=[("dd", p)])
            self.P.add("dve", (lambda o_, i_: (lambda e: e.reciprocal(out=o_, in_=i_)))(v["dd"], v["dd"]), R=[("dd", p)], W=[("dd", p)])
            self.ts("dve", v["gs"], v["dd"], -30.0, 15.0, ALU.mult, ALU.add, R=[("dd", p)], W=[("gs", p)])
            self.act(v["ee"], v["gs"][:, 8:16], AF.Exp, R=[("gs", p)], W=[("ee", p)], scale=-1.0)
            self.act(v["lfn"], v["ee"], AF.Ln, R=[("ee", p)], W=[("lfn", p)], bias=1.0)
            if late_fn is not None:
                late_fn()
            self.mm(B[6][:, 16:24], self.negtri[:], v["lfn"], True, True, R=[("lfn", p), "negtri"], W=[("B", 6)])
            self.mm(B[6][:, 24:32], self.negones[:], v["lfn"], True, True, R=[("lfn", p), "negones"], W=[("B", 6)])
            self.tt("dve", v["a"], v["gs"][:, 0:8], B[6][:, 16:24], ALU.subtract, R=[("gs", p), ("B", 6)], W=[("a", p)])
            self.act(v["eb"], B[6][:, 16:24], AF.Exp, R=[("B", 6)], W=[("eb", p)])
            bl2 = B[6][:, 24:32].rearrange("p (j two) -> p j two", two=2)
            self.copy("dve", v["gp"][0:64, :], bl2[0:64, :, 0], R=[("B", 6)], W=[("gp0", p)])
            self.copy("dve", v["gp"][64:128, :], bl2[64:128, :, 1], R=[("B", 6)], W=[("gp1", p)])
            self.act(v["ea"], v["a"], AF.Exp, R=[("a", p)], W=[("ea", p)])
            self.copy("dve", eabf[p][:, 0:8], v["ea"], R=[("ea", p)], W=[("eabf", p)])
            self.act(v["gsel"], v["gp"], AF.Exp, R=[("gp0", p), ("gp1", p)], W=[("gsel", p)])

        def A3(t):
            p = t % 2
            v = SV[p]
            for j in range(4):
                self.tr(psT[:, j * 128:(j + 1) * 128], q_bf[:, j * 128:(j + 1) * 128], self.ident_bf[:],
                        R=["q_bf", "ident_bf"], W=[("B", 7)])
            for j in range(4):
                self.tr(psT[:, (4 + j) * 128:(5 + j) * 128], k_bf[:, j * 128:(j + 1) * 128], self.ident_bf[:],
                        R=["k_bf", "ident_bf"], W=[("B", 7)])
            self.act(qkT[p], psT3, AF.Copy, R=[("B", 7)], W=[("qkT", p)])
            for i in range(2):
                proj(t, 2 + i, 4 + i)
                self.tt("dve", vs[p][:, 4 * i:4 * i + 4, :], b4(4 + i),
                        v["ea"][:, 4 * i:4 * i + 4].unsqueeze(2).to_broadcast([128, 4, 128]), ALU.mult,
                        R=[("B", 4 + i), ("ea", p)], W=[("vs", p, i)])
            for i in range(2):
                proj(t, 4 + i, 4 + i)
                self.act(eo[p][:, i * 512:(i + 1) * 512], B[4 + i][:], AF.Exp, R=[("B", 4 + i)], W=[("eo", p)], scale=-1.0)

        def A3b(t):
            p = t % 2
            self.act(eo[p], eo[p], AF.Ln, R=[("eo", p)], W=[("eo", p)], bias=1.0)
            self.act(eo[p], eo[p], AF.Exp, R=[("eo", p)], W=[("eo", p)], scale=-1.0)
            self.tt("dve", eo[p], eo[p], hgain, ALU.mult, R=[("eo", p), "hgain"], W=[("eo", p)])

        def B1(t):
            p = t % 2
            for h in range(H):
                p0 = (h % 2) * 64
                self.mm(b4(h % 2)[:, h // 2, :], qkT[p][p0:p0 + 64, 4 + h // 2, :], qkT[p][p0:p0 + 64, h // 2, :], True, True,
                        R=[("qkT", p)], W=[("B", h % 2)])
            PT4 = PT.rearrange("p (j two) s -> p j two s", two=2)
            for i in range(2):
                self.tt("dve", PT4[:, :, i, :], b4(i), self.causal[:].unsqueeze(1).to_broadcast([128, 4, 128]), ALU.mult,
                        R=[("B", i), "causal"], W=[("PT", i)])

        def B2(t):
            p = t % 2
            v = SV[p]
            cb_ = Cbf[p]
            for h in range(H):
                p0 = (h % 2) * 64
                qTh = qkT[p][p0:p0 + 64, h // 2, :]
                self.mm(b4(2 + h // 4)[:, h % 4, :], PT[:, h, :], vs[p][:, h, :], True, False,
                        R=[("PT", h % 2), ("vs", p, h // 4)], W=[("B", 2 + h // 4)])
                self.mm(b4(2 + h // 4)[:, h % 4, :], qTh, cb_[p0:p0 + 64, h // 2, 0:128], False, True,
                        R=[("qkT", p), ("Cbf", p)], W=[("B", 2 + h // 4)])
                self.mm(B[6][:, 32 + h:33 + h], PT[:, h, :], eabf[p][:, h:h + 1], True, False,
                        R=[("PT", h % 2), ("eabf", p)], W=[("B", 6)])
                self.mm(B[6][:, 32 + h:33 + h], qTh, cb_[p0:p0 + 64, h // 2, 128:129], False, True,
                        R=[("qkT", p), ("Cbf", p)], W=[("B", 6)])
            for j in range(4):
                self.mm(b4(0)[:, j, :], kE[p][:, j * 128:(j + 1) * 128], vs[p][:, 2 * j, :], True, False,
                        R=[("kE", p), ("vs", p, j // 2)], W=[("B", 0)])
                self.mm(b4(0)[:, j, :], kO[p][:, j * 128:(j + 1) * 128], vs[p][:, 2 * j + 1, :], False, True,
                        R=[("kO", p), ("vs", p, j // 2)], W=[("B", 0)])
                self.mm(B[6][:, 40 + j:41 + j], kE[p][:, j * 128:(j + 1) * 128], eabf[p][:, 2 * j:2 * j + 1], True, False,
                        R=[("kE", p), ("eabf", p)], W=[("B", 6)])
                self.mm(B[6][:, 40 + j:41 + j], kO[p][:, j * 128:(j + 1) * 128], eabf[p][:, 2 * j + 1:2 * j + 2], False, True,
                        R=[("kO", p), ("eabf", p)], W=[("B", 6)])
            self.tt("dve", d1, B[6][:, 32:40], v["eb"], ALU.mult, R=[("B", 6), ("eb", p)], W=["d1"])
            self.stt("dve", d2, d1, -1.0, d1, ALU.mult, ALU.max, R=["d1"], W=["d2"])
            self.ts("dve", d2, d2, 1.0, None, ALU.max, R=["d2"], W=["d2"])
            self.P.add("dve", lambda e: e.reciprocal(out=d2, in_=d2), R=["d2"], W=["d2"])
            self.tt("dve", scl, d2, v["eb"], ALU.mult, R=["d2", ("eb", p)], W=["scl"])
            hc3 = hcs[p].rearrange("p (h s) -> p h s", h=H)
            for i in range(2):
                self.tt("dve", hc3[:, 4 * i:4 * i + 4, :], b4(2 + i),
                        scl[:, 4 * i:4 * i + 4].unsqueeze(2).to_broadcast([128, 4, 128]), ALU.mult,
                        R=[("B", 2 + i), "scl"], W=[("hc", p)])
            self.tt("dve", C32[:, :, 0:128], C32[:, :, 0:128], b4(0), ALU.add, R=["C32", ("B", 0)], W=["C32"])
            self.tt("dve", C32[:, :, 128], C32[:, :, 128], B[6][:, 40:44], ALU.add, R=["C32", ("B", 6)], W=["C32"])
            self.tt("dve", C32, C32, v["gsel"].unsqueeze(2).to_broadcast([128, 4, 132]), ALU.mult, R=["C32", ("gsel", p)], W=["C32"])
            self.act(Cbf[1 - p], C32, AF.Copy, R=["C32"], W=[("Cbf", 1 - p)])

        def B3(t):
            p = t % 2
            hc = hcs[p]
            hc3 = hc.rearrange("p (h s) -> p h s", h=H)
            self.act(sqb, hc, AF.Square, R=[("hc", p)], W=["sqb"])
            self.P.add("dve", lambda e: e.reduce_sum(out=ssh, in_=sqb.rearrange("p (h s) -> p h s", h=H), axis=AX.X),
                       R=["sqb"], W=["ssh"])
            self.act(l2, ssh, AF.Ln, R=["ssh"], W=["l2"], scale=1.0 / 128.0, bias=EPS)
            self.act(rs, l2, AF.Exp, R=["l2"], W=["rs"], scale=-0.5)
            self.tt("dve", hc3, hc3, rs.unsqueeze(2).to_broadcast([128, H, 128]), ALU.mult,
                    R=[("hc", p), "rs"], W=[("hc", p)])
            self.tt("dve", outbs[p], hc, eo[p], ALU.mult, R=[("hc", p), ("eo", p)], W=[("outb", p)])

        def B3pe(t):
            p = t % 2
            for j in range(8):
                self.tr(psT[:, j * 128:(j + 1) * 128], outbs[p][:, j * 128:(j + 1) * 128], self.ident_bf[:],
                        R=[("outb", p), "ident_bf"], W=[("B", 7)])
            self.act(outTs[p], psT3, AF.Copy, R=[("B", 7)], W=[("outT", p)])

        def B4(t):
            p = t % 2
            for half in range(2):
                bk = half
                for vc in range(8):
                    self.mm(B[bk][:], outTs[p][:, vc, :], wout[:, vc, half * 512:(half + 1) * 512], vc == 0, vc == 7,
                            R=[("outT", p), "wout"], W=[("B", bk)])
                xs = self.x[:, t, half * 512:(half + 1) * 512]
                self.tt("dve", xs, xs, B[bk][:], ALU.add, R=[("B", bk), ("x", t)], W=[("x", t)])
            if NTL == NT:
                self.emit_ss(t, jkD)

        A0(0)
        if NTL > 1:
            A0(1)
        A1(0)
        A2(0)
        A3(0)
        for t in range(NTL + 1):
            cur = t < NTL
            nxt = t + 1 < NTL
            late = t >= 1
            if cur:
                B1(t)
            if nxt:
                A1(t + 1)
            if t + 2 < NTL:
                A0(t + 2)
            if late:
                B3(t - 1)
            if cur:
                B2(t)
            if late:
                B3pe(t - 1)
            if nxt:
                A2(t + 1, late_fn=(lambda tt=t: B4(tt - 1)) if late else None)
            elif late:
                B4(t - 1)
            if nxt:
                A3(t + 1)
            if cur:
                A3b(t)
        self.stats_ready = (NTL == NT)

    def mlstm_phase_v1(self, l):
        P = self.P
        slot = l // 2
        P.barrier()
        self.off = 0
        hT = self.carve([128, KC, S], BF16)
        win = self.carve([128, KC, 3088], BF16)
        wout = self.carve([128, KC, D], BF16)
        hb = [self.carve([128, D], BF16) for _ in range(2)]
        q_bf = self.carve([128, 512], BF16)
        k_bf = self.carve([128, 512], BF16)
        kE = self.carve([128, 512], BF16)
        kO = self.carve([128, 512], BF16)
        qkT = self.carve([128, 8, 128], BF16)
        vs = self.carve([128, H, 128], BF16)
        PT = self.carve([128, H, 128], BF16)
        sig = self.carve([128, D], F32)
        hc = self.carve([128, D], F32)
        outT = self.carve([128, 8, 128], BF16)
        C32 = self.carve([128, 4, 132], F32)
        Cbf = self.carve([128, 4, 132], BF16)
        hgain = self.carve([128, D], F32)
        bias_bc = self.carve([128, 16], F32)
        eabf = self.carve([128, 8], BF16)
        jk = self.carve([128, 128], BF16)
        outb = hb[0]
        sm = self.small
        g1, th, gs, ee, lfn = sm[:, 64:80], sm[:, 80:96], sm[:, 96:112], sm[:, 112:120], sm[:, 120:128]
        a_, ea, eb, gp, gsel = sm[:, 128:136], sm[:, 136:144], sm[:, 144:152], sm[:, 152:156], sm[:, 156:160]
        d1, d2, scl, ssh, l2, rs = sm[:, 160:168], sm[:, 168:176], sm[:, 176:184], sm[:, 184:192], sm[:, 192:200], sm[:, 200:208]
        B = self.B

        w_in = self.din("mlstm_w_in", [2, D, 3088])[slot].rearrange("(k p) n -> p k n", p=128)
        w_out = self.din("mlstm_w_out", [2, D, D])[slot].rearrange("(k p) n -> p k n", p=128)
        pieces = [(0, 1024), (1024, 2048), (2048, 3072), (3072, 3088)]
        for i, (c0, c1) in enumerate(pieces):
            self.dma("pool", win[:, :, c0:c1], w_in[:, :, c0:c1], slot=("win", i), W=[("win", i)])
        self.dma("pool", wout, w_out, slot=("wout",), W=["wout"])
        self.dma("sp", hgain, self.din("mlstm_head_gain", [2, D])[slot:slot + 1, :].partition_broadcast(128),
                 slot=("hgain",), W=["hgain"])
        self.dma("sp", bias_bc, self.din("mlstm_gate_bias", [2, 16])[slot:slot + 1, :].partition_broadcast(128),
                 slot=("gbias",), W=["gbias"])
        self.memset("dve", C32, 0.0, W=["C32"])
        self.memset("dve", Cbf, 0.0, W=["Cbf"])
        self.memset("dve", kE, 0.0, W=["kE"])
        self.memset("dve", kO, 0.0, W=["kO"])

        self.norm_to_hT(self.din("norm_mix", [DEPTH, D])[l:l + 1, :], hT, hb)

        psT = B[7][:].bitcast(BF16)
        psT3 = psT.rearrange("p (k s) -> p k s", k=8)

        def b4(i):
            return B[i][:].rearrange("p (h s) -> p h s", h=4)

        wpiece = {0: 0, 1: 0, 2: 1, 3: 1, 4: 2, 5: 2}
        for t in range(self.ntiles):
            tok = slice(t * 128, (t + 1) * 128)
            for cb in range(6):
                for kc in range(KC):
                    self.mm(B[cb][:], hT[:, kc, tok], win[:, kc, cb * 512:(cb + 1) * 512], kc == 0, kc == KC - 1,
                            R=[("hT", t), ("win", wpiece[cb])], W=[("B", cb)])
            for kc in range(KC):
                self.mm(B[6][:, 0:16], hT[:, kc, tok], win[:, kc, 3072:3088], kc == 0, kc == KC - 1,
                        R=[("hT", t), ("win", 3)], W=[("B", 6)])
            self.tt("dve", g1, B[6][:, 0:16], bias_bc, ALU.add, R=[("B", 6), "gbias"], W=["g1"])
            self.act(th, g1, AF.Tanh, R=["g1"], W=["th"], scale=1.0 / 15.0)
            self.ts("dve", gs, th, 15.0, None, ALU.mult, R=["th"], W=["gs"])
            self.act(ee, gs[:, 8:16], AF.Exp, R=["gs"], W=["ee"], scale=-1.0)
            self.act(lfn, ee, AF.Ln, R=["ee"], W=["lfn"], bias=1.0)
            self.mm(B[6][:, 16:24], self.negtri[:], lfn, True, True, R=["lfn", "negtri"], W=[("B", 6)])
            self.mm(B[6][:, 24:32], self.negones[:], lfn, True, True, R=["lfn", "negones"], W=[("B", 6)])
            self.tt("dve", a_, gs[:, 0:8], B[6][:, 16:24], ALU.subtract, R=["gs", ("B", 6)], W=["a"])
            self.act(ea, a_, AF.Exp, R=["a"], W=["ea"])
            self.copy("dve", eabf, ea, R=["ea"], W=["eabf"])
            self.act(eb, B[6][:, 16:24], AF.Exp, R=[("B", 6)], W=["eb"])
            bl2 = B[6][:, 24:32].rearrange("p (j two) -> p j two", two=2)
            self.copy("dve", gp[0:64, :], bl2[0:64, :, 0], R=[("B", 6)], W=["gp0"])
            self.copy("dve", gp[64:128, :], bl2[64:128, :, 1], R=[("B", 6)], W=["gp1"])
            self.act(gsel, gp, AF.Exp, R=["gp0", "gp1"], W=["gsel"])
            self.act(q_bf, B[0][:], AF.Copy, R=[("B", 0)], W=["q_bf"])
            self.act(k_bf, B[1][:], AF.Copy, R=[("B", 1)], W=["k_bf"], scale=0.125)
            k4 = B[1][:].rearrange("p (j two c) -> p j two c", two=2, c=64)
            kE4 = kE.rearrange("p (j two c) -> p j two c", two=2, c=64)
            kO4 = kO.rearrange("p (j two c) -> p j two c", two=2, c=64)
            self.ts("dve", kE4[:, :, 0, :], k4[:, :, 0, :], 0.125, None, ALU.mult, R=[("B", 1)], W=["kE"])
            self.ts("dve", kO4[:, :, 1, :], k4[:, :, 1, :], 0.125, None, ALU.mult, R=[("B", 1)], W=["kO"])
            for j in range(4):
                self.tr(psT[:, j * 128:(j + 1) * 128], q_bf[:, j * 128:(j + 1) * 128], self.ident_bf[:],
                        R=["q_bf", "ident_bf"], W=[("B", 7)])
            for j in range(4):
                self.tr(psT[:, (4 + j) * 128:(5 + j) * 128], k_bf[:, j * 128:(j + 1) * 128], self.ident_bf[:],
                        R=["k_bf", "ident_bf"], W=[("B", 7)])
            self.act(qkT, psT3, AF.Copy, R=[("B", 7)], W=["qkT"])
            for i in range(2):
                self.tt("dve", vs[:, 4 * i:4 * i + 4, :], b4(2 + i),
                        ea[:, 4 * i:4 * i + 4].unsqueeze(2).to_broadcast([128, 4, 128]), ALU.mult,
                        R=[("B", 2 + i), "ea"], W=[("vs", i)])
            for i in range(2):
                self.act(sig[:, i * 512:(i + 1) * 512], B[4 + i][:], AF.Tanh, R=[("B", 4 + i)], W=[("sig", i)], scale=0.5)
            for h in range(H):
                p0 = (h % 2) * 64
                self.mm(b4(h % 2)[:, h // 2, :], qkT[p0:p0 + 64, 4 + h // 2, :], qkT[p0:p0 + 64, h // 2, :], True, True,
                        R=["qkT"], W=[("B", h % 2)])
            PT4 = PT.rearrange("p (j two) s -> p j two s", two=2)
            for i in range(2):
                self.tt("dve", PT4[:, :, i, :], b4(i),
                        self.causal[:].unsqueeze(1).to_broadcast([128, 4, 128]), ALU.mult,
                        R=[("B", i), "causal"], W=[("PT", i)])
            for h in range(H):
                p0 = (h % 2) * 64
                qTh = qkT[p0:p0 + 64, h // 2, :]
                self.mm(b4(2 + h // 4)[:, h % 4, :], PT[:, h, :], vs[:, h, :], True, False,
                        R=[("PT", h % 2), ("vs", h // 4)], W=[("B", 2 + h // 4)])
                self.mm(b4(2 + h // 4)[:, h % 4, :], qTh, Cbf[p0:p0 + 64, h // 2, 0:128], False, True,
                        R=["qkT", "Cbf"], W=[("B", 2 + h // 4)])
                self.mm(B[6][:, 32 + h:33 + h], PT[:, h, :], eabf[:, h:h + 1], True, False,
                        R=[("PT", h % 2), "eabf"], W=[("B", 6)])
                self.mm(B[6][:, 32 + h:33 + h], qTh, Cbf[p0:p0 + 64, h // 2, 128:129], False, True,
                        R=["qkT", "Cbf"], W=[("B", 6)])
            for j in range(4):
                self.mm(b4(4)[:, j, :], kE[:, j * 128:(j + 1) * 128], vs[:, 2 * j, :], True, False,
                        R=["kE", ("vs", j // 2)], W=[("B", 4)])
                self.mm(b4(4)[:, j, :], kO[:, j * 128:(j + 1) * 128], vs[:, 2 * j + 1, :], False, True,
                        R=["kO", ("vs", j // 2)], W=[("B", 4)])
                self.mm(B[6][:, 40 + j:41 + j], kE[:, j * 128:(j + 1) * 128], eabf[:, 2 * j:2 * j + 1], True, False,
                        R=["kE", "eabf"], W=[("B", 6)])
                self.mm(B[6][:, 40 + j:41 + j], kO[:, j * 128:(j + 1) * 128], eabf[:, 2 * j + 1:2 * j + 2], False, True,
                        R=["kO", "eabf"], W=[("B", 6)])
            self.tt("dve", C32[:, :, 0:128], C32[:, :, 0:128], b4(4), ALU.add, R=["C32", ("B", 4)], W=["C32"])
            self.tt("dve", C32[:, :, 128], C32[:, :, 128], B[6][:, 40:44], ALU.add, R=["C32", ("B", 6)], W=["C32"])
            self.tt("dve", C32, C32, gsel.unsqueeze(2).to_broadcast([128, 4, 132]), ALU.mult, R=["C32", "gsel"], W=["C32"])
            self.act(Cbf, C32, AF.Copy, R=["C32"], W=["Cbf"])
            self.tt("dve", d1, B[6][:, 32:40], eb, ALU.mult, R=[("B", 6), "eb"], W=["d1"])
            self.stt("dve", d2, d1, -1.0, d1, ALU.mult, ALU.max, R=["d1"], W=["d2"])
            self.ts("dve", d2, d2, 1.0, None, ALU.max, R=["d2"], W=["d2"])
            self.P.add("dve", lambda e: e.reciprocal(out=d2, in_=d2), R=["d2"], W=["d2"])
            self.tt("dve", scl, d2, eb, ALU.mult, R=["d2", "eb"], W=["scl"])
            hc3 = hc.rearrange("p (h s) -> p h s", h=H)
            for i in range(2):
                self.tt("dve", hc3[:, 4 * i:4 * i + 4, :], b4(2 + i),
                        scl[:, 4 * i:4 * i + 4].unsqueeze(2).to_broadcast([128, 4, 128]), ALU.mult,
                        R=[("B", 2 + i), "scl"], W=[("hc", i)])
            for h in range(H):
                self.act(jk, hc3[:, h, :], AF.Square, R=[("hc", h // 4)], W=["jk", "ssh"], accum_out=ssh[:, h:h + 1])
            self.act(l2, ssh, AF.Ln, R=["ssh"], W=["l2"], scale=1.0 / 128.0, bias=EPS)
            self.act(rs, l2, AF.Exp, R=["l2"], W=["rs"], scale=-0.5, bias=float(np.log(0.5)))
            self.tt("dve", hc3, hc3, rs.unsqueeze(2).to_broadcast([128, H, 128]), ALU.mult,
                    R=[("hc", 0), ("hc", 1), "rs"], W=[("hc", 0), ("hc", 1)])
            self.tt("pool", hc, hc, hgain, ALU.mult, R=[("hc", 0), ("hc", 1), "hgain"], W=[("hc", 0), ("hc", 1)])
            self.stt("dve", outb, sig, 1.0, hc, ALU.add, ALU.mult,
                     R=[("sig", 0), ("sig", 1), ("hc", 0), ("hc", 1)], W=[("hb", 0)])
            for j in range(8):
                self.tr(psT[:, j * 128:(j + 1) * 128], outb[:, j * 128:(j + 1) * 128], self.ident_bf[:],
                        R=[("hb", 0), "ident_bf"], W=[("B", 7)])
            self.act(outT, psT3, AF.Copy, R=[("B", 7)], W=["outT"])
            for half in range(2):
                bk = 5 if half == 0 else 1
                for vc in range(8):
                    self.mm(B[bk][:], outT[:, vc, :], wout[:, vc, half * 512:(half + 1) * 512], vc == 0, vc == 7,
                            R=["outT", "wout"], W=[("B", bk)])
                xs = self.x[:, t, half * 512:(half + 1) * 512]
                self.tt("dve", xs, xs, B[bk][:], ALU.add, R=[("B", bk), ("x", t)], W=[("x", t)])

    def pool_phase(self, l):
        P = self.P
        slot = l // 2
        P.barrier()
        self.off = 0
        band = [self.carve([128, 128], F32) for _ in range(4)]
        band0 = [self.carve([128, 128], F32) for _ in range(4)]
        offm = [self.carve([128, 128], F32) for _ in range(4)]
        sc16 = self.carve([128, 4, 16], F32)
        h32 = [self.carve([128, D], F32) for _ in range(3)]
        yT = [self.carve([128, KC, 128], BF16) for _ in range(2)]
        wp = self.carve([128, 4, 2, 256], BF16)
        pscale = self.carve([128, D], F32)
        tmp = [self.carve([128, 512], F32) for _ in range(2)]
        jkD = self.carve([128, D], BF16)
        B = self.B
        wsrc = self.din("pool_w_group", [2, 4, 256, 256])[slot]
        for g in range(4):
            self.dma("pool", wp[:, g, :, :], wsrc[g].rearrange("(k p) d -> p k d", p=128), slot=("wp", g), W=[("wp", g)])
        self.dma("sp", pscale, self.din("pool_scale", [2, D])[slot:slot + 1, :].partition_broadcast(128),
                 slot=("pscale",), W=["pscale"])
        gi = self.load_gain(self.din("norm_mix", [DEPTH, D])[l:l + 1, :])

        def sel(ap, pattern, cm, base, key):
            self.P.add("pool", lambda e: e.affine_select(out=ap, in_=ap, compare_op=ALU.is_ge, fill=0.0, base=base,
                                                         pattern=pattern, channel_multiplier=cm), W=[key])
        for wi, w in enumerate(WINS):
            self.memset("pool", band[wi], 1.0 / w, W=[("band", wi)])
            sel(band[wi], [[1, 128]], -1, 0, ("band", wi))
            sel(band[wi], [[-1, 128]], 1, w - 1, ("band", wi))
            self.memset("pool", offm[wi], 1.0 / w, W=[("off", wi)])
            sel(offm[wi], [[-1, 128]], 1, w - 129, ("off", wi))
            self.ts("dve", sc16[:, wi, :], self.invcnt[:, wi, :], float(w), None, ALU.mult, R=["invcnt"], W=[("sc16", wi)])
            self.copy("dve", band0[wi], band[wi], R=[("band", wi)], W=[("band0", wi)])
            self.tt("dve", band0[wi][:, 0:16], band[wi][:, 0:16], sc16[:, wi, :], ALU.mult,
                    R=[("band", wi), ("sc16", wi)], W=[("band0", wi)])
            self.tt("dve", band0[wi], band0[wi], self.ident_f[:], ALU.subtract, R=[("band0", wi), "ident_f"], W=[("band0", wi)])
            self.tt("dve", band[wi], band[wi], self.ident_f[:], ALU.subtract, R=[("band", wi), "ident_f"], W=[("band", wi)])

        self.norm_stats(jkD)

        def b4(i):
            return B[i][:].rearrange("p (h s) -> p h s", h=4)

        def norm_tile(t):
            self.stt("dve", h32[t % 3], self.x[:, t, :], self.rstd[:, t:t + 1], self.gain_bc[gi][:], ALU.mult, ALU.mult,
                     R=[("x", t), "rstd", ("gain", gi)], W=[("h32", t % 3)] + (["junk"] if t == 0 else []))

        def band_mm(t):
            par = t % 2
            for c in range(KC):
                wi = c // 2
                bk = 2 * par + c // 4
                out = b4(bk)[:, c % 4, :]
                cs = slice(c * 128, (c + 1) * 128)
                if t == 0:
                    self.mm(out, h32[0][:, cs], band0[wi], True, True, R=[("h32", 0), ("band0", wi)], W=[("B", bk)])
                else:
                    self.mm(out, h32[t % 3][:, cs], band[wi], True, False, R=[("h32", t % 3), ("band", wi)], W=[("B", bk)])
                    self.mm(out[:, 0:16], h32[(t - 1) % 3][:, cs], offm[wi][:, 0:16], False, True,
                            R=[("h32", (t - 1) % 3), ("off", wi)], W=[("B", bk)])

        def evac_y(t):
            par = t % 2
            self.act(yT[par][:, 0:4, :], b4(2 * par), AF.Copy, R=[("B", 2 * par)], W=[("yT", par, 0)])
            self.copy("dve", yT[par][:, 4:8, :], b4(2 * par + 1), R=[("B", 2 * par + 1)], W=[("yT", par, 1)])

        def pool_mm(t):
            par = t % 2
            for g in range(4):
                bk = 4 + 2 * par + g // 2
                for kc in range(2):
                    self.mm(B[bk][:, (g % 2) * 256:(g % 2 + 1) * 256], yT[par][:, 2 * g + kc, :], wp[:, g, kc, :], kc == 0, kc == 1,
                            R=[("yT", par, g // 2), ("wp", g)], W=[("B", bk)])

        def final(t):
            par = t % 2
            for half in range(2):
                bk = 4 + 2 * par + half
                self.tt("dve", tmp[half], B[bk][:], pscale[:, half * 512:(half + 1) * 512], ALU.mult,
                        R=[("B", bk), "pscale"], W=[("tmp", half)])
                xs = self.x[:, t, half * 512:(half + 1) * 512]
                self.tt("dve", xs, xs, tmp[half], ALU.add, R=[("tmp", half), ("x", t)], W=[("x", t)])
            self.emit_ss(t, jkD)

        norm_tile(0)
        norm_tile(1)
        band_mm(0)
        evac_y(0)
        for t in range(NT):
            if t + 2 < NT:
                norm_tile(t + 2)
            if t + 1 < NT:
                band_mm(t + 1)
            pool_mm(t)
            if t + 1 < NT:
                evac_y(t + 1)
            final(t)
        self.stats_ready = True

    def pool_phase_v1(self, l):
        P = self.P
        slot = l // 2
        P.barrier()
        self.off = 0
        hT32 = self.carve([128, KC, S], F32)
        yT = self.carve([128, KC, S], BF16)
        sbuf_ = [self.carve([128, S], F32) for _ in range(2)]
        wp = self.carve([128, 4, 2, 256], BF16)
        pscale = self.carve([128, D], F32)
        h32 = [self.carve([128, D], F32) for _ in range(2)]
        t16 = self.carve([128, 16], F32)
        B = self.B
        wsrc = self.din("pool_w_group", [2, 4, 256, 256])[slot]
        for g in range(4):
            self.dma("pool", wp[:, g, :, :], wsrc[g].rearrange("(k p) d -> p k d", p=128), slot=("wp", g), W=[("wp", g)])
        self.dma("sp", pscale, self.din("pool_scale", [2, D])[slot:slot + 1, :].partition_broadcast(128),
                 slot=("pscale",), W=["pscale"])
        gi = self.load_gain(self.din("norm_mix", [DEPTH, D])[l:l + 1, :])
        self.norm_stats(h32[0])
        for t in range(NT):
            ht = h32[t % 2]
            self.stt("dve", ht, self.x[:, t, :], self.rstd[:, t:t + 1], self.gain_bc[gi][:], ALU.mult, ALU.mult,
                     R=[("x", t), "rstd", ("gain", gi)], W=[("h32", t % 2), "junk"] if t % 2 == 0 else [("h32", t % 2)])
            for kc in range(KC):
                bk = 6 + kc // 4
                self.tr(B[bk][:, (kc % 4) * 128:(kc % 4 + 1) * 128], ht[:, kc * 128:(kc + 1) * 128], self.ident_f[:],
                        R=[("h32", t % 2), "ident_f"], W=[("B", bk)])
            self.act(hT32[:, 0:4, t * 128:(t + 1) * 128], B[6][:].rearrange("p (k s) -> p k s", k=4), AF.Copy,
                     R=[("B", 6)], W=[("hT32", c) for c in range(0, 4)])
            self.copy("dve", hT32[:, 4:8, t * 128:(t + 1) * 128], B[7][:].rearrange("p (k s) -> p k s", k=4),
                      R=[("B", 7)], W=[("hT32", c) for c in range(4, 8)])
        for c in range(KC):
            wi = c // 2
            win_ = WINS[wi]
            src = hT32[:, c, :]
            cur, ckey = src, ("hT32", c)
            sh, n = 1, 0
            while sh < win_:
                dst = sbuf_[n % 2]
                dkey = ("sb", n % 2)
                self.tt("dve", dst[:, sh:], cur[:, sh:], cur[:, 0:S - sh], ALU.add, R=[ckey], W=[dkey])
                self.copy("dve", dst[:, 0:sh], cur[:, 0:sh], R=[ckey], W=[dkey])
                cur, ckey = dst, dkey
                sh *= 2
                n += 1
            self.stt("dve", yT[:, c, :], cur, 1.0 / win_, src, ALU.mult, ALU.subtract, R=[ckey, ("hT32", c)], W=[("yT", c)])
            self.tt("dve", t16, cur[:, 0:16], self.invcnt[:, wi, :], ALU.mult, R=[ckey, "invcnt"], W=["t16"])
            self.tt("dve", yT[:, c, 0:16], t16, src[:, 0:16], ALU.subtract, R=["t16", ("hT32", c)], W=[("yT", c)])
        for t in range(NT):
            tok = slice(t * 128, (t + 1) * 128)
            for g in range(4):
                bk = (t % 2) * 2 + g // 2
                for kc in range(2):
                    self.mm(B[bk][:, (g % 2) * 256:(g % 2 + 1) * 256], yT[:, 2 * g + kc, tok], wp[:, g, kc, :], kc == 0, kc == 1,
                            R=[("yT", 2 * g + kc), ("wp", g)], W=[("B", bk)])
            for half in range(2):
                bk = (t % 2) * 2 + half
                tmp = h32[half][:, 0:512]
                self.tt("dve", tmp, B[bk][:], pscale[:, half * 512:(half + 1) * 512], ALU.mult,
                        R=[("B", bk), "pscale"], W=[("h32", half)])
                xs = self.x[:, t, half * 512:(half + 1) * 512]
                self.tt("dve", xs, xs, tmp, ALU.add, R=[("h32", half), ("x", t)], W=[("x", t)])

    def ffn_phase(self, l):
        P = self.P
        P.barrier()
        self.off = 0
        hT = self.carve([128, KC, S], BF16)
        abuf = [self.carve([128, 4, S], BF16) for _ in range(2)]
        wgu = [self.carve([128, KC, 2, 256], BF16) for _ in range(3)]
        wd = [self.carve([128, 4, D], BF16) for _ in range(2)]
        hb = [self.carve([128, D], BF16) for _ in range(2)]
        sl = [self.carve([128, 512], BF16) for _ in range(2)]
        jkD = self.carve([128, D], BF16)
        nxt_needs_stats = (l % 2 == 0) or (l == self.layers[-1])
        B = self.B
        wgu_src = self.din("ffn_w_gate_up", [DEPTH, D, 2 * DFF])[l].rearrange("(k p) n -> p k n", p=128)
        wd_src = self.din("ffn_w_down", [DEPTH, DFF, D])[l].rearrange("(j p) d -> p j d", p=128)
        groups = [(j0, min(4, NJ - j0)) for j0 in range(0, NJ, 4)]

        def load_gu(n):
            s_ = n % 3
            self.dma("pool", wgu[s_][:, :, 0, :], wgu_src[:, :, n * 256:(n + 1) * 256], slot=("wg", s_), W=[("wg", s_)])
            self.dma("pool", wgu[s_][:, :, 1, :], wgu_src[:, :, DFF + n * 256:DFF + (n + 1) * 256], slot=("wu", s_), W=[("wu", s_)])

        def load_d(gi):
            j0, nj = groups[gi]
            self.dma("pool", wd[gi % 2][:, 0:nj, :], wd_src[:, j0:j0 + nj, :], slot=("wd", gi % 2), W=[("wd", gi % 2)])

        load_gu(0)
        load_gu(1)
        load_gu(2)
        load_d(0)
        self.norm_to_hT(self.din("norm_ffn", [DEPTH, D])[l:l + 1, :], hT, hb)

        cnt = [0]

        def gateup(gi):
            j0, nj = groups[gi]
            ab = abuf[gi % 2]
            for jj in range(nj):
                j = j0 + jj
                n, sub = j // 2, j % 2
                s_ = n % 3
                for tg in range(4):
                    pr = cnt[0] % 2
                    cnt[0] += 1
                    bg, bu = B[2 * pr], B[2 * pr + 1]
                    rk = [("hT", 4 * tg + i) for i in range(4)]
                    for kc in range(KC):
                        self.mm(bg[:], wgu[s_][:, kc, 0, sub * 128:(sub + 1) * 128], hT[:, kc, tg * 512:(tg + 1) * 512],
                                kc == 0, kc == KC - 1, R=rk + [("wg", s_)], W=[("B", 2 * pr)])
                    for kc in range(KC):
                        self.mm(bu[:], wgu[s_][:, kc, 1, sub * 128:(sub + 1) * 128], hT[:, kc, tg * 512:(tg + 1) * 512],
                                kc == 0, kc == KC - 1, R=rk + [("wu", s_)], W=[("B", 2 * pr + 1)])
                    self.act(sl[pr], bg[:], AF.Silu, R=[("B", 2 * pr)], W=[("sl", pr)])
                    self.tt("dve", ab[:, jj, tg * 512:(tg + 1) * 512], sl[pr], bu[:], ALU.mult,
                            R=[("sl", pr), ("B", 2 * pr + 1)], W=[("ab", gi % 2, tg)])
                if sub == 1 or j == NJ - 1:
                    if n + 3 < NJ // 2:
                        load_gu(n + 3)

        def down(gi):
            j0, nj = groups[gi]
            ab = abuf[gi % 2]
            for t in range(NT):
                for half in range(2):
                    bk = 4 + (2 * t + half) % 4
                    for jj in range(nj):
                        self.mm(B[bk][:], ab[:, jj, t * 128:(t + 1) * 128], wd[gi % 2][:, jj, half * 512:(half + 1) * 512],
                                jj == 0, jj == nj - 1, R=[("ab", gi % 2, t // 4), ("wd", gi % 2)], W=[("B", bk)])
                    xs = self.x[:, t, half * 512:(half + 1) * 512]
                    self.tt("dve", xs, xs, B[bk][:], ALU.add, R=[("B", bk), ("x", t)], W=[("x", t)])
                if gi == len(groups) - 1 and nxt_needs_stats:
                    self.emit_ss(t, jkD)
            if gi + 2 < len(groups):
                load_d(gi + 2)

        gateup(0)
        load_d(1)
        for gi in range(len(groups)):
            if gi + 1 < len(groups):
                gateup(gi + 1)
            down(gi)
        self.stats_ready = nxt_needs_stats

    def final_phase(self):
        P = self.P
        P.barrier()
        self.off = 0
        ob = [self.carve([128, D], F32) for _ in range(2)]
        if self.final_norm:
            gi = self.load_gain(self.din("final_norm", [1, D])[0:1, :])
            self.norm_stats(ob[0])
        stores = []
        for t in range(NT):
            o = ob[t % 2]
            if self.final_norm:
                self.stt("dve", o, self.x[:, t, :], self.rstd[:, t:t + 1], self.gain_bc[gi][:], ALU.mult, ALU.mult,
                         R=[("x", t), "rstd", ("gain", gi)], W=[("ob", t % 2), "junk"] if t % 2 == 0 else [("ob", t % 2)])
                src, rk = o, [("ob", t % 2)]
            else:
                src, rk = self.x[:, t, :], [("x", t)]
            stores.append(self.dma("sp", self.out[t * 128:(t + 1) * 128, :], src, slot=("st", t), R=rk, W=[("outd", t)]))
        P.add("sp", None, extra=stores)


_CACHE = {}


def _get_prog(layers, final_norm):
    key = (tuple(layers), final_norm)
    if key not in _CACHE:
        b = Builder(layers, final_norm)
        nc = b.build()
        _CACHE[key] = (nc, sorted(b.dram.keys()))
    return _CACHE[key]


def _run(layers, final_norm, xs, inputs):
    nc, names = _get_prog(layers, final_norm)
    in_maps = []
    for c in range(N_CORES):
        m = {}
        for n in names:
            if n == "x":
                m[n] = np.ascontiguousarray(xs[c])
            elif n == "final_norm":
                m[n] = np.ascontiguousarray(inputs[n]).reshape(1, D)
            else:
                m[n] = np.ascontiguousarray(inputs[n])
        in_maps.append(m)
    res = run_bass_kernel_spmd(nc, in_maps, core_ids=list(range(N_CORES)))
    return [res.results[c]["out"] for c in range(N_CORES)]


LAUNCH_GROUPS = [[0, 1, 2, 3]]


def kernel(**inputs):
    inputs = {k: np.asarray(v, dtype=np.float32) for k, v in inputs.items()}
    xs = [inputs["x"][b] for b in range(N_CORES)]
    for gi, grp in enumerate(LAUNCH_GROUPS):
        xs = _run(grp, gi == len(LAUNCH_GROUPS) - 1, xs, inputs)
    return np.stack(xs, axis=0).astype(np.float32)
```

```python
import numpy as np
from contextlib import ExitStack
import concourse.bass as bass
import concourse.mybir as mybir
from concourse.bass_utils import run_bass_kernel_spmd

F32 = mybir.dt.float32
BF16 = mybir.dt.bfloat16
U8 = mybir.dt.uint8
AF = mybir.ActivationFunctionType
ALU = mybir.AluOpType
AX = mybir.AxisListType

S = 2048
D = 1024
NT = 16
KC = 8
H = 8
DFF = 2816
NJ = 22
EPS = 1e-6
N_CORES = 8
DEPTH = 4
WINS = (2, 4, 8, 16)


class _Op:
    __slots__ = ("eng", "fn", "waits", "sig", "pos", "gid", "slot", "cnt", "known", "sidx")


class Prog:
    ENGS = ("pe", "act", "dve", "pool", "sp")

    def __init__(self, self_sync=True):
        self.streams = {e: [] for e in self.ENGS}
        self.known = {e: {} for e in self.ENGS}
        self.last_w = {}
        self.readers = {}
        self.slot_cnt = {}
        self.slot_last = {}
        self.gid = 0
        self.self_sync = self_sync

    def add(self, eng, fn, R=(), W=(), slot=None, extra=()):
        op = _Op()
        op.eng, op.fn, op.waits, op.sig, op.slot, op.cnt, op.sidx = eng, fn, [], False, slot, 0, 0
        op.gid = self.gid
        self.gid += 1
        st = self.streams[eng]
        op.pos = len(st)
        st.append(op)
        psum_r = [k for k in R if isinstance(k, tuple) and k[0] == "B"]
        if psum_r:
            R = [k for k in R if not (isinstance(k, tuple) and k[0] == "B")]
            W = list(W) + psum_r
        deps = {}
        for k in R:
            lw = self.last_w.get(k)
            if lw is not None:
                deps[lw.gid] = lw
        for k in W:
            lw = self.last_w.get(k)
            if lw is not None:
                deps[lw.gid] = lw
            for r in self.readers.get(k, ()):
                deps[r.gid] = r
        for d in extra:
            deps[d.gid] = d
        kn = self.known[eng]
        for g in sorted(deps, reverse=True):
            d = deps[g]
            if d.slot is not None:
                src, val = d.slot, d.cnt
            else:
                if d.eng == eng and (eng == "pe" or not self.self_sync):
                    continue
                src, val = d.eng, d.pos
            if kn.get(src, -1) >= val:
                continue
            assert d.fn is not None
            op.waits.append(d)
            d.sig = True
            for s_, v_ in d.known.items():
                if kn.get(s_, -1) < v_:
                    kn[s_] = v_
        if slot is not None:
            c = self.slot_cnt.get(slot, 0) + 1
            self.slot_cnt[slot] = c
            op.cnt = c
            prev = self.slot_last.get(slot)
            if prev is not None:
                assert kn.get(slot, -1) >= prev.cnt, ("two DMAs in flight on slot", slot)
            self.slot_last[slot] = op
            op.known = dict(kn)
            op.known[slot] = c
        else:
            op.known = dict(kn)
            op.known[eng] = op.pos
        for k in R:
            self.readers.setdefault(k, []).append(op)
        for k in W:
            self.last_w[k] = op
            self.readers[k] = []
        return op

    def barrier(self):
        if not getattr(self, "_had_barrier", False):
            self._had_barrier = True
            return
        lasts = []
        for e in self.ENGS:
            for op in reversed(self.streams[e]):
                if op.fn is not None and op.slot is None:
                    lasts.append(op)
                    break
        lasts += list(self.slot_last.values())
        for e in self.ENGS:
            self.add(e, None, extra=lasts)

    def emit(self, block, sems, slot_sems):
        for e in self.ENGS:
            n = 0
            for op in self.streams[e]:
                if op.slot is None and op.sig:
                    n += 1
                    op.sidx = n

        def body_for(e):
            st = self.streams[e]

            def body(eng):
                for op in st:
                    for d in op.waits:
                        if d.slot is not None:
                            eng.wait_ge(slot_sems[d.slot], 16 * d.cnt)
                        else:
                            eng.wait_ge(sems[d.eng], d.sidx)
                    if op.fn is not None:
                        ins = op.fn(eng)
                        if op.slot is not None:
                            ins.then_inc(slot_sems[op.slot], 16)
                        elif op.sig:
                            ins.then_inc(sems[e], 1)
            return body

        block.tensor(body_for("pe"))
        block.scalar(body_for("act"))
        block.vector(body_for("dve"))
        block.gpsimd(body_for("pool"))
        block.sync(body_for("sp"))


def _dsize(dt):
    return {F32: 4, BF16: 2, U8: 1}[dt]


class Builder:
    def __init__(self, layers, final_norm, self_sync=True, parts=("mixer", "ffn"), ntiles=NT):
        self.parts = parts
        self.ntiles = ntiles
        self.layers = list(layers)
        self.final_norm = final_norm
        self.nc = bass.Bass("TRN2", target_bir_lowering=False)
        self.P = Prog(self_sync=self_sync)
        self.dram = {}
        self.slots = set()

    def din(self, name, shape):
        if name not in self.dram:
            self.dram[name] = self.nc.dram_tensor(name, list(shape), F32, kind="ExternalInput").ap()
        return self.dram[name]

    def view(self, off, shape, dt):
        n = 1
        for s_ in shape[1:]:
            n *= s_
        nb = n * _dsize(dt)
        assert off % 32 == 0 and off + nb <= self.arena_bytes, (off, nb, self.arena_bytes)
        ap = self.arena[:, off:off + nb].bitcast(dt)
        if len(shape) == 3:
            ap = ap.rearrange("p (a b) -> p a b", a=shape[1])
        elif len(shape) == 4:
            ap = ap.rearrange("p (a b c) -> p a b c", a=shape[1], b=shape[2])
        return ap

    def carve(self, shape, dt):
        n = 1
        for s_ in shape[1:]:
            n *= s_
        nb = (n * _dsize(dt) + 31) // 32 * 32
        v = self.view(self.off, shape, dt)
        self.off += nb
        return v

    def dma(self, eng, out, in_, slot, R=(), W=()):
        self.slots.add(slot)
        return self.P.add(eng, lambda e: e.dma_start(out=out, in_=in_), R=R, W=W, slot=slot)

    def mm(self, out, lhsT, rhs, start, stop, R=(), W=()):
        return self.P.add("pe", lambda e: e.matmul(out, lhsT=lhsT, rhs=rhs, start=start, stop=stop), R=R, W=W)

    def tr(self, out, in_, ident, R=(), W=()):
        return self.P.add("pe", lambda e: e.transpose(out, in_, ident), R=R, W=W)

    def act(self, out, in_, func, R=(), W=(), **kw):
        return self.P.add("act", lambda e: e.activation(out=out, in_=in_, func=func, **kw), R=R, W=W)

    def tt(self, eng, out, in0, in1, op, R=(), W=()):
        return self.P.add(eng, lambda e: e.tensor_tensor(out=out, in0=in0, in1=in1, op=op), R=R, W=W)

    def ts(self, eng, out, in0, s1, s2, op0, op1=None, R=(), W=()):
        if op1 is None:
            return self.P.add(eng, lambda e: e.tensor_scalar(out=out, in0=in0, scalar1=s1, scalar2=None, op0=op0), R=R, W=W)
        return self.P.add(eng, lambda e: e.tensor_scalar(out=out, in0=in0, scalar1=s1, scalar2=s2, op0=op0, op1=op1), R=R, W=W)

    def stt(self, eng, out, in0, scalar, in1, op0, op1, R=(), W=()):
        return self.P.add(eng, lambda e: e.scalar_tensor_tensor(out=out, in0=in0, scalar=scalar, in1=in1, op0=op0, op1=op1), R=R, W=W)

    def copy(self, eng, out, in_, R=(), W=()):
        return self.P.add(eng, lambda e: e.tensor_copy(out, in_), R=R, W=W)

    def memset(self, eng, ap, val, R=(), W=()):
        return self.P.add(eng, lambda e: e.memset(ap, val), R=R, W=W)

    def build(self):
        nc = self.nc
        P = self.P
        with ExitStack() as es:
            def sb(name, shape, dt):
                return es.enter_context(nc.sbuf_tensor(name, shape, dt))

            self.x = sb("x_res", [128, NT, D], F32)
            self.ident_bf = sb("ident_bf", [128, 128], BF16)
            self.ident_f = sb("ident_f", [128, 128], F32)
            self.causal = sb("causal", [128, 128], BF16)
            self.negtri = sb("negtri", [128, 128], F32)
            self.negones = sb("negones", [128, 128], F32)
            self.invcnt = sb("invcnt", [128, 4, 16], F32)
            self.small = sb("small", [128, 512], F32)
            self.gain_bc = [sb(f"gain_bc{i}", [128, D], F32) for i in range(2)]
            self.gain_n = 0
            self.arena_bytes = (nc.sbuf_bytes_remaining - 256) // 64 * 64
            self.arena = sb("arena", [128, self.arena_bytes], U8)
            self.B = [es.enter_context(nc.psum_tensor(f"B{i}", [128, 512], F32)) for i in range(8)]
            self.out = nc.dram_tensor("out", [S, D], F32, kind="ExternalOutput").ap()
            xin = self.din("x", [S, D])

            sm = self.small
            self.ss = sm[:, 0:16]
            self.lnv = sm[:, 16:32]
            self.rstd = sm[:, 32:48]

            self.setup_consts()
            for t in range(NT):
                self.dma("sp", self.x[:, t, :], xin[t * 128:(t + 1) * 128, :], slot=("xld", t), W=[("x", t)])

            for l in self.layers:
                if "mixer" in self.parts:
                    if l % 2 == 0:
                        self.mlstm_phase(l)
                    else:
                        self.pool_phase(l)
                if "ffn" in self.parts:
                    self.ffn_phase(l)
            self.final_phase()

            sems = {e: es.enter_context(nc.semaphore(f"s_{e}")) for e in Prog.ENGS}
            slot_sems = {}
            for i, sl in enumerate(sorted(self.slots, key=str)):
                slot_sems[sl] = es.enter_context(nc.semaphore(f"d_{i}"))
            block = es.enter_context(nc.Block())
            P.emit(block, sems, slot_sems)
        return nc

    def setup_consts(self):
        def sel(ap, cmp, pattern, cm, key):
            self.P.add("pool", lambda e: e.affine_select(out=ap, in_=ap, compare_op=cmp, fill=0.0, base=0,
                                                         pattern=pattern, channel_multiplier=cm), W=[key])
        self.memset("pool", self.ident_bf[:], 1.0, W=["ident_bf"])
        sel(self.ident_bf[:], ALU.is_equal, [[-1, 128]], 1, "ident_bf")
        self.memset("pool", self.ident_f[:], 1.0, W=["ident_f"])
        sel(self.ident_f[:], ALU.is_equal, [[-1, 128]], 1, "ident_f")
        self.memset("pool", self.causal[:], 1.0, W=["causal"])
        sel(self.causal[:], ALU.is_ge, [[1, 128]], -1, "causal")
        self.memset("pool", self.negtri[:], -1.0, W=["negtri"])
        sel(self.negtri[:], ALU.is_ge, [[1, 128]], -1, "negtri")
        self.memset("pool", self.negones[:], -1.0, W=["negones"])
        for wi, w in enumerate(WINS):
            self.memset("pool", self.invcnt[:, wi, :], 1.0 / w, W=["invcnt"])
            for t in range(w - 1):
                self.memset("pool", self.invcnt[:, wi, t:t + 1], 1.0 / (t + 1), W=["invcnt"])

    def load_gain(self, row_ap):
        i = self.gain_n % 2
        self.gain_n += 1
        self.dma("sp", self.gain_bc[i][:], row_ap.partition_broadcast(128), slot=("gain", i), W=[("gain", i)])
        return i

    def emit_ss(self, t, junk):
        self.act(junk, self.x[:, t, :], AF.Square, R=[("x", t)], W=["junk", ("ss", t)], accum_out=self.ss[:, t:t + 1])

    def norm_stats(self, junk):
        if not getattr(self, "stats_ready", False):
            for t in range(NT):
                self.emit_ss(t, junk)
        self.stats_ready = False
        self.act(self.lnv, self.ss, AF.Ln, R=[("ss", t) for t in range(NT)], W=["lnv"], scale=1.0 / D, bias=EPS)
        self.act(self.rstd, self.lnv, AF.Exp, R=["lnv"], W=["rstd"], scale=-0.5)

    def norm_to_hT(self, gain_row, hT, hb):
        gi = self.load_gain(gain_row)
        self.norm_stats(hb[0])
        for t in range(NT):
            hbt = hb[t % 2]
            bk = 6 + t % 2
            psT = self.B[bk][:].bitcast(BF16)
            self.stt("dve", hbt, self.x[:, t, :], self.rstd[:, t:t + 1], self.gain_bc[gi][:], ALU.mult, ALU.mult,
                     R=[("x", t), "rstd", ("gain", gi)], W=[("hb", t % 2), "junk"] if t % 2 == 0 else [("hb", t % 2)])
            for kc in range(KC):
                self.tr(psT[:, kc * 128:(kc + 1) * 128], hbt[:, kc * 128:(kc + 1) * 128], self.ident_bf[:],
                        R=[("hb", t % 2), "ident_bf"], W=[("B", bk)])
            self.act(hT[:, :, t * 128:(t + 1) * 128], psT.rearrange("p (k s) -> p k s", k=KC), AF.Copy,
                     R=[("B", bk)], W=[("hT", t)])

    def mlstm_phase(self, l):
        P = self.P
        slot = l // 2
        P.barrier()
        self.off = 0
        NTL = self.ntiles
        win = self.carve([128, KC, 3088], BF16)
        wout = self.carve([128, KC, D], BF16)
        hgain = self.carve([128, D], F32)
        bias_bc = self.carve([128, 16], F32)
        hb = [self.carve([128, D], BF16) for _ in range(2)]
        hTt = [self.carve([128, KC, 128], BF16) for _ in range(2)]
        q_bf = self.carve([128, 512], BF16)
        k_bf = self.carve([128, 512], BF16)
        kE = [self.carve([128, 512], BF16) for _ in range(2)]
        kO = [self.carve([128, 512], BF16) for _ in range(2)]
        qkT = [self.carve([128, 8, 128], BF16) for _ in range(2)]
        vs = [self.carve([128, H, 128], BF16) for _ in range(2)]
        eo = [self.carve([128, D], F32) for _ in range(2)]
        PT = self.carve([128, H, 128], BF16)
        hcs = [self.carve([128, D], F32) for _ in range(2)]
        outbs = [self.carve([128, D], BF16) for _ in range(2)]
        outTs = [self.carve([128, 8, 128], BF16) for _ in range(2)]
        C32 = self.carve([128, 4, 132], F32)
        Cbf = [self.carve([128, 4, 132], BF16) for _ in range(2)]
        eabf = [self.carve([128, 16], BF16) for _ in range(2)]
        sqb = self.carve([128, D], F32)
        jkD = self.carve([128, D], BF16)
        sm = self.small
        def smv(p):
            o = 64 + p * 120
            names = [("ssA", 1), ("lnA", 1), ("rsA", 1), ("g1", 16), ("e2", 16), ("dd", 16), ("gs", 16), ("ee", 8),
                     ("lfn", 8), ("a", 8), ("ea", 8), ("eb", 8), ("gp", 4), ("gsel", 4)]
            d_ = {}
            for n_, w_ in names:
                d_[n_] = sm[:, o:o + w_]
                o += w_
            return d_
        SV = [smv(0), smv(1)]
        d1, d2, scl, ssh, l2, rs = (sm[:, 320:328], sm[:, 328:336], sm[:, 336:344], sm[:, 344:352], sm[:, 352:360], sm[:, 360:368])
        B = self.B

        w_in = self.din("mlstm_w_in", [2, D, 3088])[slot].rearrange("(k p) n -> p k n", p=128)
        w_out = self.din("mlstm_w_out", [2, D, D])[slot].rearrange("(k p) n -> p k n", p=128)
        pieces = [(0, 1024), (3072, 3088), (1024, 2048), (2048, 3072)]
        pkey = {0: 0, 1: 0, 2: 2, 3: 2, 4: 3, 5: 3}
        for i, (c0, c1) in enumerate(pieces):
            self.dma("pool", win[:, :, c0:c1], w_in[:, :, c0:c1], slot=("win", i), W=[("win", i)])
        self.dma("pool", wout, w_out, slot=("wout",), W=["wout"])
        self.dma("sp", hgain, self.din("mlstm_head_gain", [2, D])[slot:slot + 1, :].partition_broadcast(128),
                 slot=("hgain",), W=["hgain"])
        self.dma("sp", bias_bc, self.din("mlstm_gate_bias", [2, 16])[slot:slot + 1, :].partition_broadcast(128),
                 slot=("gbias",), W=["gbias"])
        gi = self.load_gain(self.din("norm_mix", [DEPTH, D])[l:l + 1, :])
        self.memset("dve", C32, 0.0, W=["C32"])
        self.memset("dve", Cbf[0], 0.0, W=[("Cbf", 0)])
        for p in range(2):
            self.memset("dve", kE[p], 0.0, W=[("kE", p)])
            self.memset("dve", kO[p], 0.0, W=[("kO", p)])

        psT = B[7][:].bitcast(BF16)
        psT3 = psT.rearrange("p (k s) -> p k s", k=8)

        def b4(i):
            return B[i][:].rearrange("p (h s) -> p h s", h=4)

        def A0(t):
            p = t % 2
            v = SV[p]
            self.act(hb[p], self.x[:, t, :], AF.Square, R=[("x", t)], W=[("hb", p)], accum_out=v["ssA"])
            self.act(v["lnA"], v["ssA"], AF.Ln, R=[("hb", p)], W=[("lnA", p)], scale=1.0 / D, bias=EPS)
            self.act(v["rsA"], v["lnA"], AF.Exp, R=[("lnA", p)], W=[("rsA", p)], scale=-0.5)
            self.stt("dve", hb[p], self.x[:, t, :], v["rsA"], self.gain_bc[gi][:], ALU.mult, ALU.mult,
                     R=[("x", t), ("rsA", p), ("gain", gi)], W=[("hb", p)])

        def A1(t):
            p = t % 2
            for kc in range(KC):
                self.tr(psT[:, kc * 128:(kc + 1) * 128], hb[p][:, kc * 128:(kc + 1) * 128], self.ident_bf[:],
                        R=[("hb", p), "ident_bf"], W=[("B", 7)])
            self.act(hTt[p], psT3, AF.Copy, R=[("B", 7)], W=[("hTt", p)])

        def proj(t, cb, bank):
            p = t % 2
            for kc in range(KC):
                self.mm(B[bank][:], hTt[p][:, kc, :], win[:, kc, cb * 512:(cb + 1) * 512], kc == 0, kc == KC - 1,
                        R=[("hTt", p), ("win", pkey[cb])], W=[("B", bank)])

        def A2(t, late_fn=None):
            p = t % 2
            v = SV[p]
            proj(t, 0, 4)
            self.act(q_bf, B[4][:], AF.Copy, R=[("B", 4)], W=["q_bf"])
            proj(t, 1, 5)
            for kc in range(KC):
                self.mm(B[6][:, 0:16], hTt[p][:, kc, :], win[:, kc, 3072:3088], kc == 0, kc == KC - 1,
                        R=[("hTt", p), ("win", 1)], W=[("B", 6)])
            self.act(k_bf, B[5][:], AF.Copy, R=[("B", 5)], W=["k_bf"], scale=0.125)
            k4 = B[5][:].rearrange("p (j two c) -> p j two c", two=2, c=64)
            kE4 = kE[p].rearrange("p (j two c) -> p j two c", two=2, c=64)
            kO4 = kO[p].rearrange("p (j two c) -> p j two c", two=2, c=64)
            self.ts("dve", kE4[:, :, 0, :], k4[:, :, 0, :], 0.125, None, ALU.mult, R=[("B", 5)], W=[("kE", p)])
            self.ts("dve", kO4[:, :, 1, :], k4[:, :, 1, :], 0.125, None, ALU.mult, R=[("B", 5)], W=[("kO", p)])
            self.tt("dve", v["g1"], B[6][:, 0:16], bias_bc, ALU.add, R=[("B", 6), "gbias"], W=[("g1", p)])
            self.act(v["e2"], v["g1"], AF.Exp, R=[("g1", p)], W=[("e2", p)], scale=2.0 / 15.0)
            self.ts("dve", v["dd"], v["e2"], 1.0, None, ALU.add, R=[("e2", p)], W=[("dd", p)])
            self.P.add("dve", (lambda o_, i_: (lambda e: e.reciprocal(out=o_, in_=i_)))(v["dd"], v["dd"]), R=[("dd", p)], W=[("dd", p)])
            self.ts("dve", v["gs"], v["dd"], -30.0, 15.0, ALU.mult, ALU.add, R=[("dd", p)], W=[("gs", p)])
            self.act(v["ee"], v["gs"][:, 8:16], AF.Exp, R=[("gs", p)], W=[("ee", p)], scale=-1.0)
            self.act(v["lfn"], v["ee"], AF.Ln, R=[("ee", p)], W=[("lfn", p)], bias=1.0)
            if late_fn is not None:
                late_fn()
            self.mm(B[6][:, 16:24], self.negtri[:], v["lfn"], True, True, R=[("lfn", p), "negtri"], W=[("B", 6)])
            self.mm(B[6][:, 24:32], self.negones[:], v["lfn"], True, True, R=[("lfn", p), "negones"], W=[("B", 6)])
            self.tt("dve", v["a"], v["gs"][:, 0:8], B[6][:, 16:24], ALU.subtract, R=[("gs", p), ("B", 6)], W=[("a", p)])
            self.act(v["eb"], B[6][:, 16:24], AF.Exp, R=[("B", 6)], W=[("eb", p)])
            bl2 = B[6][:, 24:32].rearrange("p (j two) -> p j two", two=2)
            self.copy("dve", v["gp"][0:64, :], bl2[0:64, :, 0], R=[("B", 6)], W=[("gp0", p)])
            self.copy("dve", v["gp"][64:128, :], bl2[64:128, :, 1], R=[("B", 6)], W=[("gp1", p)])
            self.act(v["ea"], v["a"], AF.Exp, R=[("a", p)], W=[("ea", p)])
            self.copy("dve", eabf[p][:, 0:8], v["ea"], R=[("ea", p)], W=[("eabf", p)])
            self.act(v["gsel"], v["gp"], AF.Exp, R=[("gp0", p), ("gp1", p)], W=[("gsel", p)])

        def A3(t):
            p = t % 2
            v = SV[p]
            for j in range(4):
                self.tr(psT[:, j * 128:(j + 1) * 128], q_bf[:, j * 128:(j + 1) * 128], self.ident_bf[:],
                        R=["q_bf", "ident_bf"], W=[("B", 7)])
            for j in range(4):
                self.tr(psT[:, (4 + j) * 128:(5 + j) * 128], k_bf[:, j * 128:(j + 1) * 128], self.ident_bf[:],
                        R=["k_bf", "ident_bf"], W=[("B", 7)])
            self.act(qkT[p], psT3, AF.Copy, R=[("B", 7)], W=[("qkT", p)])
            for i in range(2):
                proj(t, 2 + i, 4 + i)
                self.tt("dve", vs[p][:, 4 * i:4 * i + 4, :], b4(4 + i),
                        v["ea"][:, 4 * i:4 * i + 4].unsqueeze(2).to_broadcast([128, 4, 128]), ALU.mult,
                        R=[("B", 4 + i), ("ea", p)], W=[("vs", p, i)])
            for i in range(2):
                proj(t, 4 + i, 4 + i)
                self.act(eo[p][:, i * 512:(i + 1) * 512], B[4 + i][:], AF.Exp, R=[("B", 4 + i)], W=[("eo", p)], scale=-1.0)

        def A3b(t):
            p = t % 2
            self.act(eo[p], eo[p], AF.Ln, R=[("eo", p)], W=[("eo", p)], bias=1.0)
            self.act(eo[p], eo[p], AF.Exp, R=[("eo", p)], W=[("eo", p)], scale=-1.0)

        def A3c(t):
            p = t % 2
            self.tt("dve", eo[p], eo[p], hgain, ALU.mult, R=[("eo", p), "hgain"], W=[("eo", p)])

        def B1(t):
            p = t % 2
            for h in range(H):
                p0 = (h % 2) * 64
                self.mm(b4(h % 2)[:, h // 2, :], qkT[p][p0:p0 + 64, 4 + h // 2, :], qkT[p][p0:p0 + 64, h // 2, :], True, True,
                        R=[("qkT", p)], W=[("B", h % 2)])
            PT4 = PT.rearrange("p (j two) s -> p j two s", two=2)
            for i in range(2):
                self.tt("dve", PT4[:, :, i, :], b4(i), self.causal[:].unsqueeze(1).to_broadcast([128, 4, 128]), ALU.mult,
                        R=[("B", i), "causal"], W=[("PT", i)])

        def B2(t):
            p = t % 2
            v = SV[p]
            cb_ = Cbf[p]
            for h in range(H):
                p0 = (h % 2) * 64
                qTh = qkT[p][p0:p0 + 64, h // 2, :]
                self.mm(b4(2 + h // 4)[:, h % 4, :], PT[:, h, :], vs[p][:, h, :], True, False,
                        R=[("PT", h % 2), ("vs", p, h // 4)], W=[("B", 2 + h // 4)])
                self.mm(b4(2 + h // 4)[:, h % 4, :], qTh, cb_[p0:p0 + 64, h // 2, 0:128], False, True,
                        R=[("qkT", p), ("Cbf", p)], W=[("B", 2 + h // 4)])
                self.mm(B[6][:, 32 + h:33 + h], PT[:, h, :], eabf[p][:, h:h + 1], True, False,
                        R=[("PT", h % 2), ("eabf", p)], W=[("B", 6)])
                self.mm(B[6][:, 32 + h:33 + h], qTh, cb_[p0:p0 + 64, h // 2, 128:129], False, True,
                        R=[("qkT", p), ("Cbf", p)], W=[("B", 6)])
            for j in range(4):
                self.mm(b4(0)[:, j, :], kE[p][:, j * 128:(j + 1) * 128], vs[p][:, 2 * j, :], True, False,
                        R=[("kE", p), ("vs", p, j // 2)], W=[("B", 0)])
                self.mm(b4(0)[:, j, :], kO[p][:, j * 128:(j + 1) * 128], vs[p][:, 2 * j + 1, :], False, True,
                        R=[("kO", p), ("vs", p, j // 2)], W=[("B", 0)])
                self.mm(B[6][:, 40 + j:41 + j], kE[p][:, j * 128:(j + 1) * 128], eabf[p][:, 2 * j:2 * j + 1], True, False,
                        R=[("kE", p), ("eabf", p)], W=[("B", 6)])
                self.mm(B[6][:, 40 + j:41 + j], kO[p][:, j * 128:(j + 1) * 128], eabf[p][:, 2 * j + 1:2 * j + 2], False, True,
                        R=[("kO", p), ("eabf", p)], W=[("B", 6)])
            self.tt("dve", d1, B[6][:, 32:40], v["eb"], ALU.mult, R=[("B", 6), ("eb", p)], W=["d1"])
            self.stt("dve", d2, d1, -1.0, d1, ALU.mult, ALU.max, R=["d1"], W=["d2"])
            self.ts("dve", d2, d2, 1.0, None, ALU.max, R=["d2"], W=["d2"])
            self.P.add("dve", lambda e: e.reciprocal(out=d2, in_=d2), R=["d2"], W=["d2"])
            self.tt("dve", scl, d2, v["eb"], ALU.mult, R=["d2", ("eb", p)], W=["scl"])
            hc3 = hcs[p].rearrange("p (h s) -> p h s", h=H)
            for i in range(2):
                self.tt("dve", hc3[:, 4 * i:4 * i + 4, :], b4(2 + i),
                        scl[:, 4 * i:4 * i + 4].unsqueeze(2).to_broadcast([128, 4, 128]), ALU.mult,
                        R=[("B", 2 + i), "scl"], W=[("hc", p)])
            self.tt("dve", C32[:, :, 0:128], C32[:, :, 0:128], b4(0), ALU.add, R=["C32", ("B", 0)], W=["C32"])
            self.tt("dve", C32[:, :, 128], C32[:, :, 128], B[6][:, 40:44], ALU.add, R=["C32", ("B", 6)], W=["C32"])
            self.tt("dve", C32, C32, v["gsel"].unsqueeze(2).to_broadcast([128, 4, 132]), ALU.mult, R=["C32", ("gsel", p)], W=["C32"])
            self.act(Cbf[1 - p], C32, AF.Copy, R=["C32"], W=[("Cbf", 1 - p)])

        def B3(t):
            p = t % 2
            hc = hcs[p]
            hc3 = hc.rearrange("p (h s) -> p h s", h=H)
            self.act(sqb, hc, AF.Square, R=[("hc", p)], W=["sqb"])
            self.P.add("dve", lambda e: e.reduce_sum(out=ssh, in_=sqb.rearrange("p (h s) -> p h s", h=H), axis=AX.X),
                       R=["sqb"], W=["ssh"])
            self.act(l2, ssh, AF.Ln, R=["ssh"], W=["l2"], scale=1.0 / 128.0, bias=EPS)
            self.act(rs, l2, AF.Exp, R=["l2"], W=["rs"], scale=-0.5)
            self.tt("dve", hc3, hc3, rs.unsqueeze(2).to_broadcast([128, H, 128]), ALU.mult,
                    R=[("hc", p), "rs"], W=[("hc", p)])
            self.tt("dve", outbs[p], hc, eo[p], ALU.mult, R=[("hc", p), ("eo", p)], W=[("outb", p)])

        def B3pe(t):
            p = t % 2
            for j in range(8):
                self.tr(psT[:, j * 128:(j + 1) * 128], outbs[p][:, j * 128:(j + 1) * 128], self.ident_bf[:],
                        R=[("outb", p), "ident_bf"], W=[("B", 7)])
            self.act(outTs[p], psT3, AF.Copy, R=[("B", 7)], W=[("outT", p)])

        def B4(t):
            p = t % 2
            for half in range(2):
                bk = half
                for vc in range(8):
                    self.mm(B[bk][:], outTs[p][:, vc, :], wout[:, vc, half * 512:(half + 1) * 512], vc == 0, vc == 7,
                            R=[("outT", p), "wout"], W=[("B", bk)])
                xs = self.x[:, t, half * 512:(half + 1) * 512]
                self.tt("dve", xs, xs, B[bk][:], ALU.add, R=[("B", bk), ("x", t)], W=[("x", t)])
            if NTL == NT:
                self.emit_ss(t, jkD)

        A0(0)
        if NTL > 1:
            A0(1)
        A1(0)
        A2(0)
        A3(0)
        for t in range(NTL + 1):
            cur = t < NTL
            nxt = t + 1 < NTL
            late = t >= 1
            if cur:
                B1(t)
            if late:
                A3c(t - 1)
            if nxt:
                A1(t + 1)
            if t + 2 < NTL:
                A0(t + 2)
            if late:
                B3(t - 1)
            if cur:
                B2(t)
            if late:
                B3pe(t - 1)
            if nxt:
                A2(t + 1, late_fn=(lambda tt=t: B4(tt - 1)) if late else None)
            elif late:
                B4(t - 1)
            if nxt:
                A3(t + 1)
            if cur:
                A3b(t)
        self.stats_ready = (NTL == NT)

    def mlstm_phase_v1(self, l):
        P = self.P
        slot = l // 2
        P.barrier()
        self.off = 0
        hT = self.carve([128, KC, S], BF16)
        win = self.carve([128, KC, 3088], BF16)
        wout = self.carve([128, KC, D], BF16)
        hb = [self.carve([128, D], BF16) for _ in range(2)]
        q_bf = self.carve([128, 512], BF16)
        k_bf = self.carve([128, 512], BF16)
        kE = self.carve([128, 512], BF16)
        kO = self.carve([128, 512], BF16)
        qkT = self.carve([128, 8, 128], BF16)
        vs = self.carve([128, H, 128], BF16)
        PT = self.carve([128, H, 128], BF16)
        sig = self.carve([128, D], F32)
        hc = self.carve([128, D], F32)
        outT = self.carve([128, 8, 128], BF16)
        C32 = self.carve([128, 4, 132], F32)
        Cbf = self.carve([128, 4, 132], BF16)
        hgain = self.carve([128, D], F32)
        bias_bc = self.carve([128, 16], F32)
        eabf = self.carve([128, 8], BF16)
        jk = self.carve([128, 128], BF16)
        outb = hb[0]
        sm = self.small
        g1, th, gs, ee, lfn = sm[:, 64:80], sm[:, 80:96], sm[:, 96:112], sm[:, 112:120], sm[:, 120:128]
        a_, ea, eb, gp, gsel = sm[:, 128:136], sm[:, 136:144], sm[:, 144:152], sm[:, 152:156], sm[:, 156:160]
        d1, d2, scl, ssh, l2, rs = sm[:, 160:168], sm[:, 168:176], sm[:, 176:184], sm[:, 184:192], sm[:, 192:200], sm[:, 200:208]
        B = self.B

        w_in = self.din("mlstm_w_in", [2, D, 3088])[slot].rearrange("(k p) n -> p k n", p=128)
        w_out = self.din("mlstm_w_out", [2, D, D])[slot].rearrange("(k p) n -> p k n", p=128)
        pieces = [(0, 1024), (1024, 2048), (2048, 3072), (3072, 3088)]
        for i, (c0, c1) in enumerate(pieces):
            self.dma("pool", win[:, :, c0:c1], w_in[:, :, c0:c1], slot=("win", i), W=[("win", i)])
        self.dma("pool", wout, w_out, slot=("wout",), W=["wout"])
        self.dma("sp", hgain, self.din("mlstm_head_gain", [2, D])[slot:slot + 1, :].partition_broadcast(128),
                 slot=("hgain",), W=["hgain"])
        self.dma("sp", bias_bc, self.din("mlstm_gate_bias", [2, 16])[slot:slot + 1, :].partition_broadcast(128),
                 slot=("gbias",), W=["gbias"])
        self.memset("dve", C32, 0.0, W=["C32"])
        self.memset("dve", Cbf, 0.0, W=["Cbf"])
        self.memset("dve", kE, 0.0, W=["kE"])
        self.memset("dve", kO, 0.0, W=["kO"])

        self.norm_to_hT(self.din("norm_mix", [DEPTH, D])[l:l + 1, :], hT, hb)

        psT = B[7][:].bitcast(BF16)
        psT3 = psT.rearrange("p (k s) -> p k s", k=8)

        def b4(i):
            return B[i][:].rearrange("p (h s) -> p h s", h=4)

        wpiece = {0: 0, 1: 0, 2: 1, 3: 1, 4: 2, 5: 2}
        for t in range(self.ntiles):
            tok = slice(t * 128, (t + 1) * 128)
            for cb in range(6):
                for kc in range(KC):
                    self.mm(B[cb][:], hT[:, kc, tok], win[:, kc, cb * 512:(cb + 1) * 512], kc == 0, kc == KC - 1,
                            R=[("hT", t), ("win", wpiece[cb])], W=[("B", cb)])
            for kc in range(KC):
                self.mm(B[6][:, 0:16], hT[:, kc, tok], win[:, kc, 3072:3088], kc == 0, kc == KC - 1,
                        R=[("hT", t), ("win", 3)], W=[("B", 6)])
            self.tt("dve", g1, B[6][:, 0:16], bias_bc, ALU.add, R=[("B", 6), "gbias"], W=["g1"])
            self.act(th, g1, AF.Tanh, R=["g1"], W=["th"], scale=1.0 / 15.0)
            self.ts("dve", gs, th, 15.0, None, ALU.mult, R=["th"], W=["gs"])
            self.act(ee, gs[:, 8:16], AF.Exp, R=["gs"], W=["ee"], scale=-1.0)
            self.act(lfn, ee, AF.Ln, R=["ee"], W=["lfn"], bias=1.0)
            self.mm(B[6][:, 16:24], self.negtri[:], lfn, True, True, R=["lfn", "negtri"], W=[("B", 6)])
            self.mm(B[6][:, 24:32], self.negones[:], lfn, True, True, R=["lfn", "negones"], W=[("B", 6)])
            self.tt("dve", a_, gs[:, 0:8], B[6][:, 16:24], ALU.subtract, R=["gs", ("B", 6)], W=["a"])
            self.act(ea, a_, AF.Exp, R=["a"], W=["ea"])
            self.copy("dve", eabf, ea, R=["ea"], W=["eabf"])
            self.act(eb, B[6][:, 16:24], AF.Exp, R=[("B", 6)], W=["eb"])
            bl2 = B[6][:, 24:32].rearrange("p (j two) -> p j two", two=2)
            self.copy("dve", gp[0:64, :], bl2[0:64, :, 0], R=[("B", 6)], W=["gp0"])
            self.copy("dve", gp[64:128, :], bl2[64:128, :, 1], R=[("B", 6)], W=["gp1"])
            self.act(gsel, gp, AF.Exp, R=["gp0", "gp1"], W=["gsel"])
            self.act(q_bf, B[0][:], AF.Copy, R=[("B", 0)], W=["q_bf"])
            self.act(k_bf, B[1][:], AF.Copy, R=[("B", 1)], W=["k_bf"], scale=0.125)
            k4 = B[1][:].rearrange("p (j two c) -> p j two c", two=2, c=64)
            kE4 = kE.rearrange("p (j two c) -> p j two c", two=2, c=64)
            kO4 = kO.rearrange("p (j two c) -> p j two c", two=2, c=64)
            self.ts("dve", kE4[:, :, 0, :], k4[:, :, 0, :], 0.125, None, ALU.mult, R=[("B", 1)], W=["kE"])
            self.ts("dve", kO4[:, :, 1, :], k4[:, :, 1, :], 0.125, None, ALU.mult, R=[("B", 1)], W=["kO"])
            for j in range(4):
                self.tr(psT[:, j * 128:(j + 1) * 128], q_bf[:, j * 128:(j + 1) * 128], self.ident_bf[:],
                        R=["q_bf", "ident_bf"], W=[("B", 7)])
            for j in range(4):
                self.tr(psT[:, (4 + j) * 128:(5 + j) * 128], k_bf[:, j * 128:(j + 1) * 128], self.ident_bf[:],
                        R=["k_bf", "ident_bf"], W=[("B", 7)])
            self.act(qkT, psT3, AF.Copy, R=[("B", 7)], W=["qkT"])
            for i in range(2):
                self.tt("dve", vs[:, 4 * i:4 * i + 4, :], b4(2 + i),
                        ea[:, 4 * i:4 * i + 4].unsqueeze(2).to_broadcast([128, 4, 128]), ALU.mult,
                        R=[("B", 2 + i), "ea"], W=[("vs", i)])
            for i in range(2):
                self.act(sig[:, i * 512:(i + 1) * 512], B[4 + i][:], AF.Tanh, R=[("B", 4 + i)], W=[("sig", i)], scale=0.5)
            for h in range(H):
                p0 = (h % 2) * 64
                self.mm(b4(h % 2)[:, h // 2, :], qkT[p0:p0 + 64, 4 + h // 2, :], qkT[p0:p0 + 64, h // 2, :], True, True,
                        R=["qkT"], W=[("B", h % 2)])
            PT4 = PT.rearrange("p (j two) s -> p j two s", two=2)
            for i in range(2):
                self.tt("dve", PT4[:, :, i, :], b4(i),
                        self.causal[:].unsqueeze(1).to_broadcast([128, 4, 128]), ALU.mult,
                        R=[("B", i), "causal"], W=[("PT", i)])
            for h in range(H):
                p0 = (h % 2) * 64
                qTh = qkT[p0:p0 + 64, h // 2, :]
                self.mm(b4(2 + h // 4)[:, h % 4, :], PT[:, h, :], vs[:, h, :], True, False,
                        R=[("PT", h % 2), ("vs", h // 4)], W=[("B", 2 + h // 4)])
                self.mm(b4(2 + h // 4)[:, h % 4, :], qTh, Cbf[p0:p0 + 64, h // 2, 0:128], False, True,
                        R=["qkT", "Cbf"], W=[("B", 2 + h // 4)])
                self.mm(B[6][:, 32 + h:33 + h], PT[:, h, :], eabf[:, h:h + 1], True, False,
                        R=[("PT", h % 2), "eabf"], W=[("B", 6)])
                self.mm(B[6][:, 32 + h:33 + h], qTh, Cbf[p0:p0 + 64, h // 2, 128:129], False, True,
                        R=["qkT", "Cbf"], W=[("B", 6)])
            for j in range(4):
                self.mm(b4(4)[:, j, :], kE[:, j * 128:(j + 1) * 128], vs[:, 2 * j, :], True, False,
                        R=["kE", ("vs", j // 2)], W=[("B", 4)])
                self.mm(b4(4)[:, j, :], kO[:, j * 128:(j + 1) * 128], vs[:, 2 * j + 1, :], False, True,
                        R=["kO", ("vs", j // 2)], W=[("B", 4)])
                self.mm(B[6][:, 40 + j:41 + j], kE[:, j * 128:(j + 1) * 128], eabf[:, 2 * j:2 * j + 1], True, False,
                        R=["kE", "eabf"], W=[("B", 6)])
                self.mm(B[6][:, 40 + j:41 + j], kO[:, j * 128:(j + 1) * 128], eabf[:, 2 * j + 1:2 * j + 2], False, True,
                        R=["kO", "eabf"], W=[("B", 6)])
            self.tt("dve", C32[:, :, 0:128], C32[:, :, 0:128], b4(4), ALU.add, R=["C32", ("B", 4)], W=["C32"])
            self.tt("dve", C32[:, :, 128], C32[:, :, 128], B[6][:, 40:44], ALU.add, R=["C32", ("B", 6)], W=["C32"])
            self.tt("dve", C32, C32, gsel.unsqueeze(2).to_broadcast([128, 4, 132]), ALU.mult, R=["C32", "gsel"], W=["C32"])
            self.act(Cbf, C32, AF.Copy, R=["C32"], W=["Cbf"])
            self.tt("dve", d1, B[6][:, 32:40], eb, ALU.mult, R=[("B", 6), "eb"], W=["d1"])
            self.stt("dve", d2, d1, -1.0, d1, ALU.mult, ALU.max, R=["d1"], W=["d2"])
            self.ts("dve", d2, d2, 1.0, None, ALU.max, R=["d2"], W=["d2"])
            self.P.add("dve", lambda e: e.reciprocal(out=d2, in_=d2), R=["d2"], W=["d2"])
            self.tt("dve", scl, d2, eb, ALU.mult, R=["d2", "eb"], W=["scl"])
            hc3 = hc.rearrange("p (h s) -> p h s", h=H)
            for i in range(2):
                self.tt("dve", hc3[:, 4 * i:4 * i + 4, :], b4(2 + i),
                        scl[:, 4 * i:4 * i + 4].unsqueeze(2).to_broadcast([128, 4, 128]), ALU.mult,
                        R=[("B", 2 + i), "scl"], W=[("hc", i)])
            for h in range(H):
                self.act(jk, hc3[:, h, :], AF.Square, R=[("hc", h // 4)], W=["jk", "ssh"], accum_out=ssh[:, h:h + 1])
            self.act(l2, ssh, AF.Ln, R=["ssh"], W=["l2"], scale=1.0 / 128.0, bias=EPS)
            self.act(rs, l2, AF.Exp, R=["l2"], W=["rs"], scale=-0.5, bias=float(np.log(0.5)))
            self.tt("dve", hc3, hc3, rs.unsqueeze(2).to_broadcast([128, H, 128]), ALU.mult,
                    R=[("hc", 0), ("hc", 1), "rs"], W=[("hc", 0), ("hc", 1)])
            self.tt("pool", hc, hc, hgain, ALU.mult, R=[("hc", 0), ("hc", 1), "hgain"], W=[("hc", 0), ("hc", 1)])
            self.stt("dve", outb, sig, 1.0, hc, ALU.add, ALU.mult,
                     R=[("sig", 0), ("sig", 1), ("hc", 0), ("hc", 1)], W=[("hb", 0)])
            for j in range(8):
                self.tr(psT[:, j * 128:(j + 1) * 128], outb[:, j * 128:(j + 1) * 128], self.ident_bf[:],
                        R=[("hb", 0), "ident_bf"], W=[("B", 7)])
            self.act(outT, psT3, AF.Copy, R=[("B", 7)], W=["outT"])
            for half in range(2):
                bk = 5 if half == 0 else 1
                for vc in range(8):
                    self.mm(B[bk][:], outT[:, vc, :], wout[:, vc, half * 512:(half + 1) * 512], vc == 0, vc == 7,
                            R=["outT", "wout"], W=[("B", bk)])
                xs = self.x[:, t, half * 512:(half + 1) * 512]
                self.tt("dve", xs, xs, B[bk][:], ALU.add, R=[("B", bk), ("x", t)], W=[("x", t)])

    def pool_phase(self, l):
        P = self.P
        slot = l // 2
        P.barrier()
        self.off = 0
        band = [self.carve([128, 128], F32) for _ in range(4)]
        band0 = [self.carve([128, 128], F32) for _ in range(4)]
        offm = [self.carve([128, 128], F32) for _ in range(4)]
        sc16 = self.carve([128, 4, 16], F32)
        h32 = [self.carve([128, D], F32) for _ in range(3)]
        yT = [self.carve([128, KC, 128], BF16) for _ in range(2)]
        wp = self.carve([128, 4, 2, 256], BF16)
        pscale = self.carve([128, D], F32)
        tmp = [self.carve([128, 512], F32) for _ in range(2)]
        jkD = self.carve([128, D], BF16)
        B = self.B
        wsrc = self.din("pool_w_group", [2, 4, 256, 256])[slot]
        for g in range(4):
            self.dma("pool", wp[:, g, :, :], wsrc[g].rearrange("(k p) d -> p k d", p=128), slot=("wp", g), W=[("wp", g)])
        self.dma("sp", pscale, self.din("pool_scale", [2, D])[slot:slot + 1, :].partition_broadcast(128),
                 slot=("pscale",), W=["pscale"])
        gi = self.load_gain(self.din("norm_mix", [DEPTH, D])[l:l + 1, :])

        def sel(ap, pattern, cm, base, key):
            self.P.add("pool", lambda e: e.affine_select(out=ap, in_=ap, compare_op=ALU.is_ge, fill=0.0, base=base,
                                                         pattern=pattern, channel_multiplier=cm), W=[key])
        for wi, w in enumerate(WINS):
            self.memset("pool", band[wi], 1.0 / w, W=[("band", wi)])
            sel(band[wi], [[1, 128]], -1, 0, ("band", wi))
            sel(band[wi], [[-1, 128]], 1, w - 1, ("band", wi))
            self.memset("pool", offm[wi], 1.0 / w, W=[("off", wi)])
            sel(offm[wi], [[-1, 128]], 1, w - 129, ("off", wi))
            self.ts("dve", sc16[:, wi, :], self.invcnt[:, wi, :], float(w), None, ALU.mult, R=["invcnt"], W=[("sc16", wi)])
            self.copy("dve", band0[wi], band[wi], R=[("band", wi)], W=[("band0", wi)])
            self.tt("dve", band0[wi][:, 0:16], band[wi][:, 0:16], sc16[:, wi, :], ALU.mult,
                    R=[("band", wi), ("sc16", wi)], W=[("band0", wi)])
            self.tt("dve", band0[wi], band0[wi], self.ident_f[:], ALU.subtract, R=[("band0", wi), "ident_f"], W=[("band0", wi)])
            self.tt("dve", band[wi], band[wi], self.ident_f[:], ALU.subtract, R=[("band", wi), "ident_f"], W=[("band", wi)])

        self.norm_stats(jkD)

        def b4(i):
            return B[i][:].rearrange("p (h s) -> p h s", h=4)

        def norm_tile(t):
            self.stt("dve", h32[t % 3], self.x[:, t, :], self.rstd[:, t:t + 1], self.gain_bc[gi][:], ALU.mult, ALU.mult,
                     R=[("x", t), "rstd", ("gain", gi)], W=[("h32", t % 3)] + (["junk"] if t == 0 else []))

        def band_mm(t):
            par = t % 2
            for c in range(KC):
                wi = c // 2
                bk = 2 * par + c // 4
                out = b4(bk)[:, c % 4, :]
                cs = slice(c * 128, (c + 1) * 128)
                if t == 0:
                    self.mm(out, h32[0][:, cs], band0[wi], True, True, R=[("h32", 0), ("band0", wi)], W=[("B", bk)])
                else:
                    self.mm(out, h32[t % 3][:, cs], band[wi], True, False, R=[("h32", t % 3), ("band", wi)], W=[("B", bk)])
                    self.mm(out[:, 0:16], h32[(t - 1) % 3][:, cs], offm[wi][:, 0:16], False, True,
                            R=[("h32", (t - 1) % 3), ("off", wi)], W=[("B", bk)])

        def evac_y(t):
            par = t % 2
            self.act(yT[par][:, 0:4, :], b4(2 * par), AF.Copy, R=[("B", 2 * par)], W=[("yT", par, 0)])
            self.copy("dve", yT[par][:, 4:8, :], b4(2 * par + 1), R=[("B", 2 * par + 1)], W=[("yT", par, 1)])

        def pool_mm(t):
            par = t % 2
            for g in range(4):
                bk = 4 + 2 * par + g // 2
                for kc in range(2):
                    self.mm(B[bk][:, (g % 2) * 256:(g % 2 + 1) * 256], yT[par][:, 2 * g + kc, :], wp[:, g, kc, :], kc == 0, kc == 1,
                            R=[("yT", par, g // 2), ("wp", g)], W=[("B", bk)])

        def final(t):
            par = t % 2
            for half in range(2):
                bk = 4 + 2 * par + half
                self.tt("dve", tmp[half], B[bk][:], pscale[:, half * 512:(half + 1) * 512], ALU.mult,
                        R=[("B", bk), "pscale"], W=[("tmp", half)])
                xs = self.x[:, t, half * 512:(half + 1) * 512]
                self.tt("dve", xs, xs, tmp[half], ALU.add, R=[("tmp", half), ("x", t)], W=[("x", t)])
            self.emit_ss(t, jkD)

        norm_tile(0)
        norm_tile(1)
        band_mm(0)
        evac_y(0)
        for t in range(NT):
            if t + 2 < NT:
                norm_tile(t + 2)
            if t + 1 < NT:
                band_mm(t + 1)
            pool_mm(t)
            if t + 1 < NT:
                evac_y(t + 1)
            final(t)
        self.stats_ready = True

    def pool_phase_v1(self, l):
        P = self.P
        slot = l // 2
        P.barrier()
        self.off = 0
        hT32 = self.carve([128, KC, S], F32)
        yT = self.carve([128, KC, S], BF16)
        sbuf_ = [self.carve([128, S], F32) for _ in range(2)]
        wp = self.carve([128, 4, 2, 256], BF16)
        pscale = self.carve([128, D], F32)
        h32 = [self.carve([128, D], F32) for _ in range(2)]
        t16 = self.carve([128, 16], F32)
        B = self.B
        wsrc = self.din("pool_w_group", [2, 4, 256, 256])[slot]
        for g in range(4):
            self.dma("pool", wp[:, g, :, :], wsrc[g].rearrange("(k p) d -> p k d", p=128), slot=("wp", g), W=[("wp", g)])
        self.dma("sp", pscale, self.din("pool_scale", [2, D])[slot:slot + 1, :].partition_broadcast(128),
                 slot=("pscale",), W=["pscale"])
        gi = self.load_gain(self.din("norm_mix", [DEPTH, D])[l:l + 1, :])
        self.norm_stats(h32[0])
        for t in range(NT):
            ht = h32[t % 2]
            self.stt("dve", ht, self.x[:, t, :], self.rstd[:, t:t + 1], self.gain_bc[gi][:], ALU.mult, ALU.mult,
                     R=[("x", t), "rstd", ("gain", gi)], W=[("h32", t % 2), "junk"] if t % 2 == 0 else [("h32", t % 2)])
            for kc in range(KC):
                bk = 6 + kc // 4
                self.tr(B[bk][:, (kc % 4) * 128:(kc % 4 + 1) * 128], ht[:, kc * 128:(kc + 1) * 128], self.ident_f[:],
                        R=[("h32", t % 2), "ident_f"], W=[("B", bk)])
            self.act(hT32[:, 0:4, t * 128:(t + 1) * 128], B[6][:].rearrange("p (k s) -> p k s", k=4), AF.Copy,
                     R=[("B", 6)], W=[("hT32", c) for c in range(0, 4)])
            self.copy("dve", hT32[:, 4:8, t * 128:(t + 1) * 128], B[7][:].rearrange("p (k s) -> p k s", k=4),
                      R=[("B", 7)], W=[("hT32", c) for c in range(4, 8)])
        for c in range(KC):
            wi = c // 2
            win_ = WINS[wi]
            src = hT32[:, c, :]
            cur, ckey = src, ("hT32", c)
            sh, n = 1, 0
            while sh < win_:
                dst = sbuf_[n % 2]
                dkey = ("sb", n % 2)
                self.tt("dve", dst[:, sh:], cur[:, sh:], cur[:, 0:S - sh], ALU.add, R=[ckey], W=[dkey])
                self.copy("dve", dst[:, 0:sh], cur[:, 0:sh], R=[ckey], W=[dkey])
                cur, ckey = dst, dkey
                sh *= 2
                n += 1
            self.stt("dve", yT[:, c, :], cur, 1.0 / win_, src, ALU.mult, ALU.subtract, R=[ckey, ("hT32", c)], W=[("yT", c)])
            self.tt("dve", t16, cur[:, 0:16], self.invcnt[:, wi, :], ALU.mult, R=[ckey, "invcnt"], W=["t16"])
            self.tt("dve", yT[:, c, 0:16], t16, src[:, 0:16], ALU.subtract, R=["t16", ("hT32", c)], W=[("yT", c)])
        for t in range(NT):
            tok = slice(t * 128, (t + 1) * 128)
            for g in range(4):
                bk = (t % 2) * 2 + g // 2
                for kc in range(2):
                    self.mm(B[bk][:, (g % 2) * 256:(g % 2 + 1) * 256], yT[:, 2 * g + kc, tok], wp[:, g, kc, :], kc == 0, kc == 1,
                            R=[("yT", 2 * g + kc), ("wp", g)], W=[("B", bk)])
            for half in range(2):
                bk = (t % 2) * 2 + half
                tmp = h32[half][:, 0:512]
                self.tt("dve", tmp, B[bk][:], pscale[:, half * 512:(half + 1) * 512], ALU.mult,
                        R=[("B", bk), "pscale"], W=[("h32", half)])
                xs = self.x[:, t, half * 512:(half + 1) * 512]
                self.tt("dve", xs, xs, tmp, ALU.add, R=[("h32", half), ("x", t)], W=[("x", t)])

    def ffn_phase(self, l):
        P = self.P
        P.barrier()
        self.off = 0
        hT = self.carve([128, KC, S], BF16)
        abuf = [self.carve([128, 4, S], BF16) for _ in range(2)]
        wgu = [self.carve([128, KC, 2, 256], BF16) for _ in range(3)]
        wd = [self.carve([128, 4, D], BF16) for _ in range(2)]
        hb = [self.carve([128, D], BF16) for _ in range(2)]
        sl = [self.carve([128, 512], BF16) for _ in range(2)]
        jkD = self.carve([128, D], BF16)
        nxt_needs_stats = (l % 2 == 0) or (l == self.layers[-1])
        B = self.B
        wgu_src = self.din("ffn_w_gate_up", [DEPTH, D, 2 * DFF])[l].rearrange("(k p) n -> p k n", p=128)
        wd_src = self.din("ffn_w_down", [DEPTH, DFF, D])[l].rearrange("(j p) d -> p j d", p=128)
        groups = [(j0, min(4, NJ - j0)) for j0 in range(0, NJ, 4)]

        def load_gu(n):
            s_ = n % 3
            self.dma("pool", wgu[s_][:, :, 0, :], wgu_src[:, :, n * 256:(n + 1) * 256], slot=("wg", s_), W=[("wg", s_)])
            self.dma("pool", wgu[s_][:, :, 1, :], wgu_src[:, :, DFF + n * 256:DFF + (n + 1) * 256], slot=("wu", s_), W=[("wu", s_)])

        def load_d(gi):
            j0, nj = groups[gi]
            self.dma("pool", wd[gi % 2][:, 0:nj, :], wd_src[:, j0:j0 + nj, :], slot=("wd", gi % 2), W=[("wd", gi % 2)])

        load_gu(0)
        load_gu(1)
        load_gu(2)
        load_d(0)
        self.norm_to_hT(self.din("norm_ffn", [DEPTH, D])[l:l + 1, :], hT, hb)

        cnt = [0]

        def gateup(gi):
            j0, nj = groups[gi]
            ab = abuf[gi % 2]
            for jj in range(nj):
                j = j0 + jj
                n, sub = j // 2, j % 2
                s_ = n % 3
                for tg in range(4):
                    pr = cnt[0] % 2
                    cnt[0] += 1
                    bg, bu = B[2 * pr], B[2 * pr + 1]
                    rk = [("hT", 4 * tg + i) for i in range(4)]
                    for kc in range(KC):
                        self.mm(bg[:], wgu[s_][:, kc, 0, sub * 128:(sub + 1) * 128], hT[:, kc, tg * 512:(tg + 1) * 512],
                                kc == 0, kc == KC - 1, R=rk + [("wg", s_)], W=[("B", 2 * pr)])
                    for kc in range(KC):
                        self.mm(bu[:], wgu[s_][:, kc, 1, sub * 128:(sub + 1) * 128], hT[:, kc, tg * 512:(tg + 1) * 512],
                                kc == 0, kc == KC - 1, R=rk + [("wu", s_)], W=[("B", 2 * pr + 1)])
                    self.act(sl[pr], bg[:], AF.Silu, R=[("B", 2 * pr)], W=[("sl", pr)])
                    self.tt("dve", ab[:, jj, tg * 512:(tg + 1) * 512], sl[pr], bu[:], ALU.mult,
                            R=[("sl", pr), ("B", 2 * pr + 1)], W=[("ab", gi % 2, tg)])
                if sub == 1 or j == NJ - 1:
                    if n + 3 < NJ // 2:
                        load_gu(n + 3)

        def down(gi):
            j0, nj = groups[gi]
            ab = abuf[gi % 2]
            for t in range(NT):
                for half in range(2):
                    bk = 4 + (2 * t + half) % 4
                    for jj in range(nj):
                        self.mm(B[bk][:], ab[:, jj, t * 128:(t + 1) * 128], wd[gi % 2][:, jj, half * 512:(half + 1) * 512],
                                jj == 0, jj == nj - 1, R=[("ab", gi % 2, t // 4), ("wd", gi % 2)], W=[("B", bk)])
                    xs = self.x[:, t, half * 512:(half + 1) * 512]
                    self.tt("dve", xs, xs, B[bk][:], ALU.add, R=[("B", bk), ("x", t)], W=[("x", t)])
                if gi == len(groups) - 1 and nxt_needs_stats:
                    self.emit_ss(t, jkD)
            if gi + 2 < len(groups):
                load_d(gi + 2)

        gateup(0)
        load_d(1)
        for gi in range(len(groups)):
            if gi + 1 < len(groups):
                gateup(gi + 1)
            down(gi)
        self.stats_ready = nxt_needs_stats

    def final_phase(self):
        P = self.P
        P.barrier()
        self.off = 0
        ob = [self.carve([128, D], F32) for _ in range(2)]
        if self.final_norm:
            gi = self.load_gain(self.din("final_norm", [1, D])[0:1, :])
            self.norm_stats(ob[0])
        stores = []
        for t in range(NT):
            o = ob[t % 2]
            if self.final_norm:
                self.stt("dve", o, self.x[:, t, :], self.rstd[:, t:t + 1], self.gain_bc[gi][:], ALU.mult, ALU.mult,
                         R=[("x", t), "rstd", ("gain", gi)], W=[("ob", t % 2), "junk"] if t % 2 == 0 else [("ob", t % 2)])
                src, rk = o, [("ob", t % 2)]
            else:
                src, rk = self.x[:, t, :], [("x", t)]
            stores.append(self.dma("sp", self.out[t * 128:(t + 1) * 128, :], src, slot=("st", t), R=rk, W=[("outd", t)]))
        P.add("sp", None, extra=stores)


_CACHE = {}


def _get_prog(layers, final_norm):
    key = (tuple(layers), final_norm)
    if key not in _CACHE:
        b = Builder(layers, final_norm)
        nc = b.build()
        _CACHE[key] = (nc, sorted(b.dram.keys()))
    return _CACHE[key]


def _run(layers, final_norm, xs, inputs):
    nc, names = _get_prog(layers, final_norm)
    in_maps = []
    for c in range(N_CORES):
        m = {}
        for n in names:
            if n == "x":
                m[n] = np.ascontiguousarray(xs[c])
            elif n == "final_norm":
                m[n] = np.ascontiguousarray(inputs[n]).reshape(1, D)
            else:
                m[n] = np.ascontiguousarray(inputs[n])
        in_maps.append(m)
    res = run_bass_kernel_spmd(nc, in_maps, core_ids=list(range(N_CORES)))
    return [res.results[c]["out"] for c in range(N_CORES)]


LAUNCH_GROUPS = [[0, 1, 2, 3]]


def kernel(**inputs):
    inputs = {k: np.asarray(v, dtype=np.float32) for k, v in inputs.items()}
    xs = [inputs["x"][b] for b in range(N_CORES)]
    for gi, grp in enumerate(LAUNCH_GROUPS):
        xs = _run(grp, gi == len(LAUNCH_GROUPS) - 1, xs, inputs)
    return np.stack(xs, axis=0).astype(np.float32)
```

```python
import numpy as np
from contextlib import ExitStack
import concourse.bass as bass
import concourse.mybir as mybir
from concourse.bass_utils import run_bass_kernel_spmd

F32 = mybir.dt.float32
BF16 = mybir.dt.bfloat16
U8 = mybir.dt.uint8
AF = mybir.ActivationFunctionType
ALU = mybir.AluOpType
AX = mybir.AxisListType

S = 2048
D = 1024
NT = 16
KC = 8
H = 8
DFF = 2816
NJ = 22
EPS = 1e-6
N_CORES = 8
DEPTH = 4
WINS = (2, 4, 8, 16)


class _Op:
    __slots__ = ("eng", "fn", "waits", "sig", "pos", "gid", "slot", "cnt", "known", "sidx")


class Prog:
    ENGS = ("pe", "act", "dve", "pool", "sp")

    def __init__(self, self_sync=True):
        self.streams = {e: [] for e in self.ENGS}
        self.known = {e: {} for e in self.ENGS}
        self.last_w = {}
        self.readers = {}
        self.slot_cnt = {}
        self.slot_last = {}
        self.gid = 0
        self.self_sync = self_sync

    def add(self, eng, fn, R=(), W=(), slot=None, extra=()):
        op = _Op()
        op.eng, op.fn, op.waits, op.sig, op.slot, op.cnt, op.sidx = eng, fn, [], False, slot, 0, 0
        op.gid = self.gid
        self.gid += 1
        st = self.streams[eng]
        op.pos = len(st)
        st.append(op)
        psum_r = [k for k in R if isinstance(k, tuple) and k[0] == "B"]
        if psum_r:
            R = [k for k in R if not (isinstance(k, tuple) and k[0] == "B")]
            W = list(W) + psum_r
        deps = {}
        for k in R:
            lw = self.last_w.get(k)
            if lw is not None:
                deps[lw.gid] = lw
        for k in W:
            lw = self.last_w.get(k)
            if lw is not None:
                deps[lw.gid] = lw
            for r in self.readers.get(k, ()):
                deps[r.gid] = r
        for d in extra:
            deps[d.gid] = d
        kn = self.known[eng]
        for g in sorted(deps, reverse=True):
            d = deps[g]
            if d.slot is not None:
                src, val = d.slot, d.cnt
            else:
                if d.eng == eng and (eng == "pe" or not self.self_sync):
                    continue
                src, val = d.eng, d.pos
            if kn.get(src, -1) >= val:
                continue
            assert d.fn is not None
            op.waits.append(d)
            d.sig = True
            for s_, v_ in d.known.items():
                if kn.get(s_, -1) < v_:
                    kn[s_] = v_
        if slot is not None:
            c = self.slot_cnt.get(slot, 0) + 1
            self.slot_cnt[slot] = c
            op.cnt = c
            prev = self.slot_last.get(slot)
            if prev is not None:
                assert kn.get(slot, -1) >= prev.cnt, ("two DMAs in flight on slot", slot)
            self.slot_last[slot] = op
            op.known = dict(kn)
            op.known[slot] = c
        else:
            op.known = dict(kn)
            op.known[eng] = op.pos
        for k in R:
            self.readers.setdefault(k, []).append(op)
        for k in W:
            self.last_w[k] = op
            self.readers[k] = []
        return op

    def barrier(self):
        if not getattr(self, "_had_barrier", False):
            self._had_barrier = True
            return
        lasts = []
        for e in self.ENGS:
            for op in reversed(self.streams[e]):
                if op.fn is not None and op.slot is None:
                    lasts.append(op)
                    break
        lasts += list(self.slot_last.values())
        for e in self.ENGS:
            self.add(e, None, extra=lasts)

    def emit(self, block, sems, slot_sems):
        for e in self.ENGS:
            n = 0
            for op in self.streams[e]:
                if op.slot is None and op.sig:
                    n += 1
                    op.sidx = n

        def body_for(e):
            st = self.streams[e]

            def body(eng):
                for op in st:
                    for d in op.waits:
                        if d.slot is not None:
                            eng.wait_ge(slot_sems[d.slot], 16 * d.cnt)
                        else:
                            eng.wait_ge(sems[d.eng], d.sidx)
                    if op.fn is not None:
                        ins = op.fn(eng)
                        if op.slot is not None:
                            ins.then_inc(slot_sems[op.slot], 16)
                        elif op.sig:
                            ins.then_inc(sems[e], 1)
            return body

        block.tensor(body_for("pe"))
        block.scalar(body_for("act"))
        block.vector(body_for("dve"))
        block.gpsimd(body_for("pool"))
        block.sync(body_for("sp"))


def _dsize(dt):
    return {F32: 4, BF16: 2, U8: 1}[dt]


class Builder:
    def __init__(self, layers, final_norm, self_sync=True, parts=("mixer", "ffn"), ntiles=NT):
        self.parts = parts
        self.ntiles = ntiles
        self.layers = list(layers)
        self.final_norm = final_norm
        self.nc = bass.Bass("TRN2", target_bir_lowering=False)
        self.P = Prog(self_sync=self_sync)
        self.dram = {}
        self.slots = set()

    def din(self, name, shape):
        if name not in self.dram:
            self.dram[name] = self.nc.dram_tensor(name, list(shape), F32, kind="ExternalInput").ap()
        return self.dram[name]

    def view(self, off, shape, dt):
        n = 1
        for s_ in shape[1:]:
            n *= s_
        nb = n * _dsize(dt)
        assert off % 32 == 0 and off + nb <= self.arena_bytes, (off, nb, self.arena_bytes)
        ap = self.arena[:, off:off + nb].bitcast(dt)
        if len(shape) == 3:
            ap = ap.rearrange("p (a b) -> p a b", a=shape[1])
        elif len(shape) == 4:
            ap = ap.rearrange("p (a b c) -> p a b c", a=shape[1], b=shape[2])
        return ap

    def carve(self, shape, dt):
        n = 1
        for s_ in shape[1:]:
            n *= s_
        nb = (n * _dsize(dt) + 31) // 32 * 32
        v = self.view(self.off, shape, dt)
        self.off += nb
        return v

    def dma(self, eng, out, in_, slot, R=(), W=()):
        self.slots.add(slot)
        return self.P.add(eng, lambda e: e.dma_start(out=out, in_=in_), R=R, W=W, slot=slot)

    def mm(self, out, lhsT, rhs, start, stop, R=(), W=()):
        return self.P.add("pe", lambda e: e.matmul(out, lhsT=lhsT, rhs=rhs, start=start, stop=stop), R=R, W=W)

    def tr(self, out, in_, ident, R=(), W=()):
        return self.P.add("pe", lambda e: e.transpose(out, in_, ident), R=R, W=W)

    def act(self, out, in_, func, R=(), W=(), **kw):
        return self.P.add("act", lambda e: e.activation(out=out, in_=in_, func=func, **kw), R=R, W=W)

    def tt(self, eng, out, in0, in1, op, R=(), W=()):
        return self.P.add(eng, lambda e: e.tensor_tensor(out=out, in0=in0, in1=in1, op=op), R=R, W=W)

    def ts(self, eng, out, in0, s1, s2, op0, op1=None, R=(), W=()):
        if op1 is None:
            return self.P.add(eng, lambda e: e.tensor_scalar(out=out, in0=in0, scalar1=s1, scalar2=None, op0=op0), R=R, W=W)
        return self.P.add(eng, lambda e: e.tensor_scalar(out=out, in0=in0, scalar1=s1, scalar2=s2, op0=op0, op1=op1), R=R, W=W)

    def stt(self, eng, out, in0, scalar, in1, op0, op1, R=(), W=()):
        return self.P.add(eng, lambda e: e.scalar_tensor_tensor(out=out, in0=in0, scalar=scalar, in1=in1, op0=op0, op1=op1), R=R, W=W)

    def copy(self, eng, out, in_, R=(), W=()):
        return self.P.add(eng, lambda e: e.tensor_copy(out, in_), R=R, W=W)

    def memset(self, eng, ap, val, R=(), W=()):
        return self.P.add(eng, lambda e: e.memset(ap, val), R=R, W=W)

    def build(self):
        nc = self.nc
        P = self.P
        with ExitStack() as es:
            def sb(name, shape, dt):
                return es.enter_context(nc.sbuf_tensor(name, shape, dt))

            self.x = sb("x_res", [128, NT, D], F32)
            self.ident_bf = sb("ident_bf", [128, 128], BF16)
            self.ident_f = sb("ident_f", [128, 128], F32)
            self.causal = sb("causal", [128, 128], BF16)
            self.negtri = sb("negtri", [128, 128], F32)
            self.negones = sb("negones", [128, 128], F32)
            self.invcnt = sb("invcnt", [128, 4, 16], F32)
            self.small = sb("small", [128, 512], F32)
            self.gain_bc = [sb(f"gain_bc{i}", [128, D], F32) for i in range(2)]
            self.gain_n = 0
            self.arena_bytes = (nc.sbuf_bytes_remaining - 256) // 64 * 64
            self.arena = sb("arena", [128, self.arena_bytes], U8)
            self.B = [es.enter_context(nc.psum_tensor(f"B{i}", [128, 512], F32)) for i in range(8)]
            self.out = nc.dram_tensor("out", [S, D], F32, kind="ExternalOutput").ap()
            xin = self.din("x", [S, D])

            sm = self.small
            self.ss = sm[:, 0:16]
            self.lnv = sm[:, 16:32]
            self.rstd = sm[:, 32:48]

            self.setup_consts()
            for t in range(NT):
                self.dma("sp", self.x[:, t, :], xin[t * 128:(t + 1) * 128, :], slot=("xld", t), W=[("x", t)])

            for l in self.layers:
                if "mixer" in self.parts:
                    if l % 2 == 0:
                        self.mlstm_phase(l)
                    else:
                        self.pool_phase(l)
                if "ffn" in self.parts:
                    self.ffn_phase(l)
            self.final_phase()

            sems = {e: es.enter_context(nc.semaphore(f"s_{e}")) for e in Prog.ENGS}
            slot_sems = {}
            for i, sl in enumerate(sorted(self.slots, key=str)):
                slot_sems[sl] = es.enter_context(nc.semaphore(f"d_{i}"))
            block = es.enter_context(nc.Block())
            P.emit(block, sems, slot_sems)
        return nc

    def setup_consts(self):
        def sel(ap, cmp, pattern, cm, key):
            self.P.add("pool", lambda e: e.affine_select(out=ap, in_=ap, compare_op=cmp, fill=0.0, base=0,
                                                         pattern=pattern, channel_multiplier=cm), W=[key])
        self.memset("pool", self.ident_bf[:], 1.0, W=["ident_bf"])
        sel(self.ident_bf[:], ALU.is_equal, [[-1, 128]], 1, "ident_bf")
        self.memset("pool", self.ident_f[:], 1.0, W=["ident_f"])
        sel(self.ident_f[:], ALU.is_equal, [[-1, 128]], 1, "ident_f")
        self.memset("pool", self.causal[:], 1.0, W=["causal"])
        sel(self.causal[:], ALU.is_ge, [[1, 128]], -1, "causal")
        self.memset("pool", self.negtri[:], -1.0, W=["negtri"])
        sel(self.negtri[:], ALU.is_ge, [[1, 128]], -1, "negtri")
        self.memset("pool", self.negones[:], -1.0, W=["negones"])
        for wi, w in enumerate(WINS):
            self.memset("pool", self.invcnt[:, wi, :], 1.0 / w, W=["invcnt"])
            for t in range(w - 1):
                self.memset("pool", self.invcnt[:, wi, t:t + 1], 1.0 / (t + 1), W=["invcnt"])

    def load_gain(self, row_ap):
        i = self.gain_n % 2
        self.gain_n += 1
        self.dma("sp", self.gain_bc[i][:], row_ap.partition_broadcast(128), slot=("gain", i), W=[("gain", i)])
        return i

    def emit_ss(self, t, junk):
        self.act(junk, self.x[:, t, :], AF.Square, R=[("x", t)], W=["junk", ("ss", t)], accum_out=self.ss[:, t:t + 1])

    def norm_stats(self, junk):
        if not getattr(self, "stats_ready", False):
            for t in range(NT):
                self.emit_ss(t, junk)
        self.stats_ready = False
        self.act(self.lnv, self.ss, AF.Ln, R=[("ss", t) for t in range(NT)], W=["lnv"], scale=1.0 / D, bias=EPS)
        self.act(self.rstd, self.lnv, AF.Exp, R=["lnv"], W=["rstd"], scale=-0.5)

    def norm_to_hT(self, gain_row, hT, hb):
        gi = self.load_gain(gain_row)
        self.norm_stats(hb[0])
        for t in range(NT):
            hbt = hb[t % 2]
            bk = 6 + t % 2
            psT = self.B[bk][:].bitcast(BF16)
            self.stt("dve", hbt, self.x[:, t, :], self.rstd[:, t:t + 1], self.gain_bc[gi][:], ALU.mult, ALU.mult,
                     R=[("x", t), "rstd", ("gain", gi)], W=[("hb", t % 2), "junk"] if t % 2 == 0 else [("hb", t % 2)])
            for kc in range(KC):
                self.tr(psT[:, kc * 128:(kc + 1) * 128], hbt[:, kc * 128:(kc + 1) * 128], self.ident_bf[:],
                        R=[("hb", t % 2), "ident_bf"], W=[("B", bk)])
            self.act(hT[:, :, t * 128:(t + 1) * 128], psT.rearrange("p (k s) -> p k s", k=KC), AF.Copy,
                     R=[("B", bk)], W=[("hT", t)])

    def mlstm_phase(self, l):
        P = self.P
        slot = l // 2
        P.barrier()
        self.off = 0
        NTL = self.ntiles
        win = self.carve([128, KC, 3088], BF16)
        wout = self.carve([128, KC, D], BF16)
        hgain = self.carve([128, D], F32)
        bias_bc = self.carve([128, 16], F32)
        hb = [self.carve([128, D], BF16) for _ in range(2)]
        hTt = [self.carve([128, KC, 128], BF16) for _ in range(2)]
        q_bf = self.carve([128, 512], BF16)
        k_bf = self.carve([128, 512], BF16)
        kE = [self.carve([128, 512], BF16) for _ in range(2)]
        kO = [self.carve([128, 512], BF16) for _ in range(2)]
        qkT = [self.carve([128, 8, 128], BF16) for _ in range(2)]
        vs = [self.carve([128, H, 128], BF16) for _ in range(2)]
        eo = [self.carve([128, D], F32) for _ in range(2)]
        PT = self.carve([128, H, 128], BF16)
        hcs = [self.carve([128, D], F32) for _ in range(2)]
        outbs = [self.carve([128, D], BF16) for _ in range(2)]
        outTs = [self.carve([128, 8, 128], BF16) for _ in range(2)]
        C32 = self.carve([128, 4, 132], F32)
        Cbf = [self.carve([128, 4, 132], BF16) for _ in range(2)]
        eabf = [self.carve([128, 16], BF16) for _ in range(2)]
        sqb = self.carve([128, D], F32)
        jkD = self.carve([128, D], BF16)
        sm = self.small
        def smv(p):
            o = 64 + p * 120
            names = [("ssA", 1), ("lnA", 1), ("rsA", 1), ("g1", 16), ("e2", 16), ("dd", 16), ("gs", 16), ("ee", 8),
                     ("lfn", 8), ("a", 8), ("ea", 8), ("eb", 8), ("gp", 4), ("gsel", 4)]
            d_ = {}
            for n_, w_ in names:
                d_[n_] = sm[:, o:o + w_]
                o += w_
            return d_
        SV = [smv(0), smv(1)]
        d1, d2, scl, ssh, l2, rs = (sm[:, 320:328], sm[:, 328:336], sm[:, 336:344], sm[:, 344:352], sm[:, 352:360], sm[:, 360:368])
        B = self.B

        w_in = self.din("mlstm_w_in", [2, D, 3088])[slot].rearrange("(k p) n -> p k n", p=128)
        w_out = self.din("mlstm_w_out", [2, D, D])[slot].rearrange("(k p) n -> p k n", p=128)
        pieces = [(0, 1024), (3072, 3088), (1024, 2048), (2048, 3072)]
        pkey = {0: 0, 1: 0, 2: 2, 3: 2, 4: 3, 5: 3}
        for i, (c0, c1) in enumerate(pieces):
            self.dma("pool", win[:, :, c0:c1], w_in[:, :, c0:c1], slot=("win", i), W=[("win", i)])
        self.dma("pool", wout, w_out, slot=("wout",), W=["wout"])
        self.dma("sp", hgain, self.din("mlstm_head_gain", [2, D])[slot:slot + 1, :].partition_broadcast(128),
                 slot=("hgain",), W=["hgain"])
        self.dma("sp", bias_bc, self.din("mlstm_gate_bias", [2, 16])[slot:slot + 1, :].partition_broadcast(128),
                 slot=("gbias",), W=["gbias"])
        gi = self.load_gain(self.din("norm_mix", [DEPTH, D])[l:l + 1, :])
        self.memset("dve", C32, 0.0, W=["C32"])
        self.memset("dve", Cbf[0], 0.0, W=[("Cbf", 0)])
        for p in range(2):
            self.memset("dve", kE[p], 0.0, W=[("kE", p)])
            self.memset("dve", kO[p], 0.0, W=[("kO", p)])

        psT = B[7][:].bitcast(BF16)
        psT3 = psT.rearrange("p (k s) -> p k s", k=8)

        def b4(i):
            return B[i][:].rearrange("p (h s) -> p h s", h=4)

        def A0(t):
            p = t % 2
            v = SV[p]
            self.act(hb[p], self.x[:, t, :], AF.Square, R=[("x", t)], W=[("hb", p)], accum_out=v["ssA"])
            self.act(v["lnA"], v["ssA"], AF.Ln, R=[("hb", p)], W=[("lnA", p)], scale=1.0 / D, bias=EPS)
            self.act(v["rsA"], v["lnA"], AF.Exp, R=[("lnA", p)], W=[("rsA", p)], scale=-0.5)
            self.stt("dve", hb[p], self.x[:, t, :], v["rsA"], self.gain_bc[gi][:], ALU.mult, ALU.mult,
                     R=[("x", t), ("rsA", p), ("gain", gi)], W=[("hb", p)])

        def A1(t):
            p = t % 2
            for kc in range(KC):
                self.tr(psT[:, kc * 128:(kc + 1) * 128], hb[p][:, kc * 128:(kc + 1) * 128], self.ident_bf[:],
                        R=[("hb", p), "ident_bf"], W=[("B", 7)])
            self.act(hTt[p], psT3, AF.Copy, R=[("B", 7)], W=[("hTt", p)])

        def proj(t, cb, bank):
            p = t % 2
            for kc in range(KC):
                self.mm(B[bank][:], hTt[p][:, kc, :], win[:, kc, cb * 512:(cb + 1) * 512], kc == 0, kc == KC - 1,
                        R=[("hTt", p), ("win", pkey[cb])], W=[("B", bank)])

        def A2(t, late_fn=None):
            p = t % 2
            v = SV[p]
            proj(t, 0, 4)
            self.act(q_bf, B[4][:], AF.Copy, R=[("B", 4)], W=["q_bf"])
            proj(t, 1, 5)
            for kc in range(KC):
                self.mm(B[6][:, 0:16], hTt[p][:, kc, :], win[:, kc, 3072:3088], kc == 0, kc == KC - 1,
                        R=[("hTt", p), ("win", 1)], W=[("B", 6)])
            self.act(k_bf, B[5][:], AF.Copy, R=[("B", 5)], W=["k_bf"], scale=0.125)
            k4 = B[5][:].rearrange("p (j two c) -> p j two c", two=2, c=64)
            kE4 = kE[p].rearrange("p (j two c) -> p j two c", two=2, c=64)
            kO4 = kO[p].rearrange("p (j two c) -> p j two c", two=2, c=64)
            self.ts("dve", kE4[:, :, 0, :], k4[:, :, 0, :], 0.125, None, ALU.mult, R=[("B", 5)], W=[("kE", p)])
            self.ts("dve", kO4[:, :, 1, :], k4[:, :, 1, :], 0.125, None, ALU.mult, R=[("B", 5)], W=[("kO", p)])
            self.tt("dve", v["g1"], B[6][:, 0:16], bias_bc, ALU.add, R=[("B", 6), "gbias"], W=[("g1", p)])
            self.act(v["e2"], v["g1"], AF.Exp, R=[("g1", p)], W=[("e2", p)], scale=2.0 / 15.0)
            self.ts("dve", v["dd"], v["e2"], 1.0, None, ALU.add, R=[("e2", p)], W=[("dd", p)])
            self.P.add("dve", (lambda o_, i_: (lambda e: e.reciprocal(out=o_, in_=i_)))(v["dd"], v["dd"]), R=[("dd", p)], W=[("dd", p)])
            self.ts("dve", v["gs"], v["dd"], -30.0, 15.0, ALU.mult, ALU.add, R=[("dd", p)], W=[("gs", p)])
            self.act(v["ee"], v["gs"][:, 8:16], AF.Exp, R=[("gs", p)], W=[("ee", p)], scale=-1.0)
            self.act(v["lfn"], v["ee"], AF.Ln, R=[("ee", p)], W=[("lfn", p)], bias=1.0)
            if late_fn is not None:
                late_fn()
            self.mm(B[6][:, 16:24], self.negtri[:], v["lfn"], True, True, R=[("lfn", p), "negtri"], W=[("B", 6)])
            self.mm(B[6][:, 24:32], self.negones[:], v["lfn"], True, True, R=[("lfn", p), "negones"], W=[("B", 6)])
            self.tt("dve", v["a"], v["gs"][:, 0:8], B[6][:, 16:24], ALU.subtract, R=[("gs", p), ("B", 6)], W=[("a", p)])
            self.act(v["eb"], B[6][:, 16:24], AF.Exp, R=[("B", 6)], W=[("eb", p)])
            bl2 = B[6][:, 24:32].rearrange("p (j two) -> p j two", two=2)
            self.copy("dve", v["gp"][0:64, :], bl2[0:64, :, 0], R=[("B", 6)], W=[("gp0", p)])
            self.copy("dve", v["gp"][64:128, :], bl2[64:128, :, 1], R=[("B", 6)], W=[("gp1", p)])
            self.act(v["ea"], v["a"], AF.Exp, R=[("a", p)], W=[("ea", p)])
            self.copy("dve", eabf[p][:, 0:8], v["ea"], R=[("ea", p)], W=[("eabf", p)])
            self.act(v["gsel"], v["gp"], AF.Exp, R=[("gp0", p), ("gp1", p)], W=[("gsel", p)])

        def A3(t):
            p = t % 2
            v = SV[p]
            for j in range(4):
                self.tr(psT[:, j * 128:(j + 1) * 128], q_bf[:, j * 128:(j + 1) * 128], self.ident_bf[:],
                        R=["q_bf", "ident_bf"], W=[("B", 7)])
            for j in range(4):
                self.tr(psT[:, (4 + j) * 128:(5 + j) * 128], k_bf[:, j * 128:(j + 1) * 128], self.ident_bf[:],
                        R=["k_bf", "ident_bf"], W=[("B", 7)])
            self.act(qkT[p], psT3, AF.Copy, R=[("B", 7)], W=[("qkT", p)])
            for i in range(2):
                proj(t, 2 + i, 4 + i)
                self.tt("dve", vs[p][:, 4 * i:4 * i + 4, :], b4(4 + i),
                        v["ea"][:, 4 * i:4 * i + 4].unsqueeze(2).to_broadcast([128, 4, 128]), ALU.mult,
                        R=[("B", 4 + i), ("ea", p)], W=[("vs", p, i)])
            for i in range(2):
                proj(t, 4 + i, 4 + i)
                self.act(eo[p][:, i * 512:(i + 1) * 512], B[4 + i][:], AF.Exp, R=[("B", 4 + i)], W=[("eo", p)], scale=-1.0)

        def A3b(t):
            p = t % 2
            self.act(eo[p], eo[p], AF.Ln, R=[("eo", p)], W=[("eo", p)], bias=1.0)
            self.act(eo[p], eo[p], AF.Exp, R=[("eo", p)], W=[("eo", p)], scale=-1.0)

        def A3c(t):
            p = t % 2
            self.tt("dve", eo[p], eo[p], hgain, ALU.mult, R=[("eo", p), "hgain"], W=[("eo", p)])

        def B1(t):
            p = t % 2
            for h in range(H):
                p0 = (h % 2) * 64
                self.mm(b4(h % 2)[:, h // 2, :], qkT[p][p0:p0 + 64, 4 + h // 2, :], qkT[p][p0:p0 + 64, h // 2, :], True, True,
                        R=[("qkT", p)], W=[("B", h % 2)])
            PT4 = PT.rearrange("p (j two) s -> p j two s", two=2)
            for i in range(2):
                self.tt("dve", PT4[:, :, i, :], b4(i), self.causal[:].unsqueeze(1).to_broadcast([128, 4, 128]), ALU.mult,
                        R=[("B", i), "causal"], W=[("PT", i)])

        def B2(t):
            p = t % 2
            v = SV[p]
            cb_ = Cbf[p]
            for h in range(H):
                p0 = (h % 2) * 64
                qTh = qkT[p][p0:p0 + 64, h // 2, :]
                self.mm(b4(2 + h // 4)[:, h % 4, :], PT[:, h, :], vs[p][:, h, :], True, False,
                        R=[("PT", h % 2), ("vs", p, h // 4)], W=[("B", 2 + h // 4)])
                self.mm(b4(2 + h // 4)[:, h % 4, :], qTh, cb_[p0:p0 + 64, h // 2, 0:128], False, True,
                        R=[("qkT", p), ("Cbf", p)], W=[("B", 2 + h // 4)])
                self.mm(B[6][:, 32 + h:33 + h], PT[:, h, :], eabf[p][:, h:h + 1], True, False,
                        R=[("PT", h % 2), ("eabf", p)], W=[("B", 6)])
                self.mm(B[6][:, 32 + h:33 + h], qTh, cb_[p0:p0 + 64, h // 2, 128:129], False, True,
                        R=[("qkT", p), ("Cbf", p)], W=[("B", 6)])
            for j in range(4):
                self.mm(b4(0)[:, j, :], kE[p][:, j * 128:(j + 1) * 128], vs[p][:, 2 * j, :], True, False,
                        R=[("kE", p), ("vs", p, j // 2)], W=[("B", 0)])
                self.mm(b4(0)[:, j, :], kO[p][:, j * 128:(j + 1) * 128], vs[p][:, 2 * j + 1, :], False, True,
                        R=[("kO", p), ("vs", p, j // 2)], W=[("B", 0)])
                self.mm(B[6][:, 40 + j:41 + j], kE[p][:, j * 128:(j + 1) * 128], eabf[p][:, 2 * j:2 * j + 1], True, False,
                        R=[("kE", p), ("eabf", p)], W=[("B", 6)])
                self.mm(B[6][:, 40 + j:41 + j], kO[p][:, j * 128:(j + 1) * 128], eabf[p][:, 2 * j + 1:2 * j + 2], False, True,
                        R=[("kO", p), ("eabf", p)], W=[("B", 6)])
            self.tt("dve", d1, B[6][:, 32:40], v["eb"], ALU.mult, R=[("B", 6), ("eb", p)], W=["d1"])
            self.stt("dve", d2, d1, -1.0, d1, ALU.mult, ALU.max, R=["d1"], W=["d2"])
            self.ts("dve", d2, d2, 1.0, None, ALU.max, R=["d2"], W=["d2"])
            self.P.add("dve", lambda e: e.reciprocal(out=d2, in_=d2), R=["d2"], W=["d2"])
            self.tt("dve", scl, d2, v["eb"], ALU.mult, R=["d2", ("eb", p)], W=["scl"])
            hc3 = hcs[p].rearrange("p (h s) -> p h s", h=H)
            for i in range(2):
                self.tt("dve", hc3[:, 4 * i:4 * i + 4, :], b4(2 + i),
                        scl[:, 4 * i:4 * i + 4].unsqueeze(2).to_broadcast([128, 4, 128]), ALU.mult,
                        R=[("B", 2 + i), "scl"], W=[("hc", p)])
            self.tt("dve", C32[:, :, 0:128], C32[:, :, 0:128], b4(0), ALU.add, R=["C32", ("B", 0)], W=["C32"])
            self.tt("dve", C32[:, :, 128], C32[:, :, 128], B[6][:, 40:44], ALU.add, R=["C32", ("B", 6)], W=["C32"])
            self.tt("dve", C32, C32, v["gsel"].unsqueeze(2).to_broadcast([128, 4, 132]), ALU.mult, R=["C32", ("gsel", p)], W=["C32"])
            self.act(Cbf[1 - p], C32, AF.Copy, R=["C32"], W=[("Cbf", 1 - p)])

        def B3(t):
            p = t % 2
            hc = hcs[p]
            hc3 = hc.rearrange("p (h s) -> p h s", h=H)
            self.act(sqb, hc, AF.Square, R=[("hc", p)], W=["sqb"])
            self.P.add("dve", lambda e: e.reduce_sum(out=ssh, in_=sqb.rearrange("p (h s) -> p h s", h=H), axis=AX.X),
                       R=["sqb"], W=["ssh"])
            self.act(l2, ssh, AF.Ln, R=["ssh"], W=["l2"], scale=1.0 / 128.0, bias=EPS)
            self.act(rs, l2, AF.Exp, R=["l2"], W=["rs"], scale=-0.5)
            self.tt("dve", hc3, hc3, rs.unsqueeze(2).to_broadcast([128, H, 128]), ALU.mult,
                    R=[("hc", p), "rs"], W=[("hc", p)])
            self.tt("dve", outbs[p], hc, eo[p], ALU.mult, R=[("hc", p), ("eo", p)], W=[("outb", p)])

        def B3pe(t):
            p = t % 2
            for j in range(8):
                self.tr(psT[:, j * 128:(j + 1) * 128], outbs[p][:, j * 128:(j + 1) * 128], self.ident_bf[:],
                        R=[("outb", p), "ident_bf"], W=[("B", 7)])
            self.act(outTs[p], psT3, AF.Copy, R=[("B", 7)], W=[("outT", p)])

        def B4(t):
            p = t % 2
            for half in range(2):
                bk = half
                for vc in range(8):
                    self.mm(B[bk][:], outTs[p][:, vc, :], wout[:, vc, half * 512:(half + 1) * 512], vc == 0, vc == 7,
                            R=[("outT", p), "wout"], W=[("B", bk)])
                xs = self.x[:, t, half * 512:(half + 1) * 512]
                self.tt("dve", xs, xs, B[bk][:], ALU.add, R=[("B", bk), ("x", t)], W=[("x", t)])
            if NTL == NT:
                self.emit_ss(t, jkD)

        A0(0)
        if NTL > 1:
            A0(1)
        A1(0)
        A2(0)
        A3(0)
        for t in range(NTL + 1):
            cur = t < NTL
            nxt = t + 1 < NTL
            late = t >= 1
            if cur:
                B1(t)
            if late:
                A3c(t - 1)
                B3(t - 1)
            if nxt:
                A1(t + 1)
            if t + 2 < NTL:
                A0(t + 2)
            if cur:
                B2(t)
            if late:
                B3pe(t - 1)
            if nxt:
                A2(t + 1, late_fn=(lambda tt=t: B4(tt - 1)) if late else None)
            elif late:
                B4(t - 1)
            if nxt:
                A3(t + 1)
            if cur:
                A3b(t)
        self.stats_ready = (NTL == NT)

    def mlstm_phase_v1(self, l):
        P = self.P
        slot = l // 2
        P.barrier()
        self.off = 0
        hT = self.carve([128, KC, S], BF16)
        win = self.carve([128, KC, 3088], BF16)
        wout = self.carve([128, KC, D], BF16)
        hb = [self.carve([128, D], BF16) for _ in range(2)]
        q_bf = self.carve([128, 512], BF16)
        k_bf = self.carve([128, 512], BF16)
        kE = self.carve([128, 512], BF16)
        kO = self.carve([128, 512], BF16)
        qkT = self.carve([128, 8, 128], BF16)
        vs = self.carve([128, H, 128], BF16)
        PT = self.carve([128, H, 128], BF16)
        sig = self.carve([128, D], F32)
        hc = self.carve([128, D], F32)
        outT = self.carve([128, 8, 128], BF16)
        C32 = self.carve([128, 4, 132], F32)
        Cbf = self.carve([128, 4, 132], BF16)
        hgain = self.carve([128, D], F32)
        bias_bc = self.carve([128, 16], F32)
        eabf = self.carve([128, 8], BF16)
        jk = self.carve([128, 128], BF16)
        outb = hb[0]
        sm = self.small
        g1, th, gs, ee, lfn = sm[:, 64:80], sm[:, 80:96], sm[:, 96:112], sm[:, 112:120], sm[:, 120:128]
        a_, ea, eb, gp, gsel = sm[:, 128:136], sm[:, 136:144], sm[:, 144:152], sm[:, 152:156], sm[:, 156:160]
        d1, d2, scl, ssh, l2, rs = sm[:, 160:168], sm[:, 168:176], sm[:, 176:184], sm[:, 184:192], sm[:, 192:200], sm[:, 200:208]
        B = self.B

        w_in = self.din("mlstm_w_in", [2, D, 3088])[slot].rearrange("(k p) n -> p k n", p=128)
        w_out = self.din("mlstm_w_out", [2, D, D])[slot].rearrange("(k p) n -> p k n", p=128)
        pieces = [(0, 1024), (1024, 2048), (2048, 3072), (3072, 3088)]
        for i, (c0, c1) in enumerate(pieces):
            self.dma("pool", win[:, :, c0:c1], w_in[:, :, c0:c1], slot=("win", i), W=[("win", i)])
        self.dma("pool", wout, w_out, slot=("wout",), W=["wout"])
        self.dma("sp", hgain, self.din("mlstm_head_gain", [2, D])[slot:slot + 1, :].partition_broadcast(128),
                 slot=("hgain",), W=["hgain"])
        self.dma("sp", bias_bc, self.din("mlstm_gate_bias", [2, 16])[slot:slot + 1, :].partition_broadcast(128),
                 slot=("gbias",), W=["gbias"])
        self.memset("dve", C32, 0.0, W=["C32"])
        self.memset("dve", Cbf, 0.0, W=["Cbf"])
        self.memset("dve", kE, 0.0, W=["kE"])
        self.memset("dve", kO, 0.0, W=["kO"])

        self.norm_to_hT(self.din("norm_mix", [DEPTH, D])[l:l + 1, :], hT, hb)

        psT = B[7][:].bitcast(BF16)
        psT3 = psT.rearrange("p (k s) -> p k s", k=8)

        def b4(i):
            return B[i][:].rearrange("p (h s) -> p h s", h=4)

        wpiece = {0: 0, 1: 0, 2: 1, 3: 1, 4: 2, 5: 2}
        for t in range(self.ntiles):
            tok = slice(t * 128, (t + 1) * 128)
            for cb in range(6):
                for kc in range(KC):
                    self.mm(B[cb][:], hT[:, kc, tok], win[:, kc, cb * 512:(cb + 1) * 512], kc == 0, kc == KC - 1,
                            R=[("hT", t), ("win", wpiece[cb])], W=[("B", cb)])
            for kc in range(KC):
                self.mm(B[6][:, 0:16], hT[:, kc, tok], win[:, kc, 3072:3088], kc == 0, kc == KC - 1,
                        R=[("hT", t), ("win", 3)], W=[("B", 6)])
            self.tt("dve", g1, B[6][:, 0:16], bias_bc, ALU.add, R=[("B", 6), "gbias"], W=["g1"])
            self.act(th, g1, AF.Tanh, R=["g1"], W=["th"], scale=1.0 / 15.0)
            self.ts("dve", gs, th, 15.0, None, ALU.mult, R=["th"], W=["gs"])
            self.act(ee, gs[:, 8:16], AF.Exp, R=["gs"], W=["ee"], scale=-1.0)
            self.act(lfn, ee, AF.Ln, R=["ee"], W=["lfn"], bias=1.0)
            self.mm(B[6][:, 16:24], self.negtri[:], lfn, True, True, R=["lfn", "negtri"], W=[("B", 6)])
            self.mm(B[6][:, 24:32], self.negones[:], lfn, True, True, R=["lfn", "negones"], W=[("B", 6)])
            self.tt("dve", a_, gs[:, 0:8], B[6][:, 16:24], ALU.subtract, R=["gs", ("B", 6)], W=["a"])
            self.act(ea, a_, AF.Exp, R=["a"], W=["ea"])
            self.copy("dve", eabf, ea, R=["ea"], W=["eabf"])
            self.act(eb, B[6][:, 16:24], AF.Exp, R=[("B", 6)], W=["eb"])
            bl2 = B[6][:, 24:32].rearrange("p (j two) -> p j two", two=2)
            self.copy("dve", gp[0:64, :], bl2[0:64, :, 0], R=[("B", 6)], W=["gp0"])
            self.copy("dve", gp[64:128, :], bl2[64:128, :, 1], R=[("B", 6)], W=["gp1"])
            self.act(gsel, gp, AF.Exp, R=["gp0", "gp1"], W=["gsel"])
            self.act(q_bf, B[0][:], AF.Copy, R=[("B", 0)], W=["q_bf"])
            self.act(k_bf, B[1][:], AF.Copy, R=[("B", 1)], W=["k_bf"], scale=0.125)
            k4 = B[1][:].rearrange("p (j two c) -> p j two c", two=2, c=64)
            kE4 = kE.rearrange("p (j two c) -> p j two c", two=2, c=64)
            kO4 = kO.rearrange("p (j two c) -> p j two c", two=2, c=64)
            self.ts("dve", kE4[:, :, 0, :], k4[:, :, 0, :], 0.125, None, ALU.mult, R=[("B", 1)], W=["kE"])
            self.ts("dve", kO4[:, :, 1, :], k4[:, :, 1, :], 0.125, None, ALU.mult, R=[("B", 1)], W=["kO"])
            for j in range(4):
                self.tr(psT[:, j * 128:(j + 1) * 128], q_bf[:, j * 128:(j + 1) * 128], self.ident_bf[:],
                        R=["q_bf", "ident_bf"], W=[("B", 7)])
            for j in range(4):
                self.tr(psT[:, (4 + j) * 128:(5 + j) * 128], k_bf[:, j * 128:(j + 1) * 128], self.ident_bf[:],
                        R=["k_bf", "ident_bf"], W=[("B", 7)])
            self.act(qkT, psT3, AF.Copy, R=[("B", 7)], W=["qkT"])
            for i in range(2):
                self.tt("dve", vs[:, 4 * i:4 * i + 4, :], b4(2 + i),
                        ea[:, 4 * i:4 * i + 4].unsqueeze(2).to_broadcast([128, 4, 128]), ALU.mult,
                        R=[("B", 2 + i), "ea"], W=[("vs", i)])
            for i in range(2):
                self.act(sig[:, i * 512:(i + 1) * 512], B[4 + i][:], AF.Tanh, R=[("B", 4 + i)], W=[("sig", i)], scale=0.5)
            for h in range(H):
                p0 = (h % 2) * 64
                self.mm(b4(h % 2)[:, h // 2, :], qkT[p0:p0 + 64, 4 + h // 2, :], qkT[p0:p0 + 64, h // 2, :], True, True,
                        R=["qkT"], W=[("B", h % 2)])
            PT4 = PT.rearrange("p (j two) s -> p j two s", two=2)
            for i in range(2):
                self.tt("dve", PT4[:, :, i, :], b4(i),
                        self.causal[:].unsqueeze(1).to_broadcast([128, 4, 128]), ALU.mult,
                        R=[("B", i), "causal"], W=[("PT", i)])
            for h in range(H):
                p0 = (h % 2) * 64
                qTh = qkT[p0:p0 + 64, h // 2, :]
                self.mm(b4(2 + h // 4)[:, h % 4, :], PT[:, h, :], vs[:, h, :], True, False,
                        R=[("PT", h % 2), ("vs", h // 4)], W=[("B", 2 + h // 4)])
                self.mm(b4(2 + h // 4)[:, h % 4, :], qTh, Cbf[p0:p0 + 64, h // 2, 0:128], False, True,
                        R=["qkT", "Cbf"], W=[("B", 2 + h // 4)])
                self.mm(B[6][:, 32 + h:33 + h], PT[:, h, :], eabf[:, h:h + 1], True, False,
                        R=[("PT", h % 2), "eabf"], W=[("B", 6)])
                self.mm(B[6][:, 32 + h:33 + h], qTh, Cbf[p0:p0 + 64, h // 2, 128:129], False, True,
                        R=["qkT", "Cbf"], W=[("B", 6)])
            for j in range(4):
                self.mm(b4(4)[:, j, :], kE[:, j * 128:(j + 1) * 128], vs[:, 2 * j, :], True, False,
                        R=["kE", ("vs", j // 2)], W=[("B", 4)])
                self.mm(b4(4)[:, j, :], kO[:, j * 128:(j + 1) * 128], vs[:, 2 * j + 1, :], False, True,
                        R=["kO", ("vs", j // 2)], W=[("B", 4)])
                self.mm(B[6][:, 40 + j:41 + j], kE[:, j * 128:(j + 1) * 128], eabf[:, 2 * j:2 * j + 1], True, False,
                        R=["kE", "eabf"], W=[("B", 6)])
                self.mm(B[6][:, 40 + j:41 + j], kO[:, j * 128:(j + 1) * 128], eabf[:, 2 * j + 1:2 * j + 2], False, True,
                        R=["kO", "eabf"], W=[("B", 6)])
            self.tt("dve", C32[:, :, 0:128], C32[:, :, 0:128], b4(4), ALU.add, R=["C32", ("B", 4)], W=["C32"])
            self.tt("dve", C32[:, :, 128], C32[:, :, 128], B[6][:, 40:44], ALU.add, R=["C32", ("B", 6)], W=["C32"])
            self.tt("dve", C32, C32, gsel.unsqueeze(2).to_broadcast([128, 4, 132]), ALU.mult, R=["C32", "gsel"], W=["C32"])
            self.act(Cbf, C32, AF.Copy, R=["C32"], W=["Cbf"])
            self.tt("dve", d1, B[6][:, 32:40], eb, ALU.mult, R=[("B", 6), "eb"], W=["d1"])
            self.stt("dve", d2, d1, -1.0, d1, ALU.mult, ALU.max, R=["d1"], W=["d2"])
            self.ts("dve", d2, d2, 1.0, None, ALU.max, R=["d2"], W=["d2"])
            self.P.add("dve", lambda e: e.reciprocal(out=d2, in_=d2), R=["d2"], W=["d2"])
            self.tt("dve", scl, d2, eb, ALU.mult, R=["d2", "eb"], W=["scl"])
            hc3 = hc.rearrange("p (h s) -> p h s", h=H)
            for i in range(2):
                self.tt("dve", hc3[:, 4 * i:4 * i + 4, :], b4(2 + i),
                        scl[:, 4 * i:4 * i + 4].unsqueeze(2).to_broadcast([128, 4, 128]), ALU.mult,
                        R=[("B", 2 + i), "scl"], W=[("hc", i)])
            for h in range(H):
                self.act(jk, hc3[:, h, :], AF.Square, R=[("hc", h // 4)], W=["jk", "ssh"], accum_out=ssh[:, h:h + 1])
            self.act(l2, ssh, AF.Ln, R=["ssh"], W=["l2"], scale=1.0 / 128.0, bias=EPS)
            self.act(rs, l2, AF.Exp, R=["l2"], W=["rs"], scale=-0.5, bias=float(np.log(0.5)))
            self.tt("dve", hc3, hc3, rs.unsqueeze(2).to_broadcast([128, H, 128]), ALU.mult,
                    R=[("hc", 0), ("hc", 1), "rs"], W=[("hc", 0), ("hc", 1)])
            self.tt("pool", hc, hc, hgain, ALU.mult, R=[("hc", 0), ("hc", 1), "hgain"], W=[("hc", 0), ("hc", 1)])
            self.stt("dve", outb, sig, 1.0, hc, ALU.add, ALU.mult,
                     R=[("sig", 0), ("sig", 1), ("hc", 0), ("hc", 1)], W=[("hb", 0)])
            for j in range(8):
                self.tr(psT[:, j * 128:(j + 1) * 128], outb[:, j * 128:(j + 1) * 128], self.ident_bf[:],
                        R=[("hb", 0), "ident_bf"], W=[("B", 7)])
            self.act(outT, psT3, AF.Copy, R=[("B", 7)], W=["outT"])
            for half in range(2):
                bk = 5 if half == 0 else 1
                for vc in range(8):
                    self.mm(B[bk][:], outT[:, vc, :], wout[:, vc, half * 512:(half + 1) * 512], vc == 0, vc == 7,
                            R=["outT", "wout"], W=[("B", bk)])
                xs = self.x[:, t, half * 512:(half + 1) * 512]
                self.tt("dve", xs, xs, B[bk][:], ALU.add, R=[("B", bk), ("x", t)], W=[("x", t)])

    def pool_phase(self, l):
        P = self.P
        slot = l // 2
        P.barrier()
        self.off = 0
        band = [self.carve([128, 128], F32) for _ in range(4)]
        band0 = [self.carve([128, 128], F32) for _ in range(4)]
        offm = [self.carve([128, 128], F32) for _ in range(4)]
        sc16 = self.carve([128, 4, 16], F32)
        h32 = [self.carve([128, D], F32) for _ in range(3)]
        yT = [self.carve([128, KC, 128], BF16) for _ in range(2)]
        wp = self.carve([128, 4, 2, 256], BF16)
        pscale = self.carve([128, D], F32)
        tmp = [self.carve([128, 512], F32) for _ in range(2)]
        jkD = self.carve([128, D], BF16)
        B = self.B
        wsrc = self.din("pool_w_group", [2, 4, 256, 256])[slot]
        for g in range(4):
            self.dma("pool", wp[:, g, :, :], wsrc[g].rearrange("(k p) d -> p k d", p=128), slot=("wp", g), W=[("wp", g)])
        self.dma("sp", pscale, self.din("pool_scale", [2, D])[slot:slot + 1, :].partition_broadcast(128),
                 slot=("pscale",), W=["pscale"])
        gi = self.load_gain(self.din("norm_mix", [DEPTH, D])[l:l + 1, :])

        def sel(ap, pattern, cm, base, key):
            self.P.add("pool", lambda e: e.affine_select(out=ap, in_=ap, compare_op=ALU.is_ge, fill=0.0, base=base,
                                                         pattern=pattern, channel_multiplier=cm), W=[key])
        for wi, w in enumerate(WINS):
            self.memset("pool", band[wi], 1.0 / w, W=[("band", wi)])
            sel(band[wi], [[1, 128]], -1, 0, ("band", wi))
            sel(band[wi], [[-1, 128]], 1, w - 1, ("band", wi))
            self.memset("pool", offm[wi], 1.0 / w, W=[("off", wi)])
            sel(offm[wi], [[-1, 128]], 1, w - 129, ("off", wi))
            self.ts("dve", sc16[:, wi, :], self.invcnt[:, wi, :], float(w), None, ALU.mult, R=["invcnt"], W=[("sc16", wi)])
            self.copy("dve", band0[wi], band[wi], R=[("band", wi)], W=[("band0", wi)])
            self.tt("dve", band0[wi][:, 0:16], band[wi][:, 0:16], sc16[:, wi, :], ALU.mult,
                    R=[("band", wi), ("sc16", wi)], W=[("band0", wi)])
            self.tt("dve", band0[wi], band0[wi], self.ident_f[:], ALU.subtract, R=[("band0", wi), "ident_f"], W=[("band0", wi)])
            self.tt("dve", band[wi], band[wi], self.ident_f[:], ALU.subtract, R=[("band", wi), "ident_f"], W=[("band", wi)])

        self.norm_stats(jkD)

        def b4(i):
            return B[i][:].rearrange("p (h s) -> p h s", h=4)

        def norm_tile(t):
            self.stt("dve", h32[t % 3], self.x[:, t, :], self.rstd[:, t:t + 1], self.gain_bc[gi][:], ALU.mult, ALU.mult,
                     R=[("x", t), "rstd", ("gain", gi)], W=[("h32", t % 3)] + (["junk"] if t == 0 else []))

        def band_mm(t):
            par = t % 2
            for c in range(KC):
                wi = c // 2
                bk = 2 * par + c // 4
                out = b4(bk)[:, c % 4, :]
                cs = slice(c * 128, (c + 1) * 128)
                if t == 0:
                    self.mm(out, h32[0][:, cs], band0[wi], True, True, R=[("h32", 0), ("band0", wi)], W=[("B", bk)])
                else:
                    self.mm(out, h32[t % 3][:, cs], band[wi], True, False, R=[("h32", t % 3), ("band", wi)], W=[("B", bk)])
                    self.mm(out[:, 0:16], h32[(t - 1) % 3][:, cs], offm[wi][:, 0:16], False, True,
                            R=[("h32", (t - 1) % 3), ("off", wi)], W=[("B", bk)])

        def evac_y(t):
            par = t % 2
            self.act(yT[par][:, 0:4, :], b4(2 * par), AF.Copy, R=[("B", 2 * par)], W=[("yT", par, 0)])
            self.copy("dve", yT[par][:, 4:8, :], b4(2 * par + 1), R=[("B", 2 * par + 1)], W=[("yT", par, 1)])

        def pool_mm(t):
            par = t % 2
            for g in range(4):
                bk = 4 + 2 * par + g // 2
                for kc in range(2):
                    self.mm(B[bk][:, (g % 2) * 256:(g % 2 + 1) * 256], yT[par][:, 2 * g + kc, :], wp[:, g, kc, :], kc == 0, kc == 1,
                            R=[("yT", par, g // 2), ("wp", g)], W=[("B", bk)])

        def final(t):
            par = t % 2
            for half in range(2):
                bk = 4 + 2 * par + half
                self.tt("dve", tmp[half], B[bk][:], pscale[:, half * 512:(half + 1) * 512], ALU.mult,
                        R=[("B", bk), "pscale"], W=[("tmp", half)])
                xs = self.x[:, t, half * 512:(half + 1) * 512]
                self.tt("dve", xs, xs, tmp[half], ALU.add, R=[("tmp", half), ("x", t)], W=[("x", t)])
            self.emit_ss(t, jkD)

        norm_tile(0)
        norm_tile(1)
        band_mm(0)
        evac_y(0)
        for t in range(NT):
            if t + 2 < NT:
                norm_tile(t + 2)
            if t + 1 < NT:
                band_mm(t + 1)
            pool_mm(t)
            if t + 1 < NT:
                evac_y(t + 1)
            final(t)
        self.stats_ready = True

    def pool_phase_v1(self, l):
        P = self.P
        slot = l // 2
        P.barrier()
        self.off = 0
        hT32 = self.carve([128, KC, S], F32)
        yT = self.carve([128, KC, S], BF16)
        sbuf_ = [self.carve([128, S], F32) for _ in range(2)]
        wp = self.carve([128, 4, 2, 256], BF16)
        pscale = self.carve([128, D], F32)
        h32 = [self.carve([128, D], F32) for _ in range(2)]
        t16 = self.carve([128, 16], F32)
        B = self.B
        wsrc = self.din("pool_w_group", [2, 4, 256, 256])[slot]
        for g in range(4):
            self.dma("pool", wp[:, g, :, :], wsrc[g].rearrange("(k p) d -> p k d", p=128), slot=("wp", g), W=[("wp", g)])
        self.dma("sp", pscale, self.din("pool_scale", [2, D])[slot:slot + 1, :].partition_broadcast(128),
                 slot=("pscale",), W=["pscale"])
        gi = self.load_gain(self.din("norm_mix", [DEPTH, D])[l:l + 1, :])
        self.norm_stats(h32[0])
        for t in range(NT):
            ht = h32[t % 2]
            self.stt("dve", ht, self.x[:, t, :], self.rstd[:, t:t + 1], self.gain_bc[gi][:], ALU.mult, ALU.mult,
                     R=[("x", t), "rstd", ("gain", gi)], W=[("h32", t % 2), "junk"] if t % 2 == 0 else [("h32", t % 2)])
            for kc in range(KC):
                bk = 6 + kc // 4
                self.tr(B[bk][:, (kc % 4) * 128:(kc % 4 + 1) * 128], ht[:, kc * 128:(kc + 1) * 128], self.ident_f[:],
                        R=[("h32", t % 2), "ident_f"], W=[("B", bk)])
            self.act(hT32[:, 0:4, t * 128:(t + 1) * 128], B[6][:].rearrange("p (k s) -> p k s", k=4), AF.Copy,
                     R=[("B", 6)], W=[("hT32", c) for c in range(0, 4)])
            self.copy("dve", hT32[:, 4:8, t * 128:(t + 1) * 128], B[7][:].rearrange("p (k s) -> p k s", k=4),
                      R=[("B", 7)], W=[("hT32", c) for c in range(4, 8)])
        for c in range(KC):
            wi = c // 2
            win_ = WINS[wi]
            src = hT32[:, c, :]
            cur, ckey = src, ("hT32", c)
            sh, n = 1, 0
            while sh < win_:
                dst = sbuf_[n % 2]
                dkey = ("sb", n % 2)
                self.tt("dve", dst[:, sh:], cur[:, sh:], cur[:, 0:S - sh], ALU.add, R=[ckey], W=[dkey])
                self.copy("dve", dst[:, 0:sh], cur[:, 0:sh], R=[ckey], W=[dkey])
                cur, ckey = dst, dkey
                sh *= 2
                n += 1
            self.stt("dve", yT[:, c, :], cur, 1.0 / win_, src, ALU.mult, ALU.subtract, R=[ckey, ("hT32", c)], W=[("yT", c)])
            self.tt("dve", t16, cur[:, 0:16], self.invcnt[:, wi, :], ALU.mult, R=[ckey, "invcnt"], W=["t16"])
            self.tt("dve", yT[:, c, 0:16], t16, src[:, 0:16], ALU.subtract, R=["t16", ("hT32", c)], W=[("yT", c)])
        for t in range(NT):
            tok = slice(t * 128, (t + 1) * 128)
            for g in range(4):
                bk = (t % 2) * 2 + g // 2
                for kc in range(2):
                    self.mm(B[bk][:, (g % 2) * 256:(g % 2 + 1) * 256], yT[:, 2 * g + kc, tok], wp[:, g, kc, :], kc == 0, kc == 1,
                            R=[("yT", 2 * g + kc), ("wp", g)], W=[("B", bk)])
            for half in range(2):
                bk = (t % 2) * 2 + half
                tmp = h32[half][:, 0:512]
                self.tt("dve", tmp, B[bk][:], pscale[:, half * 512:(half + 1) * 512], ALU.mult,
                        R=[("B", bk), "pscale"], W=[("h32", half)])
                xs = self.x[:, t, half * 512:(half + 1) * 512]
                self.tt("dve", xs, xs, tmp, ALU.add, R=[("h32", half), ("x", t)], W=[("x", t)])

    def ffn_phase(self, l):
        P = self.P
        P.barrier()
        self.off = 0
        hT = self.carve([128, KC, S], BF16)
        abuf = [self.carve([128, 4, S], BF16) for _ in range(2)]
        wgu = [self.carve([128, KC, 2, 256], BF16) for _ in range(3)]
        wd = [self.carve([128, 4, D], BF16) for _ in range(2)]
        hb = [self.carve([128, D], BF16) for _ in range(2)]
        sl = [self.carve([128, 512], BF16) for _ in range(2)]
        jkD = self.carve([128, D], BF16)
        nxt_needs_stats = (l % 2 == 0) or (l == self.layers[-1])
        B = self.B
        wgu_src = self.din("ffn_w_gate_up", [DEPTH, D, 2 * DFF])[l].rearrange("(k p) n -> p k n", p=128)
        wd_src = self.din("ffn_w_down", [DEPTH, DFF, D])[l].rearrange("(j p) d -> p j d", p=128)
        groups = [(j0, min(4, NJ - j0)) for j0 in range(0, NJ, 4)]

        def load_gu(n):
            s_ = n % 3
            self.dma("pool", wgu[s_][:, :, 0, :], wgu_src[:, :, n * 256:(n + 1) * 256], slot=("wg", s_), W=[("wg", s_)])
            self.dma("pool", wgu[s_][:, :, 1, :], wgu_src[:, :, DFF + n * 256:DFF + (n + 1) * 256], slot=("wu", s_), W=[("wu", s_)])

        def load_d(gi):
            j0, nj = groups[gi]
            self.dma("pool", wd[gi % 2][:, 0:nj, :], wd_src[:, j0:j0 + nj, :], slot=("wd", gi % 2), W=[("wd", gi % 2)])

        load_gu(0)
        load_gu(1)
        load_gu(2)
        load_d(0)
        self.norm_to_hT(self.din("norm_ffn", [DEPTH, D])[l:l + 1, :], hT, hb)

        cnt = [0]

        def gateup(gi):
            j0, nj = groups[gi]
            ab = abuf[gi % 2]
            for jj in range(nj):
                j = j0 + jj
                n, sub = j // 2, j % 2
                s_ = n % 3
                for tg in range(4):
                    pr = cnt[0] % 2
                    cnt[0] += 1
                    bg, bu = B[2 * pr], B[2 * pr + 1]
                    rk = [("hT", 4 * tg + i) for i in range(4)]
                    for kc in range(KC):
                        self.mm(bg[:], wgu[s_][:, kc, 0, sub * 128:(sub + 1) * 128], hT[:, kc, tg * 512:(tg + 1) * 512],
                                kc == 0, kc == KC - 1, R=rk + [("wg", s_)], W=[("B", 2 * pr)])
                    for kc in range(KC):
                        self.mm(bu[:], wgu[s_][:, kc, 1, sub * 128:(sub + 1) * 128], hT[:, kc, tg * 512:(tg + 1) * 512],
                                kc == 0, kc == KC - 1, R=rk + [("wu", s_)], W=[("B", 2 * pr + 1)])
                    self.act(sl[pr], bg[:], AF.Silu, R=[("B", 2 * pr)], W=[("sl", pr)])
                    self.tt("dve", ab[:, jj, tg * 512:(tg + 1) * 512], sl[pr], bu[:], ALU.mult,
                            R=[("sl", pr), ("B", 2 * pr + 1)], W=[("ab", gi % 2, tg)])
                if sub == 1 or j == NJ - 1:
                    if n + 3 < NJ // 2:
                        load_gu(n + 3)

        def down(gi):
            j0, nj = groups[gi]
            ab = abuf[gi % 2]
            for t in range(NT):
                for half in range(2):
                    bk = 4 + (2 * t + half) % 4
                    for jj in range(nj):
                        self.mm(B[bk][:], ab[:, jj, t * 128:(t + 1) * 128], wd[gi % 2][:, jj, half * 512:(half + 1) * 512],
                                jj == 0, jj == nj - 1, R=[("ab", gi % 2, t // 4), ("wd", gi % 2)], W=[("B", bk)])
                    xs = self.x[:, t, half * 512:(half + 1) * 512]
                    self.tt("dve", xs, xs, B[bk][:], ALU.add, R=[("B", bk), ("x", t)], W=[("x", t)])
                if gi == len(groups) - 1 and nxt_needs_stats:
                    self.emit_ss(t, jkD)
            if gi + 2 < len(groups):
                load_d(gi + 2)

        gateup(0)
        load_d(1)
        for gi in range(len(groups)):
            if gi + 1 < len(groups):
                gateup(gi + 1)
            down(gi)
        self.stats_ready = nxt_needs_stats

    def final_phase(self):
        P = self.P
        P.barrier()
        self.off = 0
        ob = [self.carve([128, D], F32) for _ in range(2)]
        if self.final_norm:
            gi = self.load_gain(self.din("final_norm", [1, D])[0:1, :])
            self.norm_stats(ob[0])
        stores = []
        for t in range(NT):
            o = ob[t % 2]
            if self.final_norm:
                self.stt("dve", o, self.x[:, t, :], self.rstd[:, t:t + 1], self.gain_bc[gi][:], ALU.mult, ALU.mult,
                         R=[("x", t), "rstd", ("gain", gi)], W=[("ob", t % 2), "junk"] if t % 2 == 0 else [("ob", t % 2)])
                src, rk = o, [("ob", t % 2)]
            else:
                src, rk = self.x[:, t, :], [("x", t)]
            stores.append(self.dma("sp", self.out[t * 128:(t + 1) * 128, :], src, slot=("st", t), R=rk, W=[("outd", t)]))
        P.add("sp", None, extra=stores)


_CACHE = {}


def _get_prog(layers, final_norm):
    key = (tuple(layers), final_norm)
    if key not in _CACHE:
        b = Builder(layers, final_norm)
        nc = b.build()
        _CACHE[key] = (nc, sorted(b.dram.keys()))
    return _CACHE[key]


def _run(layers, final_norm, xs, inputs):
    nc, names = _get_prog(layers, final_norm)
    in_maps = []
    for c in range(N_CORES):
        m = {}
        for n in names:
            if n == "x":
                m[n] = np.ascontiguousarray(xs[c])
            elif n == "final_norm":
                m[n] = np.ascontiguousarray(inputs[n]).reshape(1, D)
            else:
                m[n] = np.ascontiguousarray(inputs[n])
        in_maps.append(m)
    res = run_bass_kernel_spmd(nc, in_maps, core_ids=list(range(N_CORES)))
    return [res.results[c]["out"] for c in range(N_CORES)]


LAUNCH_GROUPS = [[0, 1, 2, 3]]


def kernel(**inputs):
    inputs = {k: np.asarray(v, dtype=np.float32) for k, v in inputs.items()}
    xs = [inputs["x"][b] for b in range(N_CORES)]
    for gi, grp in enumerate(LAUNCH_GROUPS):
        xs = _run(grp, gi == len(LAUNCH_GROUPS) - 1, xs, inputs)
    return np.stack(xs, axis=0).astype(np.float32)
```

```python
import numpy as np
from contextlib import ExitStack
import concourse.bass as bass
import concourse.mybir as mybir
from concourse.bass_utils import run_bass_kernel_spmd

F32 = mybir.dt.float32
BF16 = mybir.dt.bfloat16
U8 = mybir.dt.uint8
AF = mybir.ActivationFunctionType
ALU = mybir.AluOpType
AX = mybir.AxisListType

S = 2048
D = 1024
NT = 16
KC = 8
H = 8
DFF = 2816
NJ = 22
EPS = 1e-6
N_CORES = 8
DEPTH = 4
WINS = (2, 4, 8, 16)


class _Op:
    __slots__ = ("eng", "fn", "waits", "sig", "pos", "gid", "slot", "cnt", "known", "sidx")


class Prog:
    ENGS = ("pe", "act", "dve", "pool", "sp")

    def __init__(self, self_sync=True):
        self.streams = {e: [] for e in self.ENGS}
        self.known = {e: {} for e in self.ENGS}
        self.last_w = {}
        self.readers = {}
        self.slot_cnt = {}
        self.slot_last = {}
        self.gid = 0
        self.self_sync = self_sync

    def add(self, eng, fn, R=(), W=(), slot=None, extra=()):
        op = _Op()
        op.eng, op.fn, op.waits, op.sig, op.slot, op.cnt, op.sidx = eng, fn, [], False, slot, 0, 0
        op.gid = self.gid
        self.gid += 1
        st = self.streams[eng]
        op.pos = len(st)
        st.append(op)
        psum_r = [k for k in R if isinstance(k, tuple) and k[0] == "B"]
        if psum_r:
            R = [k for k in R if not (isinstance(k, tuple) and k[0] == "B")]
            W = list(W) + psum_r
        deps = {}
        for k in R:
            lw = self.last_w.get(k)
            if lw is not None:
                deps[lw.gid] = lw
        for k in W:
            lw = self.last_w.get(k)
            if lw is not None:
                deps[lw.gid] = lw
            for r in self.readers.get(k, ()):
                deps[r.gid] = r
        for d in extra:
            deps[d.gid] = d
        kn = self.known[eng]
        for g in sorted(deps, reverse=True):
            d = deps[g]
            if d.slot is not None:
                src, val = d.slot, d.cnt
            else:
                if d.eng == eng and (eng == "pe" or not self.self_sync):
                    continue
                src, val = d.eng, d.pos
            if kn.get(src, -1) >= val:
                continue
            assert d.fn is not None
            op.waits.append(d)
            d.sig = True
            for s_, v_ in d.known.items():
                if kn.get(s_, -1) < v_:
                    kn[s_] = v_
        if slot is not None:
            c = self.slot_cnt.get(slot, 0) + 1
            self.slot_cnt[slot] = c
            op.cnt = c
            prev = self.slot_last.get(slot)
            if prev is not None:
                assert kn.get(slot, -1) >= prev.cnt, ("two DMAs in flight on slot", slot)
            self.slot_last[slot] = op
            op.known = dict(kn)
            op.known[slot] = c
        else:
            op.known = dict(kn)
            op.known[eng] = op.pos
        for k in R:
            self.readers.setdefault(k, []).append(op)
        for k in W:
            self.last_w[k] = op
            self.readers[k] = []
        return op

    def barrier(self):
        if not getattr(self, "_had_barrier", False):
            self._had_barrier = True
            return
        lasts = []
        for e in self.ENGS:
            for op in reversed(self.streams[e]):
                if op.fn is not None and op.slot is None:
                    lasts.append(op)
                    break
        lasts += list(self.slot_last.values())
        for e in self.ENGS:
            self.add(e, None, extra=lasts)

    def emit(self, block, sems, slot_sems):
        for e in self.ENGS:
            n = 0
            for op in self.streams[e]:
                if op.slot is None and op.sig:
                    n += 1
                    op.sidx = n

        def body_for(e):
            st = self.streams[e]

            def body(eng):
                for op in st:
                    for d in op.waits:
                        if d.slot is not None:
                            eng.wait_ge(slot_sems[d.slot], 16 * d.cnt)
                        else:
                            eng.wait_ge(sems[d.eng], d.sidx)
                    if op.fn is not None:
                        ins = op.fn(eng)
                        if op.slot is not None:
                            ins.then_inc(slot_sems[op.slot], 16)
                        elif op.sig:
                            ins.then_inc(sems[e], 1)
            return body

        block.tensor(body_for("pe"))
        block.scalar(body_for("act"))
        block.vector(body_for("dve"))
        block.gpsimd(body_for("pool"))
        block.sync(body_for("sp"))


def _dsize(dt):
    return {F32: 4, BF16: 2, U8: 1}[dt]


class Builder:
    def __init__(self, layers, final_norm, self_sync=True, parts=("mixer", "ffn"), ntiles=NT):
        self.parts = parts
        self.ntiles = ntiles
        self.layers = list(layers)
        self.final_norm = final_norm
        self.nc = bass.Bass("TRN2", target_bir_lowering=False)
        self.P = Prog(self_sync=self_sync)
        self.dram = {}
        self.slots = set()

    def din(self, name, shape):
        if name not in self.dram:
            self.dram[name] = self.nc.dram_tensor(name, list(shape), F32, kind="ExternalInput").ap()
        return self.dram[name]

    def view(self, off, shape, dt):
        n = 1
        for s_ in shape[1:]:
            n *= s_
        nb = n * _dsize(dt)
        assert off % 32 == 0 and off + nb <= self.arena_bytes, (off, nb, self.arena_bytes)
        ap = self.arena[:, off:off + nb].bitcast(dt)
        if len(shape) == 3:
            ap = ap.rearrange("p (a b) -> p a b", a=shape[1])
        elif len(shape) == 4:
            ap = ap.rearrange("p (a b c) -> p a b c", a=shape[1], b=shape[2])
        return ap

    def carve(self, shape, dt):
        n = 1
        for s_ in shape[1:]:
            n *= s_
        nb = (n * _dsize(dt) + 31) // 32 * 32
        v = self.view(self.off, shape, dt)
        self.off += nb
        return v

    def dma(self, eng, out, in_, slot, R=(), W=()):
        self.slots.add(slot)
        return self.P.add(eng, lambda e: e.dma_start(out=out, in_=in_), R=R, W=W, slot=slot)

    def mm(self, out, lhsT, rhs, start, stop, R=(), W=()):
        return self.P.add("pe", lambda e: e.matmul(out, lhsT=lhsT, rhs=rhs, start=start, stop=stop), R=R, W=W)

    def tr(self, out, in_, ident, R=(), W=()):
        return self.P.add("pe", lambda e: e.transpose(out, in_, ident), R=R, W=W)

    def act(self, out, in_, func, R=(), W=(), **kw):
        return self.P.add("act", lambda e: e.activation(out=out, in_=in_, func=func, **kw), R=R, W=W)

    def tt(self, eng, out, in0, in1, op, R=(), W=()):
        return self.P.add(eng, lambda e: e.tensor_tensor(out=out, in0=in0, in1=in1, op=op), R=R, W=W)

    def ts(self, eng, out, in0, s1, s2, op0, op1=None, R=(), W=()):
        if op1 is None:
            return self.P.add(eng, lambda e: e.tensor_scalar(out=out, in0=in0, scalar1=s1, scalar2=None, op0=op0), R=R, W=W)
        return self.P.add(eng, lambda e: e.tensor_scalar(out=out, in0=in0, scalar1=s1, scalar2=s2, op0=op0, op1=op1), R=R, W=W)

    def stt(self, eng, out, in0, scalar, in1, op0, op1, R=(), W=()):
        return self.P.add(eng, lambda e: e.scalar_tensor_tensor(out=out, in0=in0, scalar=scalar, in1=in1, op0=op0, op1=op1), R=R, W=W)

    def copy(self, eng, out, in_, R=(), W=()):
        return self.P.add(eng, lambda e: e.tensor_copy(out, in_), R=R, W=W)

    def memset(self, eng, ap, val, R=(), W=()):
        return self.P.add(eng, lambda e: e.memset(ap, val), R=R, W=W)

    def build(self):
        nc = self.nc
        P = self.P
        with ExitStack() as es:
            def sb(name, shape, dt):
                return es.enter_context(nc.sbuf_tensor(name, shape, dt))

            self.x = sb("x_res", [128, NT, D], F32)
            self.ident_bf = sb("ident_bf", [128, 128], BF16)
            self.ident_f = sb("ident_f", [128, 128], F32)
            self.causal = sb("causal", [128, 128], BF16)
            self.negtri = sb("negtri", [128, 128], F32)
            self.negones = sb("negones", [128, 128], F32)
            self.invcnt = sb("invcnt", [128, 4, 16], F32)
            self.small = sb("small", [128, 512], F32)
            self.gain_bc = [sb(f"gain_bc{i}", [128, D], F32) for i in range(2)]
            self.gain_n = 0
            self.arena_bytes = (nc.sbuf_bytes_remaining - 256) // 64 * 64
            self.arena = sb("arena", [128, self.arena_bytes], U8)
            self.B = [es.enter_context(nc.psum_tensor(f"B{i}", [128, 512], F32)) for i in range(8)]
            self.out = nc.dram_tensor("out", [S, D], F32, kind="ExternalOutput").ap()
            xin = self.din("x", [S, D])

            sm = self.small
            self.ss = sm[:, 0:16]
            self.lnv = sm[:, 16:32]
            self.rstd = sm[:, 32:48]

            self.setup_consts()
            for t in range(NT):
                self.dma("sp", self.x[:, t, :], xin[t * 128:(t + 1) * 128, :], slot=("xld", t), W=[("x", t)])

            for l in self.layers:
                if "mixer" in self.parts:
                    if l % 2 == 0:
                        self.mlstm_phase(l)
                    else:
                        self.pool_phase(l)
                if "ffn" in self.parts:
                    self.ffn_phase(l)
            self.final_phase()

            sems = {e: es.enter_context(nc.semaphore(f"s_{e}")) for e in Prog.ENGS}
            slot_sems = {}
            for i, sl in enumerate(sorted(self.slots, key=str)):
                slot_sems[sl] = es.enter_context(nc.semaphore(f"d_{i}"))
            block = es.enter_context(nc.Block())
            P.emit(block, sems, slot_sems)
        return nc

    def setup_consts(self):
        def sel(ap, cmp, pattern, cm, key):
            self.P.add("pool", lambda e: e.affine_select(out=ap, in_=ap, compare_op=cmp, fill=0.0, base=0,
                                                         pattern=pattern, channel_multiplier=cm), W=[key])
        self.memset("pool", self.ident_bf[:], 1.0, W=["ident_bf"])
        sel(self.ident_bf[:], ALU.is_equal, [[-1, 128]], 1, "ident_bf")
        self.memset("pool", self.ident_f[:], 1.0, W=["ident_f"])
        sel(self.ident_f[:], ALU.is_equal, [[-1, 128]], 1, "ident_f")
        self.memset("pool", self.causal[:], 1.0, W=["causal"])
        sel(self.causal[:], ALU.is_ge, [[1, 128]], -1, "causal")
        self.memset("pool", self.negtri[:], -1.0, W=["negtri"])
        sel(self.negtri[:], ALU.is_ge, [[1, 128]], -1, "negtri")
        self.memset("pool", self.negones[:], -1.0, W=["negones"])
        for wi, w in enumerate(WINS):
            self.memset("pool", self.invcnt[:, wi, :], 1.0 / w, W=["invcnt"])
            for t in range(w - 1):
                self.memset("pool", self.invcnt[:, wi, t:t + 1], 1.0 / (t + 1), W=["invcnt"])

    def load_gain(self, row_ap):
        i = self.gain_n % 2
        self.gain_n += 1
        self.dma("sp", self.gain_bc[i][:], row_ap.partition_broadcast(128), slot=("gain", i), W=[("gain", i)])
        return i

    def emit_ss(self, t, junk):
        self.act(junk, self.x[:, t, :], AF.Square, R=[("x", t)], W=["junk", ("ss", t)], accum_out=self.ss[:, t:t + 1])

    def norm_stats(self, junk):
        if not getattr(self, "stats_ready", False):
            for t in range(NT):
                self.emit_ss(t, junk)
        self.stats_ready = False
        self.act(self.lnv, self.ss, AF.Ln, R=[("ss", t) for t in range(NT)], W=["lnv"], scale=1.0 / D, bias=EPS)
        self.act(self.rstd, self.lnv, AF.Exp, R=["lnv"], W=["rstd"], scale=-0.5)

    def norm_to_hT(self, gain_row, hT, hb):
        gi = self.load_gain(gain_row)
        self.norm_stats(hb[0])
        for t in range(NT):
            hbt = hb[t % 2]
            bk = 6 + t % 2
            psT = self.B[bk][:].bitcast(BF16)
            self.stt("dve", hbt, self.x[:, t, :], self.rstd[:, t:t + 1], self.gain_bc[gi][:], ALU.mult, ALU.mult,
                     R=[("x", t), "rstd", ("gain", gi)], W=[("hb", t % 2), "junk"] if t % 2 == 0 else [("hb", t % 2)])
            for kc in range(KC):
                self.tr(psT[:, kc * 128:(kc + 1) * 128], hbt[:, kc * 128:(kc + 1) * 128], self.ident_bf[:],
                        R=[("hb", t % 2), "ident_bf"], W=[("B", bk)])
            self.act(hT[:, :, t * 128:(t + 1) * 128], psT.rearrange("p (k s) -> p k s", k=KC), AF.Copy,
                     R=[("B", bk)], W=[("hT", t)])

    def mlstm_phase(self, l):
        P = self.P
        slot = l // 2
        P.barrier()
        self.off = 0
        NTL = self.ntiles
        win = self.carve([128, KC, 3088], BF16)
        wout = self.carve([128, KC, D], BF16)
        hgain = self.carve([128, D], F32)
        bias_bc = self.carve([128, 16], F32)
        hb = [self.carve([128, D], BF16) for _ in range(2)]
        hTt = [self.carve([128, KC, 128], BF16) for _ in range(2)]
        q_bf = self.carve([128, 512], BF16)
        k_bf = self.carve([128, 512], BF16)
        kE = [self.carve([128, 512], BF16) for _ in range(2)]
        kO = [self.carve([128, 512], BF16) for _ in range(2)]
        qkT = [self.carve([128, 8, 128], BF16) for _ in range(2)]
        vs = [self.carve([128, H, 128], BF16) for _ in range(2)]
        eo = [self.carve([128, D], F32) for _ in range(2)]
        PT = self.carve([128, H, 128], BF16)
        hcs = [self.carve([128, D], F32) for _ in range(2)]
        outbs = [self.carve([128, D], BF16) for _ in range(2)]
        outTs = [self.carve([128, 8, 128], BF16) for _ in range(2)]
        C32 = self.carve([128, 4, 132], F32)
        Cbf = [self.carve([128, 4, 132], BF16) for _ in range(2)]
        eabf = [self.carve([128, 16], BF16) for _ in range(2)]
        sqb = self.carve([128, D], F32)
        jkD = self.carve([128, D], BF16)
        sm = self.small
        def smv(p):
            o = 64 + p * 120
            names = [("ssA", 1), ("lnA", 1), ("rsA", 1), ("g1", 16), ("e2", 16), ("dd", 16), ("gs", 16), ("ee", 8),
                     ("lfn", 8), ("a", 8), ("ea", 8), ("eb", 8), ("gp", 4), ("gsel", 4)]
            d_ = {}
            for n_, w_ in names:
                d_[n_] = sm[:, o:o + w_]
                o += w_
            return d_
        SV = [smv(0), smv(1)]
        d1, d2, scl, ssh, l2, rs = (sm[:, 320:328], sm[:, 328:336], sm[:, 336:344], sm[:, 344:352], sm[:, 352:360], sm[:, 360:368])
        B = self.B

        w_in = self.din("mlstm_w_in", [2, D, 3088])[slot].rearrange("(k p) n -> p k n", p=128)
        w_out = self.din("mlstm_w_out", [2, D, D])[slot].rearrange("(k p) n -> p k n", p=128)
        pieces = [(0, 1024), (3072, 3088), (1024, 2048), (2048, 3072)]
        pkey = {0: 0, 1: 0, 2: 2, 3: 2, 4: 3, 5: 3}
        for i, (c0, c1) in enumerate(pieces):
            self.dma("pool", win[:, :, c0:c1], w_in[:, :, c0:c1], slot=("win", i), W=[("win", i)])
        self.dma("pool", wout, w_out, slot=("wout",), W=["wout"])
        self.dma("sp", hgain, self.din("mlstm_head_gain", [2, D])[slot:slot + 1, :].partition_broadcast(128),
                 slot=("hgain",), W=["hgain"])
        self.dma("sp", bias_bc, self.din("mlstm_gate_bias", [2, 16])[slot:slot + 1, :].partition_broadcast(128),
                 slot=("gbias",), W=["gbias"])
        gi = self.load_gain(self.din("norm_mix", [DEPTH, D])[l:l + 1, :])
        self.memset("dve", C32, 0.0, W=["C32"])
        self.memset("dve", Cbf[0], 0.0, W=[("Cbf", 0)])
        for p in range(2):
            self.memset("dve", kE[p], 0.0, W=[("kE", p)])
            self.memset("dve", kO[p], 0.0, W=[("kO", p)])

        psT = B[7][:].bitcast(BF16)
        psT3 = psT.rearrange("p (k s) -> p k s", k=8)

        def b4(i):
            return B[i][:].rearrange("p (h s) -> p h s", h=4)

        self.norm_stats(jkD)

        def A0(t):
            p = t % 2
            self.stt("dve", hb[p], self.x[:, t, :], self.rstd[:, t:t + 1], self.gain_bc[gi][:], ALU.mult, ALU.mult,
                     R=[("x", t), "rstd", ("gain", gi)], W=[("hb", p)])

        def A1(t):
            p = t % 2
            for kc in range(KC):
                self.tr(psT[:, kc * 128:(kc + 1) * 128], hb[p][:, kc * 128:(kc + 1) * 128], self.ident_bf[:],
                        R=[("hb", p), "ident_bf"], W=[("B", 7)])
            self.act(hTt[p], psT3, AF.Copy, R=[("B", 7)], W=[("hTt", p)])

        def proj(t, cb, bank):
            p = t % 2
            for kc in range(KC):
                self.mm(B[bank][:], hTt[p][:, kc, :], win[:, kc, cb * 512:(cb + 1) * 512], kc == 0, kc == KC - 1,
                        R=[("hTt", p), ("win", pkey[cb])], W=[("B", bank)])

        def A2(t, late_fn=None):
            p = t % 2
            v = SV[p]
            proj(t, 0, 4)
            self.act(q_bf, B[4][:], AF.Copy, R=[("B", 4)], W=["q_bf"])
            proj(t, 1, 5)
            for kc in range(KC):
                self.mm(B[6][:, 0:16], hTt[p][:, kc, :], win[:, kc, 3072:3088], kc == 0, kc == KC - 1,
                        R=[("hTt", p), ("win", 1)], W=[("B", 6)])
            self.act(k_bf, B[5][:], AF.Copy, R=[("B", 5)], W=["k_bf"], scale=0.125)
            k4 = B[5][:].rearrange("p (j two c) -> p j two c", two=2, c=64)
            kE4 = kE[p].rearrange("p (j two c) -> p j two c", two=2, c=64)
            kO4 = kO[p].rearrange("p (j two c) -> p j two c", two=2, c=64)
            self.ts("dve", kE4[:, :, 0, :], k4[:, :, 0, :], 0.125, None, ALU.mult, R=[("B", 5)], W=[("kE", p)])
            self.ts("dve", kO4[:, :, 1, :], k4[:, :, 1, :], 0.125, None, ALU.mult, R=[("B", 5)], W=[("kO", p)])
            self.tt("dve", v["g1"], B[6][:, 0:16], bias_bc, ALU.add, R=[("B", 6), "gbias"], W=[("g1", p)])
            self.act(v["e2"], v["g1"], AF.Exp, R=[("g1", p)], W=[("e2", p)], scale=2.0 / 15.0)
            self.ts("dve", v["dd"], v["e2"], 1.0, None, ALU.add, R=[("e2", p)], W=[("dd", p)])
            self.P.add("dve", (lambda o_, i_: (lambda e: e.reciprocal(out=o_, in_=i_)))(v["dd"], v["dd"]), R=[("dd", p)], W=[("dd", p)])
            self.ts("dve", v["gs"], v["dd"], -30.0, 15.0, ALU.mult, ALU.add, R=[("dd", p)], W=[("gs", p)])
            self.act(v["ee"], v["gs"][:, 8:16], AF.Exp, R=[("gs", p)], W=[("ee", p)], scale=-1.0)
            self.act(v["lfn"], v["ee"], AF.Ln, R=[("ee", p)], W=[("lfn", p)], bias=1.0)
            if late_fn is not None:
                late_fn()
            self.mm(B[6][:, 16:24], self.negtri[:], v["lfn"], True, True, R=[("lfn", p), "negtri"], W=[("B", 6)])
            self.mm(B[6][:, 24:32], self.negones[:], v["lfn"], True, True, R=[("lfn", p), "negones"], W=[("B", 6)])
            self.tt("dve", v["a"], v["gs"][:, 0:8], B[6][:, 16:24], ALU.subtract, R=[("gs", p), ("B", 6)], W=[("a", p)])
            self.act(v["eb"], B[6][:, 16:24], AF.Exp, R=[("B", 6)], W=[("eb", p)])
            bl2 = B[6][:, 24:32].rearrange("p (j two) -> p j two", two=2)
            self.copy("dve", v["gp"][0:64, :], bl2[0:64, :, 0], R=[("B", 6)], W=[("gp0", p)])
            self.copy("dve", v["gp"][64:128, :], bl2[64:128, :, 1], R=[("B", 6)], W=[("gp1", p)])
            self.act(v["ea"], v["a"], AF.Exp, R=[("a", p)], W=[("ea", p)])
            self.copy("dve", eabf[p][:, 0:8], v["ea"], R=[("ea", p)], W=[("eabf", p)])
            self.act(v["gsel"], v["gp"], AF.Exp, R=[("gp0", p), ("gp1", p)], W=[("gsel", p)])

        def A3(t):
            p = t % 2
            v = SV[p]
            for j in range(4):
                self.tr(psT[:, j * 128:(j + 1) * 128], q_bf[:, j * 128:(j + 1) * 128], self.ident_bf[:],
                        R=["q_bf", "ident_bf"], W=[("B", 7)])
            for j in range(4):
                self.tr(psT[:, (4 + j) * 128:(5 + j) * 128], k_bf[:, j * 128:(j + 1) * 128], self.ident_bf[:],
                        R=["k_bf", "ident_bf"], W=[("B", 7)])
            self.act(qkT[p], psT3, AF.Copy, R=[("B", 7)], W=[("qkT", p)])
            for i in range(2):
                proj(t, 2 + i, 4 + i)
                self.tt("dve", vs[p][:, 4 * i:4 * i + 4, :], b4(4 + i),
                        v["ea"][:, 4 * i:4 * i + 4].unsqueeze(2).to_broadcast([128, 4, 128]), ALU.mult,
                        R=[("B", 4 + i), ("ea", p)], W=[("vs", p, i)])
            for i in range(2):
                proj(t, 4 + i, 4 + i)
                self.act(eo[p][:, i * 512:(i + 1) * 512], B[4 + i][:], AF.Exp, R=[("B", 4 + i)], W=[("eo", p)], scale=-1.0)

        def A3b(t):
            p = t % 2
            self.act(eo[p], eo[p], AF.Ln, R=[("eo", p)], W=[("eo", p)], bias=1.0)
            self.act(eo[p], eo[p], AF.Exp, R=[("eo", p)], W=[("eo", p)], scale=-1.0)

        def A3c(t):
            p = t % 2
            self.tt("dve", eo[p], eo[p], hgain, ALU.mult, R=[("eo", p), "hgain"], W=[("eo", p)])

        def B1(t):
            p = t % 2
            for h in range(H):
                p0 = (h % 2) * 64
                self.mm(b4(h % 2)[:, h // 2, :], qkT[p][p0:p0 + 64, 4 + h // 2, :], qkT[p][p0:p0 + 64, h // 2, :], True, True,
                        R=[("qkT", p)], W=[("B", h % 2)])
            PT4 = PT.rearrange("p (j two) s -> p j two s", two=2)
            for i in range(2):
                self.tt("dve", PT4[:, :, i, :], b4(i), self.causal[:].unsqueeze(1).to_broadcast([128, 4, 128]), ALU.mult,
                        R=[("B", i), "causal"], W=[("PT", i)])

        def B2(t):
            p = t % 2
            v = SV[p]
            cb_ = Cbf[p]
            for h in range(H):
                p0 = (h % 2) * 64
                qTh = qkT[p][p0:p0 + 64, h // 2, :]
                self.mm(b4(2 + h // 4)[:, h % 4, :], PT[:, h, :], vs[p][:, h, :], True, False,
                        R=[("PT", h % 2), ("vs", p, h // 4)], W=[("B", 2 + h // 4)])
                self.mm(b4(2 + h // 4)[:, h % 4, :], qTh, cb_[p0:p0 + 64, h // 2, 0:128], False, True,
                        R=[("qkT", p), ("Cbf", p)], W=[("B", 2 + h // 4)])
                self.mm(B[6][:, 32 + h:33 + h], PT[:, h, :], eabf[p][:, h:h + 1], True, False,
                        R=[("PT", h % 2), ("eabf", p)], W=[("B", 6)])
                self.mm(B[6][:, 32 + h:33 + h], qTh, cb_[p0:p0 + 64, h // 2, 128:129], False, True,
                        R=[("qkT", p), ("Cbf", p)], W=[("B", 6)])
            for j in range(4):
                self.mm(b4(0)[:, j, :], kE[p][:, j * 128:(j + 1) * 128], vs[p][:, 2 * j, :], True, False,
                        R=[("kE", p), ("vs", p, j // 2)], W=[("B", 0)])
                self.mm(b4(0)[:, j, :], kO[p][:, j * 128:(j + 1) * 128], vs[p][:, 2 * j + 1, :], False, True,
                        R=[("kO", p), ("vs", p, j // 2)], W=[("B", 0)])
                self.mm(B[6][:, 40 + j:41 + j], kE[p][:, j * 128:(j + 1) * 128], eabf[p][:, 2 * j:2 * j + 1], True, False,
                        R=[("kE", p), ("eabf", p)], W=[("B", 6)])
                self.mm(B[6][:, 40 + j:41 + j], kO[p][:, j * 128:(j + 1) * 128], eabf[p][:, 2 * j + 1:2 * j + 2], False, True,
                        R=[("kO", p), ("eabf", p)], W=[("B", 6)])
            self.tt("dve", d1, B[6][:, 32:40], v["eb"], ALU.mult, R=[("B", 6), ("eb", p)], W=["d1"])
            self.stt("dve", d2, d1, -1.0, d1, ALU.mult, ALU.max, R=["d1"], W=["d2"])
            self.ts("dve", d2, d2, 1.0, None, ALU.max, R=["d2"], W=["d2"])
            self.P.add("dve", lambda e: e.reciprocal(out=d2, in_=d2), R=["d2"], W=["d2"])
            self.tt("dve", scl, d2, v["eb"], ALU.mult, R=["d2", ("eb", p)], W=["scl"])
            hc3 = hcs[p].rearrange("p (h s) -> p h s", h=H)
            for i in range(2):
                self.tt("dve", hc3[:, 4 * i:4 * i + 4, :], b4(2 + i),
                        scl[:, 4 * i:4 * i + 4].unsqueeze(2).to_broadcast([128, 4, 128]), ALU.mult,
                        R=[("B", 2 + i), "scl"], W=[("hc", p)])
            self.tt("dve", C32[:, :, 0:128], C32[:, :, 0:128], b4(0), ALU.add, R=["C32", ("B", 0)], W=["C32"])
            self.tt("dve", C32[:, :, 128], C32[:, :, 128], B[6][:, 40:44], ALU.add, R=["C32", ("B", 6)], W=["C32"])
            self.tt("dve", C32, C32, v["gsel"].unsqueeze(2).to_broadcast([128, 4, 132]), ALU.mult, R=["C32", ("gsel", p)], W=["C32"])
            self.act(Cbf[1 - p], C32, AF.Copy, R=["C32"], W=[("Cbf", 1 - p)])

        def B3(t):
            p = t % 2
            hc = hcs[p]
            hc3 = hc.rearrange("p (h s) -> p h s", h=H)
            self.act(sqb, hc, AF.Square, R=[("hc", p)], W=["sqb"])
            self.P.add("dve", lambda e: e.reduce_sum(out=ssh, in_=sqb.rearrange("p (h s) -> p h s", h=H), axis=AX.X),
                       R=["sqb"], W=["ssh"])
            self.act(l2, ssh, AF.Ln, R=["ssh"], W=["l2"], scale=1.0 / 128.0, bias=EPS)
            self.act(rs, l2, AF.Exp, R=["l2"], W=["rs"], scale=-0.5)
            self.tt("dve", hc3, hc3, rs.unsqueeze(2).to_broadcast([128, H, 128]), ALU.mult,
                    R=[("hc", p), "rs"], W=[("hc", p)])
            self.tt("dve", outbs[p], hc, eo[p], ALU.mult, R=[("hc", p), ("eo", p)], W=[("outb", p)])

        def B3pe(t):
            p = t % 2
            for j in range(8):
                self.tr(psT[:, j * 128:(j + 1) * 128], outbs[p][:, j * 128:(j + 1) * 128], self.ident_bf[:],
                        R=[("outb", p), "ident_bf"], W=[("B", 7)])
            self.act(outTs[p], psT3, AF.Copy, R=[("B", 7)], W=[("outT", p)])

        def B4(t):
            p = t % 2
            for half in range(2):
                bk = half
                for vc in range(8):
                    self.mm(B[bk][:], outTs[p][:, vc, :], wout[:, vc, half * 512:(half + 1) * 512], vc == 0, vc == 7,
                            R=[("outT", p), "wout"], W=[("B", bk)])
                xs = self.x[:, t, half * 512:(half + 1) * 512]
                self.tt("dve", xs, xs, B[bk][:], ALU.add, R=[("B", bk), ("x", t)], W=[("x", t)])
            if NTL == NT:
                self.emit_ss(t, jkD)

        A0(0)
        if NTL > 1:
            A0(1)
        A1(0)
        A2(0)
        A3(0)
        for t in range(NTL + 1):
            cur = t < NTL
            nxt = t + 1 < NTL
            late = t >= 1
            if cur:
                B1(t)
            if late:
                A3c(t - 1)
                B3(t - 1)
            if nxt:
                A1(t + 1)
            if t + 2 < NTL:
                A0(t + 2)
            if cur:
                B2(t)
            if late:
                B3pe(t - 1)
            if nxt:
                A2(t + 1, late_fn=(lambda tt=t: B4(tt - 1)) if late else None)
            elif late:
                B4(t - 1)
            if nxt:
                A3(t + 1)
            if cur:
                A3b(t)
        self.stats_ready = (NTL == NT)

    def mlstm_phase_v1(self, l):
        P = self.P
        slot = l // 2
        P.barrier()
        self.off = 0
        hT = self.carve([128, KC, S], BF16)
        win = self.carve([128, KC, 3088], BF16)
        wout = self.carve([128, KC, D], BF16)
        hb = [self.carve([128, D], BF16) for _ in range(2)]
        q_bf = self.carve([128, 512], BF16)
        k_bf = self.carve([128, 512], BF16)
        kE = self.carve([128, 512], BF16)
        kO = self.carve([128, 512], BF16)
        qkT = self.carve([128, 8, 128], BF16)
        vs = self.carve([128, H, 128], BF16)
        PT = self.carve([128, H, 128], BF16)
        sig = self.carve([128, D], F32)
        hc = self.carve([128, D], F32)
        outT = self.carve([128, 8, 128], BF16)
        C32 = self.carve([128, 4, 132], F32)
        Cbf = self.carve([128, 4, 132], BF16)
        hgain = self.carve([128, D], F32)
        bias_bc = self.carve([128, 16], F32)
        eabf = self.carve([128, 8], BF16)
        jk = self.carve([128, 128], BF16)
        outb = hb[0]
        sm = self.small
        g1, th, gs, ee, lfn = sm[:, 64:80], sm[:, 80:96], sm[:, 96:112], sm[:, 112:120], sm[:, 120:128]
        a_, ea, eb, gp, gsel = sm[:, 128:136], sm[:, 136:144], sm[:, 144:152], sm[:, 152:156], sm[:, 156:160]
        d1, d2, scl, ssh, l2, rs = sm[:, 160:168], sm[:, 168:176], sm[:, 176:184], sm[:, 184:192], sm[:, 192:200], sm[:, 200:208]
        B = self.B

        w_in = self.din("mlstm_w_in", [2, D, 3088])[slot].rearrange("(k p) n -> p k n", p=128)
        w_out = self.din("mlstm_w_out", [2, D, D])[slot].rearrange("(k p) n -> p k n", p=128)
        pieces = [(0, 1024), (1024, 2048), (2048, 3072), (3072, 3088)]
        for i, (c0, c1) in enumerate(pieces):
            self.dma("pool", win[:, :, c0:c1], w_in[:, :, c0:c1], slot=("win", i), W=[("win", i)])
        self.dma("pool", wout, w_out, slot=("wout",), W=["wout"])
        self.dma("sp", hgain, self.din("mlstm_head_gain", [2, D])[slot:slot + 1, :].partition_broadcast(128),
                 slot=("hgain",), W=["hgain"])
        self.dma("sp", bias_bc, self.din("mlstm_gate_bias", [2, 16])[slot:slot + 1, :].partition_broadcast(128),
                 slot=("gbias",), W=["gbias"])
        self.memset("dve", C32, 0.0, W=["C32"])
        self.memset("dve", Cbf, 0.0, W=["Cbf"])
        self.memset("dve", kE, 0.0, W=["kE"])
        self.memset("dve", kO, 0.0, W=["kO"])

        self.norm_to_hT(self.din("norm_mix", [DEPTH, D])[l:l + 1, :], hT, hb)

        psT = B[7][:].bitcast(BF16)
        psT3 = psT.rearrange("p (k s) -> p k s", k=8)

        def b4(i):
            return B[i][:].rearrange("p (h s) -> p h s", h=4)

        wpiece = {0: 0, 1: 0, 2: 1, 3: 1, 4: 2, 5: 2}
        for t in range(self.ntiles):
            tok = slice(t * 128, (t + 1) * 128)
            for cb in range(6):
                for kc in range(KC):
                    self.mm(B[cb][:], hT[:, kc, tok], win[:, kc, cb * 512:(cb + 1) * 512], kc == 0, kc == KC - 1,
                            R=[("hT", t), ("win", wpiece[cb])], W=[("B", cb)])
            for kc in range(KC):
                self.mm(B[6][:, 0:16], hT[:, kc, tok], win[:, kc, 3072:3088], kc == 0, kc == KC - 1,
                        R=[("hT", t), ("win", 3)], W=[("B", 6)])
            self.tt("dve", g1, B[6][:, 0:16], bias_bc, ALU.add, R=[("B", 6), "gbias"], W=["g1"])
            self.act(th, g1, AF.Tanh, R=["g1"], W=["th"], scale=1.0 / 15.0)
            self.ts("dve", gs, th, 15.0, None, ALU.mult, R=["th"], W=["gs"])
            self.act(ee, gs[:, 8:16], AF.Exp, R=["gs"], W=["ee"], scale=-1.0)
            self.act(lfn, ee, AF.Ln, R=["ee"], W=["lfn"], bias=1.0)
            self.mm(B[6][:, 16:24], self.negtri[:], lfn, True, True, R=["lfn", "negtri"], W=[("B", 6)])
            self.mm(B[6][:, 24:32], self.negones[:], lfn, True, True, R=["lfn", "negones"], W=[("B", 6)])
            self.tt("dve", a_, gs[:, 0:8], B[6][:, 16:24], ALU.subtract, R=["gs", ("B", 6)], W=["a"])
            self.act(ea, a_, AF.Exp, R=["a"], W=["ea"])
            self.copy("dve", eabf, ea, R=["ea"], W=["eabf"])
            self.act(eb, B[6][:, 16:24], AF.Exp, R=[("B", 6)], W=["eb"])
            bl2 = B[6][:, 24:32].rearrange("p (j two) -> p j two", two=2)
            self.copy("dve", gp[0:64, :], bl2[0:64, :, 0], R=[("B", 6)], W=["gp0"])
            self.copy("dve", gp[64:128, :], bl2[64:128, :, 1], R=[("B", 6)], W=["gp1"])
            self.act(gsel, gp, AF.Exp, R=["gp0", "gp1"], W=["gsel"])
            self.act(q_bf, B[0][:], AF.Copy, R=[("B", 0)], W=["q_bf"])
            self.act(k_bf, B[1][:], AF.Copy, R=[("B", 1)], W=["k_bf"], scale=0.125)
            k4 = B[1][:].rearrange("p (j two c) -> p j two c", two=2, c=64)
            kE4 = kE.rearrange("p (j two c) -> p j two c", two=2, c=64)
            kO4 = kO.rearrange("p (j two c) -> p j two c", two=2, c=64)
            self.ts("dve", kE4[:, :, 0, :], k4[:, :, 0, :], 0.125, None, ALU.mult, R=[("B", 1)], W=["kE"])
            self.ts("dve", kO4[:, :, 1, :], k4[:, :, 1, :], 0.125, None, ALU.mult, R=[("B", 1)], W=["kO"])
            for j in range(4):
                self.tr(psT[:, j * 128:(j + 1) * 128], q_bf[:, j * 128:(j + 1) * 128], self.ident_bf[:],
                        R=["q_bf", "ident_bf"], W=[("B", 7)])
            for j in range(4):
                self.tr(psT[:, (4 + j) * 128:(5 + j) * 128], k_bf[:, j * 128:(j + 1) * 128], self.ident_bf[:],
                        R=["k_bf", "ident_bf"], W=[("B", 7)])
            self.act(qkT, psT3, AF.Copy, R=[("B", 7)], W=["qkT"])
            for i in range(2):
                self.tt("dve", vs[:, 4 * i:4 * i + 4, :], b4(2 + i),
                        ea[:, 4 * i:4 * i + 4].unsqueeze(2).to_broadcast([128, 4, 128]), ALU.mult,
                        R=[("B", 2 + i), "ea"], W=[("vs", i)])
            for i in range(2):
                self.act(sig[:, i * 512:(i + 1) * 512], B[4 + i][:], AF.Tanh, R=[("B", 4 + i)], W=[("sig", i)], scale=0.5)
            for h in range(H):
                p0 = (h % 2) * 64
                self.mm(b4(h % 2)[:, h // 2, :], qkT[p0:p0 + 64, 4 + h // 2, :], qkT[p0:p0 + 64, h // 2, :], True, True,
                        R=["qkT"], W=[("B", h % 2)])
            PT4 = PT.rearrange("p (j two) s -> p j two s", two=2)
            for i in range(2):
                self.tt("dve", PT4[:, :, i, :], b4(i),
                        self.causal[:].unsqueeze(1).to_broadcast([128, 4, 128]), ALU.mult,
                        R=[("B", i), "causal"], W=[("PT", i)])
            for h in range(H):
                p0 = (h % 2) * 64
                qTh = qkT[p0:p0 + 64, h // 2, :]
                self.mm(b4(2 + h // 4)[:, h % 4, :], PT[:, h, :], vs[:, h, :], True, False,
                        R=[("PT", h % 2), ("vs", h // 4)], W=[("B", 2 + h // 4)])
                self.mm(b4(2 + h // 4)[:, h % 4, :], qTh, Cbf[p0:p0 + 64, h // 2, 0:128], False, True,
                        R=["qkT", "Cbf"], W=[("B", 2 + h // 4)])
                self.mm(B[6][:, 32 + h:33 + h], PT[:, h, :], eabf[:, h:h + 1], True, False,
                        R=[("PT", h % 2), "eabf"], W=[("B", 6)])
                self.mm(B[6][:, 32 + h:33 + h], qTh, Cbf[p0:p0 + 64, h // 2, 128:129], False, True,
                        R=["qkT", "Cbf"], W=[("B", 6)])
            for j in range(4):
                self.mm(b4(4)[:, j, :], kE[:, j * 128:(j + 1) * 128], vs[:, 2 * j, :], True, False,
                        R=["kE", ("vs", j // 2)], W=[("B", 4)])
                self.mm(b4(4)[:, j, :], kO[:, j * 128:(j + 1) * 128], vs[:, 2 * j + 1, :], False, True,
                        R=["kO", ("vs", j // 2)], W=[("B", 4)])
                self.mm(B[6][:, 40 + j:41 + j], kE[:, j * 128:(j + 1) * 128], eabf[:, 2 * j:2 * j + 1], True, False,
                        R=["kE", "eabf"], W=[("B", 6)])
                self.mm(B[6][:, 40 + j:41 + j], kO[:, j * 128:(j + 1) * 128], eabf[:, 2 * j + 1:2 * j + 2], False, True,
                        R=["kO", "eabf"], W=[("B", 6)])
            self.tt("dve", C32[:, :, 0:128], C32[:, :, 0:128], b4(4), ALU.add, R=["C32", ("B", 4)], W=["C32"])
            self.tt("dve", C32[:, :, 128], C32[:, :, 128], B[6][:, 40:44], ALU.add, R=["C32", ("B", 6)], W=["C32"])
            self.tt("dve", C32, C32, gsel.unsqueeze(2).to_broadcast([128, 4, 132]), ALU.mult, R=["C32", "gsel"], W=["C32"])
            self.act(Cbf, C32, AF.Copy, R=["C32"], W=["Cbf"])
            self.tt("dve", d1, B[6][:, 32:40], eb, ALU.mult, R=[("B", 6), "eb"], W=["d1"])
            self.stt("dve", d2, d1, -1.0, d1, ALU.mult, ALU.max, R=["d1"], W=["d2"])
            self.ts("dve", d2, d2, 1.0, None, ALU.max, R=["d2"], W=["d2"])
            self.P.add("dve", lambda e: e.reciprocal(out=d2, in_=d2), R=["d2"], W=["d2"])
            self.tt("dve", scl, d2, eb, ALU.mult, R=["d2", "eb"], W=["scl"])
            hc3 = hc.rearrange("p (h s) -> p h s", h=H)
            for i in range(2):
                self.tt("dve", hc3[:, 4 * i:4 * i + 4, :], b4(2 + i),
                        scl[:, 4 * i:4 * i + 4].unsqueeze(2).to_broadcast([128, 4, 128]), ALU.mult,
                        R=[("B", 2 + i), "scl"], W=[("hc", i)])
            for h in range(H):
                self.act(jk, hc3[:, h, :], AF.Square, R=[("hc", h // 4)], W=["jk", "ssh"], accum_out=ssh[:, h:h + 1])
            self.act(l2, ssh, AF.Ln, R=["ssh"], W=["l2"], scale=1.0 / 128.0, bias=EPS)
            self.act(rs, l2, AF.Exp, R=["l2"], W=["rs"], scale=-0.5, bias=float(np.log(0.5)))
            self.tt("dve", hc3, hc3, rs.unsqueeze(2).to_broadcast([128, H, 128]), ALU.mult,
                    R=[("hc", 0), ("hc", 1), "rs"], W=[("hc", 0), ("hc", 1)])
            self.tt("pool", hc, hc, hgain, ALU.mult, R=[("hc", 0), ("hc", 1), "hgain"], W=[("hc", 0), ("hc", 1)])
            self.stt("dve", outb, sig, 1.0, hc, ALU.add, ALU.mult,
                     R=[("sig", 0), ("sig", 1), ("hc", 0), ("hc", 1)], W=[("hb", 0)])
            for j in range(8):
                self.tr(psT[:, j * 128:(j + 1) * 128], outb[:, j * 128:(j + 1) * 128], self.ident_bf[:],
                        R=[("hb", 0), "ident_bf"], W=[("B", 7)])
            self.act(outT, psT3, AF.Copy, R=[("B", 7)], W=["outT"])
            for half in range(2):
                bk = 5 if half == 0 else 1
                for vc in range(8):
                    self.mm(B[bk][:], outT[:, vc, :], wout[:, vc, half * 512:(half + 1) * 512], vc == 0, vc == 7,
                            R=["outT", "wout"], W=[("B", bk)])
                xs = self.x[:, t, half * 512:(half + 1) * 512]
                self.tt("dve", xs, xs, B[bk][:], ALU.add, R=[("B", bk), ("x", t)], W=[("x", t)])

    def pool_phase(self, l):
        P = self.P
        slot = l // 2
        P.barrier()
        self.off = 0
        band = [self.carve([128, 128], F32) for _ in range(4)]
        band0 = [self.carve([128, 128], F32) for _ in range(4)]
        offm = [self.carve([128, 128], F32) for _ in range(4)]
        sc16 = self.carve([128, 4, 16], F32)
        h32 = [self.carve([128, D], F32) for _ in range(3)]
        yT = [self.carve([128, KC, 128], BF16) for _ in range(2)]
        wp = self.carve([128, 4, 2, 256], BF16)
        pscale = self.carve([128, D], F32)
        tmp = [self.carve([128, 512], F32) for _ in range(2)]
        jkD = self.carve([128, D], BF16)
        B = self.B
        wsrc = self.din("pool_w_group", [2, 4, 256, 256])[slot]
        for g in range(4):
            self.dma("pool", wp[:, g, :, :], wsrc[g].rearrange("(k p) d -> p k d", p=128), slot=("wp", g), W=[("wp", g)])
        self.dma("sp", pscale, self.din("pool_scale", [2, D])[slot:slot + 1, :].partition_broadcast(128),
                 slot=("pscale",), W=["pscale"])
        gi = self.load_gain(self.din("norm_mix", [DEPTH, D])[l:l + 1, :])

        def sel(ap, pattern, cm, base, key):
            self.P.add("pool", lambda e: e.affine_select(out=ap, in_=ap, compare_op=ALU.is_ge, fill=0.0, base=base,
                                                         pattern=pattern, channel_multiplier=cm), W=[key])
        for wi, w in enumerate(WINS):
            self.memset("pool", band[wi], 1.0 / w, W=[("band", wi)])
            sel(band[wi], [[1, 128]], -1, 0, ("band", wi))
            sel(band[wi], [[-1, 128]], 1, w - 1, ("band", wi))
            self.memset("pool", offm[wi], 1.0 / w, W=[("off", wi)])
            sel(offm[wi], [[-1, 128]], 1, w - 129, ("off", wi))
            self.ts("dve", sc16[:, wi, :], self.invcnt[:, wi, :], float(w), None, ALU.mult, R=["invcnt"], W=[("sc16", wi)])
            self.copy("dve", band0[wi], band[wi], R=[("band", wi)], W=[("band0", wi)])
            self.tt("dve", band0[wi][:, 0:16], band[wi][:, 0:16], sc16[:, wi, :], ALU.mult,
                    R=[("band", wi), ("sc16", wi)], W=[("band0", wi)])
            self.tt("dve", band0[wi], band0[wi], self.ident_f[:], ALU.subtract, R=[("band0", wi), "ident_f"], W=[("band0", wi)])
            self.tt("dve", band[wi], band[wi], self.ident_f[:], ALU.subtract, R=[("band", wi), "ident_f"], W=[("band", wi)])

        self.norm_stats(jkD)

        def b4(i):
            return B[i][:].rearrange("p (h s) -> p h s", h=4)

        def norm_tile(t):
            self.stt("dve", h32[t % 3], self.x[:, t, :], self.rstd[:, t:t + 1], self.gain_bc[gi][:], ALU.mult, ALU.mult,
                     R=[("x", t), "rstd", ("gain", gi)], W=[("h32", t % 3)] + (["junk"] if t == 0 else []))

        def band_mm(t):
            par = t % 2
            for c in range(KC):
                wi = c // 2
                bk = 2 * par + c // 4
                out = b4(bk)[:, c % 4, :]
                cs = slice(c * 128, (c + 1) * 128)
                if t == 0:
                    self.mm(out, h32[0][:, cs], band0[wi], True, True, R=[("h32", 0), ("band0", wi)], W=[("B", bk)])
                else:
                    self.mm(out, h32[t % 3][:, cs], band[wi], True, False, R=[("h32", t % 3), ("band", wi)], W=[("B", bk)])
                    self.mm(out[:, 0:16], h32[(t - 1) % 3][:, cs], offm[wi][:, 0:16], False, True,
                            R=[("h32", (t - 1) % 3), ("off", wi)], W=[("B", bk)])

        def evac_y(t):
            par = t % 2
            self.act(yT[par][:, 0:4, :], b4(2 * par), AF.Copy, R=[("B", 2 * par)], W=[("yT", par, 0)])
            self.copy("dve", yT[par][:, 4:8, :], b4(2 * par + 1), R=[("B", 2 * par + 1)], W=[("yT", par, 1)])

        def pool_mm(t):
            par = t % 2
            for g in range(4):
                bk = 4 + 2 * par + g // 2
                for kc in range(2):
                    self.mm(B[bk][:, (g % 2) * 256:(g % 2 + 1) * 256], yT[par][:, 2 * g + kc, :], wp[:, g, kc, :], kc == 0, kc == 1,
                            R=[("yT", par, g // 2), ("wp", g)], W=[("B", bk)])

        def final(t):
            par = t % 2
            for half in range(2):
                bk = 4 + 2 * par + half
                self.tt("dve", tmp[half], B[bk][:], pscale[:, half * 512:(half + 1) * 512], ALU.mult,
                        R=[("B", bk), "pscale"], W=[("tmp", half)])
                xs = self.x[:, t, half * 512:(half + 1) * 512]
                self.tt("dve", xs, xs, tmp[half], ALU.add, R=[("tmp", half), ("x", t)], W=[("x", t)])
            self.emit_ss(t, jkD)

        norm_tile(0)
        norm_tile(1)
        band_mm(0)
        evac_y(0)
        for t in range(NT):
            if t + 2 < NT:
                norm_tile(t + 2)
            if t + 1 < NT:
                band_mm(t + 1)
            pool_mm(t)
            if t + 1 < NT:
                evac_y(t + 1)
            final(t)
        self.stats_ready = True

    def pool_phase_v1(self, l):
        P = self.P
        slot = l // 2
        P.barrier()
        self.off = 0
        hT32 = self.carve([128, KC, S], F32)
        yT = self.carve([128, KC, S], BF16)
        sbuf_ = [self.carve([128, S], F32) for _ in range(2)]
        wp = self.carve([128, 4, 2, 256], BF16)
        pscale = self.carve([128, D], F32)
        h32 = [self.carve([128, D], F32) for _ in range(2)]
        t16 = self.carve([128, 16], F32)
        B = self.B
        wsrc = self.din("pool_w_group", [2, 4, 256, 256])[slot]
        for g in range(4):
            self.dma("pool", wp[:, g, :, :], wsrc[g].rearrange("(k p) d -> p k d", p=128), slot=("wp", g), W=[("wp", g)])
        self.dma("sp", pscale, self.din("pool_scale", [2, D])[slot:slot + 1, :].partition_broadcast(128),
                 slot=("pscale",), W=["pscale"])
        gi = self.load_gain(self.din("norm_mix", [DEPTH, D])[l:l + 1, :])
        self.norm_stats(h32[0])
        for t in range(NT):
            ht = h32[t % 2]
            self.stt("dve", ht, self.x[:, t, :], self.rstd[:, t:t + 1], self.gain_bc[gi][:], ALU.mult, ALU.mult,
                     R=[("x", t), "rstd", ("gain", gi)], W=[("h32", t % 2), "junk"] if t % 2 == 0 else [("h32", t % 2)])
            for kc in range(KC):
                bk = 6 + kc // 4
                self.tr(B[bk][:, (kc % 4) * 128:(kc % 4 + 1) * 128], ht[:, kc * 128:(kc + 1) * 128], self.ident_f[:],
                        R=[("h32", t % 2), "ident_f"], W=[("B", bk)])
            self.act(hT32[:, 0:4, t * 128:(t + 1) * 128], B[6][:].rearrange("p (k s) -> p k s", k=4), AF.Copy,
                     R=[("B", 6)], W=[("hT32", c) for c in range(0, 4)])
            self.copy("dve", hT32[:, 4:8, t * 128:(t + 1) * 128], B[7][:].rearrange("p (k s) -> p k s", k=4),
                      R=[("B", 7)], W=[("hT32", c) for c in range(4, 8)])
        for c in range(KC):
            wi = c // 2
            win_ = WINS[wi]
            src = hT32[:, c, :]
            cur, ckey = src, ("hT32", c)
            sh, n = 1, 0
            while sh < win_:
                dst = sbuf_[n % 2]
                dkey = ("sb", n % 2)
                self.tt("dve", dst[:, sh:], cur[:, sh:], cur[:, 0:S - sh], ALU.add, R=[ckey], W=[dkey])
                self.copy("dve", dst[:, 0:sh], cur[:, 0:sh], R=[ckey], W=[dkey])
                cur, ckey = dst, dkey
                sh *= 2
                n += 1
            self.stt("dve", yT[:, c, :], cur, 1.0 / win_, src, ALU.mult, ALU.subtract, R=[ckey, ("hT32", c)], W=[("yT", c)])
            self.tt("dve", t16, cur[:, 0:16], self.invcnt[:, wi, :], ALU.mult, R=[ckey, "invcnt"], W=["t16"])
            self.tt("dve", yT[:, c, 0:16], t16, src[:, 0:16], ALU.subtract, R=["t16", ("hT32", c)], W=[("yT", c)])
        for t in range(NT):
            tok = slice(t * 128, (t + 1) * 128)
            for g in range(4):
                bk = (t % 2) * 2 + g // 2
                for kc in range(2):
                    self.mm(B[bk][:, (g % 2) * 256:(g % 2 + 1) * 256], yT[:, 2 * g + kc, tok], wp[:, g, kc, :], kc == 0, kc == 1,
                            R=[("yT", 2 * g + kc), ("wp", g)], W=[("B", bk)])
            for half in range(2):
                bk = (t % 2) * 2 + half
                tmp = h32[half][:, 0:512]
                self.tt("dve", tmp, B[bk][:], pscale[:, half * 512:(half + 1) * 512], ALU.mult,
                        R=[("B", bk), "pscale"], W=[("h32", half)])
                xs = self.x[:, t, half * 512:(half + 1) * 512]
                self.tt("dve", xs, xs, tmp, ALU.add, R=[("h32", half), ("x", t)], W=[("x", t)])

    def ffn_phase(self, l):
        P = self.P
        P.barrier()
        self.off = 0
        hT = self.carve([128, KC, S], BF16)
        abuf = [self.carve([128, 4, S], BF16) for _ in range(2)]
        wgu = [self.carve([128, KC, 2, 256], BF16) for _ in range(3)]
        wd = [self.carve([128, 4, D], BF16) for _ in range(2)]
        hb = [self.carve([128, D], BF16) for _ in range(2)]
        sl = [self.carve([128, 512], BF16) for _ in range(2)]
        jkD = self.carve([128, D], BF16)
        nxt_needs_stats = True
        B = self.B
        wgu_src = self.din("ffn_w_gate_up", [DEPTH, D, 2 * DFF])[l].rearrange("(k p) n -> p k n", p=128)
        wd_src = self.din("ffn_w_down", [DEPTH, DFF, D])[l].rearrange("(j p) d -> p j d", p=128)
        groups = [(j0, min(4, NJ - j0)) for j0 in range(0, NJ, 4)]

        def load_gu(n):
            s_ = n % 3
            self.dma("pool", wgu[s_][:, :, 0, :], wgu_src[:, :, n * 256:(n + 1) * 256], slot=("wg", s_), W=[("wg", s_)])
            self.dma("pool", wgu[s_][:, :, 1, :], wgu_src[:, :, DFF + n * 256:DFF + (n + 1) * 256], slot=("wu", s_), W=[("wu", s_)])

        def load_d(gi):
            j0, nj = groups[gi]
            self.dma("pool", wd[gi % 2][:, 0:nj, :], wd_src[:, j0:j0 + nj, :], slot=("wd", gi % 2), W=[("wd", gi % 2)])

        load_gu(0)
        load_gu(1)
        load_gu(2)
        load_d(0)
        self.norm_to_hT(self.din("norm_ffn", [DEPTH, D])[l:l + 1, :], hT, hb)

        cnt = [0]

        def gateup(gi):
            j0, nj = groups[gi]
            ab = abuf[gi % 2]
            for jj in range(nj):
                j = j0 + jj
                n, sub = j // 2, j % 2
                s_ = n % 3
                for tg in range(4):
                    pr = cnt[0] % 2
                    cnt[0] += 1
                    bg, bu = B[2 * pr], B[2 * pr + 1]
                    rk = [("hT", 4 * tg + i) for i in range(4)]
                    for kc in range(KC):
                        self.mm(bg[:], wgu[s_][:, kc, 0, sub * 128:(sub + 1) * 128], hT[:, kc, tg * 512:(tg + 1) * 512],
                                kc == 0, kc == KC - 1, R=rk + [("wg", s_)], W=[("B", 2 * pr)])
                    for kc in range(KC):
                        self.mm(bu[:], wgu[s_][:, kc, 1, sub * 128:(sub + 1) * 128], hT[:, kc, tg * 512:(tg + 1) * 512],
                                kc == 0, kc == KC - 1, R=rk + [("wu", s_)], W=[("B", 2 * pr + 1)])
                    self.act(sl[pr], bg[:], AF.Silu, R=[("B", 2 * pr)], W=[("sl", pr)])
                    self.tt("dve", ab[:, jj, tg * 512:(tg + 1) * 512], sl[pr], bu[:], ALU.mult,
                            R=[("sl", pr), ("B", 2 * pr + 1)], W=[("ab", gi % 2, tg)])
                if sub == 1 or j == NJ - 1:
                    if n + 3 < NJ // 2:
                        load_gu(n + 3)

        def down(gi):
            j0, nj = groups[gi]
            ab = abuf[gi % 2]
            for t in range(NT):
                for half in range(2):
                    bk = 4 + (2 * t + half) % 4
                    for jj in range(nj):
                        self.mm(B[bk][:], ab[:, jj, t * 128:(t + 1) * 128], wd[gi % 2][:, jj, half * 512:(half + 1) * 512],
                                jj == 0, jj == nj - 1, R=[("ab", gi % 2, t // 4), ("wd", gi % 2)], W=[("B", bk)])
                    xs = self.x[:, t, half * 512:(half + 1) * 512]
                    self.tt("dve", xs, xs, B[bk][:], ALU.add, R=[("B", bk), ("x", t)], W=[("x", t)])
                if gi == len(groups) - 1 and nxt_needs_stats:
                    self.emit_ss(t, jkD)
            if gi + 2 < len(groups):
                load_d(gi + 2)

        gateup(0)
        load_d(1)
        for gi in range(len(groups)):
            if gi + 1 < len(groups):
                gateup(gi + 1)
            down(gi)
        self.stats_ready = nxt_needs_stats

    def final_phase(self):
        P = self.P
        P.barrier()
        self.off = 0
        ob = [self.carve([128, D], F32) for _ in range(2)]
        if self.final_norm:
            gi = self.load_gain(self.din("final_norm", [1, D])[0:1, :])
            self.norm_stats(ob[0])
        stores = []
        for t in range(NT):
            o = ob[t % 2]
            if self.final_norm:
                self.stt("dve", o, self.x[:, t, :], self.rstd[:, t:t + 1], self.gain_bc[gi][:], ALU.mult, ALU.mult,
                         R=[("x", t), "rstd", ("gain", gi)], W=[("ob", t % 2), "junk"] if t % 2 == 0 else [("ob", t % 2)])
                src, rk = o, [("ob", t % 2)]
            else:
                src, rk = self.x[:, t, :], [("x", t)]
            stores.append(self.dma("sp", self.out[t * 128:(t + 1) * 128, :], src, slot=("st", t), R=rk, W=[("outd", t)]))
        P.add("sp", None, extra=stores)


_CACHE = {}


def _get_prog(layers, final_norm):
    key = (tuple(layers), final_norm)
    if key not in _CACHE:
        b = Builder(layers, final_norm)
        nc = b.build()
        _CACHE[key] = (nc, sorted(b.dram.keys()))
    return _CACHE[key]


def _run(layers, final_norm, xs, inputs):
    nc, names = _get_prog(layers, final_norm)
    in_maps = []
    for c in range(N_CORES):
        m = {}
        for n in names:
            if n == "x":
                m[n] = np.ascontiguousarray(xs[c])
            elif n == "final_norm":
                m[n] = np.ascontiguousarray(inputs[n]).reshape(1, D)
            else:
                m[n] = np.ascontiguousarray(inputs[n])
        in_maps.append(m)
    res = run_bass_kernel_spmd(nc, in_maps, core_ids=list(range(N_CORES)))
    return [res.results[c]["out"] for c in range(N_CORES)]


LAUNCH_GROUPS = [[0, 1, 2, 3]]


def kernel(**inputs):
    inputs = {k: np.asarray(v, dtype=np.float32) for k, v in inputs.items()}
    xs = [inputs["x"][b] for b in range(N_CORES)]
    for gi, grp in enumerate(LAUNCH_GROUPS):
        xs = _run(grp, gi == len(LAUNCH_GROUPS) - 1, xs, inputs)
    return np.stack(xs, axis=0).astype(np.float32)
```
